# Optimizing a Trainium2 kernel written in Bass

```python
import jax, jax.numpy as jnp
from jax import lax
import numpy as np

D_MODEL = 1024
BATCH = 4
SEQ = 8192
DEPTH = 2

HEAD_DIM = 64
ROPE_THETA = 10000.0
Q_BLOCK = 128
LN_EPS = 1e-5

NSA_HEADS = 8
NSA_KV_HEADS = 2
NSA_GROUP = NSA_HEADS // NSA_KV_HEADS
CMP_BLOCK = 32
CMP_STRIDE = 16
CMP_HIDDEN = 2 * HEAD_DIM
SLC_BLOCK = 64
SLC_TOPN = 16
WINDOW = 512
FORCE_BONUS = 1.0e4

MOBA_HEADS = 8
MOBA_BLOCK = 256
MOBA_TOPK = 3
MOBA_Q_BLOCK = 32

SB_HEADS = 16

N_GROUPS = 4
EXPERTS_PER_GROUP = 4
N_EXPERTS = N_GROUPS * EXPERTS_PER_GROUP
TOPK_IN_GROUP = 2
EXPERT_HIDDEN = 256

DEEPNORM_ALPHA = float((2 * DEPTH) ** 0.25)
DEEPNORM_BETA = float((8 * DEPTH) ** -0.25)

N_EVEN = (DEPTH + 1) // 2
N_ODD = DEPTH // 2

NSA_Q_W = NSA_HEADS * HEAD_DIM
NSA_KV_W = NSA_KV_HEADS * HEAD_DIM
NSA_GATE_W = 3 * NSA_HEADS
MOBA_W = MOBA_HEADS * HEAD_DIM
EVEN_WIDTHS = (NSA_Q_W, NSA_KV_W, NSA_KV_W, NSA_KV_W, NSA_KV_W, NSA_KV_W, NSA_KV_W,
               NSA_GATE_W, MOBA_W, MOBA_W, MOBA_W)
EVEN_IN_W = sum(EVEN_WIDTHS)
EVEN_SPLITS = tuple(int(v) for v in np.cumsum(EVEN_WIDTHS)[:-1])
EVEN_OUT_W = NSA_Q_W + MOBA_W
SB_W = SB_HEADS * HEAD_DIM

kernel_name = 'hybrid_nsa_moba_stickbreak_hmoe'


def layer_norm(x, g, b):
    xf = x.astype(jnp.float32)
    mu = jnp.mean(xf, axis=-1, keepdims=True)
    var = jnp.mean(jnp.square(xf - mu), axis=-1, keepdims=True)
    return ((xf - mu) * lax.rsqrt(var + LN_EPS) * g + b).astype(x.dtype)


def rope(x, positions):
    half = x.shape[-1] // 2
    inv_freq = ROPE_THETA ** (-jnp.arange(half, dtype=jnp.float32) / half)
    ang = positions.astype(jnp.float32)[..., None] * inv_freq
    cos = jnp.cos(ang)[:, :, None, :]
    sin = jnp.sin(ang)[:, :, None, :]
    x1 = x[..., :half].astype(jnp.float32)
    x2 = x[..., half:].astype(jnp.float32)
    return jnp.concatenate([x1 * cos - x2 * sin, x2 * cos + x1 * sin], axis=-1).astype(x.dtype)


def masked_softmax(s, mask):
    s = jnp.where(mask, s.astype(jnp.float32), -jnp.inf)
    m = jnp.max(s, axis=-1, keepdims=True)
    m = jnp.where(jnp.isfinite(m), m, 0.0)
    e = jnp.where(mask, jnp.exp(s - m), 0.0)
    return e / jnp.maximum(jnp.sum(e, axis=-1, keepdims=True), 1e-30)


def nsa_attention(q, k_cmp, v_cmp, k_slc, v_slc, k_win, v_win, gates,
                  cmp_pos_k, cmp_pos_v, cmp_w1_k, cmp_w2_k, cmp_w1_v, cmp_w2_v):
    B, S = q.shape[0], q.shape[1]
    scale = HEAD_DIM ** -0.5
    n_cmp = (S - CMP_BLOCK) // CMP_STRIDE + 1
    cmp_start = jnp.arange(n_cmp) * CMP_STRIDE
    cmp_end = cmp_start + CMP_BLOCK - 1
    tok_idx = cmp_start[:, None] + jnp.arange(CMP_BLOCK)[None, :]

    def compress(kv, pos_emb, w1, w2):
        blocks = kv[:, tok_idx] + pos_emb[None, None, :, None, :]
        flat = blocks.transpose(0, 1, 3, 2, 4).reshape(B, n_cmp, NSA_KV_HEADS, CMP_BLOCK * HEAD_DIM)
        return jax.nn.gelu(flat @ w1) @ w2

    kc = compress(k_cmp, cmp_pos_k, cmp_w1_k, cmp_w2_k)
    vc = compress(v_cmp, cmp_pos_v, cmp_w1_v, cmp_w2_v)

    n_slc = S // SLC_BLOCK
    slc_start = jnp.arange(n_slc) * SLC_BLOCK
    overlap = ((cmp_start[:, None] < slc_start[None, :] + SLC_BLOCK)
               & (cmp_end[:, None] >= slc_start[None, :])).astype(jnp.float32)
    top_n = min(SLC_TOPN, n_slc)
    kb = k_slc.reshape(B, n_slc, SLC_BLOCK, NSA_KV_HEADS, HEAD_DIM).transpose(0, 3, 1, 2, 4)
    vb = v_slc.reshape(B, n_slc, SLC_BLOCK, NSA_KV_HEADS, HEAD_DIM).transpose(0, 3, 1, 2, 4)
    pad = ((0, 0), (WINDOW, 0), (0, 0), (0, 0))
    kwp = jnp.pad(k_win, pad)
    vwp = jnp.pad(v_win, pad)
    bi = jnp.arange(B)[:, None, None, None]
    hi = jnp.arange(NSA_KV_HEADS)[None, :, None, None]
    blk_ids = jnp.arange(n_slc)

    def one_block(b):
        q0 = b * Q_BLOCK
        t = q0 + jnp.arange(Q_BLOCK)
        qb = lax.dynamic_slice_in_dim(q, q0, Q_BLOCK, axis=1).reshape(
            B, Q_BLOCK, NSA_KV_HEADS, NSA_GROUP, HEAD_DIM)
        gb = lax.dynamic_slice_in_dim(gates, q0, Q_BLOCK, axis=1).reshape(
            B, Q_BLOCK, NSA_KV_HEADS, NSA_GROUP, 3)
        s_c = jnp.einsum('bqkgd,bnkd->bkgqn', qb, kc) * scale
        p_c = masked_softmax(s_c, cmp_end[None, :] <= t[:, None])
        o_c = jnp.einsum('bkgqn,bnkd->bqkgd', p_c.astype(vc.dtype), vc)
        imp = jnp.einsum('bkgqn,nj->bkqj', p_c, overlap)
        cur = t // SLC_BLOCK
        forced = ((blk_ids[None, :] == 0) | (blk_ids[None, :] == cur[:, None])
                  | (blk_ids[None, :] == cur[:, None] - 1))
        valid = blk_ids[None, :] <= cur[:, None]
        score = jnp.where(forced, imp + FORCE_BONUS, jnp.where(valid, imp, -1.0))
        _, sel = lax.top_k(score, top_n)
        sel_ok = sel <= cur[None, None, :, None]
        ks = kb[bi, hi, sel]
        vs = vb[bi, hi, sel]
        kpos = sel[..., None] * SLC_BLOCK + jnp.arange(SLC_BLOCK)
        m_s = sel_ok[..., None] & (kpos <= t[None, None, :, None, None])
        n_keys = top_n * SLC_BLOCK
        s_s = jnp.einsum('bqkgd,bkqnld->bkgqnl', qb, ks).reshape(
            B, NSA_KV_HEADS, NSA_GROUP, Q_BLOCK, n_keys) * scale
        p_s = masked_softmax(s_s, m_s.reshape(B, NSA_KV_HEADS, Q_BLOCK, n_keys)[:, :, None])
        o_s = jnp.einsum('bkgqm,bkqmd->bqkgd', p_s.astype(vs.dtype),
                         vs.reshape(B, NSA_KV_HEADS, Q_BLOCK, n_keys, HEAD_DIM))
        kw = lax.dynamic_slice_in_dim(kwp, q0, WINDOW + Q_BLOCK, axis=1)
        vw = lax.dynamic_slice_in_dim(vwp, q0, WINDOW + Q_BLOCK, axis=1)
        wpos = q0 - WINDOW + jnp.arange(WINDOW + Q_BLOCK)
        dist = t[:, None] - wpos[None, :]
        m_w = (dist >= 0) & (dist < WINDOW) & (wpos[None, :] >= 0)
        s_w = jnp.einsum('bqkgd,bskd->bkgqs', qb, kw) * scale
        p_w = masked_softmax(s_w, m_w)
        o_w = jnp.einsum('bkgqs,bskd->bqkgd', p_w.astype(vw.dtype), vw)
        o = gb[..., 0:1] * o_c + gb[..., 1:2] * o_s + gb[..., 2:3] * o_w
        return o.reshape(B, Q_BLOCK, NSA_Q_W)

    out = lax.map(one_block, jnp.arange(S // Q_BLOCK))
    return out.transpose(1, 0, 2, 3).reshape(B, S, NSA_Q_W)


def moba_attention(q, k, v):
    B, S = q.shape[0], q.shape[1]
    scale = HEAD_DIM ** -0.5
    n_blk = -(-S // MOBA_BLOCK)
    pad = n_blk * MOBA_BLOCK - S
    kp = jnp.pad(k, ((0, 0), (0, pad), (0, 0), (0, 0)))
    vp = jnp.pad(v, ((0, 0), (0, pad), (0, 0), (0, 0)))
    kb = kp.reshape(B, n_blk, MOBA_BLOCK, MOBA_HEADS, HEAD_DIM).transpose(0, 3, 1, 2, 4)
    vb = vp.reshape(B, n_blk, MOBA_BLOCK, MOBA_HEADS, HEAD_DIM).transpose(0, 3, 1, 2, 4)
    k_mean = jnp.mean(kb.astype(jnp.float32), axis=3).astype(k.dtype)
    top_k = min(MOBA_TOPK, n_blk)
    bi = jnp.arange(B)[:, None, None, None]
    hi = jnp.arange(MOBA_HEADS)[None, :, None, None]
    blk_ids = jnp.arange(n_blk)
    n_sel = top_k * MOBA_BLOCK

    def one_block(b):
        q0 = b * MOBA_Q_BLOCK
        t = q0 + jnp.arange(MOBA_Q_BLOCK)
        cur = q0 // MOBA_BLOCK
        qb = lax.dynamic_slice_in_dim(q, q0, MOBA_Q_BLOCK, axis=1)
        gate = jnp.einsum('bqhd,bhnd->bhqn', qb, k_mean).astype(jnp.float32)
        gate = jnp.where(blk_ids < cur, gate, -jnp.inf)
        _, sel = lax.top_k(gate, top_k)
        sel_ok = sel < cur
        ks = kb[bi, hi, sel]
        vs = vb[bi, hi, sel]
        s_sel = jnp.einsum('bqhd,bhqnld->bhqnl', qb, ks).reshape(B, MOBA_HEADS, MOBA_Q_BLOCK, n_sel) * scale
        m_sel = jnp.broadcast_to(sel_ok[..., None], sel.shape + (MOBA_BLOCK,)).reshape(
            B, MOBA_HEADS, MOBA_Q_BLOCK, n_sel)
        k_own = lax.dynamic_index_in_dim(kb, cur, axis=2, keepdims=False)
        v_own = lax.dynamic_index_in_dim(vb, cur, axis=2, keepdims=False)
        s_own = jnp.einsum('bqhd,bhld->bhql', qb, k_own) * scale
        own_pos = cur * MOBA_BLOCK + jnp.arange(MOBA_BLOCK)
        m_own = jnp.broadcast_to(own_pos[None, :] <= t[:, None], (B, MOBA_HEADS, MOBA_Q_BLOCK, MOBA_BLOCK))
        p = masked_softmax(jnp.concatenate([s_sel, s_own], axis=-1),
                           jnp.concatenate([m_sel, m_own], axis=-1)).astype(v.dtype)
        o = (jnp.einsum('bhqm,bhqmd->bqhd', p[..., :n_sel],
                        vs.reshape(B, MOBA_HEADS, MOBA_Q_BLOCK, n_sel, HEAD_DIM))
             + jnp.einsum('bhql,bhld->bqhd', p[..., n_sel:], v_own))
        return o.reshape(B, MOBA_Q_BLOCK, MOBA_W)

    out = lax.map(one_block, jnp.arange(S // MOBA_Q_BLOCK))
    return out.transpose(1, 0, 2, 3).reshape(B, S, MOBA_W)


def stick_breaking_attention(q, k, v):
    B, S = q.shape[0], q.shape[1]
    scale = HEAD_DIM ** -0.5
    kpos = jnp.arange(S)

    def one_block(b):
        q0 = b * Q_BLOCK
        t = q0 + jnp.arange(Q_BLOCK)
        qb = lax.dynamic_slice_in_dim(q, q0, Q_BLOCK, axis=1)
        z = jnp.einsum('bqhd,bshd->bhqs', qb, k).astype(jnp.float32) * scale
        mask = kpos[None, :] < t[:, None]
        log_1m = jnp.where(mask, jax.nn.log_sigmoid(-z), 0.0)
        between = lax.cumsum(log_1m, axis=3, reverse=True) - log_1m
        w = jnp.where(mask, jnp.exp(jax.nn.log_sigmoid(z) + between), 0.0)
        o = jnp.einsum('bhqs,bshd->bqhd', w.astype(v.dtype), v)
        return o.reshape(B, Q_BLOCK, SB_W)

    out = lax.map(one_block, jnp.arange(S // Q_BLOCK))
    return out.transpose(1, 0, 2, 3).reshape(B, S, SB_W)


def nsa_moba_mixer(x, positions, w_in, w_out, cmp_pos_k, cmp_pos_v, cmp_w1_k, cmp_w2_k, cmp_w1_v, cmp_w2_v):
    B, S = x.shape[0], x.shape[1]
    proj = x @ w_in
    qa, kc, vc, ksl, vsl, kw, vw, ga, qm, km, vm = jnp.split(proj, EVEN_SPLITS, axis=-1)
    heads = lambda a, h: a.reshape(B, S, h, HEAD_DIM)
    qa = rope(heads(qa, NSA_HEADS), positions)
    kc = rope(heads(kc, NSA_KV_HEADS), positions)
    ksl = rope(heads(ksl, NSA_KV_HEADS), positions)
    kw = rope(heads(kw, NSA_KV_HEADS), positions)
    gates = jax.nn.sigmoid(ga.astype(jnp.float32)).astype(x.dtype).reshape(B, S, NSA_HEADS, 3)
    o_a = nsa_attention(qa, kc, heads(vc, NSA_KV_HEADS), ksl, heads(vsl, NSA_KV_HEADS),
                        kw, heads(vw, NSA_KV_HEADS), gates,
                        cmp_pos_k, cmp_pos_v, cmp_w1_k, cmp_w2_k, cmp_w1_v, cmp_w2_v)
    o_b = moba_attention(rope(heads(qm, MOBA_HEADS), positions),
                         rope(heads(km, MOBA_HEADS), positions), heads(vm, MOBA_HEADS))
    return jnp.concatenate([o_a, o_b], axis=-1) @ w_out


def stick_breaking_mixer(x, w_in, w_out):
    B, S = x.shape[0], x.shape[1]
    q, k, v = jnp.split(x @ w_in, 3, axis=-1)
    heads = lambda a: a.reshape(B, S, SB_HEADS, HEAD_DIM)
    return stick_breaking_attention(heads(q), heads(k), heads(v)) @ w_out


def hierarchical_moe(x, w_grp, b_grp, w_rt, b_rt, w_gate, w_up, w_down):
    p_grp = jax.nn.softmax((x @ w_grp + b_grp).astype(jnp.float32), axis=-1)
    g = jnp.argmax(p_grp, axis=-1)
    w_g = jnp.max(p_grp, axis=-1)
    logits_all = jnp.einsum('bsd,gde->bsge', x, w_rt) + b_rt
    logits = jnp.take_along_axis(logits_all, g[..., None, None], axis=2)[..., 0, :].astype(jnp.float32)
    top_val, top_idx = lax.top_k(logits, TOPK_IN_GROUP)
    w_e = jax.nn.softmax(top_val, axis=-1) * w_g[..., None]
    expert_id = g[..., None] * EXPERTS_PER_GROUP + top_idx
    combine = jnp.sum(jax.nn.one_hot(expert_id, N_EXPERTS, dtype=jnp.float32) * w_e[..., None], axis=2)

    def per_sequence(args):
        xs, cs = args
        h = jax.nn.silu(jnp.einsum('sd,edf->sef', xs, w_gate)) * jnp.einsum('sd,edf->sef', xs, w_up)
        return jnp.einsum('sef,efd->sd', h * cs[..., None].astype(h.dtype), w_down)

    return lax.map(per_sequence, (x, combine))


def _normal(key, shape, std):
    return jax.random.normal(key, shape, jnp.float32) * std


def setup_inputs(seed: int = 0) -> dict:
    key = jax.random.key(seed)
    ks = jax.random.split(key, 24)
    D = D_MODEL
    flat_cmp = CMP_BLOCK * HEAD_DIM
    return {
        'x': jax.random.normal(ks[0], (BATCH, SEQ, D), jnp.float32),
        'positions': jnp.broadcast_to(jnp.arange(SEQ, dtype=jnp.int32), (BATCH, SEQ)),
        'ab_w_in': _normal(ks[1], (N_EVEN, D, EVEN_IN_W), D ** -0.5),
        'ab_w_out': _normal(ks[2], (N_EVEN, EVEN_OUT_W, D), DEEPNORM_BETA * EVEN_OUT_W ** -0.5),
        'nsa_cmp_pos_k': _normal(ks[3], (N_EVEN, CMP_BLOCK, HEAD_DIM), 0.02),
        'nsa_cmp_pos_v': _normal(ks[4], (N_EVEN, CMP_BLOCK, HEAD_DIM), 0.02),
        'nsa_cmp_w1_k': _normal(ks[5], (N_EVEN, flat_cmp, CMP_HIDDEN), flat_cmp ** -0.5),
        'nsa_cmp_w2_k': _normal(ks[6], (N_EVEN, CMP_HIDDEN, HEAD_DIM), CMP_HIDDEN ** -0.5),
        'nsa_cmp_w1_v': _normal(ks[7], (N_EVEN, flat_cmp, CMP_HIDDEN), flat_cmp ** -0.5),
        'nsa_cmp_w2_v': _normal(ks[8], (N_EVEN, CMP_HIDDEN, HEAD_DIM), CMP_HIDDEN ** -0.5),
        'sb_w_in': _normal(ks[9], (N_ODD, D, 3 * SB_W), D ** -0.5),
        'sb_w_out': _normal(ks[10], (N_ODD, SB_W, D), DEEPNORM_BETA * SB_W ** -0.5),
        'ln_mix_g': 1.0 + _normal(ks[11], (DEPTH, D), 0.02),
        'ln_mix_b': _normal(ks[12], (DEPTH, D), 0.02),
        'ln_ffn_g': 1.0 + _normal(ks[13], (DEPTH, D), 0.02),
        'ln_ffn_b': _normal(ks[14], (DEPTH, D), 0.02),
        'moe_w_grp': _normal(ks[15], (DEPTH, D, N_GROUPS), D ** -0.5),
        'moe_b_grp': _normal(ks[16], (DEPTH, N_GROUPS), 0.01),
        'moe_w_rt': _normal(ks[17], (DEPTH, N_GROUPS, D, EXPERTS_PER_GROUP), D ** -0.5),
        'moe_b_rt': _normal(ks[18], (DEPTH, N_GROUPS, EXPERTS_PER_GROUP), 0.01),
        'moe_w_gate': _normal(ks[19], (DEPTH, N_EXPERTS, D, EXPERT_HIDDEN), D ** -0.5),
        'moe_w_up': _normal(ks[20], (DEPTH, N_EXPERTS, D, EXPERT_HIDDEN), D ** -0.5),
        'moe_w_down': _normal(ks[21], (DEPTH, N_EXPERTS, EXPERT_HIDDEN, D), DEEPNORM_BETA * EXPERT_HIDDEN ** -0.5),
    }


def reference(x, positions, ab_w_in, ab_w_out, nsa_cmp_pos_k, nsa_cmp_pos_v, nsa_cmp_w1_k, nsa_cmp_w2_k,
              nsa_cmp_w1_v, nsa_cmp_w2_v, sb_w_in, sb_w_out, ln_mix_g, ln_mix_b, ln_ffn_g, ln_ffn_b,
              moe_w_grp, moe_b_grp, moe_w_rt, moe_b_rt, moe_w_gate, moe_w_up, moe_w_down):
    for layer in range(DEPTH):
        i = layer // 2
        if layer % 2 == 0:
            mix = nsa_moba_mixer(x, positions, ab_w_in[i], ab_w_out[i], nsa_cmp_pos_k[i], nsa_cmp_pos_v[i],
                                 nsa_cmp_w1_k[i], nsa_cmp_w2_k[i], nsa_cmp_w1_v[i], nsa_cmp_w2_v[i])
        else:
            mix = stick_breaking_mixer(x, sb_w_in[i], sb_w_out[i])
        x = layer_norm(DEEPNORM_ALPHA * x + mix, ln_mix_g[layer], ln_mix_b[layer])
        ffn = hierarchical_moe(x, moe_w_grp[layer], moe_b_grp[layer], moe_w_rt[layer], moe_b_rt[layer],
                               moe_w_gate[layer], moe_w_up[layer], moe_w_down[layer])
        x = layer_norm(DEEPNORM_ALPHA * x + ffn, ln_ffn_g[layer], ln_ffn_b[layer])
    return x
```

```python
import math
import bisect
from contextlib import ExitStack
import numpy as np
import concourse.bass as bass
import concourse.mybir as mybir

F32 = mybir.dt.float32
BF16 = mybir.dt.bfloat16
I32 = mybir.dt.int32
AF = mybir.ActivationFunctionType
ALU = mybir.AluOpType
AX = mybir.AxisListType


class V:
    __slots__ = ("tile", "ap")

    def __init__(self, tile, ap):
        self.tile = tile
        self.ap = ap

    def __getitem__(self, idx):
        return V(self.tile, self.ap[idx])

    def bc(self, shape):
        return V(self.tile, self.ap.broadcast_to(shape))

    def unsq(self, d):
        return V(self.tile, self.ap.unsqueeze(d))

    def re(self, s, **kw):
        return V(self.tile, self.ap.rearrange(s, **kw))

    def bitcast(self, dt):
        return V(self.tile, self.ap.bitcast(dt))


class Tile:
    def __init__(self, k, h, name):
        self.k = k
        self.h = h
        self.name = name
        self.w = None
        self.r = {}
        self.excl = False

    def __getitem__(self, idx):
        return V(self, self.h[idx])

    @property
    def a(self):
        return V(self, self.h[:])


class EngState:
    def __init__(self, k, name, eng, is_compute=True):
        self.k = k
        self.name = name
        self.eng = eng
        self.sem = k.nc.alloc_semaphore("s_" + name)
        self.n = 0
        self.last = None
        self.marks_idx = []
        self.marks_val = []
        self.val = 0
        self.waited = {}
        self.nwaits = 0


class DmaSem:
    def __init__(self, k, name):
        self.sem = k.nc.alloc_semaphore("d_" + name)
        self.cnt = 0
        self.name = name


class K:
    def __init__(self, nc, same_engine_sync=True):
        self.nc = nc
        self.E = {
            "pe": EngState(self, "pe", nc.tensor),
            "act": EngState(self, "act", nc.scalar),
            "dve": EngState(self, "dve", nc.vector),
            "pool": EngState(self, "pool", nc.gpsimd),
            "sp": EngState(self, "sp", nc.sync),
        }
        self.same_engine_sync = same_engine_sync
        self.dsems = {}
        self.rings = {}
        self.ring_pos = {}
        self.RING = 16
        self.n_tiles = 0
        self.stack = ExitStack()
        self.pname = ""

    def sb(self, shape, dt, name=None):
        self.n_tiles += 1
        name = "sb_" + self.pname + (name or f"t{self.n_tiles}")
        h = self.stack.enter_context(self.nc.sbuf_tensor(name, list(shape), dt))
        return Tile(self, h, name)

    def ps(self, shape, dt=F32, name=None):
        self.n_tiles += 1
        name = name or f"p{self.n_tiles}"
        h = self.nc.alloc_psum_tensor(name, list(shape), dt)
        t = Tile(self, h, name)
        t.excl = True
        return t

    def dram(self, name, shape, dt, kind="Internal"):
        h = self.nc.dram_tensor(name, list(shape), dt, kind=kind)
        return Tile(self, h, name)

    def sub(self, v, name="sub"):
        return Tile(self, v.ap, name)

    def dsem(self, name):
        if name not in self.dsems:
            self.dsems[name] = DmaSem(self, name)
        return self.dsems[name]

    def _token_value(self, tok):
        kind, obj, idx = tok
        if kind == "dma":
            return obj.sem, idx
        es = obj
        if es.marks_idx and es.marks_idx[-1] >= idx:
            j = bisect.bisect_left(es.marks_idx, idx)
            return es.sem, es.marks_val[j]
        es.val += 1
        es.last.then_inc(es.sem, 1)
        es.marks_idx.append(es.n - 1)
        es.marks_val.append(es.val)
        return es.sem, es.val

    def _wait(self, es, tok):
        if tok is None:
            return
        kind, obj, idx = tok
        if kind == "eng" and obj is es:
            if not self.same_engine_sync or es.name == "pe":
                return
        sem, val = self._token_value(tok)
        key = id(sem) if kind == "dma" else obj.name
        if es.waited.get(key, 0) >= val:
            return
        es.waited[key] = val
        es.eng.wait_ge(sem, val)
        es.nwaits += 1

    def _deps(self, es, reads, writes):
        for v in reads:
            if v is None:
                continue
            self._wait(es, v.tile.w)
            if v.tile.excl:
                for tok in v.tile.r.values():
                    self._wait(es, tok)
        for v in writes:
            if v is None:
                continue
            t = v.tile
            self._wait(es, t.w)
            for tok in t.r.values():
                self._wait(es, tok)

    def _commit(self, tok, reads, writes, rkey):
        for v in reads:
            if v is None:
                continue
            v.tile.r[rkey] = tok
        for v in writes:
            if v is None:
                continue
            v.tile.w = tok
            v.tile.r = {}

    OUT_KEYS = ("out", "accum_out", "out_ap")

    def op(self, en, fn, **kw):
        es = self.E[en]
        reads, writes = [], []
        extra_r = kw.pop("_reads", [])
        extra_w = kw.pop("_writes", [])
        mark = kw.pop("mark", None)
        if mark is None:
            mark = en != "pe"
        args = {}
        for key, val in kw.items():
            if isinstance(val, V):
                (writes if key in self.OUT_KEYS else reads).append(val)
                args[key] = val.ap
            else:
                args[key] = val
        reads += extra_r
        writes += extra_w
        self._deps(es, reads, writes)
        inst = getattr(es.eng, fn)(**args)
        es.last = inst
        es.n += 1
        tok = ("eng", es, es.n - 1)
        if mark:
            es.val += 1
            inst.then_inc(es.sem, 1)
            es.marks_idx.append(es.n - 1)
            es.marks_val.append(es.val)
        self._commit(tok, reads, writes, en)
        return inst

    def dma(self, qn, out, in_, ds=None, **kw):
        es = self.E[qn]
        ring = self.rings.setdefault(qn, [])
        pos = self.ring_pos.get(qn, 0)
        if len(ring) < self.RING:
            ring.append(self.dsem(f"{qn}_r{len(ring)}"))
        ds = ring[pos % self.RING]
        self.ring_pos[qn] = pos + 1
        if ds.cnt:
            self._wait(es, ("dma", ds, ds.cnt))
        self._deps(es, [in_], [out])
        inst = es.eng.dma_start(out=out.ap, in_=in_.ap, **kw)
        inst.then_inc(ds.sem, 16)
        ds.cnt += 16
        tok = ("dma", ds, ds.cnt)
        self._commit(tok, [in_], [out], "dma_" + ds.name)
        return inst

    def begin_phase(self, name):
        self.stack = ExitStack()
        self.pname = name + "_"

    def barrier(self):
        targets = []
        for n in ("pe", "act", "dve", "pool"):
            es = self.E[n]
            if es.n == 0:
                continue
            if not es.marks_idx or es.marks_idx[-1] < es.n - 1:
                es.val += 1
                es.last.then_inc(es.sem, 1)
                es.marks_idx.append(es.n - 1)
                es.marks_val.append(es.val)
            targets.append((n, es.sem, es.val))
        for en, es in self.E.items():
            for (n, sem, val) in targets:
                if es.waited.get(n, 0) < val:
                    es.eng.wait_ge(sem, val)
                    es.waited[n] = val
            for ds in self.dsems.values():
                if ds.cnt and es.waited.get(id(ds.sem), 0) < ds.cnt:
                    es.eng.wait_ge(ds.sem, ds.cnt)
                    es.waited[id(ds.sem)] = ds.cnt

    def end_phase(self):
        self.barrier()
        self.stack.close()
        self.stack = ExitStack()

    def finish(self, out_tiles):
        es = self.E["sp"]
        for t in out_tiles:
            self._wait(es, t.w)
        for ds in self.dsems.values():
            if ds.cnt:
                key = id(ds.sem)
                if es.waited.get(key, 0) < ds.cnt:
                    es.eng.wait_ge(ds.sem, ds.cnt)
                    es.waited[key] = ds.cnt

    def stats(self):
        return {n: (e.n, e.nwaits, e.val) for n, e in self.E.items()}


D = 1024
ALPHA = float(4 ** 0.25)
EPS = 1e-5
NE = 16
FH = 256


class Psum:
    def __init__(self, k):
        self.k = k
        self.t = k.nc.alloc_psum_tensor("psum_all", [128, 8, 512], F32)
        self.b = [Tile(k, self.t[:, i, :], f"bank{i}") for i in range(8)]
        for t in self.b:
            t.excl = True

    def v(self, i, n=1):
        if n == 1:
            return V(self.b[i], self.t[:, i, :]), []
        ap = self.t[:, i:i + n, :].rearrange("p a b -> p (a b)")
        return V(self.b[i], ap), [self.b[j].a for j in range(i + 1, i + n)]


def layer_norm_tile(k, src, dst, g_t, b_t, eps_t, tmp, st, mv, rstd, nmr):
    for i in range(2):
        k.op("dve", "bn_stats", out=st[:, i, :], in_=src[:, i * 512:(i + 1) * 512])
    k.op("dve", "bn_aggr", out=mv.a, in_=st.a.re("p a b -> p (a b)"))
    k.op("act", "activation", out=rstd.a, in_=mv[:, 1:2], func=AF.Ln, bias=eps_t.a, scale=1.0)
    k.op("act", "activation", out=rstd.a, in_=rstd.a, func=AF.Exp, scale=-0.5)
    k.op("dve", "scalar_tensor_tensor", out=nmr.a, in0=mv[:, 0:1], scalar=-1.0, in1=rstd.a,
         op0=ALU.mult, op1=ALU.mult)
    k.op("act", "activation", out=dst, in_=src, func=AF.Identity, bias=nmr.a, scale=rstd.a)
    k.op("pool", "tensor_tensor", out=dst, in0=dst, in1=g_t.a, op=ALU.mult)
    k.op("pool", "tensor_tensor", out=dst, in0=dst, in1=b_t.a, op=ALU.add)


def emit_phase_b(k, P, NT, SG, oT, xres, xo, w_out, lnp, wr, br, w_gate, w_up, w_down, ident_d):
    nsg = NT // SG
    ntile = SG // 128
    ngrp = SG // 512
    idf = k.sb([128, 128], F32, "idf")
    k.dma("sp", idf.a, ident_d.a, "ld_c")
    woT = k.sb([128, 8, 1024], BF16, "woT")
    k.dma("pool", woT.a, V(w_out, w_out.h[:, :].rearrange("(c p) n -> p c n", p=128)), "ld_c")
    lnt = []
    for i in range(4):
        t = k.sb([128, 1024], F32, f"ln{i}")
        k.dma("sp", t.a, V(lnp, lnp.h[i].partition_broadcast(128)), "ld_c")
        lnt.append(t)
    WR = k.sb([128, 8, 20], F32, "WR")
    if True:
        k.dma("sp", WR.a, V(wr, wr.h[:, :].rearrange("(c p) n -> p c n", p=128)), "ld_c")
    BR = k.sb([128, 20], F32, "BR")
    if True:
      k.dma("sp", BR.a, V(br, br.h[:].partition_broadcast(128)), "ld_c")
    eps_t = k.sb([128, 1], F32, "eps")
    k.op("pool", "memset", ap=eps_t.a, constant=EPS, _writes=[eps_t.a])
    SELT = k.sb([16, 16, 128], F32, "SELT")
    k.op("pool", "memset", ap=SELT.a, constant=1.0, _writes=[SELT.a])
    ones16 = SELT
    if True:
      k.op("pool", "affine_select", out=SELT.a, in_=ones16.a, pattern=[[-1, 16], [0, 128]],
         compare_op=ALU.is_equal, fill=0.0, base=0, channel_multiplier=1)

    acc = [k.sb([128, 1024], F32, f"acc{i}") for i in range(ntile)]
    x1T = k.sb([128, 8, SG], BF16, "x1T")
    CT = k.sb([16, SG], F32, "CT")
    oTt = [k.sb([128, 8, 128], BF16, f"oTt{i}") for i in range(2)]
    xrt = [k.sb([128, 1024], F32, f"xrt{i}") for i in range(2)]
    yt = k.sb([128, 1024], F32, "yt")
    tmp = yt
    x1t = k.sb([128, 1024], F32, "x1t")
    x1Tf = k.sb([128, 8, 128], F32, "x1Tf")
    st = k.sb([128, 2, 6], F32, "st")
    mv = k.sb([128, 2], F32, "mv")
    rstd = k.sb([128, 1], F32, "rstd")
    nmr = k.sb([128, 1], F32, "nmr")
    L = k.sb([128, 20], F32, "L")
    sm = k.sb([128, 16], F32, "sm")
    r4 = k.sb([128, 8], F32, "r4")
    r16 = [k.sb([128, 16], F32, f"r16_{i}") for i in range(4)]
    comb = k.sb([128, 16], F32, "comb")
    wgb = [k.sb([128, 8, FH], BF16, f"wgb{i}") for i in range(2)]
    wub = [k.sb([128, 8, FH], BF16, f"wub{i}") for i in range(2)]
    wdb = [k.sb([128, 2, 1024], BF16, f"wdb{i}") for i in range(2)]
    hT = [k.sb([128, 2, 512], BF16, f"hT{i}") for i in range(2)]
    sgt = [k.sb([128, 512], F32, f"sgt{i}") for i in range(2)]
    tt_ = sgt
    ot = xrt

    oT_v = oT.h[:, :].rearrange("(c p) t -> p c t", p=128)
    ld_i = 0
    for sg in range(nsg):
        for ti in range(ntile if 9.0 >= 1 else 0):
            tok0 = sg * SG + ti * 128
            o_t = oTt[ld_i % 2]
            x_t = xrt[ld_i % 2]
            ld_i += 1
            k.dma("sp", o_t.a, V(oT, oT_v[:, :, tok0:tok0 + 128]), f"ld_o{ld_i % 2}")
            k.dma("sp", x_t.a, V(xres, xres.h[tok0:tok0 + 128, :]), f"ld_x{ld_i % 2}")
            pm, pm_x = P.v(0, 2)
            for half in range(2):
                for c in range(8):
                    k.op("pe", "matmul", out=pm[:, half * 512:(half + 1) * 512], lhsT=o_t[:, c, :],
                         rhs=woT[:, c, half * 512:(half + 1) * 512], start=(c == 0), stop=(c == 7),
                         _writes=pm_x, mark=(c == 7 and half == 1))
            k.op("dve", "scalar_tensor_tensor", out=yt.a, in0=x_t.a, scalar=ALPHA, in1=pm, op0=ALU.mult,
                 op1=ALU.add, _reads=pm_x)
            if 9.0 < 1.2:
                continue
            layer_norm_tile(k, yt.a, x1t.a, lnt[0], lnt[1], eps_t, tmp.a, st, mv, rstd, nmr)
            k.op("act", "mul", out=acc[ti].a, in_=x1t.a, mul=ALPHA)
            if 9.0 < 1.4:
                continue
            pT, pT_x = P.v(2, 2)
            for c in range(8):
                k.op("pe", "transpose", out=pT[:, c * 128:(c + 1) * 128], in_=x1t[:, c * 128:(c + 1) * 128],
                     identity=idf.a, _writes=pT_x, mark=(c == 7))
            if True:
                k.op("act", "copy", out=x1Tf.a.re("p c t -> p (c t)"), in_=pT, _reads=pT_x)
            if True:
                k.op("dve", "tensor_copy", out=x1T[:, :, ti * 128:(ti + 1) * 128],
                     in_=pT.re("p (c t) -> p c t", c=8), _reads=pT_x)
            if 9.0 < 2:
                continue
            pr, _ = P.v(7)
            for c in range(8):
                k.op("pe", "matmul", out=pr[:, 0:20], lhsT=x1Tf[:, c, :], rhs=WR[:, c, :], start=(c == 0),
                     stop=(c == 7), mark=(c == 7))
            k.op("dve", "tensor_tensor", out=L.a, in0=pr[:, 0:20], in1=BR.a, op=ALU.add)
            gmax, ngmax, sumg, wg_, m1, nm1, m2, ssum, coef = [sm[:, i:i + 1] for i in range(9)]
            k.op("dve", "reduce_max", out=gmax, in_=L[:, 0:4], axis=AX.X)
            k.op("dve", "tensor_scalar", out=ngmax, in0=gmax, scalar1=-1.0, scalar2=None, op0=ALU.mult)
            k.op("act", "activation", out=r4[:, 0:4], in_=L[:, 0:4], func=AF.Exp, bias=ngmax, scale=1.0,
                 accum_out=sumg)
            k.op("dve", "reciprocal", out=wg_, in_=sumg)
            k.op("dve", "tensor_scalar", out=r4[:, 4:8], in0=L[:, 0:4], scalar1=gmax, scalar2=None,
                 op0=ALU.is_equal)
            k.op("dve", "tensor_scalar", out=r4[:, 4:8], in0=r4[:, 4:8], scalar1=-1.0, scalar2=1e30,
                 op0=ALU.add, op1=ALU.mult)
            lm = r16[0]
            k.op("dve", "tensor_tensor", out=lm.a.re("p (g e) -> p g e", g=4),
                 in0=L[:, 4:20].re("p (g e) -> p g e", g=4), in1=r4[:, 4:8].unsq(2).bc([128, 4, 4]),
                 op=ALU.add)
            k.op("dve", "reduce_max", out=m1, in_=lm.a, axis=AX.X)
            k.op("dve", "tensor_scalar", out=r16[1].a, in0=lm.a, scalar1=m1, scalar2=None, op0=ALU.is_equal)
            k.op("dve", "scalar_tensor_tensor", out=r16[1].a, in0=r16[1].a, scalar=-1e30, in1=lm.a,
                 op0=ALU.mult, op1=ALU.add)
            k.op("dve", "reduce_max", out=m2, in_=r16[1].a, axis=AX.X)
            k.op("dve", "tensor_scalar", out=r16[2].a, in0=lm.a, scalar1=m2, scalar2=None, op0=ALU.is_ge)
            k.op("dve", "tensor_scalar", out=nm1, in0=m1, scalar1=-1.0, scalar2=None, op0=ALU.mult)
            k.op("act", "activation", out=r16[3].a, in_=lm.a, func=AF.Exp, bias=nm1, scale=1.0)
            k.op("dve", "tensor_tensor", out=r16[3].a, in0=r16[3].a, in1=r16[2].a, op=ALU.mult)
            k.op("dve", "reduce_sum", out=ssum, in_=r16[3].a, axis=AX.X)
            k.op("dve", "reciprocal", out=ssum, in_=ssum)
            k.op("dve", "tensor_tensor", out=coef, in0=ssum, in1=wg_, op=ALU.mult)
            k.op("dve", "tensor_scalar", out=comb.a, in0=r16[3].a, scalar1=coef, scalar2=None, op0=ALU.mult)
            pc, _ = P.v(6)
            k.op("pe", "transpose", out=pc[0:16, 0:128], in_=comb.a, identity=idf.a, mark=True)
            k.op("act", "copy", out=CT[:, ti * 128:(ti + 1) * 128], in_=pc[0:16, 0:128])
        for e in range(NE if 9.0 >= 3 else 0):
            s = e % 2
            k.dma("pool", wgb[s].a, V(w_gate, w_gate.h[e].rearrange("(c p) f -> p c f", p=128)), f"ld_wg{s}")
            k.dma("pool", wub[s].a, V(w_up, w_up.h[e].rearrange("(c p) f -> p c f", p=128)), f"ld_wu{s}")
            k.dma("pool", wdb[s].a, V(w_down, w_down.h[e].rearrange("(c p) n -> p c n", p=128)), f"ld_wd{s}")
            for grp in range(ngrp):
                tc0 = grp * 512
                h_t = hT[(e * ngrp + grp) % 2]
                pcb, _ = P.v(6)
                k.op("pe", "matmul", out=pcb, lhsT=SELT[:, e, :], rhs=CT[:, tc0:tc0 + 512], start=True, stop=True,
                     mark=True)
                for f in range(2):
                    pg, _ = P.v(2 + 2 * f)
                    pu, _ = P.v(3 + 2 * f)
                    for c in range(8):
                        k.op("pe", "matmul", out=pg, lhsT=wgb[s][:, c, f * 128:(f + 1) * 128],
                             rhs=x1T[:, c, tc0:tc0 + 512], start=(c == 0), stop=(c == 7), mark=(c == 7))
                    for c in range(8):
                        k.op("pe", "matmul", out=pu, lhsT=wub[s][:, c, f * 128:(f + 1) * 128],
                             rhs=x1T[:, c, tc0:tc0 + 512], start=(c == 0), stop=(c == 7), mark=(c == 7))
                    k.op("act", "activation", out=sgt[f].a, in_=pg, func=AF.Silu)
                    k.op("dve", "tensor_tensor", out=tt_[f].a, in0=sgt[f].a, in1=pu, op=ALU.mult)
                    k.op("dve", "tensor_tensor", out=h_t[:, f, :], in0=tt_[f].a, in1=pcb, op=ALU.mult)
                for t4 in range(4):
                    ti = grp * 4 + t4
                    pd, pd_x = P.v(0, 2)
                    for half in range(2):
                        for f in range(2):
                            k.op("pe", "matmul", out=pd[:, half * 512:(half + 1) * 512],
                                 lhsT=h_t[:, f, t4 * 128:(t4 + 1) * 128],
                                 rhs=wdb[s][:, f, half * 512:(half + 1) * 512], start=(f == 0), stop=(f == 1),
                                 _writes=pd_x, mark=(f == 1 and half == 1))
                    k.op("dve", "tensor_tensor", out=acc[ti].a, in0=acc[ti].a, in1=pd, op=ALU.add, _reads=pd_x)
        for ti in range(ntile):
            tok0 = sg * SG + ti * 128
            o_ = ot[ti % 2]
            layer_norm_tile(k, acc[ti].a, o_.a, lnt[2], lnt[3], eps_t, tmp.a, st, mv, rstd, nmr)
            dst_ap = xo.h[tok0:tok0 + 128, :]
            k.dma("sp", V(Tile(k, dst_ap, "xo_part"), dst_ap), o_.a)


def load_xT_group(k, P, x_d, tok0, ntok, xin, xbf, xT, idb, bank):
    for t in range(ntok // 128):
        xb = xbf[t % 2]
        k.dma("pool", xb.a, V(x_d, x_d.h[tok0 + t * 128: tok0 + (t + 1) * 128, :]))
        pt, _ = P.v(bank + (t % 2))
        ptb = pt.bitcast(BF16)
        for c in range(8):
            k.op("pe", "transpose", out=ptb[:, c * 128:(c + 1) * 128], in_=xb[:, c * 128:(c + 1) * 128],
                 identity=idb.a, mark=(c == 7))
        k.op("dve", "tensor_copy", out=xT[:, :, t * 128:(t + 1) * 128], in_=ptb.re("p (c t) -> p c t", c=8))


def emit_phase_sb(k, P, S, x_d, w_d, oT_d, ident_d, nsub=2, row0=0):
    NTILE = S // 128
    NQB = S // 512
    idf = k.sb([128, 128], F32, "idf")
    k.dma("sp", idf.a, ident_d.a, "ld_c")
    idb = k.sb([128, 128], BF16, "idb")
    k.op("dve", "tensor_copy", out=idb.a, in_=idf.a)
    negones = k.sb([128, 128], BF16, "negones")
    k.op("pool", "memset", ap=negones.a, constant=-1.0, _writes=[negones.a])
    NTRI = k.sb([128, 128], BF16, "NTRI")
    k.op("pool", "affine_select", out=NTRI.a, in_=negones.a, pattern=[[-1, 128]], compare_op=ALU.is_ge,
         fill=0.0, base=0, channel_multiplier=1)
    LSTR = k.sb([128, 128], BF16, "LSTR")
    k.op("pool", "affine_select", out=LSTR.a, in_=negones.a, pattern=[[1, 128]], compare_op=ALU.is_gt,
         fill=0.0, base=0, channel_multiplier=-1)

    W = k.sb([128, 8, 768], BF16, "Wsb")
    QT = [k.sb([128, S], BF16, f"QT{i}") for i in range(2)]
    KT = [k.sb([128, S], BF16, f"KT{i}") for i in range(2)]
    Vt = k.sb([128, NTILE, 256], BF16, "Vt")
    Oa = k.sb([128, NTILE, 256], BF16, "Oa")
    xin = None
    xbf = [k.sb([128, 1024], BF16, f"xbf{i}") for i in range(2)]
    xT = [k.sb([128, 8, 512], BF16, f"xT{i}") for i in range(2)]
    NS = 2
    e_t = [k.sb([128, 512], F32, f"e{i}") for i in range(NS)]
    p_t = [[k.sb([128, 512], BF16, f"p{i}_{j}") for j in range(2)] for i in range(NS)]
    a_t = [k.sb([128, 512], BF16, f"a{i}") for i in range(NS)]
    w_t = [k.sb([128, 512], BF16, f"w{i}") for i in range(NS)]
    oTs = [k.sb([128, 2, 128], BF16, f"oTs{i}") for i in range(2)]

    wv = w_d.h[:, :].rearrange("(c p) n -> p c n", p=128)
    for sub in range(nsub):
        for j, base in enumerate((0, 512, 1024)):
            k.dma("pool", W[:, :, j * 256:(j + 1) * 256],
                  V(w_d, wv[:, :, base + sub * 256: base + (sub + 1) * 256]), "ld_w")
        for g4 in range(S // 512):
            tok0 = g4 * 512
            xT_g = xT[g4 % 2]
            load_xT_group(k, P, x_d, tok0, 512, xin, xbf, xT_g, idb, 0)
            for j in range(4):
                pq, _ = P.v(2 + (j % 2))
                for c in range(8):
                    k.op("pe", "matmul", out=pq, lhsT=W[:, c, j * 128:(j + 1) * 128], rhs=xT_g[:, c, :],
                         start=(c == 0), stop=(c == 7), mark=(c == 7))
                dst = (QT if j < 2 else KT)[j % 2]
                k.op("act", "copy", out=dst[:, tok0:tok0 + 512], in_=pq)
            for t in range(4):
                pv, _ = P.v(4 + (t % 2))
                for c in range(8):
                    k.op("pe", "matmul", out=pv[:, 0:256], lhsT=xT_g[:, c, t * 128:(t + 1) * 128],
                         rhs=W[:, c, 512:768], start=(c == 0), stop=(c == 7), mark=(c == 7))
                k.op("dve", "tensor_copy", out=Vt[:, g4 * 4 + t, :], in_=pv[:, 0:256])
        for qb in range(NQB):
            for hp in range(2):
                streams = []
                for s_ in range(NS):
                    h = hp * 2 + s_
                    streams.append(dict(h=h, qt=QT[h // 2], kt=KT[h // 2], r0=(h % 2) * 64,
                                        zb=P.v(0 + s_)[0], lab=P.v(2 + s_)[0], ob=P.v(4 + s_)[0],
                                        e=e_t[s_], a=a_t[s_], w=w_t[s_], p=p_t[s_], first=True, prev=None))
                chunks = [(4 * qb + j, 128 * j) for j in (3, 2, 1, 0)] + [(kc, 0) for kc in range(4 * qb - 1, -1, -1)]
                for ci, (kc, c0) in enumerate(chunks):
                    n = 512 - c0
                    for st in streams:
                        r0 = st["r0"]
                        zb, lab, ob = st["zb"], st["lab"], st["ob"]
                        p_cur = st["p"][ci % 2]
                        k.op("pe", "matmul", out=zb[:, c0:512], lhsT=st["kt"][r0:r0 + 64, kc * 128:(kc + 1) * 128],
                             rhs=st["qt"][r0:r0 + 64, qb * 512 + c0:(qb + 1) * 512], start=True, stop=True, mark=True)
                        k.op("act", "activation", out=st["e"][:, c0:512], in_=zb[:, c0:512], func=AF.Exp, scale=0.125)
                        if c0 > 0 or kc >= 4 * qb:
                            k.op("pool", "affine_select", out=st["e"][:, c0:c0 + 128], in_=st["e"][:, c0:c0 + 128],
                                 pattern=[[1, 128]], compare_op=ALU.is_gt, fill=0.0, base=0, channel_multiplier=-1)
                        k.op("act", "activation", out=p_cur[:, c0:512], in_=st["e"][:, c0:512], func=AF.Ln, bias=1.0,
                             scale=1.0)
                        if st["prev"] is not None:
                            pp, pc0 = st["prev"]
                            k.op("pe", "matmul", out=lab[:, pc0:512], lhsT=LSTR.a, rhs=pp[:, pc0:512], start=False,
                                 stop=False, skip_group_check=True)
                        k.op("pe", "matmul", out=lab[:, c0:512], lhsT=NTRI.a, rhs=p_cur[:, c0:512],
                             start=st["first"], stop=True, skip_group_check=True, mark=True)
                        k.op("act", "activation", out=st["a"][:, c0:512], in_=lab[:, c0:512], func=AF.Exp)
                        k.op("dve", "tensor_tensor", out=st["w"][:, c0:512], in0=st["a"][:, c0:512],
                             in1=st["e"][:, c0:512], op=ALU.mult)
                        for i in range(c0 // 128, 4):
                            k.op("pe", "matmul", out=ob[:, i * 64:(i + 1) * 64], lhsT=st["w"][:, i * 128:(i + 1) * 128],
                                 rhs=Vt[:, kc, st["h"] * 64:(st["h"] + 1) * 64], start=(st["first"] and i == c0 // 128),
                                 stop=True, skip_group_check=True, mark=(i == 3))
                        st["first"] = False
                        st["prev"] = (p_cur, c0)
                for st in streams:
                    h = st["h"]
                    k.op("act", "copy", out=Oa[:, qb * 4:(qb + 1) * 4, h * 64:(h + 1) * 64],
                         in_=st["ob"][:, 0:256].re("p (i d) -> p i d", i=4))
        for t in range(NTILE):
            pt, _ = P.v(6 + (t % 2))
            ptb = pt.bitcast(BF16)
            for c in range(2):
                k.op("pe", "transpose", out=ptb[:, c * 128:(c + 1) * 128], in_=Oa[:, t, c * 128:(c + 1) * 128],
                     identity=idb.a, mark=(c == 1))
            o_ = oTs[t % 2]
            k.op("dve", "tensor_copy", out=o_.a, in_=ptb[:, 0:256].re("p (c t) -> p c t", c=2))
            dst_ap = oT_d.h[row0 + sub * 256:row0 + (sub + 1) * 256, t * 128:(t + 1) * 128].rearrange("(c p) t -> p c t", p=128)
            k.dma("sp", V(Tile(k, dst_ap, "oT_part"), dst_ap), o_.a)


import math

SCALE = 0.125
NEG = -30000.0


def run_streams(gens, nslot):
    pending = list(gens)
    active = [None] * nslot
    while True:
        progressed = False
        for s in range(nslot):
            if active[s] is None and pending:
                active[s] = pending.pop(0)(s)
            if active[s] is not None:
                try:
                    next(active[s])
                except StopIteration:
                    active[s] = None
                progressed = True
        if not progressed and not pending:
            break


def flash_stream(k, slot, zb, ob, e_tiles, qv_fn, chunks, nv, negM, epilogue):
    first = True
    for ci, ch in enumerate(chunks):
        c0, c1 = ch["c0"], ch["c1"]
        e = e_tiles[ci % 2]
        has_mask = ch.get("mask") is not None
        k.op("pe", "matmul", out=zb[:, c0:c1], lhsT=ch["kT"], rhs=qv_fn(c0, c1), start=True, stop=not has_mask,
             mark=not has_mask, skip_group_check=True)
        if has_mask:
            ml, mr = ch["mask"]
            k.op("pe", "matmul", out=zb[:, c0:c1], lhsT=ml, rhs=mr(c0, c1), start=False, stop=True, mark=True,
                 skip_group_check=True)
        yield
        k.op("act", "activation", out=e[:, c0:c1], in_=zb[:, c0:c1], func=AF.Exp, scale=SCALE, bias=negM)
        for (lo, hi, base, cm, step, op) in ch.get("aff", []):
            k.op("pool", "affine_select", out=e[:, lo:hi], in_=e[:, lo:hi], pattern=[[step, hi - lo]],
                 compare_op=op, fill=0.0, base=base, channel_multiplier=cm)
        yield
        subs = list(range(c0 // 128, (c1 + 127) // 128))
        for i in subs:
            k.op("pe", "matmul", out=ob[:, i * nv:(i + 1) * nv], lhsT=e[:, i * 128:(i + 1) * 128], rhs=ch["v"],
                 start=first, stop=True, skip_group_check=True, mark=(i == subs[-1]))
            first = False
        yield
    epilogue()
    yield


def rope_group(k, pos_t, invf, g4, tmps):
    posf, y, yy, ki, kf, outs = tmps
    k.op("dve", "tensor_copy", out=posf.a, in_=pos_t[:, g4 * 4:(g4 + 1) * 4])
    k.op("dve", "tensor_tensor", out=y.a, in0=posf.a.unsq(2).bc([128, 4, 32]),
         in1=invf.a.unsq(1).bc([128, 4, 32]), op=ALU.mult)
    res = []
    for j, shift in enumerate((0.0, 0.25)):
        k.op("dve", "tensor_scalar", out=yy.a, in0=y.a, scalar1=shift, scalar2=None, op0=ALU.add)
        k.op("dve", "tensor_copy", out=ki.a, in_=yy.a)
        k.op("dve", "tensor_copy", out=kf.a, in_=ki.a)
        k.op("dve", "tensor_tensor", out=yy.a, in0=yy.a, in1=kf.a, op=ALU.subtract)
        k.op("dve", "tensor_scalar", out=kf.a, in0=yy.a, scalar1=0.5, scalar2=None, op0=ALU.is_gt)
        k.op("dve", "tensor_tensor", out=yy.a, in0=yy.a, in1=kf.a, op=ALU.subtract)
        k.op("dve", "tensor_scalar", out=kf.a, in0=yy.a, scalar1=-0.5, scalar2=None, op0=ALU.is_lt)
        k.op("dve", "tensor_tensor", out=yy.a, in0=yy.a, in1=kf.a, op=ALU.add)
        t = outs[g4 % 2][j]
        k.op("act", "activation", out=t.a, in_=yy.a, func=AF.Sin, scale=2.0 * math.pi * (1 - 1e-6))
        res.append(t)
    return res[1], res[0]


def rope_tmps(k):
    posf = k.sb([128, 4], F32, "posf")
    y = k.sb([128, 4, 32], F32, "rope_y")
    yy = k.sb([128, 4, 32], F32, "rope_yy")
    ki = k.sb([128, 4, 32], I32, "rope_ki")
    kf = k.sb([128, 4, 32], F32, "rope_kf")
    outs = [[k.sb([128, 4, 32], F32, f"rope_o{i}_{j}") for j in range(2)] for i in range(2)]
    return (posf, y, yy, ki, kf, outs)


def rope_apply(k, pf, nh, cos, sin, t1, t2, out_bf):
    pr = pf.re("p (h t d) -> p h t d", h=nh, t=2)
    t1v = t1[:, 0:nh * 64].re("p (h t d) -> p h t d", h=nh, t=2)
    t2v = t2[:, 0:nh * 64].re("p (h t d) -> p h t d", h=nh, t=2)
    ob = out_bf.re("p (h t d) -> p h t d", h=nh, t=2)
    cb = cos.unsq(1).unsq(1).bc([128, nh, 2, 32])
    sb_ = sin.unsq(1).bc([128, nh, 32])
    k.op("dve", "tensor_tensor", out=t1v, in0=pr, in1=cb, op=ALU.mult)
    k.op("pool", "tensor_tensor", out=t2v[:, :, 0, :], in0=pr[:, :, 1, :], in1=sb_, op=ALU.mult)
    k.op("pool", "tensor_tensor", out=t2v[:, :, 1, :], in0=pr[:, :, 0, :], in1=sb_, op=ALU.mult)
    k.op("dve", "tensor_tensor", out=ob[:, :, 0, :], in0=t1v[:, :, 0, :], in1=t2v[:, :, 0, :], op=ALU.subtract)
    k.op("dve", "tensor_tensor", out=ob[:, :, 1, :], in0=t1v[:, :, 1, :], in1=t2v[:, :, 1, :], op=ALU.add)


def bound_negM(k, P, nq_t, nk_t, idf, ones1, out_negM, bank, scratch):
    mq, mm, row = scratch
    k.op("dve", "reduce_max", out=mq[:, 0:1], in_=nq_t, axis=AX.X)
    k.op("dve", "reduce_max", out=mq[:, 1:2], in_=nk_t, axis=AX.X)
    pb, _ = P.v(bank)
    k.op("pe", "transpose", out=pb[0:2, 0:128], in_=mq.a, identity=idf.a, mark=True)
    k.op("dve", "reduce_max", out=mm.a, in_=pb[0:2, 0:128], axis=AX.X)
    k.op("pe", "transpose", out=pb[0:1, 128:130], in_=mm.a, identity=idf[0:2, 0:2], mark=True)
    k.op("dve", "tensor_copy", out=row[:, 0:2], in_=pb[0:1, 128:130])
    k.op("dve", "tensor_tensor", out=row[:, 2:3], in0=row[:, 0:1], in1=row[:, 1:2], op=ALU.mult)
    k.op("act", "activation", out=row[:, 2:3], in_=row[:, 2:3], func=AF.Ln)
    k.op("act", "activation", out=row[:, 2:3], in_=row[:, 2:3], func=AF.Exp, scale=0.5)
    k.op("dve", "tensor_scalar", out=row[:, 3:4], in0=row[:, 2:3], scalar1=-SCALE * 1.02, scalar2=None, op0=ALU.mult)
    k.op("pe", "matmul", out=pb[:, 132:133], lhsT=ones1.a, rhs=row[:, 3:4], start=True, stop=True, mark=True)
    k.op("dve", "tensor_copy", out=out_negM, in_=pb[:, 132:133])


def emit_out_T(k, P, OUT_bf, oT_d, row0, tok0, idb, oTs, bank, idx):
    dst_ap = oT_d.h[row0:row0 + 256, tok0:tok0 + 128].rearrange("(c p) t -> p c t", p=128)
    pt, _ = P.v(bank)
    ptb = pt.bitcast(BF16)
    for c in range(2):
        k.op("pe", "transpose", out=ptb[:, c * 128:(c + 1) * 128], in_=OUT_bf[:, c * 128:(c + 1) * 128],
             identity=idb.a, mark=(c == 1))
    o_ = oTs[idx % 2]
    k.op("dve", "tensor_copy", out=o_.a, in_=ptb[:, 0:256].re("p (c t) -> p c t", c=2))
    k.dma("sp", V(Tile(k, dst_ap, "oT_part"), dst_ap), o_.a)


def emit_phase_l0(k, P, S, x_d, pos_d, wn_d, wm_d, cw, consts, oT_d, do_nsa=True, do_moba=True, row_nsa=0, row_moba=256):
    NTILE = S // 128
    NQB = S // 512
    NCMP = (S - 32) // 16 + 1
    NCC = (NCMP + 127) // 128
    NSLC = S // 64
    NBLK = S // 256
    idf = k.sb([128, 128], F32, "idf")
    k.dma("sp", idf.a, consts["ident"].a, "ld_c")
    idb = k.sb([128, 128], BF16, "idb")
    k.op("dve", "tensor_copy", out=idb.a, in_=idf.a)
    ones1 = k.sb([1, 128], F32, "ones1")
    k.op("pool", "memset", ap=ones1.a, constant=1.0, _writes=[ones1.a])
    invf = k.sb([128, 32], F32, "invf")
    k.dma("sp", invf.a, consts["invf"].a, "ld_c")
    pos_t = k.sb([128, NTILE], I32, "pos_t")
    k.dma("sp", pos_t.a, pos_d.a, "ld_c")
    rtm = rope_tmps(k)
    xin = None
    xbf = [k.sb([128, 1024], BF16, f"xbf{i}") for i in range(2)]
    xT = [k.sb([128, 8, 512], BF16, "xT0")] * 2
    pf = k.sb([128, 780], F32, "pf")
    t1 = k.sb([128, 576], F32, "rt1")
    t2 = k.sb([128, 576], F32, "rt2")
    Rb = k.sb([128, 640], BF16, "Rb")
    sqj = k.sb([128, 640], F32, "sqj")
    e_t = [[k.sb([128, 512], BF16, f"e{s}_{j}") for j in range(2)] for s in range(4)]
    oTs = [k.sb([128, 2, 128], BF16, f"oTs{i}") for i in range(2)]
    OUT = k.sb([128, 4, 256], F32, "OUT")
    OUTb = k.sb([128, 4, 256], BF16, "OUTb")
    negM = [k.sb([128, 1], F32, f"negM{i}") for i in range(4)]
    sc_mq = k.sb([128, 2], F32, "sc_mq")
    sc_mk = k.sb([2, 1], F32, "sc_mk")
    sc_row = k.sb([1, 4], F32, "sc_row")
    rz = k.sb([128, 4 * 4], F32, "rz")
    coef = k.sb([128, 4 * 4], F32, "coef")
    QT = [k.sb([128, S], BF16, f"QT{i}") for i in range(2)]
    KT = [k.sb([128, S], BF16, f"KT{i}") for i in range(2)]
    CT = k.sb([128, S], BF16, "CT")
    Vall = k.sb([128, NTILE, 260], BF16, "Vall")
    k.op("pool", "memset", ap=Vall.a, constant=1.0, _writes=[Vall.a])
    Wall = k.sb([128, 8, 780], BF16, "Wall")

    if do_nsa:
        W = Wall
        k.dma("pool", W.a, V(wn_d, wn_d.h[:, :].rearrange("(c p) n -> p c n", p=128)), "ld_w")
        VS = V(Vall, Vall.h[:, :, 0:130].rearrange("p t (a d) -> p t a d", a=2))
        GL = k.sb([128, NTILE, 12], F32, "GL")
        NQ = k.sb([128, 3, NTILE], F32, "NQ")
        for g4 in range(S // 512):
            xT_g = xT[g4 % 2]
            load_xT_group(k, P, x_d, g4 * 512, 512, xin, xbf, xT_g, idb, 0)
            COS4, SIN4 = rope_group(k, pos_t, invf, g4, rtm)
            for t in range(4):
                ti = g4 * 4 + t
                pp, pp_x = P.v(2 + 2 * (t % 2), 2)
                for (a, b) in ((0, 512), (512, 780)):
                    for c in range(8):
                        k.op("pe", "matmul", out=pp[:, a:b], lhsT=xT_g[:, c, t * 128:(t + 1) * 128], rhs=W[:, c, a:b],
                             start=(c == 0), stop=(c == 7), _writes=pp_x, mark=(c == 7 and a == 512))
                k.op("act", "copy", out=pf[:, 0:780], in_=pp[:, 0:780], _reads=pp_x)
                rope_apply(k, pf[:, 0:576], 9, COS4[:, t, :], SIN4[:, t, :], t1, t2, Rb[:, 0:576])
                k.op("pool", "tensor_copy", out=Rb[:, 576:640], in_=pf[:, 576:640])
                k.op("pool", "tensor_copy", out=VS[:, ti, :, 0:64], in_=pf[:, 640:768].re("p (a d) -> p a d", a=2))
                k.op("pool", "tensor_copy", out=GL[:, ti, :], in_=pf[:, 768:780])
                for j, (a, b) in enumerate(((0, 256), (320, 448), (512, 576))):
                    k.op("act", "activation", out=sqj[:, a:b], in_=Rb[:, a:b], func=AF.Square, accum_out=NQ[:, j, ti:ti + 1])
                pt, _ = P.v(6 + (t % 2))
                ptb = pt.bitcast(BF16)
                for j in range(5):
                    k.op("pe", "transpose", out=ptb[:, j * 128:(j + 1) * 128], in_=Rb[:, j * 128:(j + 1) * 128],
                         identity=idb.a, mark=(j == 4))
                for j, dst in enumerate((QT[0], QT[1], KT[0], KT[1], CT)):
                    k.op("act" if j % 2 else "dve", "copy" if j % 2 else "tensor_copy", out=dst[:, ti * 128:(ti + 1) * 128],
                         in_=ptb[:, j * 128:(j + 1) * 128])
        G = GL
        k.op("act", "activation", out=G.a, in_=GL.a, func=AF.Sigmoid)
        W1 = k.sb([128, 32, 128], BF16, "W1")
        k.dma("pool", W1[0:64], V(cw["w1k"], cw["w1k"].h[:, :].rearrange("(l d) h -> d l h", d=64)), "ld_w")
        k.dma("pool", W1[64:128], V(cw["w1v"], cw["w1v"].h[:, :].rearrange("(l d) h -> d l h", d=64)), "ld_w")
        PF = k.sb([128, 32], BF16, "PF")
        k.dma("pool", PF.a, cw["posT"].a)
        W2 = [k.sb([128, 128], BF16, "W2k2"), k.sb([128, 64], BF16, "W2v")]
        k.dma("pool", W2[0][:, 0:64], cw["w2k"].a, "ld_w")
        k.dma("pool", W2[0][:, 64:128], cw["w2k"].a, "ld_w")
        k.dma("pool", W2[1].a, cw["w2v"].a, "ld_w")
        KCT = k.sb([128, NCC * 128], BF16, "KCT")
        k.op("pool", "memset", ap=KCT.a, constant=0.0, _writes=[KCT.a])
        NV = 193
        VCA = k.sb([128, NCC, NV], BF16, "VCA")
        k.op("pool", "memset", ap=VCA.a, constant=1.0, _writes=[VCA.a])
        for c in range(NCC):
            k.op("pool", "affine_select", out=VCA[:, c, 65:193], in_=VCA[:, c, 65:193], pattern=[[-4, 128]],
                 compare_op=ALU.is_ge, fill=0.0, base=128 * c + 1, channel_multiplier=1)
            k.op("pool", "affine_select", out=VCA[:, c, 65:193], in_=VCA[:, c, 65:193], pattern=[[4, 128]],
                 compare_op=ALU.is_ge, fill=0.0, base=3 - 128 * c, channel_multiplier=-1)
        cb = k.sb([128, 2], F32, "cbias")
        gu = [k.sb([128, 512], F32, f"gu{i}") for i in range(3)]
        gact = [k.sb([128, 512], BF16, f"gact{i}") for i in range(2)]
        k.op("pool", "memset", ap=gact[1].a, constant=0.0, _writes=[gact[1].a])
        for kv in range(2):
            r0 = kv * 64
            ph, _ = P.v(0 + kv)
            for l in range(32):
                k.op("pe", "matmul", out=ph[:, 0:NCMP], lhsT=W1[r0:r0 + 64, l, :],
                     rhs=CT[r0:r0 + 64, l:l + 16 * (NCMP - 1) + 1:16], start=(l == 0), stop=(l == 31), mark=(l == 31))
            pbias, _ = P.v(2 + kv)
            for l in range(32):
                k.op("pe", "matmul", out=pbias[:, 0:1], lhsT=W1[r0:r0 + 64, l, :], rhs=PF[r0:r0 + 64, l:l + 1], start=(l == 0),
                     stop=(l == 31), mark=(l == 31))
            k.op("dve", "tensor_copy", out=cb[:, kv:kv + 1], in_=pbias[:, 0:1])
            u, u2, th = gu
            n_ = NCMP
            k.op("dve", "tensor_scalar", out=u[:, 0:n_], in0=ph[:, 0:n_], scalar1=cb[:, kv:kv + 1], scalar2=None, op0=ALU.add)
            k.op("dve", "tensor_tensor", out=u2[:, 0:n_], in0=u[:, 0:n_], in1=u[:, 0:n_], op=ALU.mult)
            k.op("dve", "tensor_scalar", out=u2[:, 0:n_], in0=u2[:, 0:n_], scalar1=0.044715, scalar2=1.0, op0=ALU.mult, op1=ALU.add)
            k.op("dve", "tensor_tensor", out=u2[:, 0:n_], in0=u2[:, 0:n_], in1=u[:, 0:n_], op=ALU.mult)
            k.op("act", "activation", out=th[:, 0:n_], in_=u2[:, 0:n_], func=AF.Tanh, scale=0.7978845608028654)
            k.op("dve", "tensor_scalar", out=th[:, 0:n_], in0=th[:, 0:n_], scalar1=0.5, scalar2=0.5, op0=ALU.mult, op1=ALU.add)
            k.op("dve", "tensor_tensor", out=gact[kv][:, 0:n_], in0=th[:, 0:n_], in1=u[:, 0:n_], op=ALU.mult)
            if kv == 0:
                pk, _ = P.v(4)
                k.op("pe", "matmul", out=pk[:, 0:n_], lhsT=W2[0].a, rhs=gact[0][:, 0:n_], start=True, stop=True, mark=True)
                k.op("dve", "tensor_copy", out=KCT[:, 0:n_], in_=pk[:, 0:n_])
                k.op("act", "activation", out=sqj[0:64, 0:n_], in_=pk[0:64, 0:n_], func=AF.Square)
            else:
                for c in range(NCC):
                    pvv, _ = P.v(5)
                    k.op("pe", "matmul", out=pvv[:, 0:64], lhsT=gact[1][:, c * 128:(c + 1) * 128], rhs=W2[1].a, start=True,
                         stop=True, mark=True)
                    k.op("dve", "tensor_copy", out=VCA[:, c, 0:64], in_=pvv[:, 0:64])
        ones64 = k.sb([64, 1], F32, "ones64")
        k.op("pool", "memset", ap=ones64.a, constant=1.0, _writes=[ones64.a])
        pn, _ = P.v(6)
        k.op("pe", "matmul", out=pn[0:1, 0:NCMP], lhsT=ones64.a, rhs=sqj[0:64, 0:NCMP], start=True, stop=True, mark=True)
        nkc = k.sb([128, 1], F32, "nkc")
        k.op("pool", "memset", ap=nkc.a, constant=0.0, _writes=[nkc.a])
        k.op("dve", "reduce_max", out=nkc[0:1, 0:1], in_=pn[0:1, 0:NCMP], axis=AX.X)
        bound_negM(k, P, NQ[:, 0, :], NQ[:, 1, :], idf, ones1, negM[0].a, 7, (sc_mq, sc_mk, sc_row))
        bound_negM(k, P, NQ[:, 0, :], nkc.a, idf, ones1, negM[1].a, 7, (sc_mq, sc_mk, sc_row))
        WV = k.sb([128, 256], F32, "WV")
        WADD = k.sb([128, 256], F32, "WADD")
        k.dma("sp", WV.a, consts["wv"].a, "ld_c")
        k.dma("sp", WADD.a, consts["wadd"].a, "ld_c")
        G64 = CT
        k.op("pool", "memset", ap=G64.a, constant=0.0, _writes=[G64.a])
        k.dma("sp", G64[0:NSLC], consts["g64"].a, "ld_c")
        IMP = k.sb([128, 4, 128], F32, "IMP")
        NMT = k.sb([128, 512], BF16, "NMT")
        k.op("pool", "memset", ap=NMT.a, constant=0.0, _writes=[NMT.a])
        sc_s = [k.sb([128, 128], F32, f"sc_s{i}") for i in range(2)]
        m8 = k.sb([128, 16], F32, "m8")
        nmb = k.sb([128, 128], BF16, "nmb")
        for qb in range(NQB):
            q0 = qb * 512
            first_touch = {"imp": [True] * 4, "out": [[True] * 4 for _ in range(4)]}

            def mk_cmp(h, half, qb=qb, q0=q0):
                def fac(slot):
                    zb, _ = P.v(slot * 2)
                    ob, _ = P.v(slot * 2 + 1)
                    qt = QT[h // 2]
                    r0 = (h % 2) * 64
                    cbase = q0 + half * 256
                    chunks = []
                    for c in range(NCC):
                        if 16 * (128 * c) + 31 > cbase + 255:
                            continue
                        chunks.append(dict(kT=KCT[r0:r0 + 64, c * 128:(c + 1) * 128], v=VCA[:, c, :], c0=0, c1=256,
                                           aff=[(0, 256, cbase - 2048 * c - 31, -16, 1, ALU.is_ge)]))

                    def epi():
                        for i in range(2):
                            sub = half * 2 + i
                            ti = qb * 4 + sub
                            rzv = rz[:, slot * 4 + i: slot * 4 + i + 1]
                            cfv = coef[:, slot * 4 + i: slot * 4 + i + 1]
                            if not chunks:
                                if first_touch["imp"][sub]:
                                    k.op("pool", "memset", ap=IMP[:, sub, :], constant=0.0, _writes=[IMP.a])
                                    first_touch["imp"][sub] = False
                                if first_touch["out"][sub][h]:
                                    k.op("pool", "memset", ap=OUT[:, sub, h * 64:(h + 1) * 64], constant=0.0, _writes=[OUT.a])
                                    first_touch["out"][sub][h] = False
                                continue
                            acc = ob[:, i * NV:(i + 1) * NV]
                            k.op("dve", "tensor_scalar", out=rzv, in0=acc[:, 64:65], scalar1=1e-30, scalar2=None, op0=ALU.max)
                            k.op("dve", "reciprocal", out=rzv, in_=rzv)
                            k.op("dve", "tensor_tensor", out=cfv, in0=rzv, in1=G[:, ti, h * 3:h * 3 + 1], op=ALU.mult)
                            k.op("dve", "tensor_scalar", out=OUT[:, sub, h * 64:(h + 1) * 64], in0=acc[:, 0:64], scalar1=cfv,
                                 scalar2=None, op0=ALU.mult)
                            first_touch["out"][sub][h] = False
                            if first_touch["imp"][sub]:
                                k.op("dve", "tensor_scalar", out=IMP[:, sub, :], in0=acc[:, 65:193], scalar1=rzv, scalar2=None,
                                     op0=ALU.mult)
                                first_touch["imp"][sub] = False
                            else:
                                k.op("dve", "scalar_tensor_tensor", out=IMP[:, sub, :], in0=acc[:, 65:193], scalar=rzv,
                                     in1=IMP[:, sub, :], op0=ALU.mult, op1=ALU.add)
                    qv = lambda c0, c1: qt[r0:r0 + 64, cbase + c0:cbase + c1]
                    return flash_stream(k, slot, zb, ob, e_t[slot], qv, chunks, NV, negM[1].a, epi)
                return fac
            run_streams([mk_cmp(h, half) for h in range(4) for half in range(2)], 4)
            for sub in range(4):
                qt_i = qb * 4 + sub
                sc0, sc1 = sc_s
                lo = 128 - 2 * qt_i
                k.op("dve", "tensor_tensor", out=sc0.a, in0=IMP[:, sub, :], in1=WV[:, lo:lo + 128], op=ALU.mult)
                k.op("dve", "tensor_tensor", out=sc0.a, in0=sc0.a, in1=WADD[:, lo:lo + 128], op=ALU.add)
                if qt_i >= 1:
                    k.op("dve", "tensor_scalar", out=sc0[:, 0:1], in0=sc0[:, 0:1], scalar1=1.0e4, scalar2=None, op0=ALU.add)
                k.op("dve", "max", out=m8[:, 0:8], in_=sc0.a)
                k.op("dve", "match_replace", out=sc1.a, in_to_replace=m8[:, 0:8], in_values=sc0.a, imm_value=-1e30)
                k.op("dve", "max", out=m8[:, 8:16], in_=sc1.a)
                k.op("dve", "tensor_scalar", out=sc1.a, in0=sc0.a, scalar1=m8[:, 15:16], scalar2=None, op0=ALU.is_ge)
                k.op("dve", "tensor_tensor", out=sc1.a, in0=sc1.a, in1=WV[:, lo:lo + 128], op=ALU.mult)
                k.op("dve", "tensor_scalar", out=nmb.a, in0=sc1.a, scalar1=-NEG, scalar2=NEG, op0=ALU.mult, op1=ALU.add)
                pt, _ = P.v(7)
                ptb = pt.bitcast(BF16)
                k.op("pe", "transpose", out=ptb[:, 0:128], in_=nmb.a, identity=idb.a, mark=True)
                k.op("act", "copy", out=NMT[:, sub * 128:(sub + 1) * 128], in_=ptb[:, 0:128])
            def mk_flash(h, br, qb=qb, q0=q0):
                def fac(slot):
                    zb, _ = P.v(slot * 2)
                    ob, _ = P.v(slot * 2 + 1)
                    qt = QT[h // 2]
                    r0 = (h % 2) * 64
                    chunks = []
                    if br == 1:
                        for kc in range(0, 4 * qb + 4):
                            j = kc - 4 * qb
                            c0 = 128 * j if j > 0 else 0
                            aff = [(c0, c0 + 128, 0, -1, 1, ALU.is_ge)] if j >= 0 else []
                            chunks.append(dict(kT=KT[0][r0:r0 + 64, kc * 128:(kc + 1) * 128], v=VS[:, kc, 0, :], c0=c0, c1=512, aff=aff,
                                               mask=(G64[:, kc * 128:(kc + 1) * 128], lambda a, b: NMT[:, a:b])))
                    else:
                        for kc in range(max(0, 4 * qb - 4), 4 * qb + 4):
                            j = kc - 4 * qb
                            if j >= 0:
                                c0, c1 = 128 * j, 512
                                aff = [(c0, c0 + 128, 0, -1, 1, ALU.is_ge)]
                            else:
                                jp = j + 4
                                c0, c1 = 0, 128 * (jp + 1)
                                aff = [(128 * jp, 128 * jp + 128, -1, 1, -1, ALU.is_ge)]
                            chunks.append(dict(kT=KT[1][r0:r0 + 64, kc * 128:(kc + 1) * 128], v=VS[:, kc, 1, :], c0=c0, c1=c1, aff=aff))

                    def epi():
                        for sub in range(4):
                            ti = qb * 4 + sub
                            rzv = rz[:, slot * 4 + sub: slot * 4 + sub + 1]
                            cfv = coef[:, slot * 4 + sub: slot * 4 + sub + 1]
                            acc = ob[:, sub * 65:(sub + 1) * 65]
                            k.op("dve", "tensor_scalar", out=rzv, in0=acc[:, 64:65], scalar1=1e-30, scalar2=None, op0=ALU.max)
                            k.op("dve", "reciprocal", out=rzv, in_=rzv)
                            k.op("dve", "tensor_tensor", out=cfv, in0=rzv, in1=G[:, ti, h * 3 + br:h * 3 + br + 1], op=ALU.mult)
                            k.op("dve", "scalar_tensor_tensor", out=OUT[:, sub, h * 64:(h + 1) * 64], in0=acc[:, 0:64], scalar=cfv,
                                 in1=OUT[:, sub, h * 64:(h + 1) * 64], op0=ALU.mult, op1=ALU.add)
                    qv = lambda c0, c1: qt[r0:r0 + 64, q0 + c0:q0 + c1]
                    return flash_stream(k, slot, zb, ob, e_t[slot], qv, chunks, 65, negM[0].a, epi)
                return fac
            run_streams([mk_flash(h, br) for h in range(4) for br in (1, 2)], 4)
            k.op("act", "copy", out=OUTb.a, in_=OUT.a)
            for sub in range(4):
                emit_out_T(k, P, OUTb[:, sub, :], oT_d, row_nsa, (qb * 4 + sub) * 128, idb, oTs, 7, sub)

    if do_moba:
        NB = max(NBLK, 8)
        Wm = Wall
        k.dma("pool", Wm[:, :, 0:768], V(wm_d, wm_d.h[:, :].rearrange("(c p) n -> p c n", p=128)))
        VB = V(Vall, Vall.h[:, :, :].rearrange("p t (a d) -> p t a d", a=4))
        k.op("pool", "memset", ap=Vall.a, constant=1.0, _writes=[Vall.a])
        NQm = k.sb([128, 2, NTILE], F32, "NQm")
        for g4 in range(S // 512):
            xT_g = xT[g4 % 2]
            load_xT_group(k, P, x_d, g4 * 512, 512, xin, xbf, xT_g, idb, 0)
            COS4, SIN4 = rope_group(k, pos_t, invf, g4, rtm)
            for t in range(4):
                ti = g4 * 4 + t
                pp, pp_x = P.v(2 + 2 * (t % 2), 2)
                for (a, b) in ((0, 512), (512, 768)):
                    for c in range(8):
                        k.op("pe", "matmul", out=pp[:, a:b], lhsT=xT_g[:, c, t * 128:(t + 1) * 128], rhs=Wm[:, c, a:b],
                             start=(c == 0), stop=(c == 7), _writes=pp_x, mark=(c == 7 and a == 512))
                k.op("act", "copy", out=pf[:, 0:768], in_=pp[:, 0:768], _reads=pp_x)
                rope_apply(k, pf[:, 0:512], 8, COS4[:, t, :], SIN4[:, t, :], t1, t2, Rb[:, 0:512])
                k.op("pool", "tensor_copy", out=VB[:, ti, :, 0:64], in_=pf[:, 512:768].re("p (a d) -> p a d", a=4))
                for j, (a, b) in enumerate(((0, 256), (256, 512))):
                    k.op("act", "activation", out=sqj[:, a:b], in_=Rb[:, a:b], func=AF.Square, accum_out=NQm[:, j, ti:ti + 1])
                pt, _ = P.v(6 + (t % 2))
                ptb = pt.bitcast(BF16)
                for j in range(4):
                    k.op("pe", "transpose", out=ptb[:, j * 128:(j + 1) * 128], in_=Rb[:, j * 128:(j + 1) * 128],
                         identity=idb.a, mark=(j == 3))
                for j, dst in enumerate((QT[0], QT[1], KT[0], KT[1])):
                    k.op("act" if j % 2 else "dve", "copy" if j % 2 else "tensor_copy", out=dst[:, ti * 128:(ti + 1) * 128],
                         in_=ptb[:, j * 128:(j + 1) * 128])
        bound_negM(k, P, NQm[:, 0, :], NQm[:, 1, :], idf, ones1, negM[2].a, 7, (sc_mq, sc_mk, sc_row))
        KM = k.sb([128, 32], F32, "KM")
        KMt = k.sb([128, 32], F32, "KMt")
        KMhi = [k.sb([128, 32], BF16, f"KMhi{i}") for i in range(2)]
        KMlo = [k.sb([128, 32], BF16, f"KMlo{i}") for i in range(2)]
        for i in range(2):
            k.op("pool", "memset", ap=KMhi[i].a, constant=0.0, _writes=[KMhi[i].a])
            k.op("pool", "memset", ap=KMlo[i].a, constant=0.0, _writes=[KMlo[i].a])
            k.op("dve", "tensor_reduce", out=KM[:, 0:NBLK], in_=KT[i].a.re("p (n l) -> p n l", l=256), axis=AX.X, op=ALU.add)
            k.op("dve", "tensor_scalar", out=KM[:, 0:NBLK], in0=KM[:, 0:NBLK], scalar1=1.0 / 256, scalar2=None, op0=ALU.mult)
            k.op("dve", "tensor_copy", out=KMhi[i][:, 0:NBLK], in_=KM[:, 0:NBLK])
            k.op("dve", "tensor_tensor", out=KMt[:, 0:NBLK], in0=KM[:, 0:NBLK], in1=KMhi[i][:, 0:NBLK], op=ALU.subtract)
            k.op("dve", "tensor_copy", out=KMlo[i][:, 0:NBLK], in_=KMt[:, 0:NBLK])
        G256 = CT
        k.op("pool", "memset", ap=CT[0:32], constant=0.0, _writes=[CT.a])
        k.dma("sp", G256[0:NBLK], consts["g256"].a)
        PM = k.sb([128, 64], F32, "PM")
        k.dma("sp", PM.a, consts["pm"].a)
        NMTm = [k.sb([32, 512], BF16, f"NMTm{h}") for h in range(4)]
        wk32 = k.sb([128, 32], F32, "wk32")
        nm32 = k.sb([128, 32], F32, "nm32")
        nmb32 = k.sb([128, 32], BF16, "nmb32")
        k.op("pool", "memset", ap=nmb32.a, constant=0.0, _writes=[nmb32.a])
        m8m = k.sb([128, 8], F32, "m8m")
        thr2 = k.sb([128, 1], F32, "thr2")
        for qb in range(NQB):
            q0 = qb * 512
            for h in range(4):
                r0 = (h % 2) * 64
                for sub in range(4):
                    qt_i = qb * 4 + sub
                    cur = qt_i // 2
                    pg, _ = P.v(6)
                    k.op("pe", "matmul", out=pg[:, 0:32], lhsT=QT[h // 2][r0:r0 + 64, qt_i * 128:(qt_i + 1) * 128],
                         rhs=KMhi[h // 2][r0:r0 + 64, :], start=True, stop=False)
                    k.op("pe", "matmul", out=pg[:, 0:32], lhsT=QT[h // 2][r0:r0 + 64, qt_i * 128:(qt_i + 1) * 128],
                         rhs=KMlo[h // 2][r0:r0 + 64, :], start=False, stop=True, mark=True)
                    k.op("dve", "tensor_tensor", out=wk32[:, 0:NB], in0=pg[:, 0:NB], in1=PM[:, 32 - cur:32 - cur + NB], op=ALU.add)
                    k.op("dve", "max", out=m8m.a, in_=wk32[:, 0:NB])
                    k.op("dve", "tensor_scalar", out=thr2.a, in0=m8m[:, 2:3], scalar1=-1e29, scalar2=None, op0=ALU.max)
                    k.op("dve", "tensor_scalar", out=nm32[:, 0:NB], in0=wk32[:, 0:NB], scalar1=thr2.a, scalar2=None, op0=ALU.is_ge)
                    k.op("dve", "tensor_scalar", out=nm32[:, 0:NB], in0=nm32[:, 0:NB], scalar1=-NEG, scalar2=NEG, op0=ALU.mult, op1=ALU.add)
                    k.op("dve", "memset", ap=nm32[:, cur:cur + 1], constant=0.0, _writes=[nm32.a])
                    k.op("dve", "tensor_copy", out=nmb32[:, 0:NB], in_=nm32[:, 0:NB])
                    pt, _ = P.v(7)
                    ptb = pt.bitcast(BF16)
                    k.op("pe", "transpose", out=ptb[0:32, 0:128], in_=nmb32.a, identity=idb.a, mark=True)
                    k.op("act", "copy", out=NMTm[h][:, sub * 128:(sub + 1) * 128], in_=ptb[0:32, 0:128])

            def mk_moba(h, qb=qb, q0=q0):
                def fac(slot):
                    zb, _ = P.v(slot * 2)
                    ob, _ = P.v(slot * 2 + 1)
                    qt = QT[h // 2]
                    r0 = (h % 2) * 64
                    chunks = []
                    for kc in range(0, 4 * qb + 4):
                        j = kc - 4 * qb
                        c0 = 128 * j if j > 0 else 0
                        aff = [(c0, c0 + 128, 0, -1, 1, ALU.is_ge)] if j >= 0 else []
                        chunks.append(dict(kT=KT[h // 2][r0:r0 + 64, kc * 128:(kc + 1) * 128], v=VB[:, kc, h, :], c0=c0, c1=512, aff=aff,
                                           mask=(G256[0:32, kc * 128:(kc + 1) * 128], lambda a, b, h=h: NMTm[h][:, a:b])))

                    def epi():
                        for sub in range(4):
                            rzv = rz[:, slot * 4 + sub: slot * 4 + sub + 1]
                            acc = ob[:, sub * 65:(sub + 1) * 65]
                            k.op("dve", "tensor_scalar", out=rzv, in0=acc[:, 64:65], scalar1=1e-30, scalar2=None, op0=ALU.max)
                            k.op("dve", "reciprocal", out=rzv, in_=rzv)
                            k.op("dve", "tensor_scalar", out=OUT[:, sub, h * 64:(h + 1) * 64], in0=acc[:, 0:64], scalar1=rzv,
                                 scalar2=None, op0=ALU.mult)
                    qv = lambda c0, c1: qt[r0:r0 + 64, q0 + c0:q0 + c1]
                    return flash_stream(k, slot, zb, ob, e_t[slot], qv, chunks, 65, negM[2].a, epi)
                return fac
            run_streams([mk_moba(h) for h in range(4)], 4)
            k.op("act", "copy", out=OUTb.a, in_=OUT.a)
            for sub in range(4):
                emit_out_T(k, P, OUTb[:, sub, :], oT_d, row_moba, (qb * 4 + sub) * 128, idb, oTs, 7, sub)


import ml_dtypes
from concourse.bass_utils import run_bass_kernel_spmd

_PROGS = {}
EVEN_WIDTHS = (512, 128, 128, 128, 128, 128, 128, 24, 512, 512, 512)
_SP = [0] + [int(v) for v in np.cumsum(EVEN_WIDTHS)[:-1]]


def _consts(S):
    half = 32
    invf = (10000.0 ** (-np.arange(half, dtype=np.float32) / half)).astype(np.float32)
    invf_t = np.tile((invf / np.float32(2 * np.pi)).astype(np.float32)[None, :], (128, 1))
    p = np.arange(128)[:, None]
    m = np.arange(256)[None, :]
    hp = (p >= 64).astype(np.int64)
    d = m - 128
    wv = (d <= hp).astype(np.float32)
    wadd = (1e4 * ((d == hp) | (d == hp - 1)) - 1.0 * (d > hp)).astype(np.float32)
    g64 = (np.arange(S)[None, :] // 64 == np.arange(S // 64)[:, None]).astype(ml_dtypes.bfloat16)
    g256 = (np.arange(S)[None, :] // 256 == np.arange(S // 256)[:, None]).astype(ml_dtypes.bfloat16)
    pm = np.where(np.arange(64)[None, :] < 32, 0.0, -1e30).astype(np.float32) * np.ones((128, 1), np.float32)
    return dict(ident=np.eye(128, dtype=np.float32), invf=invf_t, wv=wv, wadd=wadd, g64=g64, g256=g256, pm=pm)


def build_fused(S, SG):
    nc = bass.Bass("TRN2", target_bir_lowering=False)
    k = K(nc)
    P = Psum(k)
    EI = dict(kind="ExternalInput")
    x = k.dram("x", [S, 1024], F32, **EI)
    pos = k.dram("pos", [128, S // 128], I32, **EI)
    wn = k.dram("wn", [2, 1024, 780], F32, **EI)
    wm = k.dram("wm", [2, 1024, 768], F32, **EI)
    wsb = k.dram("wsb", [2, 1024, 1536], F32, **EI)
    cw = {n: k.dram(n, sh, F32, **EI) for n, sh in
          (("w1k", [2048, 128]), ("w1v", [2048, 128]), ("w2k", [128, 64]), ("w2v", [128, 64]), ("posT", [128, 32]))}
    consts = {"ident": k.dram("ident", [128, 128], F32, **EI), "invf": k.dram("invf", [128, 32], F32, **EI),
              "wv": k.dram("wv", [128, 256], F32, **EI), "wadd": k.dram("wadd", [128, 256], F32, **EI),
              "g64": k.dram("g64", [S // 64, S], BF16, **EI), "g256": k.dram("g256", [S // 256, S], BF16, **EI),
              "pm": k.dram("pm", [128, 64], F32, **EI)}
    w_out = k.dram("w_out", [2, 1024, 1024], F32, **EI)
    lnp = k.dram("lnp", [2, 4, 1024], F32, **EI)
    wr = k.dram("wr", [2, 1024, 20], F32, **EI)
    br = k.dram("br", [2, 20], F32, **EI)
    wg = k.dram("w_gate", [2, 16, 1024, 256], F32, **EI)
    wu = k.dram("w_up", [2, 16, 1024, 256], F32, **EI)
    wd = k.dram("w_down", [2, 16, 256, 1024], F32, **EI)
    out = k.dram("out", [S, 1024], F32, kind="ExternalOutput")
    oT_s = k.dram("oT_s", [1024, S], BF16, kind="Internal")
    x1_s = k.dram("x1_s", [S, 1024], F32, kind="Internal")
    sub = lambda t, i: Tile(k, t.h[i], t.name + f"_{i}")
    for g in range(2):
        k.begin_phase(f"l0a{g}")
        emit_phase_l0(k, P, S, x, pos, sub(wn, g), sub(wm, g), cw, consts, oT_s, row_nsa=256 * g, row_moba=512 + 256 * g)
        k.end_phase()
    k.begin_phase("l0b")
    emit_phase_b(k, P, S, SG, oT_s, x, x1_s, sub(w_out, 0), sub(lnp, 0), sub(wr, 0), sub(br, 0), sub(wg, 0), sub(wu, 0),
                 sub(wd, 0), consts["ident"])
    k.end_phase()
    for g in range(2):
        k.begin_phase(f"l1a{g}")
        emit_phase_sb(k, P, S, x1_s, sub(wsb, g), oT_s, consts["ident"], row0=512 * g)
        k.end_phase()
    k.begin_phase("l1b")
    emit_phase_b(k, P, S, SG, oT_s, x1_s, out, sub(w_out, 1), sub(lnp, 1), sub(wr, 1), sub(br, 1), sub(wg, 1), sub(wu, 1),
                 sub(wd, 1), consts["ident"])
    k.end_phase()
    k.finish([out])
    return nc


def host_inputs(inp, b, S):
    w_in = inp["ab_w_in"][0]
    c_qa, c_kc, c_vc, c_ksl, c_vsl, c_kw, c_vw, c_ga, c_qm, c_km, c_vm = _SP
    wns, wms, wsbs = [], [], []
    w_sb = inp["sb_w_in"][0]
    for g in range(2):
        kvc = lambda base: w_in[:, base + g * 64: base + (g + 1) * 64]
        wns.append(np.concatenate([w_in[:, c_qa + 256 * g: c_qa + 256 * (g + 1)], kvc(c_ksl), kvc(c_ksl), kvc(c_kw), kvc(c_kw),
                                   kvc(c_kc), kvc(c_vc), kvc(c_vsl), kvc(c_vw), w_in[:, c_ga + 12 * g: c_ga + 12 * (g + 1)]], axis=1))
        wms.append(np.concatenate([w_in[:, c_qm + 256 * g: c_qm + 256 * (g + 1)], w_in[:, c_km + 256 * g: c_km + 256 * (g + 1)],
                                   w_in[:, c_vm + 256 * g: c_vm + 256 * (g + 1)]], axis=1))
        wsbs.append(np.concatenate([w_sb[:, j * 1024 + 512 * g: j * 1024 + 512 * (g + 1)] for j in range(3)], axis=1))
    m = dict(x=np.ascontiguousarray(inp["x"][b]).astype(np.float32),
             pos=np.ascontiguousarray(inp["positions"][b].reshape(S // 128, 128).T.astype(np.int32)),
             wn=np.ascontiguousarray(np.stack(wns)), wm=np.ascontiguousarray(np.stack(wms)), wsb=np.ascontiguousarray(np.stack(wsbs)),
             w1k=inp["nsa_cmp_w1_k"][0], w1v=inp["nsa_cmp_w1_v"][0], w2k=inp["nsa_cmp_w2_k"][0], w2v=inp["nsa_cmp_w2_v"][0],
             posT=np.ascontiguousarray(np.concatenate([inp["nsa_cmp_pos_k"][0].T, inp["nsa_cmp_pos_v"][0].T], axis=0)),
             w_out=np.ascontiguousarray(np.stack([inp["ab_w_out"][0], inp["sb_w_out"][0]])),
             lnp=np.ascontiguousarray(np.stack([np.stack([inp["ln_mix_g"][l], inp["ln_mix_b"][l], inp["ln_ffn_g"][l], inp["ln_ffn_b"][l]]) for l in range(2)])),
             wr=np.ascontiguousarray(np.stack([np.concatenate([inp["moe_w_grp"][l], inp["moe_w_rt"][l].transpose(1, 0, 2).reshape(1024, 16)], axis=1) for l in range(2)])),
             br=np.ascontiguousarray(np.stack([np.concatenate([inp["moe_b_grp"][l], inp["moe_b_rt"][l].reshape(16)]) for l in range(2)])),
             w_gate=inp["moe_w_gate"], w_up=inp["moe_w_up"], w_down=inp["moe_w_down"])
    m.update(_consts(S))
    return m


def kernel(**inputs):
    inp = {k_: np.asarray(v) for k_, v in inputs.items()}
    B, S, _ = inp["x"].shape
    SG = min(2048, S)
    key = ("fused", S, SG)
    if key not in _PROGS:
        _PROGS[key] = build_fused(S, SG)
    nc = _PROGS[key]
    in_maps = [host_inputs(inp, b, S) for b in range(B)]
    res = run_bass_kernel_spmd(nc, in_maps, core_ids=list(range(B)))
    return np.stack([res.results[b]["out"] for b in range(B)]).astype(np.float32)
```

```python
import math
import bisect
from contextlib import ExitStack
import numpy as np
import concourse.bass as bass
import concourse.mybir as mybir

F32 = mybir.dt.float32
BF16 = mybir.dt.bfloat16
I32 = mybir.dt.int32
AF = mybir.ActivationFunctionType
ALU = mybir.AluOpType
AX = mybir.AxisListType


class V:
    __slots__ = ("tile", "ap")

    def __init__(self, tile, ap):
        self.tile = tile
        self.ap = ap

    def __getitem__(self, idx):
        return V(self.tile, self.ap[idx])

    def bc(self, shape):
        return V(self.tile, self.ap.broadcast_to(shape))

    def unsq(self, d):
        return V(self.tile, self.ap.unsqueeze(d))

    def re(self, s, **kw):
        return V(self.tile, self.ap.rearrange(s, **kw))

    def bitcast(self, dt):
        return V(self.tile, self.ap.bitcast(dt))


class Tile:
    def __init__(self, k, h, name):
        self.k = k
        self.h = h
        self.name = name
        self.w = None
        self.r = {}
        self.excl = False

    def __getitem__(self, idx):
        return V(self, self.h[idx])

    @property
    def a(self):
        return V(self, self.h[:])


class EngState:
    def __init__(self, k, name, eng, is_compute=True):
        self.k = k
        self.name = name
        self.eng = eng
        self.sem = k.nc.alloc_semaphore("s_" + name)
        self.n = 0
        self.last = None
        self.marks_idx = []
        self.marks_val = []
        self.val = 0
        self.waited = {}
        self.nwaits = 0


class DmaSem:
    def __init__(self, k, name):
        self.sem = k.nc.alloc_semaphore("d_" + name)
        self.cnt = 0
        self.name = name


class K:
    def __init__(self, nc, same_engine_sync=True):
        self.nc = nc
        self.E = {
            "pe": EngState(self, "pe", nc.tensor),
            "act": EngState(self, "act", nc.scalar),
            "dve": EngState(self, "dve", nc.vector),
            "pool": EngState(self, "pool", nc.gpsimd),
            "sp": EngState(self, "sp", nc.sync),
        }
        self.same_engine_sync = same_engine_sync
        self.dsems = {}
        self.rings = {}
        self.ring_pos = {}
        self.RING = 16
        self.n_tiles = 0
        self.stack = ExitStack()
        self.pname = ""

    def sb(self, shape, dt, name=None):
        self.n_tiles += 1
        name = "sb_" + self.pname + (name or f"t{self.n_tiles}")
        h = self.stack.enter_context(self.nc.sbuf_tensor(name, list(shape), dt))
        return Tile(self, h, name)

    def ps(self, shape, dt=F32, name=None):
        self.n_tiles += 1
        name = name or f"p{self.n_tiles}"
        h = self.nc.alloc_psum_tensor(name, list(shape), dt)
        t = Tile(self, h, name)
        t.excl = True
        return t

    def dram(self, name, shape, dt, kind="Internal"):
        h = self.nc.dram_tensor(name, list(shape), dt, kind=kind)
        return Tile(self, h, name)

    def sub(self, v, name="sub"):
        return Tile(self, v.ap, name)

    def dsem(self, name):
        if name not in self.dsems:
            self.dsems[name] = DmaSem(self, name)
        return self.dsems[name]

    def _token_value(self, tok):
        kind, obj, idx = tok
        if kind == "dma":
            return obj.sem, idx
        es = obj
        if es.marks_idx and es.marks_idx[-1] >= idx:
            j = bisect.bisect_left(es.marks_idx, idx)
            return es.sem, es.marks_val[j]
        es.val += 1
        es.last.then_inc(es.sem, 1)
        es.marks_idx.append(es.n - 1)
        es.marks_val.append(es.val)
        return es.sem, es.val

    def _wait(self, es, tok):
        if tok is None:
            return
        kind, obj, idx = tok
        if kind == "eng" and obj is es:
            if not self.same_engine_sync or es.name == "pe":
                return
        sem, val = self._token_value(tok)
        key = id(sem) if kind == "dma" else obj.name
        if es.waited.get(key, 0) >= val:
            return
        es.waited[key] = val
        es.eng.wait_ge(sem, val)
        es.nwaits += 1

    def _deps(self, es, reads, writes):
        for v in reads:
            if v is None:
                continue
            self._wait(es, v.tile.w)
            if v.tile.excl:
                for tok in v.tile.r.values():
                    self._wait(es, tok)
        for v in writes:
            if v is None:
                continue
            t = v.tile
            self._wait(es, t.w)
            for tok in t.r.values():
                self._wait(es, tok)

    def _commit(self, tok, reads, writes, rkey):
        for v in reads:
            if v is None:
                continue
            v.tile.r[rkey] = tok
        for v in writes:
            if v is None:
                continue
            v.tile.w = tok
            v.tile.r = {}

    OUT_KEYS = ("out", "accum_out", "out_ap")

    def op(self, en, fn, **kw):
        es = self.E[en]
        reads, writes = [], []
        extra_r = kw.pop("_reads", [])
        extra_w = kw.pop("_writes", [])
        mark = kw.pop("mark", None)
        if mark is None:
            mark = en != "pe"
        args = {}
        for key, val in kw.items():
            if isinstance(val, V):
                (writes if key in self.OUT_KEYS else reads).append(val)
                args[key] = val.ap
            else:
                args[key] = val
        reads += extra_r
        writes += extra_w
        self._deps(es, reads, writes)
        inst = getattr(es.eng, fn)(**args)
        es.last = inst
        es.n += 1
        tok = ("eng", es, es.n - 1)
        if mark:
            es.val += 1
            inst.then_inc(es.sem, 1)
            es.marks_idx.append(es.n - 1)
            es.marks_val.append(es.val)
        self._commit(tok, reads, writes, en)
        return inst

    def dma(self, qn, out, in_, ds=None, **kw):
        es = self.E[qn]
        ring = self.rings.setdefault(qn, [])
        pos = self.ring_pos.get(qn, 0)
        if len(ring) < self.RING:
            ring.append(self.dsem(f"{qn}_r{len(ring)}"))
        ds = ring[pos % self.RING]
        self.ring_pos[qn] = pos + 1
        if ds.cnt:
            self._wait(es, ("dma", ds, ds.cnt))
        self._deps(es, [in_], [out])
        inst = es.eng.dma_start(out=out.ap, in_=in_.ap, **kw)
        inst.then_inc(ds.sem, 16)
        ds.cnt += 16
        tok = ("dma", ds, ds.cnt)
        self._commit(tok, [in_], [out], "dma_" + ds.name)
        return inst

    def begin_phase(self, name):
        self.stack = ExitStack()
        self.pname = name + "_"

    def barrier(self):
        targets = []
        for n in ("pe", "act", "dve", "pool"):
            es = self.E[n]
            if es.n == 0:
                continue
            if not es.marks_idx or es.marks_idx[-1] < es.n - 1:
                es.val += 1
                es.last.then_inc(es.sem, 1)
                es.marks_idx.append(es.n - 1)
                es.marks_val.append(es.val)
            targets.append((n, es.sem, es.val))
        for en, es in self.E.items():
            for (n, sem, val) in targets:
                if es.waited.get(n, 0) < val:
                    es.eng.wait_ge(sem, val)
                    es.waited[n] = val
            for ds in self.dsems.values():
                if ds.cnt and es.waited.get(id(ds.sem), 0) < ds.cnt:
                    es.eng.wait_ge(ds.sem, ds.cnt)
                    es.waited[id(ds.sem)] = ds.cnt

    def end_phase(self):
        self.barrier()
        self.stack.close()
        self.stack = ExitStack()

    def finish(self, out_tiles):
        es = self.E["sp"]
        for t in out_tiles:
            self._wait(es, t.w)
        for ds in self.dsems.values():
            if ds.cnt:
                key = id(ds.sem)
                if es.waited.get(key, 0) < ds.cnt:
                    es.eng.wait_ge(ds.sem, ds.cnt)
                    es.waited[key] = ds.cnt

    def stats(self):
        return {n: (e.n, e.nwaits, e.val) for n, e in self.E.items()}


D = 1024
ALPHA = float(4 ** 0.25)
EPS = 1e-5
NE = 16
FH = 256


class Psum:
    def __init__(self, k):
        self.k = k
        self.t = k.nc.alloc_psum_tensor("psum_all", [128, 8, 512], F32)
        self.b = [Tile(k, self.t[:, i, :], f"bank{i}") for i in range(8)]
        for t in self.b:
            t.excl = True

    def v(self, i, n=1):
        if n == 1:
            return V(self.b[i], self.t[:, i, :]), []
        ap = self.t[:, i:i + n, :].rearrange("p a b -> p (a b)")
        return V(self.b[i], ap), [self.b[j].a for j in range(i + 1, i + n)]


def layer_norm_tile(k, src, dst, g_t, b_t, eps_t, tmp, st, mv, rstd, nmr):
    for i in range(2):
        k.op("dve", "bn_stats", out=st[:, i, :], in_=src[:, i * 512:(i + 1) * 512])
    k.op("dve", "bn_aggr", out=mv.a, in_=st.a.re("p a b -> p (a b)"))
    k.op("act", "activation", out=rstd.a, in_=mv[:, 1:2], func=AF.Ln, bias=eps_t.a, scale=1.0)
    k.op("act", "activation", out=rstd.a, in_=rstd.a, func=AF.Exp, scale=-0.5)
    k.op("dve", "scalar_tensor_tensor", out=nmr.a, in0=mv[:, 0:1], scalar=-1.0, in1=rstd.a,
         op0=ALU.mult, op1=ALU.mult)
    k.op("act", "activation", out=dst, in_=src, func=AF.Identity, bias=nmr.a, scale=rstd.a)
    k.op("pool", "tensor_tensor", out=dst, in0=dst, in1=g_t.a, op=ALU.mult)
    k.op("pool", "tensor_tensor", out=dst, in0=dst, in1=b_t.a, op=ALU.add)


def emit_phase_b(k, P, NT, SG, oT, xres, xo, w_out, lnp, wr, br, w_gate, w_up, w_down, ident_d):
    nsg = NT // SG
    ntile = SG // 128
    ngrp = SG // 512
    idf = k.sb([128, 128], F32, "idf")
    k.dma("sp", idf.a, ident_d.a, "ld_c")
    woT = k.sb([128, 8, 1024], BF16, "woT")
    k.dma("pool", woT.a, V(w_out, w_out.h[:, :].rearrange("(c p) n -> p c n", p=128)), "ld_c")
    lnt = []
    for i in range(4):
        t = k.sb([128, 1024], F32, f"ln{i}")
        k.dma("sp", t.a, V(lnp, lnp.h[i].partition_broadcast(128)), "ld_c")
        lnt.append(t)
    WR = k.sb([128, 8, 20], F32, "WR")
    if True:
        k.dma("sp", WR.a, V(wr, wr.h[:, :].rearrange("(c p) n -> p c n", p=128)), "ld_c")
    BR = k.sb([128, 20], F32, "BR")
    if True:
      k.dma("sp", BR.a, V(br, br.h[:].partition_broadcast(128)), "ld_c")
    eps_t = k.sb([128, 1], F32, "eps")
    k.op("pool", "memset", ap=eps_t.a, constant=EPS, _writes=[eps_t.a])
    SELT = k.sb([16, 16, 128], F32, "SELT")
    k.op("pool", "memset", ap=SELT.a, constant=1.0, _writes=[SELT.a])
    ones16 = SELT
    if True:
      k.op("pool", "affine_select", out=SELT.a, in_=ones16.a, pattern=[[-1, 16], [0, 128]],
         compare_op=ALU.is_equal, fill=0.0, base=0, channel_multiplier=1)

    acc = [k.sb([128, 1024], F32, f"acc{i}") for i in range(ntile)]
    x1T = k.sb([128, 8, SG], BF16, "x1T")
    CT = k.sb([16, SG], F32, "CT")
    oTt = [k.sb([128, 8, 128], BF16, f"oTt{i}") for i in range(2)]
    xrt = [k.sb([128, 1024], F32, f"xrt{i}") for i in range(2)]
    yt = k.sb([128, 1024], F32, "yt")
    tmp = yt
    x1t = k.sb([128, 1024], F32, "x1t")
    x1Tf = k.sb([128, 8, 128], F32, "x1Tf")
    st = k.sb([128, 2, 6], F32, "st")
    mv = k.sb([128, 2], F32, "mv")
    rstd = k.sb([128, 1], F32, "rstd")
    nmr = k.sb([128, 1], F32, "nmr")
    L = k.sb([128, 20], F32, "L")
    sm = k.sb([128, 16], F32, "sm")
    r4 = k.sb([128, 8], F32, "r4")
    r16 = [k.sb([128, 16], F32, f"r16_{i}") for i in range(4)]
    comb = k.sb([128, 16], F32, "comb")
    wgb = [k.sb([128, 8, FH], BF16, f"wgb{i}") for i in range(2)]
    wub = [k.sb([128, 8, FH], BF16, f"wub{i}") for i in range(2)]
    wdb = [k.sb([128, 2, 1024], BF16, f"wdb{i}") for i in range(2)]
    hT = [k.sb([128, 2, 512], BF16, f"hT{i}") for i in range(2)]
    sgt = [k.sb([128, 512], F32, f"sgt{i}") for i in range(2)]
    tt_ = sgt
    ot = xrt

    oT_v = oT.h[:, :].rearrange("(c p) t -> p c t", p=128)
    dcount = [0]
    ld_i = 0
    for sg in range(nsg):
        for ti in range(ntile if 9.0 >= 1 else 0):
            tok0 = sg * SG + ti * 128
            o_t = oTt[ld_i % 2]
            x_t = xrt[ld_i % 2]
            ld_i += 1
            k.dma("sp", o_t.a, V(oT, oT_v[:, :, tok0:tok0 + 128]), f"ld_o{ld_i % 2}")
            k.dma("sp", x_t.a, V(xres, xres.h[tok0:tok0 + 128, :]), f"ld_x{ld_i % 2}")
            pm, pm_x = P.v(0, 2)
            for half in range(2):
                for c in range(8):
                    k.op("pe", "matmul", out=pm[:, half * 512:(half + 1) * 512], lhsT=o_t[:, c, :],
                         rhs=woT[:, c, half * 512:(half + 1) * 512], start=(c == 0), stop=(c == 7),
                         _writes=pm_x, mark=(c == 7 and half == 1))
            k.op("dve", "scalar_tensor_tensor", out=yt.a, in0=x_t.a, scalar=ALPHA, in1=pm, op0=ALU.mult,
                 op1=ALU.add, _reads=pm_x)
            if 9.0 < 1.2:
                continue
            layer_norm_tile(k, yt.a, x1t.a, lnt[0], lnt[1], eps_t, tmp.a, st, mv, rstd, nmr)
            k.op("act", "mul", out=acc[ti].a, in_=x1t.a, mul=ALPHA)
            if 9.0 < 1.4:
                continue
            pT, pT_x = P.v(2, 2)
            for c in range(8):
                k.op("pe", "transpose", out=pT[:, c * 128:(c + 1) * 128], in_=x1t[:, c * 128:(c + 1) * 128],
                     identity=idf.a, _writes=pT_x, mark=(c == 7))
            if True:
                k.op("act", "copy", out=x1Tf.a.re("p c t -> p (c t)"), in_=pT, _reads=pT_x)
            if True:
                k.op("dve", "tensor_copy", out=x1T[:, :, ti * 128:(ti + 1) * 128],
                     in_=pT.re("p (c t) -> p c t", c=8), _reads=pT_x)
            if 9.0 < 2:
                continue
            pr, _ = P.v(7)
            for c in range(8):
                k.op("pe", "matmul", out=pr[:, 0:20], lhsT=x1Tf[:, c, :], rhs=WR[:, c, :], start=(c == 0),
                     stop=(c == 7), mark=(c == 7))
            k.op("dve", "tensor_tensor", out=L.a, in0=pr[:, 0:20], in1=BR.a, op=ALU.add)
            gmax, ngmax, sumg, wg_, m1, nm1, m2, ssum, coef = [sm[:, i:i + 1] for i in range(9)]
            k.op("dve", "reduce_max", out=gmax, in_=L[:, 0:4], axis=AX.X)
            k.op("dve", "tensor_scalar", out=ngmax, in0=gmax, scalar1=-1.0, scalar2=None, op0=ALU.mult)
            k.op("act", "activation", out=r4[:, 0:4], in_=L[:, 0:4], func=AF.Exp, bias=ngmax, scale=1.0,
                 accum_out=sumg)
            k.op("dve", "reciprocal", out=wg_, in_=sumg)
            k.op("dve", "tensor_scalar", out=r4[:, 4:8], in0=L[:, 0:4], scalar1=gmax, scalar2=None,
                 op0=ALU.is_equal)
            k.op("dve", "tensor_scalar", out=r4[:, 4:8], in0=r4[:, 4:8], scalar1=-1.0, scalar2=1e30,
                 op0=ALU.add, op1=ALU.mult)
            lm = r16[0]
            k.op("dve", "tensor_tensor", out=lm.a.re("p (g e) -> p g e", g=4),
                 in0=L[:, 4:20].re("p (g e) -> p g e", g=4), in1=r4[:, 4:8].unsq(2).bc([128, 4, 4]),
                 op=ALU.add)
            k.op("dve", "reduce_max", out=m1, in_=lm.a, axis=AX.X)
            k.op("dve", "tensor_scalar", out=r16[1].a, in0=lm.a, scalar1=m1, scalar2=None, op0=ALU.is_equal)
            k.op("dve", "scalar_tensor_tensor", out=r16[1].a, in0=r16[1].a, scalar=-1e30, in1=lm.a,
                 op0=ALU.mult, op1=ALU.add)
            k.op("dve", "reduce_max", out=m2, in_=r16[1].a, axis=AX.X)
            k.op("dve", "tensor_scalar", out=r16[2].a, in0=lm.a, scalar1=m2, scalar2=None, op0=ALU.is_ge)
            k.op("dve", "tensor_scalar", out=nm1, in0=m1, scalar1=-1.0, scalar2=None, op0=ALU.mult)
            k.op("act", "activation", out=r16[3].a, in_=lm.a, func=AF.Exp, bias=nm1, scale=1.0)
            k.op("dve", "tensor_tensor", out=r16[3].a, in0=r16[3].a, in1=r16[2].a, op=ALU.mult)
            k.op("dve", "reduce_sum", out=ssum, in_=r16[3].a, axis=AX.X)
            k.op("dve", "reciprocal", out=ssum, in_=ssum)
            k.op("dve", "tensor_tensor", out=coef, in0=ssum, in1=wg_, op=ALU.mult)
            k.op("dve", "tensor_scalar", out=comb.a, in0=r16[3].a, scalar1=coef, scalar2=None, op0=ALU.mult)
            pc, _ = P.v(6)
            k.op("pe", "transpose", out=pc[0:16, 0:128], in_=comb.a, identity=idf.a, mark=True)
            k.op("act", "copy", out=CT[:, ti * 128:(ti + 1) * 128], in_=pc[0:16, 0:128])
        items = [(e, grp) for e in range(NE) for grp in range(ngrp)]

        def load_w(e):
            s_ = e % 2
            k.dma("pool", wgb[s_].a, V(w_gate, w_gate.h[e].rearrange("(c p) f -> p c f", p=128)))
            k.dma("pool", wub[s_].a, V(w_up, w_up.h[e].rearrange("(c p) f -> p c f", p=128)))
            k.dma("pool", wdb[s_].a, V(w_down, w_down.h[e].rearrange("(c p) n -> p c n", p=128)))

        def emit_gu(it):
            e, grp = it
            s_ = e % 2
            tc0 = grp * 512
            pcb, _ = P.v(6)
            k.op("pe", "matmul", out=pcb, lhsT=SELT[:, e, :], rhs=CT[:, tc0:tc0 + 512], start=True, stop=True, mark=True)
            for f in range(2):
                pg, _ = P.v(2 + 2 * f)
                pu, _ = P.v(3 + 2 * f)
                for c in range(8):
                    k.op("pe", "matmul", out=pg, lhsT=wgb[s_][:, c, f * 128:(f + 1) * 128],
                         rhs=x1T[:, c, tc0:tc0 + 512], start=(c == 0), stop=(c == 7), mark=(c == 7))
                for c in range(8):
                    k.op("pe", "matmul", out=pu, lhsT=wub[s_][:, c, f * 128:(f + 1) * 128],
                         rhs=x1T[:, c, tc0:tc0 + 512], start=(c == 0), stop=(c == 7), mark=(c == 7))

        def emit_elem(idx):
            h_t = hT[idx % 2]
            pcb, _ = P.v(6)
            for f in range(2):
                pg, _ = P.v(2 + 2 * f)
                pu, _ = P.v(3 + 2 * f)
                k.op("act", "activation", out=sgt[f].a, in_=pg, func=AF.Silu)
                k.op("dve", "tensor_tensor", out=sgt[f].a, in0=sgt[f].a, in1=pu, op=ALU.mult)
                k.op("dve", "tensor_tensor", out=h_t[:, f, :], in0=sgt[f].a, in1=pcb, op=ALU.mult)

        def emit_down(idx, it):
            e, grp = it
            s_ = e % 2
            h_t = hT[idx % 2]
            for t4 in range(4):
                ti = grp * 4 + t4
                for half in range(2):
                    pd, _ = P.v((0, 1, 7)[dcount[0] % 3])
                    dcount[0] += 1
                    for f in range(2):
                        k.op("pe", "matmul", out=pd, lhsT=h_t[:, f, t4 * 128:(t4 + 1) * 128],
                             rhs=wdb[s_][:, f, half * 512:(half + 1) * 512], start=(f == 0), stop=(f == 1), mark=(f == 1))
                    k.op("dve", "tensor_tensor", out=acc[ti][:, half * 512:(half + 1) * 512],
                         in0=acc[ti][:, half * 512:(half + 1) * 512], in1=pd, op=ALU.add)

        load_w(0)
        emit_gu(items[0])
        emit_elem(0)
        for idx, it in enumerate(items):
            if idx + 1 < len(items):
                nxt = items[idx + 1]
                if nxt[0] != it[0]:
                    load_w(nxt[0])
                emit_gu(nxt)
                emit_elem(idx + 1)
            emit_down(idx, it)
        for ti in range(ntile):
            tok0 = sg * SG + ti * 128
            o_ = ot[ti % 2]
            layer_norm_tile(k, acc[ti].a, o_.a, lnt[2], lnt[3], eps_t, tmp.a, st, mv, rstd, nmr)
            dst_ap = xo.h[tok0:tok0 + 128, :]
            k.dma("sp", V(Tile(k, dst_ap, "xo_part"), dst_ap), o_.a)


def load_xT_group(k, P, x_d, tok0, ntok, xin, xbf, xT, idb, bank):
    for t in range(ntok // 128):
        xb = xbf[t % 2]
        k.dma("pool", xb.a, V(x_d, x_d.h[tok0 + t * 128: tok0 + (t + 1) * 128, :]))
        pt, _ = P.v(bank + (t % 2))
        ptb = pt.bitcast(BF16)
        for c in range(8):
            k.op("pe", "transpose", out=ptb[:, c * 128:(c + 1) * 128], in_=xb[:, c * 128:(c + 1) * 128],
                 identity=idb.a, mark=(c == 7))
        k.op("dve", "tensor_copy", out=xT[:, :, t * 128:(t + 1) * 128], in_=ptb.re("p (c t) -> p c t", c=8))


def run_streams_sb(gens, nslot):
    pending = list(gens)
    active = [None] * nslot
    while True:
        progressed = False
        for s_ in range(nslot):
            if active[s_] is None and pending:
                active[s_] = pending.pop(0)(s_)
            if active[s_] is not None:
                try:
                    next(active[s_])
                except StopIteration:
                    active[s_] = None
                progressed = True
        if not progressed and not pending:
            break


def sb_stream(k, P, slot, h, qb, qt, kt, r0, Vt, Oa, NTRI, LSTR, e_s, p_s, a_s, w_s):
    zb = P.v(0 + slot)[0]
    lab = P.v(2 + slot)[0]
    ob = P.v(4 + slot)[0]
    chunks = [(4 * qb + j, 128 * j) for j in (3, 2, 1, 0)] + [(kc, 0) for kc in range(4 * qb - 1, -1, -1)]

    def zmm(kc, c0):
        k.op("pe", "matmul", out=zb[:, c0:512], lhsT=kt[r0:r0 + 64, kc * 128:(kc + 1) * 128],
             rhs=qt[r0:r0 + 64, qb * 512 + c0:(qb + 1) * 512], start=True, stop=True, mark=True)
    zmm(*chunks[0])
    yield
    prev = None
    first = True
    for ci, (kc, c0) in enumerate(chunks):
        e = e_s[ci % 2]
        p_cur = p_s[ci % 2]
        a = a_s[ci % 2]
        w = w_s[ci % 2]
        k.op("act", "activation", out=e[:, c0:512], in_=zb[:, c0:512], func=AF.Exp, scale=0.125)
        if kc >= 4 * qb:
            k.op("pool", "affine_select", out=e[:, c0:c0 + 128], in_=e[:, c0:c0 + 128],
                 pattern=[[1, 128]], compare_op=ALU.is_gt, fill=0.0, base=0, channel_multiplier=-1)
        k.op("act", "activation", out=p_cur[:, c0:512], in_=e[:, c0:512], func=AF.Ln, bias=1.0, scale=1.0)
        yield
        if prev is not None:
            pp, pc0 = prev
            k.op("pe", "matmul", out=lab[:, pc0:512], lhsT=LSTR.a, rhs=pp[:, pc0:512], start=False,
                 stop=False, skip_group_check=True)
        k.op("pe", "matmul", out=lab[:, c0:512], lhsT=NTRI.a, rhs=p_cur[:, c0:512],
             start=first, stop=True, skip_group_check=True, mark=True)
        if ci + 1 < len(chunks):
            zmm(*chunks[ci + 1])
        yield
        k.op("act", "activation", out=a[:, c0:512], in_=lab[:, c0:512], func=AF.Exp)
        k.op("dve", "tensor_tensor", out=w[:, c0:512], in0=a[:, c0:512], in1=e[:, c0:512], op=ALU.mult)
        yield
        for i in range(c0 // 128, 4):
            k.op("pe", "matmul", out=ob[:, i * 64:(i + 1) * 64], lhsT=w[:, i * 128:(i + 1) * 128],
                 rhs=Vt[:, kc, h * 64:(h + 1) * 64], start=(first and i == c0 // 128),
                 stop=True, skip_group_check=True, mark=(i == 3))
        first = False
        prev = (p_cur, c0)
        yield
    k.op("act", "copy", out=Oa[:, qb * 4:(qb + 1) * 4, h * 64:(h + 1) * 64],
         in_=ob[:, 0:256].re("p (i d) -> p i d", i=4))
    yield


def emit_phase_sb(k, P, S, x_d, w_d, oT_d, ident_d, nsub=2, row0=0):
    NTILE = S // 128
    NQB = S // 512
    idf = k.sb([128, 128], F32, "idf")
    k.dma("sp", idf.a, ident_d.a, "ld_c")
    idb = k.sb([128, 128], BF16, "idb")
    k.op("dve", "tensor_copy", out=idb.a, in_=idf.a)
    negones = k.sb([128, 128], BF16, "negones")
    k.op("pool", "memset", ap=negones.a, constant=-1.0, _writes=[negones.a])
    NTRI = k.sb([128, 128], BF16, "NTRI")
    k.op("pool", "affine_select", out=NTRI.a, in_=negones.a, pattern=[[-1, 128]], compare_op=ALU.is_ge,
         fill=0.0, base=0, channel_multiplier=1)
    LSTR = k.sb([128, 128], BF16, "LSTR")
    k.op("pool", "affine_select", out=LSTR.a, in_=negones.a, pattern=[[1, 128]], compare_op=ALU.is_gt,
         fill=0.0, base=0, channel_multiplier=-1)

    W = k.sb([128, 8, 768], BF16, "Wsb")
    QT = [k.sb([128, S], BF16, f"QT{i}") for i in range(2)]
    KT = [k.sb([128, S], BF16, f"KT{i}") for i in range(2)]
    Vt = k.sb([128, NTILE, 256], BF16, "Vt")
    Oa = k.sb([128, NTILE, 256], BF16, "Oa")
    xin = None
    xbf = [k.sb([128, 1024], BF16, f"xbf{i}") for i in range(2)]
    xT = [k.sb([128, 8, 512], BF16, f"xT{i}") for i in range(2)]
    NS = 2
    e_t = [[k.sb([128, 512], F32, f"e{i}_{j}") for j in range(2)] for i in range(NS)]
    p_t = [[k.sb([128, 512], BF16, f"p{i}_{j}") for j in range(2)] for i in range(NS)]
    a_t = [[k.sb([128, 512], BF16, f"a{i}_{j}") for j in range(2)] for i in range(NS)]
    w_t = [[k.sb([128, 512], BF16, f"w{i}_{j}") for j in range(2)] for i in range(NS)]
    oTs = [k.sb([128, 2, 128], BF16, f"oTs{i}") for i in range(2)]

    wv = w_d.h[:, :].rearrange("(c p) n -> p c n", p=128)
    for sub in range(nsub):
        for j, base in enumerate((0, 512, 1024)):
            k.dma("pool", W[:, :, j * 256:(j + 1) * 256],
                  V(w_d, wv[:, :, base + sub * 256: base + (sub + 1) * 256]), "ld_w")
        for g4 in range(S // 512):
            tok0 = g4 * 512
            xT_g = xT[g4 % 2]
            load_xT_group(k, P, x_d, tok0, 512, xin, xbf, xT_g, idb, 0)
            for j in range(4):
                pq, _ = P.v(2 + (j % 2))
                for c in range(8):
                    k.op("pe", "matmul", out=pq, lhsT=W[:, c, j * 128:(j + 1) * 128], rhs=xT_g[:, c, :],
                         start=(c == 0), stop=(c == 7), mark=(c == 7))
                dst = (QT if j < 2 else KT)[j % 2]
                k.op("act", "copy", out=dst[:, tok0:tok0 + 512], in_=pq)
            for t in range(4):
                pv, _ = P.v(4 + (t % 2))
                for c in range(8):
                    k.op("pe", "matmul", out=pv[:, 0:256], lhsT=xT_g[:, c, t * 128:(t + 1) * 128],
                         rhs=W[:, c, 512:768], start=(c == 0), stop=(c == 7), mark=(c == 7))
                k.op("dve", "tensor_copy", out=Vt[:, g4 * 4 + t, :], in_=pv[:, 0:256])
        def mk_sb(h, qb):
            def fac(slot):
                return sb_stream(k, P, slot, h, qb, QT[h // 2], KT[h // 2], (h % 2) * 64, Vt, Oa, NTRI, LSTR,
                                 e_t[slot], p_t[slot], a_t[slot], w_t[slot])
            return fac
        run_streams_sb([mk_sb(h, qb) for qb in range(NQB) for h in range(4)], NS)
        for t in range(NTILE):
            pt, _ = P.v(6 + (t % 2))
            ptb = pt.bitcast(BF16)
            for c in range(2):
                k.op("pe", "transpose", out=ptb[:, c * 128:(c + 1) * 128], in_=Oa[:, t, c * 128:(c + 1) * 128],
                     identity=idb.a, mark=(c == 1))
            o_ = oTs[t % 2]
            k.op("dve", "tensor_copy", out=o_.a, in_=ptb[:, 0:256].re("p (c t) -> p c t", c=2))
            dst_ap = oT_d.h[row0 + sub * 256:row0 + (sub + 1) * 256, t * 128:(t + 1) * 128].rearrange("(c p) t -> p c t", p=128)
            k.dma("sp", V(Tile(k, dst_ap, "oT_part"), dst_ap), o_.a)


import math

SCALE = 0.125
NEG = -30000.0


def run_streams(gens, nslot):
    pending = list(gens)
    active = [None] * nslot
    while True:
        progressed = False
        for s in range(nslot):
            if active[s] is None and pending:
                active[s] = pending.pop(0)(s)
            if active[s] is not None:
                try:
                    next(active[s])
                except StopIteration:
                    active[s] = None
                progressed = True
        if not progressed and not pending:
            break


def flash_stream(k, slot, zb, ob, e_tiles, qv_fn, chunks, nv, negM, epilogue):
    first = True
    for ci, ch in enumerate(chunks):
        c0, c1 = ch["c0"], ch["c1"]
        e = e_tiles[ci % 2]
        has_mask = ch.get("mask") is not None
        k.op("pe", "matmul", out=zb[:, c0:c1], lhsT=ch["kT"], rhs=qv_fn(c0, c1), start=True, stop=not has_mask,
             mark=not has_mask, skip_group_check=True)
        if has_mask:
            ml, mr = ch["mask"]
            k.op("pe", "matmul", out=zb[:, c0:c1], lhsT=ml, rhs=mr(c0, c1), start=False, stop=True, mark=True,
                 skip_group_check=True)
        yield
        k.op("act", "activation", out=e[:, c0:c1], in_=zb[:, c0:c1], func=AF.Exp, scale=SCALE, bias=negM)
        for (lo, hi, base, cm, step, op) in ch.get("aff", []):
            k.op("pool", "affine_select", out=e[:, lo:hi], in_=e[:, lo:hi], pattern=[[step, hi - lo]],
                 compare_op=op, fill=0.0, base=base, channel_multiplier=cm)
        yield
        subs = list(range(c0 // 128, (c1 + 127) // 128))
        for i in subs:
            k.op("pe", "matmul", out=ob[:, i * nv:(i + 1) * nv], lhsT=e[:, i * 128:(i + 1) * 128], rhs=ch["v"],
                 start=first, stop=True, skip_group_check=True, mark=(i == subs[-1]))
            first = False
        yield
    epilogue()
    yield


def rope_group(k, pos_t, invf, g4, tmps):
    posf, y, yy, ki, kf, outs = tmps
    k.op("dve", "tensor_copy", out=posf.a, in_=pos_t[:, g4 * 4:(g4 + 1) * 4])
    k.op("dve", "tensor_tensor", out=y.a, in0=posf.a.unsq(2).bc([128, 4, 32]),
         in1=invf.a.unsq(1).bc([128, 4, 32]), op=ALU.mult)
    res = []
    for j, shift in enumerate((0.0, 0.25)):
        k.op("dve", "tensor_scalar", out=yy.a, in0=y.a, scalar1=shift, scalar2=None, op0=ALU.add)
        k.op("dve", "tensor_copy", out=ki.a, in_=yy.a)
        k.op("dve", "tensor_copy", out=kf.a, in_=ki.a)
        k.op("dve", "tensor_tensor", out=yy.a, in0=yy.a, in1=kf.a, op=ALU.subtract)
        k.op("dve", "tensor_scalar", out=kf.a, in0=yy.a, scalar1=0.5, scalar2=None, op0=ALU.is_gt)
        k.op("dve", "tensor_tensor", out=yy.a, in0=yy.a, in1=kf.a, op=ALU.subtract)
        k.op("dve", "tensor_scalar", out=kf.a, in0=yy.a, scalar1=-0.5, scalar2=None, op0=ALU.is_lt)
        k.op("dve", "tensor_tensor", out=yy.a, in0=yy.a, in1=kf.a, op=ALU.add)
        t = outs[g4 % 2][j]
        k.op("act", "activation", out=t.a, in_=yy.a, func=AF.Sin, scale=2.0 * math.pi * (1 - 1e-6))
        res.append(t)
    return res[1], res[0]


def rope_tmps(k):
    posf = k.sb([128, 4], F32, "posf")
    y = k.sb([128, 4, 32], F32, "rope_y")
    yy = k.sb([128, 4, 32], F32, "rope_yy")
    ki = k.sb([128, 4, 32], I32, "rope_ki")
    kf = k.sb([128, 4, 32], F32, "rope_kf")
    outs = [[k.sb([128, 4, 32], F32, f"rope_o{i}_{j}") for j in range(2)] for i in range(2)]
    return (posf, y, yy, ki, kf, outs)


def rope_apply(k, pf, nh, cos, sin, t1, t2, out_bf):
    pr = pf.re("p (h t d) -> p h t d", h=nh, t=2)
    t1v = t1[:, 0:nh * 64].re("p (h t d) -> p h t d", h=nh, t=2)
    t2v = t2[:, 0:nh * 64].re("p (h t d) -> p h t d", h=nh, t=2)
    ob = out_bf.re("p (h t d) -> p h t d", h=nh, t=2)
    cb = cos.unsq(1).unsq(1).bc([128, nh, 2, 32])
    sb_ = sin.unsq(1).bc([128, nh, 32])
    k.op("dve", "tensor_tensor", out=t1v, in0=pr, in1=cb, op=ALU.mult)
    k.op("pool", "tensor_tensor", out=t2v[:, :, 0, :], in0=pr[:, :, 1, :], in1=sb_, op=ALU.mult)
    k.op("pool", "tensor_tensor", out=t2v[:, :, 1, :], in0=pr[:, :, 0, :], in1=sb_, op=ALU.mult)
    k.op("dve", "tensor_tensor", out=ob[:, :, 0, :], in0=t1v[:, :, 0, :], in1=t2v[:, :, 0, :], op=ALU.subtract)
    k.op("dve", "tensor_tensor", out=ob[:, :, 1, :], in0=t1v[:, :, 1, :], in1=t2v[:, :, 1, :], op=ALU.add)


def bound_negM(k, P, nq_t, nk_t, idf, ones1, out_negM, bank, scratch):
    mq, mm, row = scratch
    k.op("dve", "reduce_max", out=mq[:, 0:1], in_=nq_t, axis=AX.X)
    k.op("dve", "reduce_max", out=mq[:, 1:2], in_=nk_t, axis=AX.X)
    pb, _ = P.v(bank)
    k.op("pe", "transpose", out=pb[0:2, 0:128], in_=mq.a, identity=idf.a, mark=True)
    k.op("dve", "reduce_max", out=mm.a, in_=pb[0:2, 0:128], axis=AX.X)
    k.op("pe", "transpose", out=pb[0:1, 128:130], in_=mm.a, identity=idf[0:2, 0:2], mark=True)
    k.op("dve", "tensor_copy", out=row[:, 0:2], in_=pb[0:1, 128:130])
    k.op("dve", "tensor_tensor", out=row[:, 2:3], in0=row[:, 0:1], in1=row[:, 1:2], op=ALU.mult)
    k.op("act", "activation", out=row[:, 2:3], in_=row[:, 2:3], func=AF.Ln)
    k.op("act", "activation", out=row[:, 2:3], in_=row[:, 2:3], func=AF.Exp, scale=0.5)
    k.op("dve", "tensor_scalar", out=row[:, 3:4], in0=row[:, 2:3], scalar1=-SCALE * 1.02, scalar2=None, op0=ALU.mult)
    k.op("pe", "matmul", out=pb[:, 132:133], lhsT=ones1.a, rhs=row[:, 3:4], start=True, stop=True, mark=True)
    k.op("dve", "tensor_copy", out=out_negM, in_=pb[:, 132:133])


def emit_out_T(k, P, OUT_bf, oT_d, row0, tok0, idb, oTs, bank, idx):
    dst_ap = oT_d.h[row0:row0 + 256, tok0:tok0 + 128].rearrange("(c p) t -> p c t", p=128)
    pt, _ = P.v(bank)
    ptb = pt.bitcast(BF16)
    for c in range(2):
        k.op("pe", "transpose", out=ptb[:, c * 128:(c + 1) * 128], in_=OUT_bf[:, c * 128:(c + 1) * 128],
             identity=idb.a, mark=(c == 1))
    o_ = oTs[idx % 2]
    k.op("dve", "tensor_copy", out=o_.a, in_=ptb[:, 0:256].re("p (c t) -> p c t", c=2))
    k.dma("sp", V(Tile(k, dst_ap, "oT_part"), dst_ap), o_.a)


def emit_phase_l0(k, P, S, x_d, pos_d, wn_d, wm_d, cw, consts, oT_d, do_nsa=True, do_moba=True, row_nsa=0, row_moba=256):
    NTILE = S // 128
    NQB = S // 512
    NCMP = (S - 32) // 16 + 1
    NCC = (NCMP + 127) // 128
    NSLC = S // 64
    NBLK = S // 256
    idf = k.sb([128, 128], F32, "idf")
    k.dma("sp", idf.a, consts["ident"].a, "ld_c")
    idb = k.sb([128, 128], BF16, "idb")
    k.op("dve", "tensor_copy", out=idb.a, in_=idf.a)
    ones1 = k.sb([1, 128], F32, "ones1")
    k.op("pool", "memset", ap=ones1.a, constant=1.0, _writes=[ones1.a])
    invf = k.sb([128, 32], F32, "invf")
    k.dma("sp", invf.a, consts["invf"].a, "ld_c")
    pos_t = k.sb([128, NTILE], I32, "pos_t")
    k.dma("sp", pos_t.a, pos_d.a, "ld_c")
    rtm = rope_tmps(k)
    xin = None
    xbf = [k.sb([128, 1024], BF16, f"xbf{i}") for i in range(2)]
    xT = [k.sb([128, 8, 512], BF16, "xT0")] * 2
    pf = k.sb([128, 780], F32, "pf")
    t1 = k.sb([128, 576], F32, "rt1")
    t2 = k.sb([128, 576], F32, "rt2")
    Rb = k.sb([128, 640], BF16, "Rb")
    sqj = k.sb([128, 640], F32, "sqj")
    e_t = [[k.sb([128, 512], BF16, f"e{s}_{j}") for j in range(2)] for s in range(4)]
    oTs = [k.sb([128, 2, 128], BF16, f"oTs{i}") for i in range(2)]
    OUT = k.sb([128, 4, 256], F32, "OUT")
    OUTb = k.sb([128, 4, 256], BF16, "OUTb")
    negM = [k.sb([128, 1], F32, f"negM{i}") for i in range(4)]
    sc_mq = k.sb([128, 2], F32, "sc_mq")
    sc_mk = k.sb([2, 1], F32, "sc_mk")
    sc_row = k.sb([1, 4], F32, "sc_row")
    rz = k.sb([128, 4 * 4], F32, "rz")
    coef = k.sb([128, 4 * 4], F32, "coef")
    QT = [k.sb([128, S], BF16, f"QT{i}") for i in range(2)]
    KT = [k.sb([128, S], BF16, f"KT{i}") for i in range(2)]
    CT = k.sb([128, S], BF16, "CT")
    Vall = k.sb([128, NTILE, 260], BF16, "Vall")
    k.op("pool", "memset", ap=Vall.a, constant=1.0, _writes=[Vall.a])
    Wall = k.sb([128, 8, 780], BF16, "Wall")

    if do_nsa:
        W = Wall
        k.dma("pool", W.a, V(wn_d, wn_d.h[:, :].rearrange("(c p) n -> p c n", p=128)), "ld_w")
        VS = V(Vall, Vall.h[:, :, 0:130].rearrange("p t (a d) -> p t a d", a=2))
        GL = k.sb([128, NTILE, 12], F32, "GL")
        NQ = k.sb([128, 3, NTILE], F32, "NQ")
        for g4 in range(S // 512):
            xT_g = xT[g4 % 2]
            load_xT_group(k, P, x_d, g4 * 512, 512, xin, xbf, xT_g, idb, 0)
            COS4, SIN4 = rope_group(k, pos_t, invf, g4, rtm)
            for t in range(4):
                ti = g4 * 4 + t
                pp, pp_x = P.v(2 + 2 * (t % 2), 2)
                for (a, b) in ((0, 512), (512, 780)):
                    for c in range(8):
                        k.op("pe", "matmul", out=pp[:, a:b], lhsT=xT_g[:, c, t * 128:(t + 1) * 128], rhs=W[:, c, a:b],
                             start=(c == 0), stop=(c == 7), _writes=pp_x, mark=(c == 7 and a == 512))
                k.op("act", "copy", out=pf[:, 0:780], in_=pp[:, 0:780], _reads=pp_x)
                rope_apply(k, pf[:, 0:576], 9, COS4[:, t, :], SIN4[:, t, :], t1, t2, Rb[:, 0:576])
                k.op("pool", "tensor_copy", out=Rb[:, 576:640], in_=pf[:, 576:640])
                k.op("pool", "tensor_copy", out=VS[:, ti, :, 0:64], in_=pf[:, 640:768].re("p (a d) -> p a d", a=2))
                k.op("pool", "tensor_copy", out=GL[:, ti, :], in_=pf[:, 768:780])
                for j, (a, b) in enumerate(((0, 256), (320, 448), (512, 576))):
                    k.op("act", "activation", out=sqj[:, a:b], in_=Rb[:, a:b], func=AF.Square, accum_out=NQ[:, j, ti:ti + 1])
                pt, _ = P.v(6 + (t % 2))
                ptb = pt.bitcast(BF16)
                for j in range(5):
                    k.op("pe", "transpose", out=ptb[:, j * 128:(j + 1) * 128], in_=Rb[:, j * 128:(j + 1) * 128],
                         identity=idb.a, mark=(j == 4))
                for j, dst in enumerate((QT[0], QT[1], KT[0], KT[1], CT)):
                    k.op("act" if j % 2 else "dve", "copy" if j % 2 else "tensor_copy", out=dst[:, ti * 128:(ti + 1) * 128],
                         in_=ptb[:, j * 128:(j + 1) * 128])
        G = GL
        k.op("act", "activation", out=G.a, in_=GL.a, func=AF.Sigmoid)
        W1 = k.sb([128, 32, 128], BF16, "W1")
        k.dma("pool", W1[0:64], V(cw["w1k"], cw["w1k"].h[:, :].rearrange("(l d) h -> d l h", d=64)), "ld_w")
        k.dma("pool", W1[64:128], V(cw["w1v"], cw["w1v"].h[:, :].rearrange("(l d) h -> d l h", d=64)), "ld_w")
        PF = k.sb([128, 32], BF16, "PF")
        k.dma("pool", PF.a, cw["posT"].a)
        W2 = [k.sb([128, 128], BF16, "W2k2"), k.sb([128, 64], BF16, "W2v")]
        k.dma("pool", W2[0][:, 0:64], cw["w2k"].a, "ld_w")
        k.dma("pool", W2[0][:, 64:128], cw["w2k"].a, "ld_w")
        k.dma("pool", W2[1].a, cw["w2v"].a, "ld_w")
        KCT = k.sb([128, NCC * 128], BF16, "KCT")
        k.op("pool", "memset", ap=KCT.a, constant=0.0, _writes=[KCT.a])
        NV = 193
        VCA = k.sb([128, NCC, NV], BF16, "VCA")
        k.op("pool", "memset", ap=VCA.a, constant=1.0, _writes=[VCA.a])
        for c in range(NCC):
            k.op("pool", "affine_select", out=VCA[:, c, 65:193], in_=VCA[:, c, 65:193], pattern=[[-4, 128]],
                 compare_op=ALU.is_ge, fill=0.0, base=128 * c + 1, channel_multiplier=1)
            k.op("pool", "affine_select", out=VCA[:, c, 65:193], in_=VCA[:, c, 65:193], pattern=[[4, 128]],
                 compare_op=ALU.is_ge, fill=0.0, base=3 - 128 * c, channel_multiplier=-1)
        cb = k.sb([128, 2], F32, "cbias")
        gu = [k.sb([128, 512], F32, f"gu{i}") for i in range(3)]
        gact = [k.sb([128, 512], BF16, f"gact{i}") for i in range(2)]
        k.op("pool", "memset", ap=gact[1].a, constant=0.0, _writes=[gact[1].a])
        for kv in range(2):
            r0 = kv * 64
            ph, _ = P.v(0 + kv)
            for l in range(32):
                k.op("pe", "matmul", out=ph[:, 0:NCMP], lhsT=W1[r0:r0 + 64, l, :],
                     rhs=CT[r0:r0 + 64, l:l + 16 * (NCMP - 1) + 1:16], start=(l == 0), stop=(l == 31), mark=(l == 31))
            pbias, _ = P.v(2 + kv)
            for l in range(32):
                k.op("pe", "matmul", out=pbias[:, 0:1], lhsT=W1[r0:r0 + 64, l, :], rhs=PF[r0:r0 + 64, l:l + 1], start=(l == 0),
                     stop=(l == 31), mark=(l == 31))
            k.op("dve", "tensor_copy", out=cb[:, kv:kv + 1], in_=pbias[:, 0:1])
            u, u2, th = gu
            n_ = NCMP
            k.op("dve", "tensor_scalar", out=u[:, 0:n_], in0=ph[:, 0:n_], scalar1=cb[:, kv:kv + 1], scalar2=None, op0=ALU.add)
            k.op("dve", "tensor_tensor", out=u2[:, 0:n_], in0=u[:, 0:n_], in1=u[:, 0:n_], op=ALU.mult)
            k.op("dve", "tensor_scalar", out=u2[:, 0:n_], in0=u2[:, 0:n_], scalar1=0.044715, scalar2=1.0, op0=ALU.mult, op1=ALU.add)
            k.op("dve", "tensor_tensor", out=u2[:, 0:n_], in0=u2[:, 0:n_], in1=u[:, 0:n_], op=ALU.mult)
            k.op("act", "activation", out=th[:, 0:n_], in_=u2[:, 0:n_], func=AF.Tanh, scale=0.7978845608028654)
            k.op("dve", "tensor_scalar", out=th[:, 0:n_], in0=th[:, 0:n_], scalar1=0.5, scalar2=0.5, op0=ALU.mult, op1=ALU.add)
            k.op("dve", "tensor_tensor", out=gact[kv][:, 0:n_], in0=th[:, 0:n_], in1=u[:, 0:n_], op=ALU.mult)
            if kv == 0:
                pk, _ = P.v(4)
                k.op("pe", "matmul", out=pk[:, 0:n_], lhsT=W2[0].a, rhs=gact[0][:, 0:n_], start=True, stop=True, mark=True)
                k.op("dve", "tensor_copy", out=KCT[:, 0:n_], in_=pk[:, 0:n_])
                k.op("act", "activation", out=sqj[0:64, 0:n_], in_=pk[0:64, 0:n_], func=AF.Square)
            else:
                for c in range(NCC):
                    pvv, _ = P.v(5)
                    k.op("pe", "matmul", out=pvv[:, 0:64], lhsT=gact[1][:, c * 128:(c + 1) * 128], rhs=W2[1].a, start=True,
                         stop=True, mark=True)
                    k.op("dve", "tensor_copy", out=VCA[:, c, 0:64], in_=pvv[:, 0:64])
        ones64 = k.sb([64, 1], F32, "ones64")
        k.op("pool", "memset", ap=ones64.a, constant=1.0, _writes=[ones64.a])
        pn, _ = P.v(6)
        k.op("pe", "matmul", out=pn[0:1, 0:NCMP], lhsT=ones64.a, rhs=sqj[0:64, 0:NCMP], start=True, stop=True, mark=True)
        nkc = k.sb([128, 1], F32, "nkc")
        k.op("pool", "memset", ap=nkc.a, constant=0.0, _writes=[nkc.a])
        k.op("dve", "reduce_max", out=nkc[0:1, 0:1], in_=pn[0:1, 0:NCMP], axis=AX.X)
        bound_negM(k, P, NQ[:, 0, :], NQ[:, 1, :], idf, ones1, negM[0].a, 7, (sc_mq, sc_mk, sc_row))
        bound_negM(k, P, NQ[:, 0, :], nkc.a, idf, ones1, negM[1].a, 7, (sc_mq, sc_mk, sc_row))
        WV = k.sb([128, 256], F32, "WV")
        WADD = k.sb([128, 256], F32, "WADD")
        k.dma("sp", WV.a, consts["wv"].a, "ld_c")
        k.dma("sp", WADD.a, consts["wadd"].a, "ld_c")
        G64 = CT
        k.op("pool", "memset", ap=G64.a, constant=0.0, _writes=[G64.a])
        k.dma("sp", G64[0:NSLC], consts["g64"].a, "ld_c")
        IMP = k.sb([128, 4, 128], F32, "IMP")
        NMT = k.sb([128, 512], BF16, "NMT")
        k.op("pool", "memset", ap=NMT.a, constant=0.0, _writes=[NMT.a])
        sc_s = [k.sb([128, 128], F32, f"sc_s{i}") for i in range(2)]
        m8 = k.sb([128, 16], F32, "m8")
        nmb = k.sb([128, 128], BF16, "nmb")
        for qb in range(NQB):
            q0 = qb * 512
            first_touch = {"imp": [True] * 4, "out": [[True] * 4 for _ in range(4)]}

            def mk_cmp(h, half, qb=qb, q0=q0):
                def fac(slot):
                    zb, _ = P.v(slot * 2)
                    ob, _ = P.v(slot * 2 + 1)
                    qt = QT[h // 2]
                    r0 = (h % 2) * 64
                    cbase = q0 + half * 256
                    chunks = []
                    for c in range(NCC):
                        if 16 * (128 * c) + 31 > cbase + 255:
                            continue
                        chunks.append(dict(kT=KCT[r0:r0 + 64, c * 128:(c + 1) * 128], v=VCA[:, c, :], c0=0, c1=256,
                                           aff=[(0, 256, cbase - 2048 * c - 31, -16, 1, ALU.is_ge)]))

                    def epi():
                        for i in range(2):
                            sub = half * 2 + i
                            ti = qb * 4 + sub
                            rzv = rz[:, slot * 4 + i: slot * 4 + i + 1]
                            cfv = coef[:, slot * 4 + i: slot * 4 + i + 1]
                            if not chunks:
                                if first_touch["imp"][sub]:
                                    k.op("pool", "memset", ap=IMP[:, sub, :], constant=0.0, _writes=[IMP.a])
                                    first_touch["imp"][sub] = False
                                if first_touch["out"][sub][h]:
                                    k.op("pool", "memset", ap=OUT[:, sub, h * 64:(h + 1) * 64], constant=0.0, _writes=[OUT.a])
                                    first_touch["out"][sub][h] = False
                                continue
                            acc = ob[:, i * NV:(i + 1) * NV]
                            k.op("dve", "tensor_scalar", out=rzv, in0=acc[:, 64:65], scalar1=1e-30, scalar2=None, op0=ALU.max)
                            k.op("dve", "reciprocal", out=rzv, in_=rzv)
                            k.op("dve", "tensor_tensor", out=cfv, in0=rzv, in1=G[:, ti, h * 3:h * 3 + 1], op=ALU.mult)
                            k.op("dve", "tensor_scalar", out=OUT[:, sub, h * 64:(h + 1) * 64], in0=acc[:, 0:64], scalar1=cfv,
                                 scalar2=None, op0=ALU.mult)
                            first_touch["out"][sub][h] = False
                            if first_touch["imp"][sub]:
                                k.op("dve", "tensor_scalar", out=IMP[:, sub, :], in0=acc[:, 65:193], scalar1=rzv, scalar2=None,
                                     op0=ALU.mult)
                                first_touch["imp"][sub] = False
                            else:
                                k.op("dve", "scalar_tensor_tensor", out=IMP[:, sub, :], in0=acc[:, 65:193], scalar=rzv,
                                     in1=IMP[:, sub, :], op0=ALU.mult, op1=ALU.add)
                    qv = lambda c0, c1: qt[r0:r0 + 64, cbase + c0:cbase + c1]
                    return flash_stream(k, slot, zb, ob, e_t[slot], qv, chunks, NV, negM[1].a, epi)
                return fac
            run_streams([mk_cmp(h, half) for h in range(4) for half in range(2)], 4)
            for sub in range(4):
                qt_i = qb * 4 + sub
                sc0, sc1 = sc_s
                lo = 128 - 2 * qt_i
                k.op("dve", "tensor_tensor", out=sc0.a, in0=IMP[:, sub, :], in1=WV[:, lo:lo + 128], op=ALU.mult)
                k.op("dve", "tensor_tensor", out=sc0.a, in0=sc0.a, in1=WADD[:, lo:lo + 128], op=ALU.add)
                if qt_i >= 1:
                    k.op("dve", "tensor_scalar", out=sc0[:, 0:1], in0=sc0[:, 0:1], scalar1=1.0e4, scalar2=None, op0=ALU.add)
                k.op("dve", "max", out=m8[:, 0:8], in_=sc0.a)
                k.op("dve", "match_replace", out=sc1.a, in_to_replace=m8[:, 0:8], in_values=sc0.a, imm_value=-1e30)
                k.op("dve", "max", out=m8[:, 8:16], in_=sc1.a)
                k.op("dve", "tensor_scalar", out=sc1.a, in0=sc0.a, scalar1=m8[:, 15:16], scalar2=None, op0=ALU.is_ge)
                k.op("dve", "tensor_tensor", out=sc1.a, in0=sc1.a, in1=WV[:, lo:lo + 128], op=ALU.mult)
                k.op("dve", "tensor_scalar", out=nmb.a, in0=sc1.a, scalar1=-NEG, scalar2=NEG, op0=ALU.mult, op1=ALU.add)
                pt, _ = P.v(7)
                ptb = pt.bitcast(BF16)
                k.op("pe", "transpose", out=ptb[:, 0:128], in_=nmb.a, identity=idb.a, mark=True)
                k.op("act", "copy", out=NMT[:, sub * 128:(sub + 1) * 128], in_=ptb[:, 0:128])
            def mk_flash(h, br, qb=qb, q0=q0):
                def fac(slot):
                    zb, _ = P.v(slot * 2)
                    ob, _ = P.v(slot * 2 + 1)
                    qt = QT[h // 2]
                    r0 = (h % 2) * 64
                    chunks = []
                    if br == 1:
                        for kc in range(0, 4 * qb + 4):
                            j = kc - 4 * qb
                            c0 = 128 * j if j > 0 else 0
                            aff = [(c0, c0 + 128, 0, -1, 1, ALU.is_ge)] if j >= 0 else []
                            chunks.append(dict(kT=KT[0][r0:r0 + 64, kc * 128:(kc + 1) * 128], v=VS[:, kc, 0, :], c0=c0, c1=512, aff=aff,
                                               mask=(G64[:, kc * 128:(kc + 1) * 128], lambda a, b: NMT[:, a:b])))
                    else:
                        for kc in range(max(0, 4 * qb - 4), 4 * qb + 4):
                            j = kc - 4 * qb
                            if j >= 0:
                                c0, c1 = 128 * j, 512
                                aff = [(c0, c0 + 128, 0, -1, 1, ALU.is_ge)]
                            else:
                                jp = j + 4
                                c0, c1 = 0, 128 * (jp + 1)
                                aff = [(128 * jp, 128 * jp + 128, -1, 1, -1, ALU.is_ge)]
                            chunks.append(dict(kT=KT[1][r0:r0 + 64, kc * 128:(kc + 1) * 128], v=VS[:, kc, 1, :], c0=c0, c1=c1, aff=aff))

                    def epi():
                        for sub in range(4):
                            ti = qb * 4 + sub
                            rzv = rz[:, slot * 4 + sub: slot * 4 + sub + 1]
                            cfv = coef[:, slot * 4 + sub: slot * 4 + sub + 1]
                            acc = ob[:, sub * 65:(sub + 1) * 65]
                            k.op("dve", "tensor_scalar", out=rzv, in0=acc[:, 64:65], scalar1=1e-30, scalar2=None, op0=ALU.max)
                            k.op("dve", "reciprocal", out=rzv, in_=rzv)
                            k.op("dve", "tensor_tensor", out=cfv, in0=rzv, in1=G[:, ti, h * 3 + br:h * 3 + br + 1], op=ALU.mult)
                            k.op("dve", "scalar_tensor_tensor", out=OUT[:, sub, h * 64:(h + 1) * 64], in0=acc[:, 0:64], scalar=cfv,
                                 in1=OUT[:, sub, h * 64:(h + 1) * 64], op0=ALU.mult, op1=ALU.add)
                    qv = lambda c0, c1: qt[r0:r0 + 64, q0 + c0:q0 + c1]
                    return flash_stream(k, slot, zb, ob, e_t[slot], qv, chunks, 65, negM[0].a, epi)
                return fac
            run_streams([mk_flash(h, br) for h in range(4) for br in (1, 2)], 4)
            k.op("act", "copy", out=OUTb.a, in_=OUT.a)
            for sub in range(4):
                emit_out_T(k, P, OUTb[:, sub, :], oT_d, row_nsa, (qb * 4 + sub) * 128, idb, oTs, 7, sub)

    if do_moba:
        NB = max(NBLK, 8)
        Wm = Wall
        k.dma("pool", Wm[:, :, 0:768], V(wm_d, wm_d.h[:, :].rearrange("(c p) n -> p c n", p=128)))
        VB = V(Vall, Vall.h[:, :, :].rearrange("p t (a d) -> p t a d", a=4))
        k.op("pool", "memset", ap=Vall.a, constant=1.0, _writes=[Vall.a])
        NQm = k.sb([128, 2, NTILE], F32, "NQm")
        for g4 in range(S // 512):
            xT_g = xT[g4 % 2]
            load_xT_group(k, P, x_d, g4 * 512, 512, xin, xbf, xT_g, idb, 0)
            COS4, SIN4 = rope_group(k, pos_t, invf, g4, rtm)
            for t in range(4):
                ti = g4 * 4 + t
                pp, pp_x = P.v(2 + 2 * (t % 2), 2)
                for (a, b) in ((0, 512), (512, 768)):
                    for c in range(8):
                        k.op("pe", "matmul", out=pp[:, a:b], lhsT=xT_g[:, c, t * 128:(t + 1) * 128], rhs=Wm[:, c, a:b],
                             start=(c == 0), stop=(c == 7), _writes=pp_x, mark=(c == 7 and a == 512))
                k.op("act", "copy", out=pf[:, 0:768], in_=pp[:, 0:768], _reads=pp_x)
                rope_apply(k, pf[:, 0:512], 8, COS4[:, t, :], SIN4[:, t, :], t1, t2, Rb[:, 0:512])
                k.op("pool", "tensor_copy", out=VB[:, ti, :, 0:64], in_=pf[:, 512:768].re("p (a d) -> p a d", a=4))
                for j, (a, b) in enumerate(((0, 256), (256, 512))):
                    k.op("act", "activation", out=sqj[:, a:b], in_=Rb[:, a:b], func=AF.Square, accum_out=NQm[:, j, ti:ti + 1])
                pt, _ = P.v(6 + (t % 2))
                ptb = pt.bitcast(BF16)
                for j in range(4):
                    k.op("pe", "transpose", out=ptb[:, j * 128:(j + 1) * 128], in_=Rb[:, j * 128:(j + 1) * 128],
                         identity=idb.a, mark=(j == 3))
                for j, dst in enumerate((QT[0], QT[1], KT[0], KT[1])):
                    k.op("act" if j % 2 else "dve", "copy" if j % 2 else "tensor_copy", out=dst[:, ti * 128:(ti + 1) * 128],
                         in_=ptb[:, j * 128:(j + 1) * 128])
        bound_negM(k, P, NQm[:, 0, :], NQm[:, 1, :], idf, ones1, negM[2].a, 7, (sc_mq, sc_mk, sc_row))
        KM = k.sb([128, 32], F32, "KM")
        KMt = k.sb([128, 32], F32, "KMt")
        KMhi = [k.sb([128, 32], BF16, f"KMhi{i}") for i in range(2)]
        KMlo = [k.sb([128, 32], BF16, f"KMlo{i}") for i in range(2)]
        for i in range(2):
            k.op("pool", "memset", ap=KMhi[i].a, constant=0.0, _writes=[KMhi[i].a])
            k.op("pool", "memset", ap=KMlo[i].a, constant=0.0, _writes=[KMlo[i].a])
            k.op("dve", "tensor_reduce", out=KM[:, 0:NBLK], in_=KT[i].a.re("p (n l) -> p n l", l=256), axis=AX.X, op=ALU.add)
            k.op("dve", "tensor_scalar", out=KM[:, 0:NBLK], in0=KM[:, 0:NBLK], scalar1=1.0 / 256, scalar2=None, op0=ALU.mult)
            k.op("dve", "tensor_copy", out=KMhi[i][:, 0:NBLK], in_=KM[:, 0:NBLK])
            k.op("dve", "tensor_tensor", out=KMt[:, 0:NBLK], in0=KM[:, 0:NBLK], in1=KMhi[i][:, 0:NBLK], op=ALU.subtract)
            k.op("dve", "tensor_copy", out=KMlo[i][:, 0:NBLK], in_=KMt[:, 0:NBLK])
        G256 = CT
        k.op("pool", "memset", ap=CT.a, constant=0.0, _writes=[CT.a])
        k.dma("sp", G256[0:NBLK], consts["g256"].a)
        PM = k.sb([128, 64], F32, "PM")
        k.dma("sp", PM.a, consts["pm"].a)
        NMTm = [k.sb([128, 512], BF16, f"NMTm{h}") for h in range(4)]
        for h in range(4):
            k.op("pool", "memset", ap=NMTm[h].a, constant=0.0, _writes=[NMTm[h].a])
        wk32 = k.sb([128, 32], F32, "wk32")
        nm32 = k.sb([128, 32], F32, "nm32")
        nmb32 = k.sb([128, 32], BF16, "nmb32")
        k.op("pool", "memset", ap=nmb32.a, constant=0.0, _writes=[nmb32.a])
        m8m = k.sb([128, 8], F32, "m8m")
        thr2 = k.sb([128, 1], F32, "thr2")
        for qb in range(NQB):
            q0 = qb * 512
            for h in range(4):
                r0 = (h % 2) * 64
                for sub in range(4):
                    qt_i = qb * 4 + sub
                    cur = qt_i // 2
                    pg, _ = P.v(6)
                    k.op("pe", "matmul", out=pg[:, 0:32], lhsT=QT[h // 2][r0:r0 + 64, qt_i * 128:(qt_i + 1) * 128],
                         rhs=KMhi[h // 2][r0:r0 + 64, :], start=True, stop=False)
                    k.op("pe", "matmul", out=pg[:, 0:32], lhsT=QT[h // 2][r0:r0 + 64, qt_i * 128:(qt_i + 1) * 128],
                         rhs=KMlo[h // 2][r0:r0 + 64, :], start=False, stop=True, mark=True)
                    k.op("dve", "tensor_tensor", out=wk32[:, 0:NB], in0=pg[:, 0:NB], in1=PM[:, 32 - cur:32 - cur + NB], op=ALU.add)
                    k.op("dve", "max", out=m8m.a, in_=wk32[:, 0:NB])
                    k.op("dve", "tensor_scalar", out=thr2.a, in0=m8m[:, 2:3], scalar1=-1e29, scalar2=None, op0=ALU.max)
                    k.op("dve", "tensor_scalar", out=nm32[:, 0:NB], in0=wk32[:, 0:NB], scalar1=thr2.a, scalar2=None, op0=ALU.is_ge)
                    k.op("dve", "tensor_scalar", out=nm32[:, 0:NB], in0=nm32[:, 0:NB], scalar1=-NEG, scalar2=NEG, op0=ALU.mult, op1=ALU.add)
                    k.op("dve", "memset", ap=nm32[:, cur:cur + 1], constant=0.0, _writes=[nm32.a])
                    k.op("dve", "tensor_copy", out=nmb32[:, 0:NB], in_=nm32[:, 0:NB])
                    pt, _ = P.v(7)
                    ptb = pt.bitcast(BF16)
                    k.op("pe", "transpose", out=ptb[0:32, 0:128], in_=nmb32.a, identity=idb.a, mark=True)
                    k.op("act", "copy", out=NMTm[h][0:32, sub * 128:(sub + 1) * 128], in_=ptb[0:32, 0:128])

            def mk_moba(h, qb=qb, q0=q0):
                def fac(slot):
                    zb, _ = P.v(slot * 2)
                    ob, _ = P.v(slot * 2 + 1)
                    qt = QT[h // 2]
                    r0 = (h % 2) * 64
                    chunks = []
                    for kc in range(0, 4 * qb + 4):
                        j = kc - 4 * qb
                        c0 = 128 * j if j > 0 else 0
                        aff = [(c0, c0 + 128, 0, -1, 1, ALU.is_ge)] if j >= 0 else []
                        chunks.append(dict(kT=KT[h // 2][r0:r0 + 64, kc * 128:(kc + 1) * 128], v=VB[:, kc, h, :], c0=c0, c1=512, aff=aff,
                                           mask=(G256[:, kc * 128:(kc + 1) * 128], lambda a, b, h=h: NMTm[h][:, a:b])))

                    def epi():
                        for sub in range(4):
                            rzv = rz[:, slot * 4 + sub: slot * 4 + sub + 1]
                            acc = ob[:, sub * 65:(sub + 1) * 65]
                            k.op("dve", "tensor_scalar", out=rzv, in0=acc[:, 64:65], scalar1=1e-30, scalar2=None, op0=ALU.max)
                            k.op("dve", "reciprocal", out=rzv, in_=rzv)
                            k.op("dve", "tensor_scalar", out=OUT[:, sub, h * 64:(h + 1) * 64], in0=acc[:, 0:64], scalar1=rzv,
                                 scalar2=None, op0=ALU.mult)
                    qv = lambda c0, c1: qt[r0:r0 + 64, q0 + c0:q0 + c1]
                    return flash_stream(k, slot, zb, ob, e_t[slot], qv, chunks, 65, negM[2].a, epi)
                return fac
            run_streams([mk_moba(h) for h in range(4)], 4)
            k.op("act", "copy", out=OUTb.a, in_=OUT.a)
            for sub in range(4):
                emit_out_T(k, P, OUTb[:, sub, :], oT_d, row_moba, (qb * 4 + sub) * 128, idb, oTs, 7, sub)


import ml_dtypes
from concourse.bass_utils import run_bass_kernel_spmd

_PROGS = {}
EVEN_WIDTHS = (512, 128, 128, 128, 128, 128, 128, 24, 512, 512, 512)
_SP = [0] + [int(v) for v in np.cumsum(EVEN_WIDTHS)[:-1]]


def _consts(S):
    half = 32
    invf = (10000.0 ** (-np.arange(half, dtype=np.float32) / half)).astype(np.float32)
    invf_t = np.tile((invf / np.float32(2 * np.pi)).astype(np.float32)[None, :], (128, 1))
    p = np.arange(128)[:, None]
    m = np.arange(256)[None, :]
    hp = (p >= 64).astype(np.int64)
    d = m - 128
    wv = (d <= hp).astype(np.float32)
    wadd = (1e4 * ((d == hp) | (d == hp - 1)) - 1.0 * (d > hp)).astype(np.float32)
    g64 = (np.arange(S)[None, :] // 64 == np.arange(S // 64)[:, None]).astype(ml_dtypes.bfloat16)
    g256 = (np.arange(S)[None, :] // 256 == np.arange(S // 256)[:, None]).astype(ml_dtypes.bfloat16)
    pm = np.where(np.arange(64)[None, :] < 32, 0.0, -1e30).astype(np.float32) * np.ones((128, 1), np.float32)
    return dict(ident=np.eye(128, dtype=np.float32), invf=invf_t, wv=wv, wadd=wadd, g64=g64, g256=g256, pm=pm)


def build_fused(S, SG):
    nc = bass.Bass("TRN2", target_bir_lowering=False)
    k = K(nc)
    P = Psum(k)
    EI = dict(kind="ExternalInput")
    x = k.dram("x", [S, 1024], F32, **EI)
    pos = k.dram("pos", [128, S // 128], I32, **EI)
    wn = k.dram("wn", [2, 1024, 780], F32, **EI)
    wm = k.dram("wm", [2, 1024, 768], F32, **EI)
    wsb = k.dram("wsb", [2, 1024, 1536], F32, **EI)
    cw = {n: k.dram(n, sh, F32, **EI) for n, sh in
          (("w1k", [2048, 128]), ("w1v", [2048, 128]), ("w2k", [128, 64]), ("w2v", [128, 64]), ("posT", [128, 32]))}
    consts = {"ident": k.dram("ident", [128, 128], F32, **EI), "invf": k.dram("invf", [128, 32], F32, **EI),
              "wv": k.dram("wv", [128, 256], F32, **EI), "wadd": k.dram("wadd", [128, 256], F32, **EI),
              "g64": k.dram("g64", [S // 64, S], BF16, **EI), "g256": k.dram("g256", [S // 256, S], BF16, **EI),
              "pm": k.dram("pm", [128, 64], F32, **EI)}
    w_out = k.dram("w_out", [2, 1024, 1024], F32, **EI)
    lnp = k.dram("lnp", [2, 4, 1024], F32, **EI)
    wr = k.dram("wr", [2, 1024, 20], F32, **EI)
    br = k.dram("br", [2, 20], F32, **EI)
    wg = k.dram("w_gate", [2, 16, 1024, 256], F32, **EI)
    wu = k.dram("w_up", [2, 16, 1024, 256], F32, **EI)
    wd = k.dram("w_down", [2, 16, 256, 1024], F32, **EI)
    out = k.dram("out", [S, 1024], F32, kind="ExternalOutput")
    oT_s = k.dram("oT_s", [1024, S], BF16, kind="Internal")
    x1_s = k.dram("x1_s", [S, 1024], F32, kind="Internal")
    sub = lambda t, i: Tile(k, t.h[i], t.name + f"_{i}")
    for g in range(2):
        k.begin_phase(f"l0a{g}")
        emit_phase_l0(k, P, S, x, pos, sub(wn, g), sub(wm, g), cw, consts, oT_s, row_nsa=256 * g, row_moba=512 + 256 * g)
        k.end_phase()
    k.begin_phase("l0b")
    emit_phase_b(k, P, S, SG, oT_s, x, x1_s, sub(w_out, 0), sub(lnp, 0), sub(wr, 0), sub(br, 0), sub(wg, 0), sub(wu, 0),
                 sub(wd, 0), consts["ident"])
    k.end_phase()
    for g in range(2):
        k.begin_phase(f"l1a{g}")
        emit_phase_sb(k, P, S, x1_s, sub(wsb, g), oT_s, consts["ident"], row0=512 * g)
        k.end_phase()
    k.begin_phase("l1b")
    emit_phase_b(k, P, S, SG, oT_s, x1_s, out, sub(w_out, 1), sub(lnp, 1), sub(wr, 1), sub(br, 1), sub(wg, 1), sub(wu, 1),
                 sub(wd, 1), consts["ident"])
    k.end_phase()
    k.finish([out])
    return nc


def host_inputs(inp, b, S):
    w_in = inp["ab_w_in"][0]
    c_qa, c_kc, c_vc, c_ksl, c_vsl, c_kw, c_vw, c_ga, c_qm, c_km, c_vm = _SP
    wns, wms, wsbs = [], [], []
    w_sb = inp["sb_w_in"][0]
    for g in range(2):
        kvc = lambda base: w_in[:, base + g * 64: base + (g + 1) * 64]
        wns.append(np.concatenate([w_in[:, c_qa + 256 * g: c_qa + 256 * (g + 1)], kvc(c_ksl), kvc(c_ksl), kvc(c_kw), kvc(c_kw),
                                   kvc(c_kc), kvc(c_vc), kvc(c_vsl), kvc(c_vw), w_in[:, c_ga + 12 * g: c_ga + 12 * (g + 1)]], axis=1))
        wms.append(np.concatenate([w_in[:, c_qm + 256 * g: c_qm + 256 * (g + 1)], w_in[:, c_km + 256 * g: c_km + 256 * (g + 1)],
                                   w_in[:, c_vm + 256 * g: c_vm + 256 * (g + 1)]], axis=1))
        wsbs.append(np.concatenate([w_sb[:, j * 1024 + 512 * g: j * 1024 + 512 * (g + 1)] for j in range(3)], axis=1))
    m = dict(x=np.ascontiguousarray(inp["x"][b]).astype(np.float32),
             pos=np.ascontiguousarray(inp["positions"][b].reshape(S // 128, 128).T.astype(np.int32)),
             wn=np.ascontiguousarray(np.stack(wns)), wm=np.ascontiguousarray(np.stack(wms)), wsb=np.ascontiguousarray(np.stack(wsbs)),
             w1k=inp["nsa_cmp_w1_k"][0], w1v=inp["nsa_cmp_w1_v"][0], w2k=inp["nsa_cmp_w2_k"][0], w2v=inp["nsa_cmp_w2_v"][0],
             posT=np.ascontiguousarray(np.concatenate([inp["nsa_cmp_pos_k"][0].T, inp["nsa_cmp_pos_v"][0].T], axis=0)),
             w_out=np.ascontiguousarray(np.stack([inp["ab_w_out"][0], inp["sb_w_out"][0]])),
             lnp=np.ascontiguousarray(np.stack([np.stack([inp["ln_mix_g"][l], inp["ln_mix_b"][l], inp["ln_ffn_g"][l], inp["ln_ffn_b"][l]]) for l in range(2)])),
             wr=np.ascontiguousarray(np.stack([np.concatenate([inp["moe_w_grp"][l], inp["moe_w_rt"][l].transpose(1, 0, 2).reshape(1024, 16)], axis=1) for l in range(2)])),
             br=np.ascontiguousarray(np.stack([np.concatenate([inp["moe_b_grp"][l], inp["moe_b_rt"][l].reshape(16)]) for l in range(2)])),
             w_gate=inp["moe_w_gate"], w_up=inp["moe_w_up"], w_down=inp["moe_w_down"])
    m.update(_consts(S))
    return m


def kernel(**inputs):
    inp = {k_: np.asarray(v) for k_, v in inputs.items()}
    B, S, _ = inp["x"].shape
    SG = min(2048, S)
    key = ("fused", S, SG)
    if key not in _PROGS:
        _PROGS[key] = build_fused(S, SG)
    nc = _PROGS[key]
    in_maps = [host_inputs(inp, b, S) for b in range(B)]
    res = run_bass_kernel_spmd(nc, in_maps, core_ids=list(range(B)))
    return np.stack([res.results[b]["out"] for b in range(B)]).astype(np.float32)
```

```python
import math
import bisect
from contextlib import ExitStack
import numpy as np
import concourse.bass as bass
import concourse.mybir as mybir

F32 = mybir.dt.float32
BF16 = mybir.dt.bfloat16
I32 = mybir.dt.int32
AF = mybir.ActivationFunctionType
ALU = mybir.AluOpType
AX = mybir.AxisListType


class V:
    __slots__ = ("tile", "ap")

    def __init__(self, tile, ap):
        self.tile = tile
        self.ap = ap

    def __getitem__(self, idx):
        return V(self.tile, self.ap[idx])

    def bc(self, shape):
        return V(self.tile, self.ap.broadcast_to(shape))

    def unsq(self, d):
        return V(self.tile, self.ap.unsqueeze(d))

    def re(self, s, **kw):
        return V(self.tile, self.ap.rearrange(s, **kw))

    def bitcast(self, dt):
        return V(self.tile, self.ap.bitcast(dt))


class Tile:
    def __init__(self, k, h, name):
        self.k = k
        self.h = h
        self.name = name
        self.w = None
        self.r = {}
        self.excl = False

    def __getitem__(self, idx):
        return V(self, self.h[idx])

    @property
    def a(self):
        return V(self, self.h[:])


class EngState:
    def __init__(self, k, name, eng, is_compute=True):
        self.k = k
        self.name = name
        self.eng = eng
        self.sem = k.nc.alloc_semaphore("s_" + name)
        self.n = 0
        self.last = None
        self.marks_idx = []
        self.marks_val = []
        self.val = 0
        self.waited = {}
        self.nwaits = 0


class DmaSem:
    def __init__(self, k, name):
        self.sem = k.nc.alloc_semaphore("d_" + name)
        self.cnt = 0
        self.name = name


class K:
    def __init__(self, nc, same_engine_sync=True):
        self.nc = nc
        self.E = {
            "pe": EngState(self, "pe", nc.tensor),
            "act": EngState(self, "act", nc.scalar),
            "dve": EngState(self, "dve", nc.vector),
            "pool": EngState(self, "pool", nc.gpsimd),
            "sp": EngState(self, "sp", nc.sync),
        }
        self.same_engine_sync = same_engine_sync
        self.dsems = {}
        self.rings = {}
        self.ring_pos = {}
        self.RING = 16
        self.n_tiles = 0
        self.stack = ExitStack()
        self.pname = ""

    def sb(self, shape, dt, name=None):
        self.n_tiles += 1
        name = "sb_" + self.pname + (name or f"t{self.n_tiles}")
        h = self.stack.enter_context(self.nc.sbuf_tensor(name, list(shape), dt))
        return Tile(self, h, name)

    def ps(self, shape, dt=F32, name=None):
        self.n_tiles += 1
        name = name or f"p{self.n_tiles}"
        h = self.nc.alloc_psum_tensor(name, list(shape), dt)
        t = Tile(self, h, name)
        t.excl = True
        return t

    def dram(self, name, shape, dt, kind="Internal"):
        h = self.nc.dram_tensor(name, list(shape), dt, kind=kind)
        return Tile(self, h, name)

    def sub(self, v, name="sub"):
        return Tile(self, v.ap, name)

    def dsem(self, name):
        if name not in self.dsems:
            self.dsems[name] = DmaSem(self, name)
        return self.dsems[name]

    def _token_value(self, tok):
        kind, obj, idx = tok
        if kind == "dma":
            return obj.sem, idx
        es = obj
        if es.marks_idx and es.marks_idx[-1] >= idx:
            j = bisect.bisect_left(es.marks_idx, idx)
            return es.sem, es.marks_val[j]
        es.val += 1
        es.last.then_inc(es.sem, 1)
        es.marks_idx.append(es.n - 1)
        es.marks_val.append(es.val)
        return es.sem, es.val

    def _wait(self, es, tok):
        if tok is None:
            return
        kind, obj, idx = tok
        if kind == "eng" and obj is es:
            if not self.same_engine_sync or es.name == "pe":
                return
        sem, val = self._token_value(tok)
        key = id(sem) if kind == "dma" else obj.name
        if es.waited.get(key, 0) >= val:
            return
        es.waited[key] = val
        es.eng.wait_ge(sem, val)
        es.nwaits += 1

    def _deps(self, es, reads, writes):
        for v in reads:
            if v is None:
                continue
            self._wait(es, v.tile.w)
            if v.tile.excl:
                for tok in v.tile.r.values():
                    self._wait(es, tok)
        for v in writes:
            if v is None:
                continue
            t = v.tile
            self._wait(es, t.w)
            for tok in t.r.values():
                self._wait(es, tok)

    def _commit(self, tok, reads, writes, rkey):
        for v in reads:
            if v is None:
                continue
            v.tile.r[rkey] = tok
        for v in writes:
            if v is None:
                continue
            v.tile.w = tok
            v.tile.r = {}

    OUT_KEYS = ("out", "accum_out", "out_ap")

    def op(self, en, fn, **kw):
        es = self.E[en]
        reads, writes = [], []
        extra_r = kw.pop("_reads", [])
        extra_w = kw.pop("_writes", [])
        mark = kw.pop("mark", None)
        if mark is None:
            mark = en != "pe"
        args = {}
        for key, val in kw.items():
            if isinstance(val, V):
                (writes if key in self.OUT_KEYS else reads).append(val)
                args[key] = val.ap
            else:
                args[key] = val
        reads += extra_r
        writes += extra_w
        self._deps(es, reads, writes)
        inst = getattr(es.eng, fn)(**args)
        es.last = inst
        es.n += 1
        tok = ("eng", es, es.n - 1)
        if mark:
            es.val += 1
            inst.then_inc(es.sem, 1)
            es.marks_idx.append(es.n - 1)
            es.marks_val.append(es.val)
        self._commit(tok, reads, writes, en)
        return inst

    def dma(self, qn, out, in_, ds=None, **kw):
        es = self.E[qn]
        ring = self.rings.setdefault(qn, [])
        pos = self.ring_pos.get(qn, 0)
        if len(ring) < self.RING:
            ring.append(self.dsem(f"{qn}_r{len(ring)}"))
        ds = ring[pos % self.RING]
        self.ring_pos[qn] = pos + 1
        if ds.cnt:
            self._wait(es, ("dma", ds, ds.cnt))
        self._deps(es, [in_], [out])
        inst = es.eng.dma_start(out=out.ap, in_=in_.ap, **kw)
        inst.then_inc(ds.sem, 16)
        ds.cnt += 16
        tok = ("dma", ds, ds.cnt)
        self._commit(tok, [in_], [out], "dma_" + ds.name)
        return inst

    def begin_phase(self, name):
        self.stack = ExitStack()
        self.pname = name + "_"

    def barrier(self):
        targets = []
        for n in ("pe", "act", "dve", "pool"):
            es = self.E[n]
            if es.n == 0:
                continue
            if not es.marks_idx or es.marks_idx[-1] < es.n - 1:
                es.val += 1
                es.last.then_inc(es.sem, 1)
                es.marks_idx.append(es.n - 1)
                es.marks_val.append(es.val)
            targets.append((n, es.sem, es.val))
        for en, es in self.E.items():
            for (n, sem, val) in targets:
                if es.waited.get(n, 0) < val:
                    es.eng.wait_ge(sem, val)
                    es.waited[n] = val
            for ds in self.dsems.values():
                if ds.cnt and es.waited.get(id(ds.sem), 0) < ds.cnt:
                    es.eng.wait_ge(ds.sem, ds.cnt)
                    es.waited[id(ds.sem)] = ds.cnt

    def end_phase(self):
        self.barrier()
        self.stack.close()
        self.stack = ExitStack()

    def finish(self, out_tiles):
        es = self.E["sp"]
        for t in out_tiles:
            self._wait(es, t.w)
        for ds in self.dsems.values():
            if ds.cnt:
                key = id(ds.sem)
                if es.waited.get(key, 0) < ds.cnt:
                    es.eng.wait_ge(ds.sem, ds.cnt)
                    es.waited[key] = ds.cnt

    def stats(self):
        return {n: (e.n, e.nwaits, e.val) for n, e in self.E.items()}


D = 1024
ALPHA = float(4 ** 0.25)
EPS = 1e-5
NE = 16
FH = 256


class Psum:
    def __init__(self, k):
        self.k = k
        self.t = k.nc.alloc_psum_tensor("psum_all", [128, 8, 512], F32)
        self.b = [Tile(k, self.t[:, i, :], f"bank{i}") for i in range(8)]
        for t in self.b:
            t.excl = True

    def v(self, i, n=1):
        if n == 1:
            return V(self.b[i], self.t[:, i, :]), []
        ap = self.t[:, i:i + n, :].rearrange("p a b -> p (a b)")
        return V(self.b[i], ap), [self.b[j].a for j in range(i + 1, i + n)]


def layer_norm_tile(k, src, dst, g_t, b_t, eps_t, tmp, st, mv, rstd, nmr):
    for i in range(2):
        k.op("dve", "bn_stats", out=st[:, i, :], in_=src[:, i * 512:(i + 1) * 512])
    k.op("dve", "bn_aggr", out=mv.a, in_=st.a.re("p a b -> p (a b)"))
    k.op("act", "activation", out=rstd.a, in_=mv[:, 1:2], func=AF.Ln, bias=eps_t.a, scale=1.0)
    k.op("act", "activation", out=rstd.a, in_=rstd.a, func=AF.Exp, scale=-0.5)
    k.op("dve", "scalar_tensor_tensor", out=nmr.a, in0=mv[:, 0:1], scalar=-1.0, in1=rstd.a,
         op0=ALU.mult, op1=ALU.mult)
    k.op("act", "activation", out=dst, in_=src, func=AF.Identity, bias=nmr.a, scale=rstd.a)
    k.op("pool", "tensor_tensor", out=dst, in0=dst, in1=g_t.a, op=ALU.mult)
    k.op("pool", "tensor_tensor", out=dst, in0=dst, in1=b_t.a, op=ALU.add)


def emit_phase_b(k, P, NT, SG, oT, xres, xo, w_out, lnp, wr, br, w_gate, w_up, w_down, ident_d):
    nsg = NT // SG
    ntile = SG // 128
    ngrp = SG // 512
    idf = k.sb([128, 128], F32, "idf")
    k.dma("sp", idf.a, ident_d.a, "ld_c")
    woT = k.sb([128, 8, 1024], BF16, "woT")
    k.dma("pool", woT.a, V(w_out, w_out.h[:, :].rearrange("(c p) n -> p c n", p=128)), "ld_c")
    lnt = []
    for i in range(4):
        t = k.sb([128, 1024], F32, f"ln{i}")
        k.dma("sp", t.a, V(lnp, lnp.h[i].partition_broadcast(128)), "ld_c")
        lnt.append(t)
    WR = k.sb([128, 8, 20], F32, "WR")
    if True:
        k.dma("sp", WR.a, V(wr, wr.h[:, :].rearrange("(c p) n -> p c n", p=128)), "ld_c")
    BR = k.sb([128, 20], F32, "BR")
    if True:
      k.dma("sp", BR.a, V(br, br.h[:].partition_broadcast(128)), "ld_c")
    eps_t = k.sb([128, 1], F32, "eps")
    k.op("pool", "memset", ap=eps_t.a, constant=EPS, _writes=[eps_t.a])
    SELT = k.sb([16, 16, 128], F32, "SELT")
    k.op("pool", "memset", ap=SELT.a, constant=1.0, _writes=[SELT.a])
    ones16 = SELT
    if True:
      k.op("pool", "affine_select", out=SELT.a, in_=ones16.a, pattern=[[-1, 16], [0, 128]],
         compare_op=ALU.is_equal, fill=0.0, base=0, channel_multiplier=1)

    acc = [k.sb([128, 1024], F32, f"acc{i}") for i in range(ntile)]
    x1T = k.sb([128, 8, SG], BF16, "x1T")
    CT = k.sb([16, SG], F32, "CT")
    oTt = [k.sb([128, 8, 128], BF16, f"oTt{i}") for i in range(2)]
    xrt = [k.sb([128, 1024], F32, f"xrt{i}") for i in range(2)]
    yt = k.sb([128, 1024], F32, "yt")
    tmp = yt
    x1t = k.sb([128, 1024], F32, "x1t")
    x1Tf = k.sb([128, 8, 128], F32, "x1Tf")
    st = k.sb([128, 2, 6], F32, "st")
    mv = k.sb([128, 2], F32, "mv")
    rstd = k.sb([128, 1], F32, "rstd")
    nmr = k.sb([128, 1], F32, "nmr")
    L = k.sb([128, 20], F32, "L")
    sm = k.sb([128, 16], F32, "sm")
    r4 = k.sb([128, 8], F32, "r4")
    r16 = [k.sb([128, 16], F32, f"r16_{i}") for i in range(4)]
    comb = k.sb([128, 16], F32, "comb")
    wgb = [k.sb([128, 8, FH], BF16, f"wgb{i}") for i in range(2)]
    wub = [k.sb([128, 8, FH], BF16, f"wub{i}") for i in range(2)]
    wdb = [k.sb([128, 2, 1024], BF16, f"wdb{i}") for i in range(2)]
    hT = [k.sb([128, 2, 512], BF16, f"hT{i}") for i in range(2)]
    sgt = [k.sb([128, 512], F32, f"sgt{i}") for i in range(2)]
    tt_ = sgt
    ot = xrt

    oT_v = oT.h[:, :].rearrange("(c p) t -> p c t", p=128)
    dcount = [0]
    ld_i = 0
    for sg in range(nsg):
        for ti in range(ntile if 9.0 >= 1 else 0):
            tok0 = sg * SG + ti * 128
            o_t = oTt[ld_i % 2]
            x_t = xrt[ld_i % 2]
            ld_i += 1
            k.dma("sp", o_t.a, V(oT, oT_v[:, :, tok0:tok0 + 128]), f"ld_o{ld_i % 2}")
            k.dma("sp", x_t.a, V(xres, xres.h[tok0:tok0 + 128, :]), f"ld_x{ld_i % 2}")
            pm, pm_x = P.v(0, 2)
            for half in range(2):
                for c in range(8):
                    k.op("pe", "matmul", out=pm[:, half * 512:(half + 1) * 512], lhsT=o_t[:, c, :],
                         rhs=woT[:, c, half * 512:(half + 1) * 512], start=(c == 0), stop=(c == 7),
                         _writes=pm_x, mark=(c == 7 and half == 1))
            k.op("dve", "scalar_tensor_tensor", out=yt.a, in0=x_t.a, scalar=ALPHA, in1=pm, op0=ALU.mult,
                 op1=ALU.add, _reads=pm_x)
            if 9.0 < 1.2:
                continue
            layer_norm_tile(k, yt.a, x1t.a, lnt[0], lnt[1], eps_t, tmp.a, st, mv, rstd, nmr)
            k.op("act", "mul", out=acc[ti].a, in_=x1t.a, mul=ALPHA)
            if 9.0 < 1.4:
                continue
            pT, pT_x = P.v(2, 2)
            for c in range(8):
                k.op("pe", "transpose", out=pT[:, c * 128:(c + 1) * 128], in_=x1t[:, c * 128:(c + 1) * 128],
                     identity=idf.a, _writes=pT_x, mark=(c == 7))
            if True:
                k.op("act", "copy", out=x1Tf.a.re("p c t -> p (c t)"), in_=pT, _reads=pT_x)
            if True:
                k.op("dve", "tensor_copy", out=x1T[:, :, ti * 128:(ti + 1) * 128],
                     in_=pT.re("p (c t) -> p c t", c=8), _reads=pT_x)
            if 9.0 < 2:
                continue
            pr, _ = P.v(7)
            for c in range(8):
                k.op("pe", "matmul", out=pr[:, 0:20], lhsT=x1Tf[:, c, :], rhs=WR[:, c, :], start=(c == 0),
                     stop=(c == 7), mark=(c == 7))
            k.op("dve", "tensor_tensor", out=L.a, in0=pr[:, 0:20], in1=BR.a, op=ALU.add)
            gmax, ngmax, sumg, wg_, m1, nm1, m2, ssum, coef = [sm[:, i:i + 1] for i in range(9)]
            k.op("dve", "reduce_max", out=gmax, in_=L[:, 0:4], axis=AX.X)
            k.op("dve", "tensor_scalar", out=ngmax, in0=gmax, scalar1=-1.0, scalar2=None, op0=ALU.mult)
            k.op("act", "activation", out=r4[:, 0:4], in_=L[:, 0:4], func=AF.Exp, bias=ngmax, scale=1.0,
                 accum_out=sumg)
            k.op("dve", "reciprocal", out=wg_, in_=sumg)
            k.op("dve", "tensor_scalar", out=r4[:, 4:8], in0=L[:, 0:4], scalar1=gmax, scalar2=None,
                 op0=ALU.is_equal)
            k.op("dve", "tensor_scalar", out=r4[:, 4:8], in0=r4[:, 4:8], scalar1=-1.0, scalar2=1e30,
                 op0=ALU.add, op1=ALU.mult)
            lm = r16[0]
            k.op("dve", "tensor_tensor", out=lm.a.re("p (g e) -> p g e", g=4),
                 in0=L[:, 4:20].re("p (g e) -> p g e", g=4), in1=r4[:, 4:8].unsq(2).bc([128, 4, 4]),
                 op=ALU.add)
            k.op("dve", "reduce_max", out=m1, in_=lm.a, axis=AX.X)
            k.op("dve", "tensor_scalar", out=r16[1].a, in0=lm.a, scalar1=m1, scalar2=None, op0=ALU.is_equal)
            k.op("dve", "scalar_tensor_tensor", out=r16[1].a, in0=r16[1].a, scalar=-1e30, in1=lm.a,
                 op0=ALU.mult, op1=ALU.add)
            k.op("dve", "reduce_max", out=m2, in_=r16[1].a, axis=AX.X)
            k.op("dve", "tensor_scalar", out=r16[2].a, in0=lm.a, scalar1=m2, scalar2=None, op0=ALU.is_ge)
            k.op("dve", "tensor_scalar", out=nm1, in0=m1, scalar1=-1.0, scalar2=None, op0=ALU.mult)
            k.op("act", "activation", out=r16[3].a, in_=lm.a, func=AF.Exp, bias=nm1, scale=1.0)
            k.op("dve", "tensor_tensor", out=r16[3].a, in0=r16[3].a, in1=r16[2].a, op=ALU.mult)
            k.op("dve", "reduce_sum", out=ssum, in_=r16[3].a, axis=AX.X)
            k.op("dve", "reciprocal", out=ssum, in_=ssum)
            k.op("dve", "tensor_tensor", out=coef, in0=ssum, in1=wg_, op=ALU.mult)
            k.op("dve", "tensor_scalar", out=comb.a, in0=r16[3].a, scalar1=coef, scalar2=None, op0=ALU.mult)
            pc, _ = P.v(6)
            k.op("pe", "transpose", out=pc[0:16, 0:128], in_=comb.a, identity=idf.a, mark=True)
            k.op("act", "copy", out=CT[:, ti * 128:(ti + 1) * 128], in_=pc[0:16, 0:128])
        items = [(e, grp) for e in range(NE) for grp in range(ngrp)]

        def load_w(e):
            s_ = e % 2
            k.dma("pool", wgb[s_].a, V(w_gate, w_gate.h[e].rearrange("(c p) f -> p c f", p=128)))
            k.dma("pool", wub[s_].a, V(w_up, w_up.h[e].rearrange("(c p) f -> p c f", p=128)))
            k.dma("pool", wdb[s_].a, V(w_down, w_down.h[e].rearrange("(c p) n -> p c n", p=128)))

        def emit_gu(it):
            e, grp = it
            s_ = e % 2
            tc0 = grp * 512
            pcb, _ = P.v(6)
            k.op("pe", "matmul", out=pcb, lhsT=SELT[:, e, :], rhs=CT[:, tc0:tc0 + 512], start=True, stop=True, mark=True)
            for f in range(2):
                pg, _ = P.v(2 + 2 * f)
                pu, _ = P.v(3 + 2 * f)
                for c in range(8):
                    k.op("pe", "matmul", out=pg, lhsT=wgb[s_][:, c, f * 128:(f + 1) * 128],
                         rhs=x1T[:, c, tc0:tc0 + 512], start=(c == 0), stop=(c == 7), mark=(c == 7))
                for c in range(8):
                    k.op("pe", "matmul", out=pu, lhsT=wub[s_][:, c, f * 128:(f + 1) * 128],
                         rhs=x1T[:, c, tc0:tc0 + 512], start=(c == 0), stop=(c == 7), mark=(c == 7))

        def emit_elem(idx):
            h_t = hT[idx % 2]
            pcb, _ = P.v(6)
            for f in range(2):
                pg, _ = P.v(2 + 2 * f)
                pu, _ = P.v(3 + 2 * f)
                k.op("act", "activation", out=sgt[f].a, in_=pg, func=AF.Silu)
                k.op("dve", "tensor_tensor", out=sgt[f].a, in0=sgt[f].a, in1=pu, op=ALU.mult)
                k.op("dve", "tensor_tensor", out=h_t[:, f, :], in0=sgt[f].a, in1=pcb, op=ALU.mult)

        def emit_down(idx, it):
            e, grp = it
            s_ = e % 2
            h_t = hT[idx % 2]
            for t4 in range(4):
                ti = grp * 4 + t4
                for half in range(2):
                    pd, _ = P.v((0, 1, 7)[dcount[0] % 3])
                    dcount[0] += 1
                    for f in range(2):
                        k.op("pe", "matmul", out=pd, lhsT=h_t[:, f, t4 * 128:(t4 + 1) * 128],
                             rhs=wdb[s_][:, f, half * 512:(half + 1) * 512], start=(f == 0), stop=(f == 1), mark=(f == 1))
                    k.op("dve", "tensor_tensor", out=acc[ti][:, half * 512:(half + 1) * 512],
                         in0=acc[ti][:, half * 512:(half + 1) * 512], in1=pd, op=ALU.add)

        load_w(0)
        emit_gu(items[0])
        emit_elem(0)
        for idx, it in enumerate(items):
            if idx + 1 < len(items):
                nxt = items[idx + 1]
                if nxt[0] != it[0]:
                    load_w(nxt[0])
                emit_gu(nxt)
                emit_elem(idx + 1)
            emit_down(idx, it)
        for ti in range(ntile):
            tok0 = sg * SG + ti * 128
            o_ = ot[ti % 2]
            layer_norm_tile(k, acc[ti].a, o_.a, lnt[2], lnt[3], eps_t, tmp.a, st, mv, rstd, nmr)
            dst_ap = xo.h[tok0:tok0 + 128, :]
            k.dma("sp", V(Tile(k, dst_ap, "xo_part"), dst_ap), o_.a)


def load_xT_group(k, P, x_d, tok0, ntok, xin, xbf, xT, idb, bank):
    for t in range(ntok // 128):
        xb = xbf[t % 2]
        k.dma("pool", xb.a, V(x_d, x_d.h[tok0 + t * 128: tok0 + (t + 1) * 128, :]))
        pt, _ = P.v(bank + (t % 2))
        ptb = pt.bitcast(BF16)
        for c in range(8):
            k.op("pe", "transpose", out=ptb[:, c * 128:(c + 1) * 128], in_=xb[:, c * 128:(c + 1) * 128],
                 identity=idb.a, mark=(c == 7))
        k.op("dve", "tensor_copy", out=xT[:, :, t * 128:(t + 1) * 128], in_=ptb.re("p (c t) -> p c t", c=8))


def run_streams_sb(gens, nslot):
    pending = list(gens)
    active = [None] * nslot
    while True:
        progressed = False
        for s_ in range(nslot):
            if active[s_] is None and pending:
                active[s_] = pending.pop(0)(s_)
            if active[s_] is not None:
                try:
                    next(active[s_])
                except StopIteration:
                    active[s_] = None
                progressed = True
        if not progressed and not pending:
            break


def sb_stream(k, P, slot, h, qb, qt, kt, r0, Vt, Oa, NTRI, LSTR, e_s, p_s, a_s, w_s):
    zb = P.v(0 + slot)[0]
    lab = P.v(2 + slot)[0]
    ob = P.v(4 + slot)[0]
    chunks = [(4 * qb + j, 128 * j) for j in (3, 2, 1, 0)] + [(kc, 0) for kc in range(4 * qb - 1, -1, -1)]

    def zmm(kc, c0):
        k.op("pe", "matmul", out=zb[:, c0:512], lhsT=kt[r0:r0 + 64, kc * 128:(kc + 1) * 128],
             rhs=qt[r0:r0 + 64, qb * 512 + c0:(qb + 1) * 512], start=True, stop=True, mark=True)
    zmm(*chunks[0])
    yield
    prev = None
    first = True
    for ci, (kc, c0) in enumerate(chunks):
        e = e_s[ci % 2]
        p_cur = p_s[ci % 2]
        a = a_s[ci % 2]
        w = w_s[ci % 2]
        k.op("act", "activation", out=e[:, c0:512], in_=zb[:, c0:512], func=AF.Exp, scale=0.125)
        if kc >= 4 * qb:
            k.op("pool", "affine_select", out=e[:, c0:c0 + 128], in_=e[:, c0:c0 + 128],
                 pattern=[[1, 128]], compare_op=ALU.is_gt, fill=0.0, base=0, channel_multiplier=-1)
        k.op("act", "activation", out=p_cur[:, c0:512], in_=e[:, c0:512], func=AF.Ln, bias=1.0, scale=1.0)
        yield
        if prev is not None:
            pp, pc0 = prev
            k.op("pe", "matmul", out=lab[:, pc0:512], lhsT=LSTR.a, rhs=pp[:, pc0:512], start=False,
                 stop=False, skip_group_check=True)
        k.op("pe", "matmul", out=lab[:, c0:512], lhsT=NTRI.a, rhs=p_cur[:, c0:512],
             start=first, stop=True, skip_group_check=True, mark=True)
        if ci + 1 < len(chunks):
            zmm(*chunks[ci + 1])
        yield
        k.op("act", "activation", out=a[:, c0:512], in_=lab[:, c0:512], func=AF.Exp)
        k.op("dve", "tensor_tensor", out=w[:, c0:512], in0=a[:, c0:512], in1=e[:, c0:512], op=ALU.mult)
        yield
        for i in range(c0 // 128, 4):
            k.op("pe", "matmul", out=ob[:, i * 64:(i + 1) * 64], lhsT=w[:, i * 128:(i + 1) * 128],
                 rhs=Vt[:, kc, h * 64:(h + 1) * 64], start=(first and i == c0 // 128),
                 stop=True, skip_group_check=True, mark=(i == 3))
        first = False
        prev = (p_cur, c0)
        yield
    k.op("act", "copy", out=Oa[:, qb * 4:(qb + 1) * 4, h * 64:(h + 1) * 64],
         in_=ob[:, 0:256].re("p (i d) -> p i d", i=4))
    yield


def sb_wide(k, P, hp, qb, qt, kt, Vt, Oa, NTRI, LSTR, e_b, p_b, a_b, w_b):
    chunks = [(4 * qb + j, 128 * j) for j in (3, 2, 1, 0)] + [(kc, 0) for kc in range(4 * qb - 1, -1, -1)]
    n = len(chunks)
    T = P.t

    def pair(b0, c0):
        ap = T[:, b0:b0 + 2, c0:512]
        return V(P.b[b0], ap), [P.b[b0 + 1].a]

    def zmm(i):
        kc, c0 = chunks[i]
        b0 = 2 * (i % 2)
        for hh in range(2):
            r0 = hh * 64
            zb = V(P.b[b0 + hh], T[:, b0 + hh, c0:512])
            k.op("pe", "matmul", out=zb, lhsT=kt[r0:r0 + 64, kc * 128:(kc + 1) * 128],
                 rhs=qt[r0:r0 + 64, qb * 512 + c0:(qb + 1) * 512], start=True, stop=True, mark=(hh == 1))

    def ep(i):
        kc, c0 = chunks[i]
        zv, zx = pair(2 * (i % 2), c0)
        e = e_b[i % 3]
        k.op("act", "activation", out=e[:, :, c0:512], in_=zv, func=AF.Exp, scale=0.125, _reads=zx)
        if kc >= 4 * qb:
            k.op("pool", "affine_select", out=e[:, :, c0:c0 + 128], in_=e[:, :, c0:c0 + 128],
                 pattern=[[0, 2], [1, 128]], compare_op=ALU.is_gt, fill=0.0, base=0, channel_multiplier=-1)
        k.op("act", "activation", out=p_b[i % 3][:, :, c0:512], in_=e[:, :, c0:512], func=AF.Ln, bias=1.0, scale=1.0)

    def la(i):
        kc, c0 = chunks[i]
        for hh in range(2):
            lab = V(P.b[4 + hh], T[:, 4 + hh, :])
            if i > 0:
                pc0 = chunks[i - 1][1]
                k.op("pe", "matmul", out=lab[:, pc0:512], lhsT=LSTR.a, rhs=p_b[(i - 1) % 3][:, hh, pc0:512], start=False,
                     stop=False, skip_group_check=True)
            k.op("pe", "matmul", out=lab[:, c0:512], lhsT=NTRI.a, rhs=p_b[i % 3][:, hh, c0:512],
                 start=(i == 0), stop=True, skip_group_check=True, mark=(hh == 1))

    def aw(i):
        kc, c0 = chunks[i]
        lv, lx = pair(4, c0)
        k.op("act", "activation", out=a_b[i % 2][:, :, c0:512], in_=lv, func=AF.Exp, _reads=lx)
        k.op("dve", "tensor_tensor", out=w_b[i % 2][:, :, c0:512], in0=a_b[i % 2][:, :, c0:512],
             in1=e_b[i % 3][:, :, c0:512], op=ALU.mult)

    def pv(i):
        kc, c0 = chunks[i]
        ob = V(P.b[6], T[:, 6, :])
        for hh in range(2):
            h = 2 * hp + hh
            for j in range(c0 // 128, 4):
                col = (hh * 4 + j) * 64
                k.op("pe", "matmul", out=ob[:, col:col + 64], lhsT=w_b[i % 2][:, hh, j * 128:(j + 1) * 128],
                     rhs=Vt[:, kc, h * 64:(h + 1) * 64], start=(i == 0 and hh == 0 and j == c0 // 128),
                     stop=True, skip_group_check=True, mark=(hh == 1 and j == 3))

    zmm(0)
    ep(0)
    if n > 1:
        zmm(1)
    la(0)
    for i in range(n):
        if i + 1 < n:
            ep(i + 1)
        if i + 2 < n:
            zmm(i + 2)
        aw(i)
        if i + 1 < n:
            la(i + 1)
        pv(i)
    ob = V(P.b[6], T[:, 6, :])
    for hh in range(2):
        h = 2 * hp + hh
        k.op("act", "copy", out=Oa[:, qb * 4:(qb + 1) * 4, h * 64:(h + 1) * 64],
             in_=ob[:, hh * 256:(hh + 1) * 256].re("p (i d) -> p i d", i=4))


def emit_phase_sb(k, P, S, x_d, w_d, oT_d, ident_d, nsub=2, row0=0):
    NTILE = S // 128
    NQB = S // 512
    idf = k.sb([128, 128], F32, "idf")
    k.dma("sp", idf.a, ident_d.a, "ld_c")
    idb = k.sb([128, 128], BF16, "idb")
    k.op("dve", "tensor_copy", out=idb.a, in_=idf.a)
    negones = k.sb([128, 128], BF16, "negones")
    k.op("pool", "memset", ap=negones.a, constant=-1.0, _writes=[negones.a])
    NTRI = k.sb([128, 128], BF16, "NTRI")
    k.op("pool", "affine_select", out=NTRI.a, in_=negones.a, pattern=[[-1, 128]], compare_op=ALU.is_ge,
         fill=0.0, base=0, channel_multiplier=1)
    LSTR = k.sb([128, 128], BF16, "LSTR")
    k.op("pool", "affine_select", out=LSTR.a, in_=negones.a, pattern=[[1, 128]], compare_op=ALU.is_gt,
         fill=0.0, base=0, channel_multiplier=-1)

    W = k.sb([128, 8, 768], BF16, "Wsb")
    QT = [k.sb([128, S], BF16, f"QT{i}") for i in range(2)]
    KT = [k.sb([128, S], BF16, f"KT{i}") for i in range(2)]
    Vt = k.sb([128, NTILE, 256], BF16, "Vt")
    Oa = k.sb([128, NTILE, 256], BF16, "Oa")
    xin = None
    xbf = [k.sb([128, 1024], BF16, f"xbf{i}") for i in range(2)]
    xT = [k.sb([128, 8, 512], BF16, f"xT{i}") for i in range(2)]
    NS = 2
    e_b = [k.sb([128, 2, 512], F32, f"e{j}") for j in range(3)]
    p_b = [k.sb([128, 2, 512], BF16, f"p{j}") for j in range(3)]
    a_b = [k.sb([128, 2, 512], BF16, f"a{j}") for j in range(2)]
    w_b = [k.sb([128, 2, 512], BF16, f"w{j}") for j in range(2)]
    oTs = [k.sb([128, 2, 128], BF16, f"oTs{i}") for i in range(2)]

    wv = w_d.h[:, :].rearrange("(c p) n -> p c n", p=128)
    for sub in range(nsub):
        for j, base in enumerate((0, 512, 1024)):
            k.dma("pool", W[:, :, j * 256:(j + 1) * 256],
                  V(w_d, wv[:, :, base + sub * 256: base + (sub + 1) * 256]), "ld_w")
        for g4 in range(S // 512):
            tok0 = g4 * 512
            xT_g = xT[g4 % 2]
            load_xT_group(k, P, x_d, tok0, 512, xin, xbf, xT_g, idb, 0)
            for j in range(4):
                pq, _ = P.v(2 + (j % 2))
                for c in range(8):
                    k.op("pe", "matmul", out=pq, lhsT=W[:, c, j * 128:(j + 1) * 128], rhs=xT_g[:, c, :],
                         start=(c == 0), stop=(c == 7), mark=(c == 7))
                dst = (QT if j < 2 else KT)[j % 2]
                k.op("act", "copy", out=dst[:, tok0:tok0 + 512], in_=pq)
            for t in range(4):
                pv, _ = P.v(4 + (t % 2))
                for c in range(8):
                    k.op("pe", "matmul", out=pv[:, 0:256], lhsT=xT_g[:, c, t * 128:(t + 1) * 128],
                         rhs=W[:, c, 512:768], start=(c == 0), stop=(c == 7), mark=(c == 7))
                k.op("dve", "tensor_copy", out=Vt[:, g4 * 4 + t, :], in_=pv[:, 0:256])
        for qb in range(NQB):
            for hp in range(2):
                sb_wide(k, P, hp, qb, QT[hp], KT[hp], Vt, Oa, NTRI, LSTR, e_b, p_b, a_b, w_b)
        for t in range(NTILE):
            pt, _ = P.v(6 + (t % 2))
            ptb = pt.bitcast(BF16)
            for c in range(2):
                k.op("pe", "transpose", out=ptb[:, c * 128:(c + 1) * 128], in_=Oa[:, t, c * 128:(c + 1) * 128],
                     identity=idb.a, mark=(c == 1))
            o_ = oTs[t % 2]
            k.op("dve", "tensor_copy", out=o_.a, in_=ptb[:, 0:256].re("p (c t) -> p c t", c=2))
            dst_ap = oT_d.h[row0 + sub * 256:row0 + (sub + 1) * 256, t * 128:(t + 1) * 128].rearrange("(c p) t -> p c t", p=128)
            k.dma("sp", V(Tile(k, dst_ap, "oT_part"), dst_ap), o_.a)


import math

SCALE = 0.125
NEG = -30000.0


def run_streams(gens, nslot):
    pending = list(gens)
    active = [None] * nslot
    while True:
        progressed = False
        for s in range(nslot):
            if active[s] is None and pending:
                active[s] = pending.pop(0)(s)
            if active[s] is not None:
                try:
                    next(active[s])
                except StopIteration:
                    active[s] = None
                progressed = True
        if not progressed and not pending:
            break


def flash_stream(k, slot, zb, ob, e_tiles, qv_fn, chunks, nv, negM, epilogue):
    first = True
    for ci, ch in enumerate(chunks):
        c0, c1 = ch["c0"], ch["c1"]
        e = e_tiles[ci % 2]
        has_mask = ch.get("mask") is not None
        k.op("pe", "matmul", out=zb[:, c0:c1], lhsT=ch["kT"], rhs=qv_fn(c0, c1), start=True, stop=not has_mask,
             mark=not has_mask, skip_group_check=True)
        if has_mask:
            ml, mr = ch["mask"]
            k.op("pe", "matmul", out=zb[:, c0:c1], lhsT=ml, rhs=mr(c0, c1), start=False, stop=True, mark=True,
                 skip_group_check=True)
        yield
        k.op("act", "activation", out=e[:, c0:c1], in_=zb[:, c0:c1], func=AF.Exp, scale=SCALE, bias=negM)
        for (lo, hi, base, cm, step, op) in ch.get("aff", []):
            k.op("pool", "affine_select", out=e[:, lo:hi], in_=e[:, lo:hi], pattern=[[step, hi - lo]],
                 compare_op=op, fill=0.0, base=base, channel_multiplier=cm)
        yield
        subs = list(range(c0 // 128, (c1 + 127) // 128))
        for i in subs:
            k.op("pe", "matmul", out=ob[:, i * nv:(i + 1) * nv], lhsT=e[:, i * 128:(i + 1) * 128], rhs=ch["v"],
                 start=first, stop=True, skip_group_check=True, mark=(i == subs[-1]))
            first = False
        yield
    epilogue()
    yield


def rope_group(k, pos_t, invf, g4, tmps):
    posf, y, yy, ki, kf, outs = tmps
    k.op("dve", "tensor_copy", out=posf.a, in_=pos_t[:, g4 * 4:(g4 + 1) * 4])
    k.op("dve", "tensor_tensor", out=y.a, in0=posf.a.unsq(2).bc([128, 4, 32]),
         in1=invf.a.unsq(1).bc([128, 4, 32]), op=ALU.mult)
    res = []
    for j, shift in enumerate((0.0, 0.25)):
        k.op("dve", "tensor_scalar", out=yy.a, in0=y.a, scalar1=shift, scalar2=None, op0=ALU.add)
        k.op("dve", "tensor_copy", out=ki.a, in_=yy.a)
        k.op("dve", "tensor_copy", out=kf.a, in_=ki.a)
        k.op("dve", "tensor_tensor", out=yy.a, in0=yy.a, in1=kf.a, op=ALU.subtract)
        k.op("dve", "tensor_scalar", out=kf.a, in0=yy.a, scalar1=0.5, scalar2=None, op0=ALU.is_gt)
        k.op("dve", "tensor_tensor", out=yy.a, in0=yy.a, in1=kf.a, op=ALU.subtract)
        k.op("dve", "tensor_scalar", out=kf.a, in0=yy.a, scalar1=-0.5, scalar2=None, op0=ALU.is_lt)
        k.op("dve", "tensor_tensor", out=yy.a, in0=yy.a, in1=kf.a, op=ALU.add)
        t = outs[g4 % 2][j]
        k.op("act", "activation", out=t.a, in_=yy.a, func=AF.Sin, scale=2.0 * math.pi * (1 - 1e-6))
        res.append(t)
    return res[1], res[0]


def rope_tmps(k):
    posf = k.sb([128, 4], F32, "posf")
    y = k.sb([128, 4, 32], F32, "rope_y")
    yy = k.sb([128, 4, 32], F32, "rope_yy")
    ki = k.sb([128, 4, 32], I32, "rope_ki")
    kf = k.sb([128, 4, 32], F32, "rope_kf")
    outs = [[k.sb([128, 4, 32], F32, f"rope_o{i}_{j}") for j in range(2)] for i in range(2)]
    return (posf, y, yy, ki, kf, outs)


def rope_apply(k, pf, nh, cos, sin, t1, t2, out_bf):
    pr = pf.re("p (h t d) -> p h t d", h=nh, t=2)
    t1v = t1[:, 0:nh * 64].re("p (h t d) -> p h t d", h=nh, t=2)
    t2v = t2[:, 0:nh * 64].re("p (h t d) -> p h t d", h=nh, t=2)
    ob = out_bf.re("p (h t d) -> p h t d", h=nh, t=2)
    cb = cos.unsq(1).unsq(1).bc([128, nh, 2, 32])
    sb_ = sin.unsq(1).bc([128, nh, 32])
    k.op("dve", "tensor_tensor", out=t1v, in0=pr, in1=cb, op=ALU.mult)
    k.op("pool", "tensor_tensor", out=t2v[:, :, 0, :], in0=pr[:, :, 1, :], in1=sb_, op=ALU.mult)
    k.op("pool", "tensor_tensor", out=t2v[:, :, 1, :], in0=pr[:, :, 0, :], in1=sb_, op=ALU.mult)
    k.op("dve", "tensor_tensor", out=ob[:, :, 0, :], in0=t1v[:, :, 0, :], in1=t2v[:, :, 0, :], op=ALU.subtract)
    k.op("dve", "tensor_tensor", out=ob[:, :, 1, :], in0=t1v[:, :, 1, :], in1=t2v[:, :, 1, :], op=ALU.add)


def bound_negM(k, P, nq_t, nk_t, idf, ones1, out_negM, bank, scratch):
    mq, mm, row = scratch
    k.op("dve", "reduce_max", out=mq[:, 0:1], in_=nq_t, axis=AX.X)
    k.op("dve", "reduce_max", out=mq[:, 1:2], in_=nk_t, axis=AX.X)
    pb, _ = P.v(bank)
    k.op("pe", "transpose", out=pb[0:2, 0:128], in_=mq.a, identity=idf.a, mark=True)
    k.op("dve", "reduce_max", out=mm.a, in_=pb[0:2, 0:128], axis=AX.X)
    k.op("pe", "transpose", out=pb[0:1, 128:130], in_=mm.a, identity=idf[0:2, 0:2], mark=True)
    k.op("dve", "tensor_copy", out=row[:, 0:2], in_=pb[0:1, 128:130])
    k.op("dve", "tensor_tensor", out=row[:, 2:3], in0=row[:, 0:1], in1=row[:, 1:2], op=ALU.mult)
    k.op("act", "activation", out=row[:, 2:3], in_=row[:, 2:3], func=AF.Ln)
    k.op("act", "activation", out=row[:, 2:3], in_=row[:, 2:3], func=AF.Exp, scale=0.5)
    k.op("dve", "tensor_scalar", out=row[:, 3:4], in0=row[:, 2:3], scalar1=-SCALE * 1.02, scalar2=None, op0=ALU.mult)
    k.op("pe", "matmul", out=pb[:, 132:133], lhsT=ones1.a, rhs=row[:, 3:4], start=True, stop=True, mark=True)
    k.op("dve", "tensor_copy", out=out_negM, in_=pb[:, 132:133])


def emit_out_T(k, P, OUT_bf, oT_d, row0, tok0, idb, oTs, bank, idx):
    dst_ap = oT_d.h[row0:row0 + 256, tok0:tok0 + 128].rearrange("(c p) t -> p c t", p=128)
    pt, _ = P.v(bank)
    ptb = pt.bitcast(BF16)
    for c in range(2):
        k.op("pe", "transpose", out=ptb[:, c * 128:(c + 1) * 128], in_=OUT_bf[:, c * 128:(c + 1) * 128],
             identity=idb.a, mark=(c == 1))
    o_ = oTs[idx % 2]
    k.op("dve", "tensor_copy", out=o_.a, in_=ptb[:, 0:256].re("p (c t) -> p c t", c=2))
    k.dma("sp", V(Tile(k, dst_ap, "oT_part"), dst_ap), o_.a)


def emit_phase_l0(k, P, S, x_d, pos_d, wn_d, wm_d, cw, consts, oT_d, do_nsa=True, do_moba=True, row_nsa=0, row_moba=256):
    NTILE = S // 128
    NQB = S // 512
    NCMP = (S - 32) // 16 + 1
    NCC = (NCMP + 127) // 128
    NSLC = S // 64
    NBLK = S // 256
    idf = k.sb([128, 128], F32, "idf")
    k.dma("sp", idf.a, consts["ident"].a, "ld_c")
    idb = k.sb([128, 128], BF16, "idb")
    k.op("dve", "tensor_copy", out=idb.a, in_=idf.a)
    ones1 = k.sb([1, 128], F32, "ones1")
    k.op("pool", "memset", ap=ones1.a, constant=1.0, _writes=[ones1.a])
    invf = k.sb([128, 32], F32, "invf")
    k.dma("sp", invf.a, consts["invf"].a, "ld_c")
    pos_t = k.sb([128, NTILE], I32, "pos_t")
    k.dma("sp", pos_t.a, pos_d.a, "ld_c")
    rtm = rope_tmps(k)
    xin = None
    xbf = [k.sb([128, 1024], BF16, f"xbf{i}") for i in range(2)]
    xT = [k.sb([128, 8, 512], BF16, "xT0")] * 2
    pf = k.sb([128, 780], F32, "pf")
    t1 = k.sb([128, 576], F32, "rt1")
    t2 = k.sb([128, 576], F32, "rt2")
    Rb = k.sb([128, 640], BF16, "Rb")
    sqj = k.sb([128, 640], F32, "sqj")
    e_t = [[k.sb([128, 512], BF16, f"e{s}_{j}") for j in range(2)] for s in range(4)]
    oTs = [k.sb([128, 2, 128], BF16, f"oTs{i}") for i in range(2)]
    OUT = k.sb([128, 4, 256], F32, "OUT")
    OUTb = k.sb([128, 4, 256], BF16, "OUTb")
    negM = [k.sb([128, 1], F32, f"negM{i}") for i in range(4)]
    sc_mq = k.sb([128, 2], F32, "sc_mq")
    sc_mk = k.sb([2, 1], F32, "sc_mk")
    sc_row = k.sb([1, 4], F32, "sc_row")
    rz = k.sb([128, 4 * 4], F32, "rz")
    coef = k.sb([128, 4 * 4], F32, "coef")
    QT = [k.sb([128, S], BF16, f"QT{i}") for i in range(2)]
    KT = [k.sb([128, S], BF16, f"KT{i}") for i in range(2)]
    CT = k.sb([128, S], BF16, "CT")
    Vall = k.sb([128, NTILE, 260], BF16, "Vall")
    k.op("pool", "memset", ap=Vall.a, constant=1.0, _writes=[Vall.a])
    Wall = k.sb([128, 8, 780], BF16, "Wall")

    if do_nsa:
        W = Wall
        k.dma("pool", W.a, V(wn_d, wn_d.h[:, :].rearrange("(c p) n -> p c n", p=128)), "ld_w")
        VS = V(Vall, Vall.h[:, :, 0:130].rearrange("p t (a d) -> p t a d", a=2))
        GL = k.sb([128, NTILE, 12], F32, "GL")
        NQ = k.sb([128, 3, NTILE], F32, "NQ")
        for g4 in range(S // 512):
            xT_g = xT[g4 % 2]
            load_xT_group(k, P, x_d, g4 * 512, 512, xin, xbf, xT_g, idb, 0)
            COS4, SIN4 = rope_group(k, pos_t, invf, g4, rtm)
            for t in range(4):
                ti = g4 * 4 + t
                pp, pp_x = P.v(2 + 2 * (t % 2), 2)
                for (a, b) in ((0, 512), (512, 780)):
                    for c in range(8):
                        k.op("pe", "matmul", out=pp[:, a:b], lhsT=xT_g[:, c, t * 128:(t + 1) * 128], rhs=W[:, c, a:b],
                             start=(c == 0), stop=(c == 7), _writes=pp_x, mark=(c == 7 and a == 512))
                k.op("act", "copy", out=pf[:, 0:780], in_=pp[:, 0:780], _reads=pp_x)
                rope_apply(k, pf[:, 0:576], 9, COS4[:, t, :], SIN4[:, t, :], t1, t2, Rb[:, 0:576])
                k.op("pool", "tensor_copy", out=Rb[:, 576:640], in_=pf[:, 576:640])
                k.op("pool", "tensor_copy", out=VS[:, ti, :, 0:64], in_=pf[:, 640:768].re("p (a d) -> p a d", a=2))
                k.op("pool", "tensor_copy", out=GL[:, ti, :], in_=pf[:, 768:780])
                for j, (a, b) in enumerate(((0, 256), (320, 448), (512, 576))):
                    k.op("act", "activation", out=sqj[:, a:b], in_=Rb[:, a:b], func=AF.Square, accum_out=NQ[:, j, ti:ti + 1])
                pt, _ = P.v(6 + (t % 2))
                ptb = pt.bitcast(BF16)
                for j in range(5):
                    k.op("pe", "transpose", out=ptb[:, j * 128:(j + 1) * 128], in_=Rb[:, j * 128:(j + 1) * 128],
                         identity=idb.a, mark=(j == 4))
                for j, dst in enumerate((QT[0], QT[1], KT[0], KT[1], CT)):
                    k.op("act" if j % 2 else "dve", "copy" if j % 2 else "tensor_copy", out=dst[:, ti * 128:(ti + 1) * 128],
                         in_=ptb[:, j * 128:(j + 1) * 128])
        G = GL
        k.op("act", "activation", out=G.a, in_=GL.a, func=AF.Sigmoid)
        W1 = k.sb([128, 32, 128], BF16, "W1")
        k.dma("pool", W1[0:64], V(cw["w1k"], cw["w1k"].h[:, :].rearrange("(l d) h -> d l h", d=64)), "ld_w")
        k.dma("pool", W1[64:128], V(cw["w1v"], cw["w1v"].h[:, :].rearrange("(l d) h -> d l h", d=64)), "ld_w")
        PF = k.sb([128, 32], BF16, "PF")
        k.dma("pool", PF.a, cw["posT"].a)
        W2 = [k.sb([128, 128], BF16, "W2k2"), k.sb([128, 64], BF16, "W2v")]
        k.dma("pool", W2[0][:, 0:64], cw["w2k"].a, "ld_w")
        k.dma("pool", W2[0][:, 64:128], cw["w2k"].a, "ld_w")
        k.dma("pool", W2[1].a, cw["w2v"].a, "ld_w")
        KCT = k.sb([128, NCC * 128], BF16, "KCT")
        k.op("pool", "memset", ap=KCT.a, constant=0.0, _writes=[KCT.a])
        NV = 193
        VCA = k.sb([128, NCC, NV], BF16, "VCA")
        k.op("pool", "memset", ap=VCA.a, constant=1.0, _writes=[VCA.a])
        for c in range(NCC):
            k.op("pool", "affine_select", out=VCA[:, c, 65:193], in_=VCA[:, c, 65:193], pattern=[[-4, 128]],
                 compare_op=ALU.is_ge, fill=0.0, base=128 * c + 1, channel_multiplier=1)
            k.op("pool", "affine_select", out=VCA[:, c, 65:193], in_=VCA[:, c, 65:193], pattern=[[4, 128]],
                 compare_op=ALU.is_ge, fill=0.0, base=3 - 128 * c, channel_multiplier=-1)
        cb = k.sb([128, 2], F32, "cbias")
        gu = [k.sb([128, 512], F32, f"gu{i}") for i in range(3)]
        gact = [k.sb([128, 512], BF16, f"gact{i}") for i in range(2)]
        k.op("pool", "memset", ap=gact[1].a, constant=0.0, _writes=[gact[1].a])
        for kv in range(2):
            r0 = kv * 64
            ph, _ = P.v(0 + kv)
            for l in range(32):
                k.op("pe", "matmul", out=ph[:, 0:NCMP], lhsT=W1[r0:r0 + 64, l, :],
                     rhs=CT[r0:r0 + 64, l:l + 16 * (NCMP - 1) + 1:16], start=(l == 0), stop=(l == 31), mark=(l == 31))
            pbias, _ = P.v(2 + kv)
            for l in range(32):
                k.op("pe", "matmul", out=pbias[:, 0:1], lhsT=W1[r0:r0 + 64, l, :], rhs=PF[r0:r0 + 64, l:l + 1], start=(l == 0),
                     stop=(l == 31), mark=(l == 31))
            k.op("dve", "tensor_copy", out=cb[:, kv:kv + 1], in_=pbias[:, 0:1])
            u, u2, th = gu
            n_ = NCMP
            k.op("dve", "tensor_scalar", out=u[:, 0:n_], in0=ph[:, 0:n_], scalar1=cb[:, kv:kv + 1], scalar2=None, op0=ALU.add)
            k.op("dve", "tensor_tensor", out=u2[:, 0:n_], in0=u[:, 0:n_], in1=u[:, 0:n_], op=ALU.mult)
            k.op("dve", "tensor_scalar", out=u2[:, 0:n_], in0=u2[:, 0:n_], scalar1=0.044715, scalar2=1.0, op0=ALU.mult, op1=ALU.add)
            k.op("dve", "tensor_tensor", out=u2[:, 0:n_], in0=u2[:, 0:n_], in1=u[:, 0:n_], op=ALU.mult)
            k.op("act", "activation", out=th[:, 0:n_], in_=u2[:, 0:n_], func=AF.Tanh, scale=0.7978845608028654)
            k.op("dve", "tensor_scalar", out=th[:, 0:n_], in0=th[:, 0:n_], scalar1=0.5, scalar2=0.5, op0=ALU.mult, op1=ALU.add)
            k.op("dve", "tensor_tensor", out=gact[kv][:, 0:n_], in0=th[:, 0:n_], in1=u[:, 0:n_], op=ALU.mult)
            if kv == 0:
                pk, _ = P.v(4)
                k.op("pe", "matmul", out=pk[:, 0:n_], lhsT=W2[0].a, rhs=gact[0][:, 0:n_], start=True, stop=True, mark=True)
                k.op("dve", "tensor_copy", out=KCT[:, 0:n_], in_=pk[:, 0:n_])
                k.op("act", "activation", out=sqj[0:64, 0:n_], in_=pk[0:64, 0:n_], func=AF.Square)
            else:
                for c in range(NCC):
                    pvv, _ = P.v(5)
                    k.op("pe", "matmul", out=pvv[:, 0:64], lhsT=gact[1][:, c * 128:(c + 1) * 128], rhs=W2[1].a, start=True,
                         stop=True, mark=True)
                    k.op("dve", "tensor_copy", out=VCA[:, c, 0:64], in_=pvv[:, 0:64])
        ones64 = k.sb([64, 1], F32, "ones64")
        k.op("pool", "memset", ap=ones64.a, constant=1.0, _writes=[ones64.a])
        pn, _ = P.v(6)
        k.op("pe", "matmul", out=pn[0:1, 0:NCMP], lhsT=ones64.a, rhs=sqj[0:64, 0:NCMP], start=True, stop=True, mark=True)
        nkc = k.sb([128, 1], F32, "nkc")
        k.op("pool", "memset", ap=nkc.a, constant=0.0, _writes=[nkc.a])
        k.op("dve", "reduce_max", out=nkc[0:1, 0:1], in_=pn[0:1, 0:NCMP], axis=AX.X)
        bound_negM(k, P, NQ[:, 0, :], NQ[:, 1, :], idf, ones1, negM[0].a, 7, (sc_mq, sc_mk, sc_row))
        bound_negM(k, P, NQ[:, 0, :], nkc.a, idf, ones1, negM[1].a, 7, (sc_mq, sc_mk, sc_row))
        WV = k.sb([128, 256], F32, "WV")
        WADD = k.sb([128, 256], F32, "WADD")
        k.dma("sp", WV.a, consts["wv"].a, "ld_c")
        k.dma("sp", WADD.a, consts["wadd"].a, "ld_c")
        G64 = CT
        k.op("pool", "memset", ap=G64.a, constant=0.0, _writes=[G64.a])
        k.dma("sp", G64[0:NSLC], consts["g64"].a, "ld_c")
        IMP = k.sb([128, 4, 128], F32, "IMP")
        NMT = k.sb([128, 512], BF16, "NMT")
        k.op("pool", "memset", ap=NMT.a, constant=0.0, _writes=[NMT.a])
        sc_s = [k.sb([128, 128], F32, f"sc_s{i}") for i in range(2)]
        m8 = k.sb([128, 16], F32, "m8")
        nmb = k.sb([128, 128], BF16, "nmb")
        for qb in range(NQB):
            q0 = qb * 512
            first_touch = {"imp": [True] * 4, "out": [[True] * 4 for _ in range(4)]}

            def mk_cmp(h, half, qb=qb, q0=q0):
                def fac(slot):
                    zb, _ = P.v(slot * 2)
                    ob, _ = P.v(slot * 2 + 1)
                    qt = QT[h // 2]
                    r0 = (h % 2) * 64
                    cbase = q0 + half * 256
                    chunks = []
                    for c in range(NCC):
                        if 16 * (128 * c) + 31 > cbase + 255:
                            continue
                        chunks.append(dict(kT=KCT[r0:r0 + 64, c * 128:(c + 1) * 128], v=VCA[:, c, :], c0=0, c1=256,
                                           aff=[(0, 256, cbase - 2048 * c - 31, -16, 1, ALU.is_ge)]))

                    def epi():
                        for i in range(2):
                            sub = half * 2 + i
                            ti = qb * 4 + sub
                            rzv = rz[:, slot * 4 + i: slot * 4 + i + 1]
                            cfv = coef[:, slot * 4 + i: slot * 4 + i + 1]
                            if not chunks:
                                if first_touch["imp"][sub]:
                                    k.op("pool", "memset", ap=IMP[:, sub, :], constant=0.0, _writes=[IMP.a])
                                    first_touch["imp"][sub] = False
                                if first_touch["out"][sub][h]:
                                    k.op("pool", "memset", ap=OUT[:, sub, h * 64:(h + 1) * 64], constant=0.0, _writes=[OUT.a])
                                    first_touch["out"][sub][h] = False
                                continue
                            acc = ob[:, i * NV:(i + 1) * NV]
                            k.op("dve", "tensor_scalar", out=rzv, in0=acc[:, 64:65], scalar1=1e-30, scalar2=None, op0=ALU.max)
                            k.op("dve", "reciprocal", out=rzv, in_=rzv)
                            k.op("dve", "tensor_tensor", out=cfv, in0=rzv, in1=G[:, ti, h * 3:h * 3 + 1], op=ALU.mult)
                            k.op("dve", "tensor_scalar", out=OUT[:, sub, h * 64:(h + 1) * 64], in0=acc[:, 0:64], scalar1=cfv,
                                 scalar2=None, op0=ALU.mult)
                            first_touch["out"][sub][h] = False
                            if first_touch["imp"][sub]:
                                k.op("dve", "tensor_scalar", out=IMP[:, sub, :], in0=acc[:, 65:193], scalar1=rzv, scalar2=None,
                                     op0=ALU.mult)
                                first_touch["imp"][sub] = False
                            else:
                                k.op("dve", "scalar_tensor_tensor", out=IMP[:, sub, :], in0=acc[:, 65:193], scalar=rzv,
                                     in1=IMP[:, sub, :], op0=ALU.mult, op1=ALU.add)
                    qv = lambda c0, c1: qt[r0:r0 + 64, cbase + c0:cbase + c1]
                    return flash_stream(k, slot, zb, ob, e_t[slot], qv, chunks, NV, negM[1].a, epi)
                return fac
            run_streams([mk_cmp(h, half) for h in range(4) for half in range(2)], 4)
            for sub in range(4):
                qt_i = qb * 4 + sub
                sc0, sc1 = sc_s
                lo = 128 - 2 * qt_i
                k.op("dve", "tensor_tensor", out=sc0.a, in0=IMP[:, sub, :], in1=WV[:, lo:lo + 128], op=ALU.mult)
                k.op("dve", "tensor_tensor", out=sc0.a, in0=sc0.a, in1=WADD[:, lo:lo + 128], op=ALU.add)
                if qt_i >= 1:
                    k.op("dve", "tensor_scalar", out=sc0[:, 0:1], in0=sc0[:, 0:1], scalar1=1.0e4, scalar2=None, op0=ALU.add)
                k.op("dve", "max", out=m8[:, 0:8], in_=sc0.a)
                k.op("dve", "match_replace", out=sc1.a, in_to_replace=m8[:, 0:8], in_values=sc0.a, imm_value=-1e30)
                k.op("dve", "max", out=m8[:, 8:16], in_=sc1.a)
                k.op("dve", "tensor_scalar", out=sc1.a, in0=sc0.a, scalar1=m8[:, 15:16], scalar2=None, op0=ALU.is_ge)
                k.op("dve", "tensor_tensor", out=sc1.a, in0=sc1.a, in1=WV[:, lo:lo + 128], op=ALU.mult)
                k.op("dve", "tensor_scalar", out=nmb.a, in0=sc1.a, scalar1=-NEG, scalar2=NEG, op0=ALU.mult, op1=ALU.add)
                pt, _ = P.v(7)
                ptb = pt.bitcast(BF16)
                k.op("pe", "transpose", out=ptb[:, 0:128], in_=nmb.a, identity=idb.a, mark=True)
                k.op("act", "copy", out=NMT[:, sub * 128:(sub + 1) * 128], in_=ptb[:, 0:128])
            def mk_flash(h, br, qb=qb, q0=q0):
                def fac(slot):
                    zb, _ = P.v(slot * 2)
                    ob, _ = P.v(slot * 2 + 1)
                    qt = QT[h // 2]
                    r0 = (h % 2) * 64
                    chunks = []
                    if br == 1:
                        for kc in range(0, 4 * qb + 4):
                            j = kc - 4 * qb
                            c0 = 128 * j if j > 0 else 0
                            aff = [(c0, c0 + 128, 0, -1, 1, ALU.is_ge)] if j >= 0 else []
                            chunks.append(dict(kT=KT[0][r0:r0 + 64, kc * 128:(kc + 1) * 128], v=VS[:, kc, 0, :], c0=c0, c1=512, aff=aff,
                                               mask=(G64[:, kc * 128:(kc + 1) * 128], lambda a, b: NMT[:, a:b])))
                    else:
                        for kc in range(max(0, 4 * qb - 4), 4 * qb + 4):
                            j = kc - 4 * qb
                            if j >= 0:
                                c0, c1 = 128 * j, 512
                                aff = [(c0, c0 + 128, 0, -1, 1, ALU.is_ge)]
                            else:
                                jp = j + 4
                                c0, c1 = 0, 128 * (jp + 1)
                                aff = [(128 * jp, 128 * jp + 128, -1, 1, -1, ALU.is_ge)]
                            chunks.append(dict(kT=KT[1][r0:r0 + 64, kc * 128:(kc + 1) * 128], v=VS[:, kc, 1, :], c0=c0, c1=c1, aff=aff))

                    def epi():
                        for sub in range(4):
                            ti = qb * 4 + sub
                            rzv = rz[:, slot * 4 + sub: slot * 4 + sub + 1]
                            cfv = coef[:, slot * 4 + sub: slot * 4 + sub + 1]
                            acc = ob[:, sub * 65:(sub + 1) * 65]
                            k.op("dve", "tensor_scalar", out=rzv, in0=acc[:, 64:65], scalar1=1e-30, scalar2=None, op0=ALU.max)
                            k.op("dve", "reciprocal", out=rzv, in_=rzv)
                            k.op("dve", "tensor_tensor", out=cfv, in0=rzv, in1=G[:, ti, h * 3 + br:h * 3 + br + 1], op=ALU.mult)
                            k.op("dve", "scalar_tensor_tensor", out=OUT[:, sub, h * 64:(h + 1) * 64], in0=acc[:, 0:64], scalar=cfv,
                                 in1=OUT[:, sub, h * 64:(h + 1) * 64], op0=ALU.mult, op1=ALU.add)
                    qv = lambda c0, c1: qt[r0:r0 + 64, q0 + c0:q0 + c1]
                    return flash_stream(k, slot, zb, ob, e_t[slot], qv, chunks, 65, negM[0].a, epi)
                return fac
            run_streams([mk_flash(h, br) for h in range(4) for br in (1, 2)], 4)
            k.op("act", "copy", out=OUTb.a, in_=OUT.a)
            for sub in range(4):
                emit_out_T(k, P, OUTb[:, sub, :], oT_d, row_nsa, (qb * 4 + sub) * 128, idb, oTs, 7, sub)

    if do_moba:
        NB = max(NBLK, 8)
        Wm = Wall
        k.dma("pool", Wm[:, :, 0:768], V(wm_d, wm_d.h[:, :].rearrange("(c p) n -> p c n", p=128)))
        VB = V(Vall, Vall.h[:, :, :].rearrange("p t (a d) -> p t a d", a=4))
        k.op("pool", "memset", ap=Vall.a, constant=1.0, _writes=[Vall.a])
        NQm = k.sb([128, 2, NTILE], F32, "NQm")
        for g4 in range(S // 512):
            xT_g = xT[g4 % 2]
            load_xT_group(k, P, x_d, g4 * 512, 512, xin, xbf, xT_g, idb, 0)
            COS4, SIN4 = rope_group(k, pos_t, invf, g4, rtm)
            for t in range(4):
                ti = g4 * 4 + t
                pp, pp_x = P.v(2 + 2 * (t % 2), 2)
                for (a, b) in ((0, 512), (512, 768)):
                    for c in range(8):
                        k.op("pe", "matmul", out=pp[:, a:b], lhsT=xT_g[:, c, t * 128:(t + 1) * 128], rhs=Wm[:, c, a:b],
                             start=(c == 0), stop=(c == 7), _writes=pp_x, mark=(c == 7 and a == 512))
                k.op("act", "copy", out=pf[:, 0:768], in_=pp[:, 0:768], _reads=pp_x)
                rope_apply(k, pf[:, 0:512], 8, COS4[:, t, :], SIN4[:, t, :], t1, t2, Rb[:, 0:512])
                k.op("pool", "tensor_copy", out=VB[:, ti, :, 0:64], in_=pf[:, 512:768].re("p (a d) -> p a d", a=4))
                for j, (a, b) in enumerate(((0, 256), (256, 512))):
                    k.op("act", "activation", out=sqj[:, a:b], in_=Rb[:, a:b], func=AF.Square, accum_out=NQm[:, j, ti:ti + 1])
                pt, _ = P.v(6 + (t % 2))
                ptb = pt.bitcast(BF16)
                for j in range(4):
                    k.op("pe", "transpose", out=ptb[:, j * 128:(j + 1) * 128], in_=Rb[:, j * 128:(j + 1) * 128],
                         identity=idb.a, mark=(j == 3))
                for j, dst in enumerate((QT[0], QT[1], KT[0], KT[1])):
                    k.op("act" if j % 2 else "dve", "copy" if j % 2 else "tensor_copy", out=dst[:, ti * 128:(ti + 1) * 128],
                         in_=ptb[:, j * 128:(j + 1) * 128])
        bound_negM(k, P, NQm[:, 0, :], NQm[:, 1, :], idf, ones1, negM[2].a, 7, (sc_mq, sc_mk, sc_row))
        KM = k.sb([128, 32], F32, "KM")
        KMt = k.sb([128, 32], F32, "KMt")
        KMhi = [k.sb([128, 32], BF16, f"KMhi{i}") for i in range(2)]
        KMlo = [k.sb([128, 32], BF16, f"KMlo{i}") for i in range(2)]
        for i in range(2):
            k.op("pool", "memset", ap=KMhi[i].a, constant=0.0, _writes=[KMhi[i].a])
            k.op("pool", "memset", ap=KMlo[i].a, constant=0.0, _writes=[KMlo[i].a])
            k.op("dve", "tensor_reduce", out=KM[:, 0:NBLK], in_=KT[i].a.re("p (n l) -> p n l", l=256), axis=AX.X, op=ALU.add)
            k.op("dve", "tensor_scalar", out=KM[:, 0:NBLK], in0=KM[:, 0:NBLK], scalar1=1.0 / 256, scalar2=None, op0=ALU.mult)
            k.op("dve", "tensor_copy", out=KMhi[i][:, 0:NBLK], in_=KM[:, 0:NBLK])
            k.op("dve", "tensor_tensor", out=KMt[:, 0:NBLK], in0=KM[:, 0:NBLK], in1=KMhi[i][:, 0:NBLK], op=ALU.subtract)
            k.op("dve", "tensor_copy", out=KMlo[i][:, 0:NBLK], in_=KMt[:, 0:NBLK])
        G256 = CT
        k.op("pool", "memset", ap=CT.a, constant=0.0, _writes=[CT.a])
        k.dma("sp", G256[0:NBLK], consts["g256"].a)
        PM = k.sb([128, 64], F32, "PM")
        k.dma("sp", PM.a, consts["pm"].a)
        NMTm = [k.sb([128, 512], BF16, f"NMTm{h}") for h in range(4)]
        for h in range(4):
            k.op("pool", "memset", ap=NMTm[h].a, constant=0.0, _writes=[NMTm[h].a])
        wk32 = k.sb([128, 32], F32, "wk32")
        nm32 = k.sb([128, 32], F32, "nm32")
        nmb32 = k.sb([128, 32], BF16, "nmb32")
        k.op("pool", "memset", ap=nmb32.a, constant=0.0, _writes=[nmb32.a])
        m8m = k.sb([128, 8], F32, "m8m")
        thr2 = k.sb([128, 1], F32, "thr2")
        for qb in range(NQB):
            q0 = qb * 512
            for h in range(4):
                r0 = (h % 2) * 64
                for sub in range(4):
                    qt_i = qb * 4 + sub
                    cur = qt_i // 2
                    pg, _ = P.v(6)
                    k.op("pe", "matmul", out=pg[:, 0:32], lhsT=QT[h // 2][r0:r0 + 64, qt_i * 128:(qt_i + 1) * 128],
                         rhs=KMhi[h // 2][r0:r0 + 64, :], start=True, stop=False)
                    k.op("pe", "matmul", out=pg[:, 0:32], lhsT=QT[h // 2][r0:r0 + 64, qt_i * 128:(qt_i + 1) * 128],
                         rhs=KMlo[h // 2][r0:r0 + 64, :], start=False, stop=True, mark=True)
                    k.op("dve", "tensor_tensor", out=wk32[:, 0:NB], in0=pg[:, 0:NB], in1=PM[:, 32 - cur:32 - cur + NB], op=ALU.add)
                    k.op("dve", "max", out=m8m.a, in_=wk32[:, 0:NB])
                    k.op("dve", "tensor_scalar", out=thr2.a, in0=m8m[:, 2:3], scalar1=-1e29, scalar2=None, op0=ALU.max)
                    k.op("dve", "tensor_scalar", out=nm32[:, 0:NB], in0=wk32[:, 0:NB], scalar1=thr2.a, scalar2=None, op0=ALU.is_ge)
                    k.op("dve", "tensor_scalar", out=nm32[:, 0:NB], in0=nm32[:, 0:NB], scalar1=-NEG, scalar2=NEG, op0=ALU.mult, op1=ALU.add)
                    k.op("dve", "memset", ap=nm32[:, cur:cur + 1], constant=0.0, _writes=[nm32.a])
                    k.op("dve", "tensor_copy", out=nmb32[:, 0:NB], in_=nm32[:, 0:NB])
                    pt, _ = P.v(7)
                    ptb = pt.bitcast(BF16)
                    k.op("pe", "transpose", out=ptb[0:32, 0:128], in_=nmb32.a, identity=idb.a, mark=True)
                    k.op("act", "copy", out=NMTm[h][0:32, sub * 128:(sub + 1) * 128], in_=ptb[0:32, 0:128])

            def mk_moba(h, qb=qb, q0=q0):
                def fac(slot):
                    zb, _ = P.v(slot * 2)
                    ob, _ = P.v(slot * 2 + 1)
                    qt = QT[h // 2]
                    r0 = (h % 2) * 64
                    chunks = []
                    for kc in range(0, 4 * qb + 4):
                        j = kc - 4 * qb
                        c0 = 128 * j if j > 0 else 0
                        aff = [(c0, c0 + 128, 0, -1, 1, ALU.is_ge)] if j >= 0 else []
                        chunks.append(dict(kT=KT[h // 2][r0:r0 + 64, kc * 128:(kc + 1) * 128], v=VB[:, kc, h, :], c0=c0, c1=512, aff=aff,
                                           mask=(G256[:, kc * 128:(kc + 1) * 128], lambda a, b, h=h: NMTm[h][:, a:b])))

                    def epi():
                        for sub in range(4):
                            rzv = rz[:, slot * 4 + sub: slot * 4 + sub + 1]
                            acc = ob[:, sub * 65:(sub + 1) * 65]
                            k.op("dve", "tensor_scalar", out=rzv, in0=acc[:, 64:65], scalar1=1e-30, scalar2=None, op0=ALU.max)
                            k.op("dve", "reciprocal", out=rzv, in_=rzv)
                            k.op("dve", "tensor_scalar", out=OUT[:, sub, h * 64:(h + 1) * 64], in0=acc[:, 0:64], scalar1=rzv,
                                 scalar2=None, op0=ALU.mult)
                    qv = lambda c0, c1: qt[r0:r0 + 64, q0 + c0:q0 + c1]
                    return flash_stream(k, slot, zb, ob, e_t[slot], qv, chunks, 65, negM[2].a, epi)
                return fac
            run_streams([mk_moba(h) for h in range(4)], 4)
            k.op("act", "copy", out=OUTb.a, in_=OUT.a)
            for sub in range(4):
                emit_out_T(k, P, OUTb[:, sub, :], oT_d, row_moba, (qb * 4 + sub) * 128, idb, oTs, 7, sub)


import ml_dtypes
from concourse.bass_utils import run_bass_kernel_spmd

_PROGS = {}
EVEN_WIDTHS = (512, 128, 128, 128, 128, 128, 128, 24, 512, 512, 512)
_SP = [0] + [int(v) for v in np.cumsum(EVEN_WIDTHS)[:-1]]


def _consts(S):
    half = 32
    invf = (10000.0 ** (-np.arange(half, dtype=np.float32) / half)).astype(np.float32)
    invf_t = np.tile((invf / np.float32(2 * np.pi)).astype(np.float32)[None, :], (128, 1))
    p = np.arange(128)[:, None]
    m = np.arange(256)[None, :]
    hp = (p >= 64).astype(np.int64)
    d = m - 128
    wv = (d <= hp).astype(np.float32)
    wadd = (1e4 * ((d == hp) | (d == hp - 1)) - 1.0 * (d > hp)).astype(np.float32)
    g64 = (np.arange(S)[None, :] // 64 == np.arange(S // 64)[:, None]).astype(ml_dtypes.bfloat16)
    g256 = (np.arange(S)[None, :] // 256 == np.arange(S // 256)[:, None]).astype(ml_dtypes.bfloat16)
    pm = np.where(np.arange(64)[None, :] < 32, 0.0, -1e30).astype(np.float32) * np.ones((128, 1), np.float32)
    return dict(ident=np.eye(128, dtype=np.float32), invf=invf_t, wv=wv, wadd=wadd, g64=g64, g256=g256, pm=pm)


def build_fused(S, SG):
    nc = bass.Bass("TRN2", target_bir_lowering=False)
    k = K(nc)
    P = Psum(k)
    EI = dict(kind="ExternalInput")
    x = k.dram("x", [S, 1024], F32, **EI)
    pos = k.dram("pos", [128, S // 128], I32, **EI)
    wn = k.dram("wn", [2, 1024, 780], F32, **EI)
    wm = k.dram("wm", [2, 1024, 768], F32, **EI)
    wsb = k.dram("wsb", [2, 1024, 1536], F32, **EI)
    cw = {n: k.dram(n, sh, F32, **EI) for n, sh in
          (("w1k", [2048, 128]), ("w1v", [2048, 128]), ("w2k", [128, 64]), ("w2v", [128, 64]), ("posT", [128, 32]))}
    consts = {"ident": k.dram("ident", [128, 128], F32, **EI), "invf": k.dram("invf", [128, 32], F32, **EI),
              "wv": k.dram("wv", [128, 256], F32, **EI), "wadd": k.dram("wadd", [128, 256], F32, **EI),
              "g64": k.dram("g64", [S // 64, S], BF16, **EI), "g256": k.dram("g256", [S // 256, S], BF16, **EI),
              "pm": k.dram("pm", [128, 64], F32, **EI)}
    w_out = k.dram("w_out", [2, 1024, 1024], F32, **EI)
    lnp = k.dram("lnp", [2, 4, 1024], F32, **EI)
    wr = k.dram("wr", [2, 1024, 20], F32, **EI)
    br = k.dram("br", [2, 20], F32, **EI)
    wg = k.dram("w_gate", [2, 16, 1024, 256], F32, **EI)
    wu = k.dram("w_up", [2, 16, 1024, 256], F32, **EI)
    wd = k.dram("w_down", [2, 16, 256, 1024], F32, **EI)
    out = k.dram("out", [S, 1024], F32, kind="ExternalOutput")
    oT_s = k.dram("oT_s", [1024, S], BF16, kind="Internal")
    x1_s = k.dram("x1_s", [S, 1024], F32, kind="Internal")
    sub = lambda t, i: Tile(k, t.h[i], t.name + f"_{i}")
    for g in range(2):
        k.begin_phase(f"l0a{g}")
        emit_phase_l0(k, P, S, x, pos, sub(wn, g), sub(wm, g), cw, consts, oT_s, row_nsa=256 * g, row_moba=512 + 256 * g)
        k.end_phase()
    k.begin_phase("l0b")
    emit_phase_b(k, P, S, SG, oT_s, x, x1_s, sub(w_out, 0), sub(lnp, 0), sub(wr, 0), sub(br, 0), sub(wg, 0), sub(wu, 0),
                 sub(wd, 0), consts["ident"])
    k.end_phase()
    for g in range(2):
        k.begin_phase(f"l1a{g}")
        emit_phase_sb(k, P, S, x1_s, sub(wsb, g), oT_s, consts["ident"], row0=512 * g)
        k.end_phase()
    k.begin_phase("l1b")
    emit_phase_b(k, P, S, SG, oT_s, x1_s, out, sub(w_out, 1), sub(lnp, 1), sub(wr, 1), sub(br, 1), sub(wg, 1), sub(wu, 1),
                 sub(wd, 1), consts["ident"])
    k.end_phase()
    k.finish([out])
    return nc


def host_inputs(inp, b, S):
    w_in = inp["ab_w_in"][0]
    c_qa, c_kc, c_vc, c_ksl, c_vsl, c_kw, c_vw, c_ga, c_qm, c_km, c_vm = _SP
    wns, wms, wsbs = [], [], []
    w_sb = inp["sb_w_in"][0]
    for g in range(2):
        kvc = lambda base: w_in[:, base + g * 64: base + (g + 1) * 64]
        wns.append(np.concatenate([w_in[:, c_qa + 256 * g: c_qa + 256 * (g + 1)], kvc(c_ksl), kvc(c_ksl), kvc(c_kw), kvc(c_kw),
                                   kvc(c_kc), kvc(c_vc), kvc(c_vsl), kvc(c_vw), w_in[:, c_ga + 12 * g: c_ga + 12 * (g + 1)]], axis=1))
        wms.append(np.concatenate([w_in[:, c_qm + 256 * g: c_qm + 256 * (g + 1)], w_in[:, c_km + 256 * g: c_km + 256 * (g + 1)],
                                   w_in[:, c_vm + 256 * g: c_vm + 256 * (g + 1)]], axis=1))
        wsbs.append(np.concatenate([w_sb[:, j * 1024 + 512 * g: j * 1024 + 512 * (g + 1)] for j in range(3)], axis=1))
    m = dict(x=np.ascontiguousarray(inp["x"][b]).astype(np.float32),
             pos=np.ascontiguousarray(inp["positions"][b].reshape(S // 128, 128).T.astype(np.int32)),
             wn=np.ascontiguousarray(np.stack(wns)), wm=np.ascontiguousarray(np.stack(wms)), wsb=np.ascontiguousarray(np.stack(wsbs)),
             w1k=inp["nsa_cmp_w1_k"][0], w1v=inp["nsa_cmp_w1_v"][0], w2k=inp["nsa_cmp_w2_k"][0], w2v=inp["nsa_cmp_w2_v"][0],
             posT=np.ascontiguousarray(np.concatenate([inp["nsa_cmp_pos_k"][0].T, inp["nsa_cmp_pos_v"][0].T], axis=0)),
             w_out=np.ascontiguousarray(np.stack([inp["ab_w_out"][0], inp["sb_w_out"][0]])),
             lnp=np.ascontiguousarray(np.stack([np.stack([inp["ln_mix_g"][l], inp["ln_mix_b"][l], inp["ln_ffn_g"][l], inp["ln_ffn_b"][l]]) for l in range(2)])),
             wr=np.ascontiguousarray(np.stack([np.concatenate([inp["moe_w_grp"][l], inp["moe_w_rt"][l].transpose(1, 0, 2).reshape(1024, 16)], axis=1) for l in range(2)])),
             br=np.ascontiguousarray(np.stack([np.concatenate([inp["moe_b_grp"][l], inp["moe_b_rt"][l].reshape(16)]) for l in range(2)])),
             w_gate=inp["moe_w_gate"], w_up=inp["moe_w_up"], w_down=inp["moe_w_down"])
    m.update(_consts(S))
    return m


def kernel(**inputs):
    inp = {k_: np.asarray(v) for k_, v in inputs.items()}
    B, S, _ = inp["x"].shape
    SG = min(2048, S)
    key = ("fused", S, SG)
    if key not in _PROGS:
        _PROGS[key] = build_fused(S, SG)
    nc = _PROGS[key]
    in_maps = [host_inputs(inp, b, S) for b in range(B)]
    res = run_bass_kernel_spmd(nc, in_maps, core_ids=list(range(B)))
    return np.stack([res.results[b]["out"] for b in range(B)]).astype(np.float32)
```

```python
import math
import bisect
from contextlib import ExitStack
import numpy as np
import concourse.bass as bass
import concourse.mybir as mybir

F32 = mybir.dt.float32
BF16 = mybir.dt.bfloat16
I32 = mybir.dt.int32
AF = mybir.ActivationFunctionType
ALU = mybir.AluOpType
AX = mybir.AxisListType


class V:
    __slots__ = ("tile", "ap")

    def __init__(self, tile, ap):
        self.tile = tile
        self.ap = ap

    def __getitem__(self, idx):
        return V(self.tile, self.ap[idx])

    def bc(self, shape):
        return V(self.tile, self.ap.broadcast_to(shape))

    def unsq(self, d):
        return V(self.tile, self.ap.unsqueeze(d))

    def re(self, s, **kw):
        return V(self.tile, self.ap.rearrange(s, **kw))

    def bitcast(self, dt):
        return V(self.tile, self.ap.bitcast(dt))


class Tile:
    def __init__(self, k, h, name):
        self.k = k
        self.h = h
        self.name = name
        self.w = None
        self.r = {}
        self.excl = False

    def __getitem__(self, idx):
        return V(self, self.h[idx])

    @property
    def a(self):
        return V(self, self.h[:])


class EngState:
    def __init__(self, k, name, eng, is_compute=True):
        self.k = k
        self.name = name
        self.eng = eng
        self.sem = k.nc.alloc_semaphore("s_" + name)
        self.n = 0
        self.last = None
        self.marks_idx = []
        self.marks_val = []
        self.val = 0
        self.waited = {}
        self.nwaits = 0


class DmaSem:
    def __init__(self, k, name):
        self.sem = k.nc.alloc_semaphore("d_" + name)
        self.cnt = 0
        self.name = name


class K:
    def __init__(self, nc, same_engine_sync=True):
        self.nc = nc
        self.E = {
            "pe": EngState(self, "pe", nc.tensor),
            "act": EngState(self, "act", nc.scalar),
            "dve": EngState(self, "dve", nc.vector),
            "pool": EngState(self, "pool", nc.gpsimd),
            "sp": EngState(self, "sp", nc.sync),
        }
        self.same_engine_sync = same_engine_sync
        self.dsems = {}
        self.rings = {}
        self.ring_pos = {}
        self.RING = 16
        self.n_tiles = 0
        self.stack = ExitStack()
        self.pname = ""

    def sb(self, shape, dt, name=None):
        self.n_tiles += 1
        name = "sb_" + self.pname + (name or f"t{self.n_tiles}")
        h = self.stack.enter_context(self.nc.sbuf_tensor(name, list(shape), dt))
        return Tile(self, h, name)

    def ps(self, shape, dt=F32, name=None):
        self.n_tiles += 1
        name = name or f"p{self.n_tiles}"
        h = self.nc.alloc_psum_tensor(name, list(shape), dt)
        t = Tile(self, h, name)
        t.excl = True
        return t

    def dram(self, name, shape, dt, kind="Internal"):
        h = self.nc.dram_tensor(name, list(shape), dt, kind=kind)
        return Tile(self, h, name)

    def sub(self, v, name="sub"):
        return Tile(self, v.ap, name)

    def dsem(self, name):
        if name not in self.dsems:
            self.dsems[name] = DmaSem(self, name)
        return self.dsems[name]

    def _token_value(self, tok):
        kind, obj, idx = tok
        if kind == "dma":
            return obj.sem, idx
        es = obj
        if es.marks_idx and es.marks_idx[-1] >= idx:
            j = bisect.bisect_left(es.marks_idx, idx)
            return es.sem, es.marks_val[j]
        es.val += 1
        es.last.then_inc(es.sem, 1)
        es.marks_idx.append(es.n - 1)
        es.marks_val.append(es.val)
        return es.sem, es.val

    def _wait(self, es, tok):
        if tok is None:
            return
        kind, obj, idx = tok
        if kind == "eng" and obj is es:
            if not self.same_engine_sync or es.name == "pe":
                return
        sem, val = self._token_value(tok)
        key = id(sem) if kind == "dma" else obj.name
        if es.waited.get(key, 0) >= val:
            return
        es.waited[key] = val
        es.eng.wait_ge(sem, val)
        es.nwaits += 1

    def _deps(self, es, reads, writes):
        for v in reads:
            if v is None:
                continue
            self._wait(es, v.tile.w)
            if v.tile.excl:
                for tok in v.tile.r.values():
                    self._wait(es, tok)
        for v in writes:
            if v is None:
                continue
            t = v.tile
            self._wait(es, t.w)
            for tok in t.r.values():
                self._wait(es, tok)

    def _commit(self, tok, reads, writes, rkey):
        for v in reads:
            if v is None:
                continue
            v.tile.r[rkey] = tok
        for v in writes:
            if v is None:
                continue
            v.tile.w = tok
            v.tile.r = {}

    OUT_KEYS = ("out", "accum_out", "out_ap")

    def op(self, en, fn, **kw):
        es = self.E[en]
        reads, writes = [], []
        extra_r = kw.pop("_reads", [])
        extra_w = kw.pop("_writes", [])
        mark = kw.pop("mark", None)
        if mark is None:
            mark = en != "pe"
        args = {}
        for key, val in kw.items():
            if isinstance(val, V):
                (writes if key in self.OUT_KEYS else reads).append(val)
                args[key] = val.ap
            else:
                args[key] = val
        reads += extra_r
        writes += extra_w
        self._deps(es, reads, writes)
        inst = getattr(es.eng, fn)(**args)
        es.last = inst
        es.n += 1
        tok = ("eng", es, es.n - 1)
        if mark:
            es.val += 1
            inst.then_inc(es.sem, 1)
            es.marks_idx.append(es.n - 1)
            es.marks_val.append(es.val)
        self._commit(tok, reads, writes, en)
        return inst

    def dma(self, qn, out, in_, ds=None, **kw):
        es = self.E[qn]
        ring = self.rings.setdefault(qn, [])
        pos = self.ring_pos.get(qn, 0)
        if len(ring) < self.RING:
            ring.append(self.dsem(f"{qn}_r{len(ring)}"))
        ds = ring[pos % self.RING]
        self.ring_pos[qn] = pos + 1
        if ds.cnt:
            self._wait(es, ("dma", ds, ds.cnt))
        self._deps(es, [in_], [out])
        inst = es.eng.dma_start(out=out.ap, in_=in_.ap, **kw)
        inst.then_inc(ds.sem, 16)
        ds.cnt += 16
        tok = ("dma", ds, ds.cnt)
        self._commit(tok, [in_], [out], "dma_" + ds.name)
        return inst

    def begin_phase(self, name):
        self.stack = ExitStack()
        self.pname = name + "_"

    def barrier(self):
        targets = []
        for n in ("pe", "act", "dve", "pool"):
            es = self.E[n]
            if es.n == 0:
                continue
            if not es.marks_idx or es.marks_idx[-1] < es.n - 1:
                es.val += 1
                es.last.then_inc(es.sem, 1)
                es.marks_idx.append(es.n - 1)
                es.marks_val.append(es.val)
            targets.append((n, es.sem, es.val))
        for en, es in self.E.items():
            for (n, sem, val) in targets:
                if es.waited.get(n, 0) < val:
                    es.eng.wait_ge(sem, val)
                    es.waited[n] = val
            for ds in self.dsems.values():
                if ds.cnt and es.waited.get(id(ds.sem), 0) < ds.cnt:
                    es.eng.wait_ge(ds.sem, ds.cnt)
                    es.waited[id(ds.sem)] = ds.cnt

    def end_phase(self):
        self.barrier()
        self.stack.close()
        self.stack = ExitStack()

    def finish(self, out_tiles):
        es = self.E["sp"]
        for t in out_tiles:
            self._wait(es, t.w)
        for ds in self.dsems.values():
            if ds.cnt:
                key = id(ds.sem)
                if es.waited.get(key, 0) < ds.cnt:
                    es.eng.wait_ge(ds.sem, ds.cnt)
                    es.waited[key] = ds.cnt

    def stats(self):
        return {n: (e.n, e.nwaits, e.val) for n, e in self.E.items()}


D = 1024
ALPHA = float(4 ** 0.25)
EPS = 1e-5
NE = 16
FH = 256


class Psum:
    def __init__(self, k):
        self.k = k
        self.t = k.nc.alloc_psum_tensor("psum_all", [128, 8, 512], F32)
        self.b = [Tile(k, self.t[:, i, :], f"bank{i}") for i in range(8)]
        for t in self.b:
            t.excl = True

    def v(self, i, n=1):
        if n == 1:
            return V(self.b[i], self.t[:, i, :]), []
        ap = self.t[:, i:i + n, :].rearrange("p a b -> p (a b)")
        return V(self.b[i], ap), [self.b[j].a for j in range(i + 1, i + n)]


def layer_norm_tile(k, src, dst, g_t, b_t, eps_t, tmp, st, mv, rstd, nmr):
    for i in range(2):
        k.op("dve", "bn_stats", out=st[:, i, :], in_=src[:, i * 512:(i + 1) * 512])
    k.op("dve", "bn_aggr", out=mv.a, in_=st.a.re("p a b -> p (a b)"))
    k.op("act", "activation", out=rstd.a, in_=mv[:, 1:2], func=AF.Ln, bias=eps_t.a, scale=1.0)
    k.op("act", "activation", out=rstd.a, in_=rstd.a, func=AF.Exp, scale=-0.5)
    k.op("dve", "scalar_tensor_tensor", out=nmr.a, in0=mv[:, 0:1], scalar=-1.0, in1=rstd.a,
         op0=ALU.mult, op1=ALU.mult)
    k.op("act", "activation", out=dst, in_=src, func=AF.Identity, bias=nmr.a, scale=rstd.a)
    k.op("pool", "tensor_tensor", out=dst, in0=dst, in1=g_t.a, op=ALU.mult)
    k.op("pool", "tensor_tensor", out=dst, in0=dst, in1=b_t.a, op=ALU.add)


def emit_phase_b(k, P, NT, SG, oT, xres, xo, w_out, lnp, wr, br, w_gate, w_up, w_down, ident_d):
    nsg = NT // SG
    ntile = SG // 128
    ngrp = SG // 512
    idf = k.sb([128, 128], F32, "idf")
    k.dma("sp", idf.a, ident_d.a, "ld_c")
    woT = k.sb([128, 8, 1024], BF16, "woT")
    k.dma("pool", woT.a, V(w_out, w_out.h[:, :].rearrange("(c p) n -> p c n", p=128)), "ld_c")
    lnt = []
    for i in range(4):
        t = k.sb([128, 1024], F32, f"ln{i}")
        k.dma("sp", t.a, V(lnp, lnp.h[i].partition_broadcast(128)), "ld_c")
        lnt.append(t)
    WR = k.sb([128, 8, 20], F32, "WR")
    if True:
        k.dma("sp", WR.a, V(wr, wr.h[:, :].rearrange("(c p) n -> p c n", p=128)), "ld_c")
    BR = k.sb([128, 20], F32, "BR")
    if True:
      k.dma("sp", BR.a, V(br, br.h[:].partition_broadcast(128)), "ld_c")
    eps_t = k.sb([128, 1], F32, "eps")
    k.op("pool", "memset", ap=eps_t.a, constant=EPS, _writes=[eps_t.a])
    SELT = k.sb([16, 16, 128], F32, "SELT")
    k.op("pool", "memset", ap=SELT.a, constant=1.0, _writes=[SELT.a])
    ones16 = SELT
    if True:
      k.op("pool", "affine_select", out=SELT.a, in_=ones16.a, pattern=[[-1, 16], [0, 128]],
         compare_op=ALU.is_equal, fill=0.0, base=0, channel_multiplier=1)

    acc = [k.sb([128, 1024], F32, f"acc{i}") for i in range(ntile)]
    x1T = k.sb([128, 8, SG], BF16, "x1T")
    CT = k.sb([16, SG], F32, "CT")
    oTt = [k.sb([128, 8, 128], BF16, f"oTt{i}") for i in range(2)]
    xrt = [k.sb([128, 1024], F32, f"xrt{i}") for i in range(2)]
    yt = k.sb([128, 1024], F32, "yt")
    tmp = yt
    x1t = k.sb([128, 1024], F32, "x1t")
    x1Tf = k.sb([128, 8, 128], F32, "x1Tf")
    st = k.sb([128, 2, 6], F32, "st")
    mv = k.sb([128, 2], F32, "mv")
    rstd = k.sb([128, 1], F32, "rstd")
    nmr = k.sb([128, 1], F32, "nmr")
    L = k.sb([128, 20], F32, "L")
    sm = k.sb([128, 16], F32, "sm")
    r4 = k.sb([128, 8], F32, "r4")
    r16 = [k.sb([128, 16], F32, f"r16_{i}") for i in range(4)]
    comb = k.sb([128, 16], F32, "comb")
    combAll = k.sb([128, ntile, 16], F32, "combAll")
    wgb = [k.sb([128, 8, FH], BF16, f"wgb{i}") for i in range(2)]
    wub = [k.sb([128, 8, FH], BF16, f"wub{i}") for i in range(2)]
    wdb = [k.sb([128, 2, 1024], BF16, f"wdb{i}") for i in range(2)]
    hT = [k.sb([128, 2, 512], BF16, f"hT{i}") for i in range(2)]
    sgt = [k.sb([128, 512], F32, f"sgt{i}") for i in range(2)]
    tt_ = sgt
    ot = xrt

    oT_v = oT.h[:, :].rearrange("(c p) t -> p c t", p=128)
    dcount = [0]
    ld_i = 0
    for sg in range(nsg):
        for ti in range(ntile if 9.0 >= 1 else 0):
            tok0 = sg * SG + ti * 128
            o_t = oTt[ld_i % 2]
            x_t = xrt[ld_i % 2]
            ld_i += 1
            k.dma("sp", o_t.a, V(oT, oT_v[:, :, tok0:tok0 + 128]), f"ld_o{ld_i % 2}")
            k.dma("sp", x_t.a, V(xres, xres.h[tok0:tok0 + 128, :]), f"ld_x{ld_i % 2}")
            pm, pm_x = P.v(0, 2)
            for half in range(2):
                for c in range(8):
                    k.op("pe", "matmul", out=pm[:, half * 512:(half + 1) * 512], lhsT=o_t[:, c, :],
                         rhs=woT[:, c, half * 512:(half + 1) * 512], start=(c == 0), stop=(c == 7),
                         _writes=pm_x, mark=(c == 7 and half == 1))
            k.op("dve", "scalar_tensor_tensor", out=yt.a, in0=x_t.a, scalar=ALPHA, in1=pm, op0=ALU.mult,
                 op1=ALU.add, _reads=pm_x)
            if 9.0 < 1.2:
                continue
            layer_norm_tile(k, yt.a, x1t.a, lnt[0], lnt[1], eps_t, tmp.a, st, mv, rstd, nmr)
            k.op("act", "mul", out=acc[ti].a, in_=x1t.a, mul=ALPHA)
            if 9.0 < 1.4:
                continue
            pT, pT_x = P.v(2, 2)
            for c in range(8):
                k.op("pe", "transpose", out=pT[:, c * 128:(c + 1) * 128], in_=x1t[:, c * 128:(c + 1) * 128],
                     identity=idf.a, _writes=pT_x, mark=(c == 7))
            if True:
                k.op("act", "copy", out=x1Tf.a.re("p c t -> p (c t)"), in_=pT, _reads=pT_x)
            if True:
                k.op("dve", "tensor_copy", out=x1T[:, :, ti * 128:(ti + 1) * 128],
                     in_=pT.re("p (c t) -> p c t", c=8), _reads=pT_x)
            if 9.0 < 2:
                continue
            pr, _ = P.v(7)
            for c in range(8):
                k.op("pe", "matmul", out=pr[:, 0:20], lhsT=x1Tf[:, c, :], rhs=WR[:, c, :], start=(c == 0),
                     stop=(c == 7), mark=(c == 7))
            k.op("dve", "tensor_tensor", out=L.a, in0=pr[:, 0:20], in1=BR.a, op=ALU.add)
            gmax, ngmax, sumg, wg_, m1, nm1, m2, ssum, coef = [sm[:, i:i + 1] for i in range(9)]
            k.op("dve", "reduce_max", out=gmax, in_=L[:, 0:4], axis=AX.X)
            k.op("dve", "tensor_scalar", out=ngmax, in0=gmax, scalar1=-1.0, scalar2=None, op0=ALU.mult)
            k.op("act", "activation", out=r4[:, 0:4], in_=L[:, 0:4], func=AF.Exp, bias=ngmax, scale=1.0,
                 accum_out=sumg)
            k.op("dve", "reciprocal", out=wg_, in_=sumg)
            k.op("dve", "tensor_scalar", out=r4[:, 4:8], in0=L[:, 0:4], scalar1=gmax, scalar2=None,
                 op0=ALU.is_equal)
            k.op("dve", "tensor_scalar", out=r4[:, 4:8], in0=r4[:, 4:8], scalar1=-1.0, scalar2=1e30,
                 op0=ALU.add, op1=ALU.mult)
            lm = r16[0]
            k.op("dve", "tensor_tensor", out=lm.a.re("p (g e) -> p g e", g=4),
                 in0=L[:, 4:20].re("p (g e) -> p g e", g=4), in1=r4[:, 4:8].unsq(2).bc([128, 4, 4]),
                 op=ALU.add)
            k.op("dve", "reduce_max", out=m1, in_=lm.a, axis=AX.X)
            k.op("dve", "tensor_scalar", out=r16[1].a, in0=lm.a, scalar1=m1, scalar2=None, op0=ALU.is_equal)
            k.op("dve", "scalar_tensor_tensor", out=r16[1].a, in0=r16[1].a, scalar=-1e30, in1=lm.a,
                 op0=ALU.mult, op1=ALU.add)
            k.op("dve", "reduce_max", out=m2, in_=r16[1].a, axis=AX.X)
            k.op("dve", "tensor_scalar", out=r16[2].a, in0=lm.a, scalar1=m2, scalar2=None, op0=ALU.is_ge)
            k.op("dve", "tensor_scalar", out=nm1, in0=m1, scalar1=-1.0, scalar2=None, op0=ALU.mult)
            k.op("act", "activation", out=r16[3].a, in_=lm.a, func=AF.Exp, bias=nm1, scale=1.0)
            k.op("dve", "tensor_tensor", out=r16[3].a, in0=r16[3].a, in1=r16[2].a, op=ALU.mult)
            k.op("dve", "reduce_sum", out=ssum, in_=r16[3].a, axis=AX.X)
            k.op("dve", "reciprocal", out=ssum, in_=ssum)
            k.op("dve", "tensor_tensor", out=coef, in0=ssum, in1=wg_, op=ALU.mult)
            k.op("dve", "tensor_scalar", out=comb.a, in0=r16[3].a, scalar1=coef, scalar2=None, op0=ALU.mult)
            k.op("dve", "tensor_copy", out=combAll[:, ti, :], in_=comb.a)
        items = [(e, grp) for e in range(NE) for grp in range(ngrp)]

        def load_w(e):
            s_ = e % 2
            k.dma("pool", wgb[s_].a, V(w_gate, w_gate.h[e].rearrange("(c p) f -> p c f", p=128)))
            k.dma("pool", wub[s_].a, V(w_up, w_up.h[e].rearrange("(c p) f -> p c f", p=128)))
            k.dma("pool", wdb[s_].a, V(w_down, w_down.h[e].rearrange("(c p) n -> p c n", p=128)))

        def gu_part(it, f, which):
            e, grp = it
            s_ = e % 2
            tc0 = grp * 512
            pb_, _ = P.v(2 + 2 * f + which)
            wt = wgb[s_] if which == 0 else wub[s_]
            for c in range(8):
                k.op("pe", "matmul", out=pb_, lhsT=wt[:, c, f * 128:(f + 1) * 128],
                     rhs=x1T[:, c, tc0:tc0 + 512], start=(c == 0), stop=(c == 7), mark=(c == 7))

        def elem_part(idx, f):
            h_t = hT[idx % 2]
            pcb, _ = P.v(6)
            pg, _ = P.v(2 + 2 * f)
            pu, _ = P.v(3 + 2 * f)
            k.op("act", "activation", out=sgt[f].a, in_=pg, func=AF.Silu)
            k.op("dve", "tensor_tensor", out=h_t[:, f, :], in0=sgt[f].a, in1=pu, op=ALU.mult)

        def down_unit(idx, it, u):
            e, grp = it
            s_ = e % 2
            h_t = hT[idx % 2]
            t4, half = u // 2, u % 2
            ti = grp * 4 + t4
            pd, _ = P.v((0, 1, 7)[dcount[0] % 3])
            dcount[0] += 1
            for f in range(2):
                k.op("pe", "matmul", out=pd, lhsT=h_t[:, f, t4 * 128:(t4 + 1) * 128],
                     rhs=wdb[s_][:, f, half * 512:(half + 1) * 512], start=(f == 0), stop=(f == 1), mark=(f == 1))
            k.op("dve", "scalar_tensor_tensor", out=acc[ti][:, half * 512:(half + 1) * 512], in0=pd,
                 scalar=combAll[:, ti, e:e + 1], in1=acc[ti][:, half * 512:(half + 1) * 512], op0=ALU.mult, op1=ALU.add)

        load_w(0)
        for f in range(2):
            gu_part(items[0], f, 0)
            gu_part(items[0], f, 1)
            elem_part(0, f)
        for idx, it in enumerate(items):
            nxt = items[idx + 1] if idx + 1 < len(items) else None
            if nxt is not None and nxt[0] != it[0]:
                load_w(nxt[0])
            u = 0
            for f in range(2):
                for which in range(2):
                    if nxt is not None:
                        gu_part(nxt, f, which)
                    if which == 1 and nxt is not None:
                        elem_part(idx + 1, f)
                    down_unit(idx, it, u)
                    down_unit(idx, it, u + 1)
                    u += 2
        for ti in range(ntile):
            tok0 = sg * SG + ti * 128
            o_ = ot[ti % 2]
            layer_norm_tile(k, acc[ti].a, o_.a, lnt[2], lnt[3], eps_t, tmp.a, st, mv, rstd, nmr)
            dst_ap = xo.h[tok0:tok0 + 128, :]
            k.dma("sp", V(Tile(k, dst_ap, "xo_part"), dst_ap), o_.a)


def load_xT_group(k, P, x_d, tok0, ntok, xin, xbf, xT, idb, bank):
    for t in range(ntok // 128):
        xb = xbf[t % 2]
        k.dma("pool", xb.a, V(x_d, x_d.h[tok0 + t * 128: tok0 + (t + 1) * 128, :]))
        pt, _ = P.v(bank + (t % 2))
        ptb = pt.bitcast(BF16)
        for c in range(8):
            k.op("pe", "transpose", out=ptb[:, c * 128:(c + 1) * 128], in_=xb[:, c * 128:(c + 1) * 128],
                 identity=idb.a, mark=(c == 7))
        k.op("dve", "tensor_copy", out=xT[:, :, t * 128:(t + 1) * 128], in_=ptb.re("p (c t) -> p c t", c=8))


def run_streams_sb(gens, nslot):
    pending = list(gens)
    active = [None] * nslot
    while True:
        progressed = False
        for s_ in range(nslot):
            if active[s_] is None and pending:
                active[s_] = pending.pop(0)(s_)
            if active[s_] is not None:
                try:
                    next(active[s_])
                except StopIteration:
                    active[s_] = None
                progressed = True
        if not progressed and not pending:
            break


def sb_stream(k, P, slot, h, qb, qt, kt, r0, Vt, Oa, NTRI, LSTR, e_s, p_s, a_s, w_s):
    zb = P.v(0 + slot)[0]
    lab = P.v(2 + slot)[0]
    ob = P.v(4 + slot)[0]
    chunks = [(4 * qb + j, 128 * j) for j in (3, 2, 1, 0)] + [(kc, 0) for kc in range(4 * qb - 1, -1, -1)]

    def zmm(kc, c0):
        k.op("pe", "matmul", out=zb[:, c0:512], lhsT=kt[r0:r0 + 64, kc * 128:(kc + 1) * 128],
             rhs=qt[r0:r0 + 64, qb * 512 + c0:(qb + 1) * 512], start=True, stop=True, mark=True)
    zmm(*chunks[0])
    yield
    prev = None
    first = True
    for ci, (kc, c0) in enumerate(chunks):
        e = e_s[ci % 2]
        p_cur = p_s[ci % 2]
        a = a_s[ci % 2]
        w = w_s[ci % 2]
        k.op("act", "activation", out=e[:, c0:512], in_=zb[:, c0:512], func=AF.Exp, scale=0.125)
        if kc >= 4 * qb:
            k.op("pool", "affine_select", out=e[:, c0:c0 + 128], in_=e[:, c0:c0 + 128],
                 pattern=[[1, 128]], compare_op=ALU.is_gt, fill=0.0, base=0, channel_multiplier=-1)
        k.op("act", "activation", out=p_cur[:, c0:512], in_=e[:, c0:512], func=AF.Ln, bias=1.0, scale=1.0)
        yield
        if prev is not None:
            pp, pc0 = prev
            k.op("pe", "matmul", out=lab[:, pc0:512], lhsT=LSTR.a, rhs=pp[:, pc0:512], start=False,
                 stop=False, skip_group_check=True)
        k.op("pe", "matmul", out=lab[:, c0:512], lhsT=NTRI.a, rhs=p_cur[:, c0:512],
             start=first, stop=True, skip_group_check=True, mark=True)
        if ci + 1 < len(chunks):
            zmm(*chunks[ci + 1])
        yield
        k.op("act", "activation", out=a[:, c0:512], in_=lab[:, c0:512], func=AF.Exp)
        k.op("dve", "tensor_tensor", out=w[:, c0:512], in0=a[:, c0:512], in1=e[:, c0:512], op=ALU.mult)
        yield
        for i in range(c0 // 128, 4):
            k.op("pe", "matmul", out=ob[:, i * 64:(i + 1) * 64], lhsT=w[:, i * 128:(i + 1) * 128],
                 rhs=Vt[:, kc, h * 64:(h + 1) * 64], start=(first and i == c0 // 128),
                 stop=True, skip_group_check=True, mark=(i == 3))
        first = False
        prev = (p_cur, c0)
        yield
    k.op("act", "copy", out=Oa[:, qb * 4:(qb + 1) * 4, h * 64:(h + 1) * 64],
         in_=ob[:, 0:256].re("p (i d) -> p i d", i=4))
    yield


def sb_wide(k, P, hp, qb, qt, kt, Vt, Oa, NTRI, LSTR, e_b, p_b, a_b, w_b):
    chunks = [(4 * qb + j, 128 * j) for j in (3, 2, 1, 0)] + [(kc, 0) for kc in range(4 * qb - 1, -1, -1)]
    n = len(chunks)
    T = P.t

    def pair(b0, c0):
        ap = T[:, b0:b0 + 2, c0:512]
        return V(P.b[b0], ap), [P.b[b0 + 1].a]

    def zmm(i):
        kc, c0 = chunks[i]
        b0 = 2 * (i % 2)
        for hh in range(2):
            r0 = hh * 64
            zb = V(P.b[b0 + hh], T[:, b0 + hh, c0:512])
            k.op("pe", "matmul", out=zb, lhsT=kt[r0:r0 + 64, kc * 128:(kc + 1) * 128],
                 rhs=qt[r0:r0 + 64, qb * 512 + c0:(qb + 1) * 512], start=True, stop=True, mark=(hh == 1))

    def ep(i):
        kc, c0 = chunks[i]
        zv, zx = pair(2 * (i % 2), c0)
        e = e_b[i % 3]
        k.op("act", "activation", out=e[:, :, c0:512], in_=zv, func=AF.Exp, scale=0.125, _reads=zx)
        if kc >= 4 * qb:
            k.op("pool", "affine_select", out=e[:, :, c0:c0 + 128], in_=e[:, :, c0:c0 + 128],
                 pattern=[[0, 2], [1, 128]], compare_op=ALU.is_gt, fill=0.0, base=0, channel_multiplier=-1)
        k.op("act", "activation", out=p_b[i % 3][:, :, c0:512], in_=e[:, :, c0:512], func=AF.Ln, bias=1.0, scale=1.0)

    def la(i):
        kc, c0 = chunks[i]
        for hh in range(2):
            lab = V(P.b[4 + hh], T[:, 4 + hh, :])
            if i > 0:
                pc0 = chunks[i - 1][1]
                k.op("pe", "matmul", out=lab[:, pc0:512], lhsT=LSTR.a, rhs=p_b[(i - 1) % 3][:, hh, pc0:512], start=False,
                     stop=False, skip_group_check=True)
            k.op("pe", "matmul", out=lab[:, c0:512], lhsT=NTRI.a, rhs=p_b[i % 3][:, hh, c0:512],
                 start=(i == 0), stop=True, skip_group_check=True, mark=(hh == 1))

    def aw(i):
        kc, c0 = chunks[i]
        lv, lx = pair(4, c0)
        k.op("act", "activation", out=a_b[i % 2][:, :, c0:512], in_=lv, func=AF.Exp, _reads=lx)
        k.op("dve", "tensor_tensor", out=w_b[i % 2][:, :, c0:512], in0=a_b[i % 2][:, :, c0:512],
             in1=e_b[i % 3][:, :, c0:512], op=ALU.mult)

    def pv(i):
        kc, c0 = chunks[i]
        ob = V(P.b[6], T[:, 6, :])
        for hh in range(2):
            h = 2 * hp + hh
            for j in range(c0 // 128, 4):
                col = (hh * 4 + j) * 64
                k.op("pe", "matmul", out=ob[:, col:col + 64], lhsT=w_b[i % 2][:, hh, j * 128:(j + 1) * 128],
                     rhs=Vt[:, kc, h * 64:(h + 1) * 64], start=(i == 0 and hh == 0 and j == c0 // 128),
                     stop=True, skip_group_check=True, mark=(hh == 1 and j == 3))

    zmm(0)
    ep(0)
    if n > 1:
        zmm(1)
    la(0)
    for i in range(n):
        if i + 1 < n:
            ep(i + 1)
        if i + 2 < n:
            zmm(i + 2)
        aw(i)
        if i + 1 < n:
            la(i + 1)
        pv(i)
    ob = V(P.b[6], T[:, 6, :])
    for hh in range(2):
        h = 2 * hp + hh
        k.op("act", "copy", out=Oa[:, qb * 4:(qb + 1) * 4, h * 64:(h + 1) * 64],
             in_=ob[:, hh * 256:(hh + 1) * 256].re("p (i d) -> p i d", i=4))


def emit_phase_sb(k, P, S, x_d, w_d, oT_d, ident_d, nsub=2, row0=0):
    NTILE = S // 128
    NQB = S // 512
    idf = k.sb([128, 128], F32, "idf")
    k.dma("sp", idf.a, ident_d.a, "ld_c")
    idb = k.sb([128, 128], BF16, "idb")
    k.op("dve", "tensor_copy", out=idb.a, in_=idf.a)
    negones = k.sb([128, 128], BF16, "negones")
    k.op("pool", "memset", ap=negones.a, constant=-1.0, _writes=[negones.a])
    NTRI = k.sb([128, 128], BF16, "NTRI")
    k.op("pool", "affine_select", out=NTRI.a, in_=negones.a, pattern=[[-1, 128]], compare_op=ALU.is_ge,
         fill=0.0, base=0, channel_multiplier=1)
    LSTR = k.sb([128, 128], BF16, "LSTR")
    k.op("pool", "affine_select", out=LSTR.a, in_=negones.a, pattern=[[1, 128]], compare_op=ALU.is_gt,
         fill=0.0, base=0, channel_multiplier=-1)

    W = k.sb([128, 8, 768], BF16, "Wsb")
    QT = [k.sb([128, S], BF16, f"QT{i}") for i in range(2)]
    KT = [k.sb([128, S], BF16, f"KT{i}") for i in range(2)]
    Vt = k.sb([128, NTILE, 256], BF16, "Vt")
    Oa = k.sb([128, NTILE, 256], BF16, "Oa")
    xin = None
    xbf = [k.sb([128, 1024], BF16, f"xbf{i}") for i in range(2)]
    xT = [k.sb([128, 8, 512], BF16, f"xT{i}") for i in range(2)]
    NS = 2
    e_b = [k.sb([128, 2, 512], F32, f"e{j}") for j in range(3)]
    p_b = [k.sb([128, 2, 512], BF16, f"p{j}") for j in range(3)]
    a_b = [k.sb([128, 2, 512], BF16, f"a{j}") for j in range(2)]
    w_b = [k.sb([128, 2, 512], BF16, f"w{j}") for j in range(2)]
    oTs = [k.sb([128, 2, 128], BF16, f"oTs{i}") for i in range(2)]

    wv = w_d.h[:, :].rearrange("(c p) n -> p c n", p=128)
    for sub in range(nsub):
        for j, base in enumerate((0, 512, 1024)):
            k.dma("pool", W[:, :, j * 256:(j + 1) * 256],
                  V(w_d, wv[:, :, base + sub * 256: base + (sub + 1) * 256]), "ld_w")
        for g4 in range(S // 512):
            tok0 = g4 * 512
            xT_g = xT[g4 % 2]
            load_xT_group(k, P, x_d, tok0, 512, xin, xbf, xT_g, idb, 0)
            for j in range(4):
                pq, _ = P.v(2 + (j % 2))
                for c in range(8):
                    k.op("pe", "matmul", out=pq, lhsT=W[:, c, j * 128:(j + 1) * 128], rhs=xT_g[:, c, :],
                         start=(c == 0), stop=(c == 7), mark=(c == 7))
                dst = (QT if j < 2 else KT)[j % 2]
                k.op("act", "copy", out=dst[:, tok0:tok0 + 512], in_=pq)
            for t in range(4):
                pv, _ = P.v(4 + (t % 2))
                for c in range(8):
                    k.op("pe", "matmul", out=pv[:, 0:256], lhsT=xT_g[:, c, t * 128:(t + 1) * 128],
                         rhs=W[:, c, 512:768], start=(c == 0), stop=(c == 7), mark=(c == 7))
                k.op("dve", "tensor_copy", out=Vt[:, g4 * 4 + t, :], in_=pv[:, 0:256])
        for qb in range(NQB):
            for hp in range(2):
                sb_wide(k, P, hp, qb, QT[hp], KT[hp], Vt, Oa, NTRI, LSTR, e_b, p_b, a_b, w_b)
        for t in range(NTILE):
            pt, _ = P.v(6 + (t % 2))
            ptb = pt.bitcast(BF16)
            for c in range(2):
                k.op("pe", "transpose", out=ptb[:, c * 128:(c + 1) * 128], in_=Oa[:, t, c * 128:(c + 1) * 128],
                     identity=idb.a, mark=(c == 1))
            o_ = oTs[t % 2]
            k.op("dve", "tensor_copy", out=o_.a, in_=ptb[:, 0:256].re("p (c t) -> p c t", c=2))
            dst_ap = oT_d.h[row0 + sub * 256:row0 + (sub + 1) * 256, t * 128:(t + 1) * 128].rearrange("(c p) t -> p c t", p=128)
            k.dma("sp", V(Tile(k, dst_ap, "oT_part"), dst_ap), o_.a)


import math

SCALE = 0.125
NEG = -30000.0


def run_streams(gens, nslot):
    pending = list(gens)
    active = [None] * nslot
    while True:
        progressed = False
        for s in range(nslot):
            if active[s] is None and pending:
                active[s] = pending.pop(0)(s)
            if active[s] is not None:
                try:
                    next(active[s])
                except StopIteration:
                    active[s] = None
                progressed = True
        if not progressed and not pending:
            break


def flash_stream(k, slot, zb, ob, e_tiles, qv_fn, chunks, nv, negM, epilogue):
    first = True
    for ci, ch in enumerate(chunks):
        c0, c1 = ch["c0"], ch["c1"]
        e = e_tiles[ci % 2]
        has_mask = ch.get("mask") is not None
        k.op("pe", "matmul", out=zb[:, c0:c1], lhsT=ch["kT"], rhs=qv_fn(c0, c1), start=True, stop=not has_mask,
             mark=not has_mask, skip_group_check=True)
        if has_mask:
            ml, mr = ch["mask"]
            k.op("pe", "matmul", out=zb[:, c0:c1], lhsT=ml, rhs=mr(c0, c1), start=False, stop=True, mark=True,
                 skip_group_check=True)
        yield
        k.op("act", "activation", out=e[:, c0:c1], in_=zb[:, c0:c1], func=AF.Exp, scale=SCALE, bias=negM)
        for (lo, hi, base, cm, step, op) in ch.get("aff", []):
            k.op("pool", "affine_select", out=e[:, lo:hi], in_=e[:, lo:hi], pattern=[[step, hi - lo]],
                 compare_op=op, fill=0.0, base=base, channel_multiplier=cm)
        yield
        subs = list(range(c0 // 128, (c1 + 127) // 128))
        for i in subs:
            k.op("pe", "matmul", out=ob[:, i * nv:(i + 1) * nv], lhsT=e[:, i * 128:(i + 1) * 128], rhs=ch["v"],
                 start=first, stop=True, skip_group_check=True, mark=(i == subs[-1]))
            first = False
        yield
    epilogue()
    yield


def rope_group(k, pos_t, invf, g4, tmps):
    posf, y, yy, ki, kf, outs = tmps
    k.op("dve", "tensor_copy", out=posf.a, in_=pos_t[:, g4 * 4:(g4 + 1) * 4])
    k.op("dve", "tensor_tensor", out=y.a, in0=posf.a.unsq(2).bc([128, 4, 32]),
         in1=invf.a.unsq(1).bc([128, 4, 32]), op=ALU.mult)
    res = []
    for j, shift in enumerate((0.0, 0.25)):
        k.op("dve", "tensor_scalar", out=yy.a, in0=y.a, scalar1=shift, scalar2=None, op0=ALU.add)
        k.op("dve", "tensor_copy", out=ki.a, in_=yy.a)
        k.op("dve", "tensor_copy", out=kf.a, in_=ki.a)
        k.op("dve", "tensor_tensor", out=yy.a, in0=yy.a, in1=kf.a, op=ALU.subtract)
        k.op("dve", "tensor_scalar", out=kf.a, in0=yy.a, scalar1=0.5, scalar2=None, op0=ALU.is_gt)
        k.op("dve", "tensor_tensor", out=yy.a, in0=yy.a, in1=kf.a, op=ALU.subtract)
        k.op("dve", "tensor_scalar", out=kf.a, in0=yy.a, scalar1=-0.5, scalar2=None, op0=ALU.is_lt)
        k.op("dve", "tensor_tensor", out=yy.a, in0=yy.a, in1=kf.a, op=ALU.add)
        t = outs[g4 % 2][j]
        k.op("act", "activation", out=t.a, in_=yy.a, func=AF.Sin, scale=2.0 * math.pi * (1 - 1e-6))
        res.append(t)
    return res[1], res[0]


def rope_tmps(k):
    posf = k.sb([128, 4], F32, "posf")
    y = k.sb([128, 4, 32], F32, "rope_y")
    yy = k.sb([128, 4, 32], F32, "rope_yy")
    ki = k.sb([128, 4, 32], I32, "rope_ki")
    kf = k.sb([128, 4, 32], F32, "rope_kf")
    outs = [[k.sb([128, 4, 32], F32, f"rope_o{i}_{j}") for j in range(2)] for i in range(2)]
    return (posf, y, yy, ki, kf, outs)


def rope_apply(k, pf, nh, cos, sin, t1, t2, out_bf):
    pr = pf.re("p (h t d) -> p h t d", h=nh, t=2)
    t1v = t1[:, 0:nh * 64].re("p (h t d) -> p h t d", h=nh, t=2)
    t2v = t2[:, 0:nh * 64].re("p (h t d) -> p h t d", h=nh, t=2)
    ob = out_bf.re("p (h t d) -> p h t d", h=nh, t=2)
    cb = cos.unsq(1).unsq(1).bc([128, nh, 2, 32])
    sb_ = sin.unsq(1).bc([128, nh, 32])
    k.op("dve", "tensor_tensor", out=t1v, in0=pr, in1=cb, op=ALU.mult)
    k.op("pool", "tensor_tensor", out=t2v[:, :, 0, :], in0=pr[:, :, 1, :], in1=sb_, op=ALU.mult)
    k.op("pool", "tensor_tensor", out=t2v[:, :, 1, :], in0=pr[:, :, 0, :], in1=sb_, op=ALU.mult)
    k.op("dve", "tensor_tensor", out=ob[:, :, 0, :], in0=t1v[:, :, 0, :], in1=t2v[:, :, 0, :], op=ALU.subtract)
    k.op("dve", "tensor_tensor", out=ob[:, :, 1, :], in0=t1v[:, :, 1, :], in1=t2v[:, :, 1, :], op=ALU.add)


def bound_negM(k, P, nq_t, nk_t, idf, ones1, out_negM, bank, scratch):
    mq, mm, row = scratch
    k.op("dve", "reduce_max", out=mq[:, 0:1], in_=nq_t, axis=AX.X)
    k.op("dve", "reduce_max", out=mq[:, 1:2], in_=nk_t, axis=AX.X)
    pb, _ = P.v(bank)
    k.op("pe", "transpose", out=pb[0:2, 0:128], in_=mq.a, identity=idf.a, mark=True)
    k.op("dve", "reduce_max", out=mm.a, in_=pb[0:2, 0:128], axis=AX.X)
    k.op("pe", "transpose", out=pb[0:1, 128:130], in_=mm.a, identity=idf[0:2, 0:2], mark=True)
    k.op("dve", "tensor_copy", out=row[:, 0:2], in_=pb[0:1, 128:130])
    k.op("dve", "tensor_tensor", out=row[:, 2:3], in0=row[:, 0:1], in1=row[:, 1:2], op=ALU.mult)
    k.op("act", "activation", out=row[:, 2:3], in_=row[:, 2:3], func=AF.Ln)
    k.op("act", "activation", out=row[:, 2:3], in_=row[:, 2:3], func=AF.Exp, scale=0.5)
    k.op("dve", "tensor_scalar", out=row[:, 3:4], in0=row[:, 2:3], scalar1=-SCALE * 1.02, scalar2=None, op0=ALU.mult)
    k.op("pe", "matmul", out=pb[:, 132:133], lhsT=ones1.a, rhs=row[:, 3:4], start=True, stop=True, mark=True)
    k.op("dve", "tensor_copy", out=out_negM, in_=pb[:, 132:133])


def emit_out_T(k, P, OUT_bf, oT_d, row0, tok0, idb, oTs, bank, idx):
    dst_ap = oT_d.h[row0:row0 + 256, tok0:tok0 + 128].rearrange("(c p) t -> p c t", p=128)
    pt, _ = P.v(bank)
    ptb = pt.bitcast(BF16)
    for c in range(2):
        k.op("pe", "transpose", out=ptb[:, c * 128:(c + 1) * 128], in_=OUT_bf[:, c * 128:(c + 1) * 128],
             identity=idb.a, mark=(c == 1))
    o_ = oTs[idx % 2]
    k.op("dve", "tensor_copy", out=o_.a, in_=ptb[:, 0:256].re("p (c t) -> p c t", c=2))
    k.dma("sp", V(Tile(k, dst_ap, "oT_part"), dst_ap), o_.a)


def emit_phase_l0(k, P, S, x_d, pos_d, wn_d, wm_d, cw, consts, oT_d, do_nsa=True, do_moba=True, row_nsa=0, row_moba=256):
    NTILE = S // 128
    NQB = S // 512
    NCMP = (S - 32) // 16 + 1
    NCC = (NCMP + 127) // 128
    NSLC = S // 64
    NBLK = S // 256
    idf = k.sb([128, 128], F32, "idf")
    k.dma("sp", idf.a, consts["ident"].a, "ld_c")
    idb = k.sb([128, 128], BF16, "idb")
    k.op("dve", "tensor_copy", out=idb.a, in_=idf.a)
    ones1 = k.sb([1, 128], F32, "ones1")
    k.op("pool", "memset", ap=ones1.a, constant=1.0, _writes=[ones1.a])
    invf = k.sb([128, 32], F32, "invf")
    k.dma("sp", invf.a, consts["invf"].a, "ld_c")
    pos_t = k.sb([128, NTILE], I32, "pos_t")
    k.dma("sp", pos_t.a, pos_d.a, "ld_c")
    rtm = rope_tmps(k)
    xin = None
    xbf = [k.sb([128, 1024], BF16, f"xbf{i}") for i in range(2)]
    xT = [k.sb([128, 8, 512], BF16, "xT0")] * 2
    pf = k.sb([128, 780], F32, "pf")
    t1 = k.sb([128, 576], F32, "rt1")
    t2 = k.sb([128, 576], F32, "rt2")
    Rb = k.sb([128, 640], BF16, "Rb")
    sqj = k.sb([128, 640], F32, "sqj")
    e_t = [[k.sb([128, 512], BF16, f"e{s}_{j}") for j in range(2)] for s in range(4)]
    oTs = [k.sb([128, 2, 128], BF16, f"oTs{i}") for i in range(2)]
    OUT = k.sb([128, 4, 256], F32, "OUT")
    OUTb = k.sb([128, 4, 256], BF16, "OUTb")
    negM = [k.sb([128, 1], F32, f"negM{i}") for i in range(4)]
    sc_mq = k.sb([128, 2], F32, "sc_mq")
    sc_mk = k.sb([2, 1], F32, "sc_mk")
    sc_row = k.sb([1, 4], F32, "sc_row")
    rz = k.sb([128, 4 * 4], F32, "rz")
    coef = k.sb([128, 4 * 4], F32, "coef")
    QT = [k.sb([128, S], BF16, f"QT{i}") for i in range(2)]
    KT = [k.sb([128, S], BF16, f"KT{i}") for i in range(2)]
    CT = k.sb([128, S], BF16, "CT")
    Vall = k.sb([128, NTILE, 260], BF16, "Vall")
    k.op("pool", "memset", ap=Vall.a, constant=1.0, _writes=[Vall.a])
    Wall = k.sb([128, 8, 780], BF16, "Wall")

    if do_nsa:
        W = Wall
        k.dma("pool", W.a, V(wn_d, wn_d.h[:, :].rearrange("(c p) n -> p c n", p=128)), "ld_w")
        VS = V(Vall, Vall.h[:, :, 0:130].rearrange("p t (a d) -> p t a d", a=2))
        GL = k.sb([128, NTILE, 12], F32, "GL")
        NQ = k.sb([128, 3, NTILE], F32, "NQ")
        for g4 in range(S // 512):
            xT_g = xT[g4 % 2]
            load_xT_group(k, P, x_d, g4 * 512, 512, xin, xbf, xT_g, idb, 0)
            COS4, SIN4 = rope_group(k, pos_t, invf, g4, rtm)
            for t in range(4):
                ti = g4 * 4 + t
                pp, pp_x = P.v(2 + 2 * (t % 2), 2)
                for (a, b) in ((0, 512), (512, 780)):
                    for c in range(8):
                        k.op("pe", "matmul", out=pp[:, a:b], lhsT=xT_g[:, c, t * 128:(t + 1) * 128], rhs=W[:, c, a:b],
                             start=(c == 0), stop=(c == 7), _writes=pp_x, mark=(c == 7 and a == 512))
                k.op("act", "copy", out=pf[:, 0:780], in_=pp[:, 0:780], _reads=pp_x)
                rope_apply(k, pf[:, 0:576], 9, COS4[:, t, :], SIN4[:, t, :], t1, t2, Rb[:, 0:576])
                k.op("pool", "tensor_copy", out=Rb[:, 576:640], in_=pf[:, 576:640])
                k.op("pool", "tensor_copy", out=VS[:, ti, :, 0:64], in_=pf[:, 640:768].re("p (a d) -> p a d", a=2))
                k.op("pool", "tensor_copy", out=GL[:, ti, :], in_=pf[:, 768:780])
                for j, (a, b) in enumerate(((0, 256), (320, 448), (512, 576))):
                    k.op("act", "activation", out=sqj[:, a:b], in_=Rb[:, a:b], func=AF.Square, accum_out=NQ[:, j, ti:ti + 1])
                pt, _ = P.v(6 + (t % 2))
                ptb = pt.bitcast(BF16)
                for j in range(5):
                    k.op("pe", "transpose", out=ptb[:, j * 128:(j + 1) * 128], in_=Rb[:, j * 128:(j + 1) * 128],
                         identity=idb.a, mark=(j == 4))
                for j, dst in enumerate((QT[0], QT[1], KT[0], KT[1], CT)):
                    k.op("act" if j % 2 else "dve", "copy" if j % 2 else "tensor_copy", out=dst[:, ti * 128:(ti + 1) * 128],
                         in_=ptb[:, j * 128:(j + 1) * 128])
        G = GL
        k.op("act", "activation", out=G.a, in_=GL.a, func=AF.Sigmoid)
        W1 = k.sb([128, 32, 128], BF16, "W1")
        k.dma("pool", W1[0:64], V(cw["w1k"], cw["w1k"].h[:, :].rearrange("(l d) h -> d l h", d=64)), "ld_w")
        k.dma("pool", W1[64:128], V(cw["w1v"], cw["w1v"].h[:, :].rearrange("(l d) h -> d l h", d=64)), "ld_w")
        PF = k.sb([128, 32], BF16, "PF")
        k.dma("pool", PF.a, cw["posT"].a)
        W2 = [k.sb([128, 128], BF16, "W2k2"), k.sb([128, 64], BF16, "W2v")]
        k.dma("pool", W2[0][:, 0:64], cw["w2k"].a, "ld_w")
        k.dma("pool", W2[0][:, 64:128], cw["w2k"].a, "ld_w")
        k.dma("pool", W2[1].a, cw["w2v"].a, "ld_w")
        KCT = k.sb([128, NCC * 128], BF16, "KCT")
        k.op("pool", "memset", ap=KCT.a, constant=0.0, _writes=[KCT.a])
        NV = 193
        VCA = k.sb([128, NCC, NV], BF16, "VCA")
        k.op("pool", "memset", ap=VCA.a, constant=1.0, _writes=[VCA.a])
        for c in range(NCC):
            k.op("pool", "affine_select", out=VCA[:, c, 65:193], in_=VCA[:, c, 65:193], pattern=[[-4, 128]],
                 compare_op=ALU.is_ge, fill=0.0, base=128 * c + 1, channel_multiplier=1)
            k.op("pool", "affine_select", out=VCA[:, c, 65:193], in_=VCA[:, c, 65:193], pattern=[[4, 128]],
                 compare_op=ALU.is_ge, fill=0.0, base=3 - 128 * c, channel_multiplier=-1)
        cb = k.sb([128, 2], F32, "cbias")
        gu = [k.sb([128, 512], F32, f"gu{i}") for i in range(3)]
        gact = [k.sb([128, 512], BF16, f"gact{i}") for i in range(2)]
        k.op("pool", "memset", ap=gact[1].a, constant=0.0, _writes=[gact[1].a])
        for kv in range(2):
            r0 = kv * 64
            ph, _ = P.v(0 + kv)
            for l in range(32):
                k.op("pe", "matmul", out=ph[:, 0:NCMP], lhsT=W1[r0:r0 + 64, l, :],
                     rhs=CT[r0:r0 + 64, l:l + 16 * (NCMP - 1) + 1:16], start=(l == 0), stop=(l == 31), mark=(l == 31))
            pbias, _ = P.v(2 + kv)
            for l in range(32):
                k.op("pe", "matmul", out=pbias[:, 0:1], lhsT=W1[r0:r0 + 64, l, :], rhs=PF[r0:r0 + 64, l:l + 1], start=(l == 0),
                     stop=(l == 31), mark=(l == 31))
            k.op("dve", "tensor_copy", out=cb[:, kv:kv + 1], in_=pbias[:, 0:1])
            u, u2, th = gu
            n_ = NCMP
            k.op("dve", "tensor_scalar", out=u[:, 0:n_], in0=ph[:, 0:n_], scalar1=cb[:, kv:kv + 1], scalar2=None, op0=ALU.add)
            k.op("dve", "tensor_tensor", out=u2[:, 0:n_], in0=u[:, 0:n_], in1=u[:, 0:n_], op=ALU.mult)
            k.op("dve", "tensor_scalar", out=u2[:, 0:n_], in0=u2[:, 0:n_], scalar1=0.044715, scalar2=1.0, op0=ALU.mult, op1=ALU.add)
            k.op("dve", "tensor_tensor", out=u2[:, 0:n_], in0=u2[:, 0:n_], in1=u[:, 0:n_], op=ALU.mult)
            k.op("act", "activation", out=th[:, 0:n_], in_=u2[:, 0:n_], func=AF.Tanh, scale=0.7978845608028654)
            k.op("dve", "tensor_scalar", out=th[:, 0:n_], in0=th[:, 0:n_], scalar1=0.5, scalar2=0.5, op0=ALU.mult, op1=ALU.add)
            k.op("dve", "tensor_tensor", out=gact[kv][:, 0:n_], in0=th[:, 0:n_], in1=u[:, 0:n_], op=ALU.mult)
            if kv == 0:
                pk, _ = P.v(4)
                k.op("pe", "matmul", out=pk[:, 0:n_], lhsT=W2[0].a, rhs=gact[0][:, 0:n_], start=True, stop=True, mark=True)
                k.op("dve", "tensor_copy", out=KCT[:, 0:n_], in_=pk[:, 0:n_])
                k.op("act", "activation", out=sqj[0:64, 0:n_], in_=pk[0:64, 0:n_], func=AF.Square)
            else:
                for c in range(NCC):
                    pvv, _ = P.v(5)
                    k.op("pe", "matmul", out=pvv[:, 0:64], lhsT=gact[1][:, c * 128:(c + 1) * 128], rhs=W2[1].a, start=True,
                         stop=True, mark=True)
                    k.op("dve", "tensor_copy", out=VCA[:, c, 0:64], in_=pvv[:, 0:64])
        ones64 = k.sb([64, 1], F32, "ones64")
        k.op("pool", "memset", ap=ones64.a, constant=1.0, _writes=[ones64.a])
        pn, _ = P.v(6)
        k.op("pe", "matmul", out=pn[0:1, 0:NCMP], lhsT=ones64.a, rhs=sqj[0:64, 0:NCMP], start=True, stop=True, mark=True)
        nkc = k.sb([128, 1], F32, "nkc")
        k.op("pool", "memset", ap=nkc.a, constant=0.0, _writes=[nkc.a])
        k.op("dve", "reduce_max", out=nkc[0:1, 0:1], in_=pn[0:1, 0:NCMP], axis=AX.X)
        bound_negM(k, P, NQ[:, 0, :], NQ[:, 1, :], idf, ones1, negM[0].a, 7, (sc_mq, sc_mk, sc_row))
        bound_negM(k, P, NQ[:, 0, :], nkc.a, idf, ones1, negM[1].a, 7, (sc_mq, sc_mk, sc_row))
        WV = k.sb([128, 256], F32, "WV")
        WADD = k.sb([128, 256], F32, "WADD")
        k.dma("sp", WV.a, consts["wv"].a, "ld_c")
        k.dma("sp", WADD.a, consts["wadd"].a, "ld_c")
        G64 = CT
        k.op("pool", "memset", ap=G64.a, constant=0.0, _writes=[G64.a])
        k.dma("sp", G64[0:NSLC], consts["g64"].a, "ld_c")
        IMP = k.sb([128, 4, 128], F32, "IMP")
        NMT = k.sb([128, 512], BF16, "NMT")
        k.op("pool", "memset", ap=NMT.a, constant=0.0, _writes=[NMT.a])
        sc_s = [k.sb([128, 128], F32, f"sc_s{i}") for i in range(2)]
        m8 = k.sb([128, 16], F32, "m8")
        nmb = k.sb([128, 128], BF16, "nmb")
        for qb in range(NQB):
            q0 = qb * 512
            first_touch = {"imp": [True] * 4, "out": [[True] * 4 for _ in range(4)]}

            def mk_cmp(h, half, qb=qb, q0=q0):
                def fac(slot):
                    zb, _ = P.v(slot * 2)
                    ob, _ = P.v(slot * 2 + 1)
                    qt = QT[h // 2]
                    r0 = (h % 2) * 64
                    cbase = q0 + half * 256
                    chunks = []
                    for c in range(NCC):
                        if 16 * (128 * c) + 31 > cbase + 255:
                            continue
                        chunks.append(dict(kT=KCT[r0:r0 + 64, c * 128:(c + 1) * 128], v=VCA[:, c, :], c0=0, c1=256,
                                           aff=[(0, 256, cbase - 2048 * c - 31, -16, 1, ALU.is_ge)]))

                    def epi():
                        for i in range(2):
                            sub = half * 2 + i
                            ti = qb * 4 + sub
                            rzv = rz[:, slot * 4 + i: slot * 4 + i + 1]
                            cfv = coef[:, slot * 4 + i: slot * 4 + i + 1]
                            if not chunks:
                                if first_touch["imp"][sub]:
                                    k.op("pool", "memset", ap=IMP[:, sub, :], constant=0.0, _writes=[IMP.a])
                                    first_touch["imp"][sub] = False
                                if first_touch["out"][sub][h]:
                                    k.op("pool", "memset", ap=OUT[:, sub, h * 64:(h + 1) * 64], constant=0.0, _writes=[OUT.a])
                                    first_touch["out"][sub][h] = False
                                continue
                            acc = ob[:, i * NV:(i + 1) * NV]
                            k.op("dve", "tensor_scalar", out=rzv, in0=acc[:, 64:65], scalar1=1e-30, scalar2=None, op0=ALU.max)
                            k.op("dve", "reciprocal", out=rzv, in_=rzv)
                            k.op("dve", "tensor_tensor", out=cfv, in0=rzv, in1=G[:, ti, h * 3:h * 3 + 1], op=ALU.mult)
                            k.op("dve", "tensor_scalar", out=OUT[:, sub, h * 64:(h + 1) * 64], in0=acc[:, 0:64], scalar1=cfv,
                                 scalar2=None, op0=ALU.mult)
                            first_touch["out"][sub][h] = False
                            if first_touch["imp"][sub]:
                                k.op("dve", "tensor_scalar", out=IMP[:, sub, :], in0=acc[:, 65:193], scalar1=rzv, scalar2=None,
                                     op0=ALU.mult)
                                first_touch["imp"][sub] = False
                            else:
                                k.op("dve", "scalar_tensor_tensor", out=IMP[:, sub, :], in0=acc[:, 65:193], scalar=rzv,
                                     in1=IMP[:, sub, :], op0=ALU.mult, op1=ALU.add)
                    qv = lambda c0, c1: qt[r0:r0 + 64, cbase + c0:cbase + c1]
                    return flash_stream(k, slot, zb, ob, e_t[slot], qv, chunks, NV, negM[1].a, epi)
                return fac
            run_streams([mk_cmp(h, half) for h in range(4) for half in range(2)], 4)
            for sub in range(4):
                qt_i = qb * 4 + sub
                sc0, sc1 = sc_s
                lo = 128 - 2 * qt_i
                k.op("dve", "tensor_tensor", out=sc0.a, in0=IMP[:, sub, :], in1=WV[:, lo:lo + 128], op=ALU.mult)
                k.op("dve", "tensor_tensor", out=sc0.a, in0=sc0.a, in1=WADD[:, lo:lo + 128], op=ALU.add)
                if qt_i >= 1:
                    k.op("dve", "tensor_scalar", out=sc0[:, 0:1], in0=sc0[:, 0:1], scalar1=1.0e4, scalar2=None, op0=ALU.add)
                k.op("dve", "max", out=m8[:, 0:8], in_=sc0.a)
                k.op("dve", "match_replace", out=sc1.a, in_to_replace=m8[:, 0:8], in_values=sc0.a, imm_value=-1e30)
                k.op("dve", "max", out=m8[:, 8:16], in_=sc1.a)
                k.op("dve", "tensor_scalar", out=sc1.a, in0=sc0.a, scalar1=m8[:, 15:16], scalar2=None, op0=ALU.is_ge)
                k.op("dve", "tensor_tensor", out=sc1.a, in0=sc1.a, in1=WV[:, lo:lo + 128], op=ALU.mult)
                k.op("dve", "tensor_scalar", out=nmb.a, in0=sc1.a, scalar1=-NEG, scalar2=NEG, op0=ALU.mult, op1=ALU.add)
                pt, _ = P.v(7)
                ptb = pt.bitcast(BF16)
                k.op("pe", "transpose", out=ptb[:, 0:128], in_=nmb.a, identity=idb.a, mark=True)
                k.op("act", "copy", out=NMT[:, sub * 128:(sub + 1) * 128], in_=ptb[:, 0:128])
            def mk_flash(h, br, qb=qb, q0=q0):
                def fac(slot):
                    zb, _ = P.v(slot * 2)
                    ob, _ = P.v(slot * 2 + 1)
                    qt = QT[h // 2]
                    r0 = (h % 2) * 64
                    chunks = []
                    if br == 1:
                        for kc in range(0, 4 * qb + 4):
                            j = kc - 4 * qb
                            c0 = 128 * j if j > 0 else 0
                            aff = [(c0, c0 + 128, 0, -1, 1, ALU.is_ge)] if j >= 0 else []
                            chunks.append(dict(kT=KT[0][r0:r0 + 64, kc * 128:(kc + 1) * 128], v=VS[:, kc, 0, :], c0=c0, c1=512, aff=aff,
                                               mask=(G64[:, kc * 128:(kc + 1) * 128], lambda a, b: NMT[:, a:b])))
                    else:
                        for kc in range(max(0, 4 * qb - 4), 4 * qb + 4):
                            j = kc - 4 * qb
                            if j >= 0:
                                c0, c1 = 128 * j, 512
                                aff = [(c0, c0 + 128, 0, -1, 1, ALU.is_ge)]
                            else:
                                jp = j + 4
                                c0, c1 = 0, 128 * (jp + 1)
                                aff = [(128 * jp, 128 * jp + 128, -1, 1, -1, ALU.is_ge)]
                            chunks.append(dict(kT=KT[1][r0:r0 + 64, kc * 128:(kc + 1) * 128], v=VS[:, kc, 1, :], c0=c0, c1=c1, aff=aff))

                    def epi():
                        for sub in range(4):
                            ti = qb * 4 + sub
                            rzv = rz[:, slot * 4 + sub: slot * 4 + sub + 1]
                            cfv = coef[:, slot * 4 + sub: slot * 4 + sub + 1]
                            acc = ob[:, sub * 65:(sub + 1) * 65]
                            k.op("dve", "tensor_scalar", out=rzv, in0=acc[:, 64:65], scalar1=1e-30, scalar2=None, op0=ALU.max)
                            k.op("dve", "reciprocal", out=rzv, in_=rzv)
                            k.op("dve", "tensor_tensor", out=cfv, in0=rzv, in1=G[:, ti, h * 3 + br:h * 3 + br + 1], op=ALU.mult)
                            k.op("dve", "scalar_tensor_tensor", out=OUT[:, sub, h * 64:(h + 1) * 64], in0=acc[:, 0:64], scalar=cfv,
                                 in1=OUT[:, sub, h * 64:(h + 1) * 64], op0=ALU.mult, op1=ALU.add)
                    qv = lambda c0, c1: qt[r0:r0 + 64, q0 + c0:q0 + c1]
                    return flash_stream(k, slot, zb, ob, e_t[slot], qv, chunks, 65, negM[0].a, epi)
                return fac
            run_streams([mk_flash(h, br) for h in range(4) for br in (1, 2)], 4)
            k.op("act", "copy", out=OUTb.a, in_=OUT.a)
            for sub in range(4):
                emit_out_T(k, P, OUTb[:, sub, :], oT_d, row_nsa, (qb * 4 + sub) * 128, idb, oTs, 7, sub)

    if do_moba:
        NB = max(NBLK, 8)
        Wm = Wall
        k.dma("pool", Wm[:, :, 0:768], V(wm_d, wm_d.h[:, :].rearrange("(c p) n -> p c n", p=128)))
        VB = V(Vall, Vall.h[:, :, :].rearrange("p t (a d) -> p t a d", a=4))
        k.op("pool", "memset", ap=Vall.a, constant=1.0, _writes=[Vall.a])
        NQm = k.sb([128, 2, NTILE], F32, "NQm")
        for g4 in range(S // 512):
            xT_g = xT[g4 % 2]
            load_xT_group(k, P, x_d, g4 * 512, 512, xin, xbf, xT_g, idb, 0)
            COS4, SIN4 = rope_group(k, pos_t, invf, g4, rtm)
            for t in range(4):
                ti = g4 * 4 + t
                pp, pp_x = P.v(2 + 2 * (t % 2), 2)
                for (a, b) in ((0, 512), (512, 768)):
                    for c in range(8):
                        k.op("pe", "matmul", out=pp[:, a:b], lhsT=xT_g[:, c, t * 128:(t + 1) * 128], rhs=Wm[:, c, a:b],
                             start=(c == 0), stop=(c == 7), _writes=pp_x, mark=(c == 7 and a == 512))
                k.op("act", "copy", out=pf[:, 0:768], in_=pp[:, 0:768], _reads=pp_x)
                rope_apply(k, pf[:, 0:512], 8, COS4[:, t, :], SIN4[:, t, :], t1, t2, Rb[:, 0:512])
                k.op("pool", "tensor_copy", out=VB[:, ti, :, 0:64], in_=pf[:, 512:768].re("p (a d) -> p a d", a=4))
                for j, (a, b) in enumerate(((0, 256), (256, 512))):
                    k.op("act", "activation", out=sqj[:, a:b], in_=Rb[:, a:b], func=AF.Square, accum_out=NQm[:, j, ti:ti + 1])
                pt, _ = P.v(6 + (t % 2))
                ptb = pt.bitcast(BF16)
                for j in range(4):
                    k.op("pe", "transpose", out=ptb[:, j * 128:(j + 1) * 128], in_=Rb[:, j * 128:(j + 1) * 128],
                         identity=idb.a, mark=(j == 3))
                for j, dst in enumerate((QT[0], QT[1], KT[0], KT[1])):
                    k.op("act" if j % 2 else "dve", "copy" if j % 2 else "tensor_copy", out=dst[:, ti * 128:(ti + 1) * 128],
                         in_=ptb[:, j * 128:(j + 1) * 128])
        bound_negM(k, P, NQm[:, 0, :], NQm[:, 1, :], idf, ones1, negM[2].a, 7, (sc_mq, sc_mk, sc_row))
        KM = k.sb([128, 32], F32, "KM")
        KMt = k.sb([128, 32], F32, "KMt")
        KMhi = [k.sb([128, 32], BF16, f"KMhi{i}") for i in range(2)]
        KMlo = [k.sb([128, 32], BF16, f"KMlo{i}") for i in range(2)]
        for i in range(2):
            k.op("pool", "memset", ap=KMhi[i].a, constant=0.0, _writes=[KMhi[i].a])
            k.op("pool", "memset", ap=KMlo[i].a, constant=0.0, _writes=[KMlo[i].a])
            k.op("dve", "tensor_reduce", out=KM[:, 0:NBLK], in_=KT[i].a.re("p (n l) -> p n l", l=256), axis=AX.X, op=ALU.add)
            k.op("dve", "tensor_scalar", out=KM[:, 0:NBLK], in0=KM[:, 0:NBLK], scalar1=1.0 / 256, scalar2=None, op0=ALU.mult)
            k.op("dve", "tensor_copy", out=KMhi[i][:, 0:NBLK], in_=KM[:, 0:NBLK])
            k.op("dve", "tensor_tensor", out=KMt[:, 0:NBLK], in0=KM[:, 0:NBLK], in1=KMhi[i][:, 0:NBLK], op=ALU.subtract)
            k.op("dve", "tensor_copy", out=KMlo[i][:, 0:NBLK], in_=KMt[:, 0:NBLK])
        G256 = CT
        k.op("pool", "memset", ap=CT.a, constant=0.0, _writes=[CT.a])
        k.dma("sp", G256[0:NBLK], consts["g256"].a)
        PM = k.sb([128, 64], F32, "PM")
        k.dma("sp", PM.a, consts["pm"].a)
        NMTm = [k.sb([128, 512], BF16, f"NMTm{h}") for h in range(4)]
        for h in range(4):
            k.op("pool", "memset", ap=NMTm[h].a, constant=0.0, _writes=[NMTm[h].a])
        wk4 = [k.sb([128, 32], F32, f"wk4_{h}") for h in range(4)]
        nm4 = [k.sb([128, 32], F32, f"nm4_{h}") for h in range(4)]
        nmb4 = [k.sb([128, 32], BF16, f"nmb4_{h}") for h in range(4)]
        for h in range(4):
            k.op("pool", "memset", ap=nmb4[h].a, constant=0.0, _writes=[nmb4[h].a])
        m84 = [k.sb([128, 8], F32, f"m84_{h}") for h in range(4)]
        thr4 = [k.sb([128, 1], F32, f"thr4_{h}") for h in range(4)]
        for qb in range(NQB):
            q0 = qb * 512
            for sub in range(4):
                qt_i = qb * 4 + sub
                cur = qt_i // 2
                pgs = [P.v(2 + h)[0] for h in range(4)]
                for h in range(4):
                    r0 = (h % 2) * 64
                    k.op("pe", "matmul", out=pgs[h][:, 0:32], lhsT=QT[h // 2][r0:r0 + 64, qt_i * 128:(qt_i + 1) * 128],
                         rhs=KMhi[h // 2][r0:r0 + 64, :], start=True, stop=False)
                    k.op("pe", "matmul", out=pgs[h][:, 0:32], lhsT=QT[h // 2][r0:r0 + 64, qt_i * 128:(qt_i + 1) * 128],
                         rhs=KMlo[h // 2][r0:r0 + 64, :], start=False, stop=True, mark=True)
                for h in range(4):
                    k.op("dve", "tensor_tensor", out=wk4[h][:, 0:NB], in0=pgs[h][:, 0:NB], in1=PM[:, 32 - cur:32 - cur + NB], op=ALU.add)
                for h in range(4):
                    k.op("dve", "max", out=m84[h].a, in_=wk4[h][:, 0:NB])
                for h in range(4):
                    k.op("dve", "tensor_scalar", out=thr4[h].a, in0=m84[h][:, 2:3], scalar1=-1e29, scalar2=None, op0=ALU.max)
                for h in range(4):
                    k.op("dve", "tensor_scalar", out=nm4[h][:, 0:NB], in0=wk4[h][:, 0:NB], scalar1=thr4[h].a, scalar2=None, op0=ALU.is_ge)
                for h in range(4):
                    k.op("dve", "tensor_scalar", out=nm4[h][:, 0:NB], in0=nm4[h][:, 0:NB], scalar1=-NEG, scalar2=NEG, op0=ALU.mult, op1=ALU.add)
                for h in range(4):
                    k.op("dve", "memset", ap=nm4[h][:, cur:cur + 1], constant=0.0, _writes=[nm4[h].a])
                for h in range(4):
                    k.op("dve", "tensor_copy", out=nmb4[h][:, 0:NB], in_=nm4[h][:, 0:NB])
                pt, _ = P.v(7)
                ptb = pt.bitcast(BF16)
                for h in range(4):
                    k.op("pe", "transpose", out=ptb[0:32, h * 128:(h + 1) * 128], in_=nmb4[h].a, identity=idb.a, mark=(h == 3))
                for h in range(4):
                    k.op("act", "copy", out=NMTm[h][0:32, sub * 128:(sub + 1) * 128], in_=ptb[0:32, h * 128:(h + 1) * 128])

            def mk_moba(h, qb=qb, q0=q0):
                def fac(slot):
                    zb, _ = P.v(slot * 2)
                    ob, _ = P.v(slot * 2 + 1)
                    qt = QT[h // 2]
                    r0 = (h % 2) * 64
                    chunks = []
                    for kc in range(0, 4 * qb + 4):
                        j = kc - 4 * qb
                        c0 = 128 * j if j > 0 else 0
                        aff = [(c0, c0 + 128, 0, -1, 1, ALU.is_ge)] if j >= 0 else []
                        chunks.append(dict(kT=KT[h // 2][r0:r0 + 64, kc * 128:(kc + 1) * 128], v=VB[:, kc, h, :], c0=c0, c1=512, aff=aff,
                                           mask=(G256[:, kc * 128:(kc + 1) * 128], lambda a, b, h=h: NMTm[h][:, a:b])))

                    def epi():
                        for sub in range(4):
                            rzv = rz[:, slot * 4 + sub: slot * 4 + sub + 1]
                            acc = ob[:, sub * 65:(sub + 1) * 65]
                            k.op("dve", "tensor_scalar", out=rzv, in0=acc[:, 64:65], scalar1=1e-30, scalar2=None, op0=ALU.max)
                            k.op("dve", "reciprocal", out=rzv, in_=rzv)
                            k.op("dve", "tensor_scalar", out=OUT[:, sub, h * 64:(h + 1) * 64], in0=acc[:, 0:64], scalar1=rzv,
                                 scalar2=None, op0=ALU.mult)
                    qv = lambda c0, c1: qt[r0:r0 + 64, q0 + c0:q0 + c1]
                    return flash_stream(k, slot, zb, ob, e_t[slot], qv, chunks, 65, negM[2].a, epi)
                return fac
            run_streams([mk_moba(h) for h in range(4)], 4)
            k.op("act", "copy", out=OUTb.a, in_=OUT.a)
            for sub in range(4):
                emit_out_T(k, P, OUTb[:, sub, :], oT_d, row_moba, (qb * 4 + sub) * 128, idb, oTs, 7, sub)


import ml_dtypes
from concourse.bass_utils import run_bass_kernel_spmd

_PROGS = {}
EVEN_WIDTHS = (512, 128, 128, 128, 128, 128, 128, 24, 512, 512, 512)
_SP = [0] + [int(v) for v in np.cumsum(EVEN_WIDTHS)[:-1]]


def _consts(S):
    half = 32
    invf = (10000.0 ** (-np.arange(half, dtype=np.float32) / half)).astype(np.float32)
    invf_t = np.tile((invf / np.float32(2 * np.pi)).astype(np.float32)[None, :], (128, 1))
    p = np.arange(128)[:, None]
    m = np.arange(256)[None, :]
    hp = (p >= 64).astype(np.int64)
    d = m - 128
    wv = (d <= hp).astype(np.float32)
    wadd = (1e4 * ((d == hp) | (d == hp - 1)) - 1.0 * (d > hp)).astype(np.float32)
    g64 = (np.arange(S)[None, :] // 64 == np.arange(S // 64)[:, None]).astype(ml_dtypes.bfloat16)
    g256 = (np.arange(S)[None, :] // 256 == np.arange(S // 256)[:, None]).astype(ml_dtypes.bfloat16)
    pm = np.where(np.arange(64)[None, :] < 32, 0.0, -1e30).astype(np.float32) * np.ones((128, 1), np.float32)
    return dict(ident=np.eye(128, dtype=np.float32), invf=invf_t, wv=wv, wadd=wadd, g64=g64, g256=g256, pm=pm)


def build_fused(S, SG):
    nc = bass.Bass("TRN2", target_bir_lowering=False)
    k = K(nc)
    P = Psum(k)
    EI = dict(kind="ExternalInput")
    x = k.dram("x", [S, 1024], F32, **EI)
    pos = k.dram("pos", [128, S // 128], I32, **EI)
    wn = k.dram("wn", [2, 1024, 780], F32, **EI)
    wm = k.dram("wm", [2, 1024, 768], F32, **EI)
    wsb = k.dram("wsb", [2, 1024, 1536], F32, **EI)
    cw = {n: k.dram(n, sh, F32, **EI) for n, sh in
          (("w1k", [2048, 128]), ("w1v", [2048, 128]), ("w2k", [128, 64]), ("w2v", [128, 64]), ("posT", [128, 32]))}
    consts = {"ident": k.dram("ident", [128, 128], F32, **EI), "invf": k.dram("invf", [128, 32], F32, **EI),
              "wv": k.dram("wv", [128, 256], F32, **EI), "wadd": k.dram("wadd", [128, 256], F32, **EI),
              "g64": k.dram("g64", [S // 64, S], BF16, **EI), "g256": k.dram("g256", [S // 256, S], BF16, **EI),
              "pm": k.dram("pm", [128, 64], F32, **EI)}
    w_out = k.dram("w_out", [2, 1024, 1024], F32, **EI)
    lnp = k.dram("lnp", [2, 4, 1024], F32, **EI)
    wr = k.dram("wr", [2, 1024, 20], F32, **EI)
    br = k.dram("br", [2, 20], F32, **EI)
    wg = k.dram("w_gate", [2, 16, 1024, 256], F32, **EI)
    wu = k.dram("w_up", [2, 16, 1024, 256], F32, **EI)
    wd = k.dram("w_down", [2, 16, 256, 1024], F32, **EI)
    out = k.dram("out", [S, 1024], F32, kind="ExternalOutput")
    oT_s = k.dram("oT_s", [1024, S], BF16, kind="Internal")
    x1_s = k.dram("x1_s", [S, 1024], F32, kind="Internal")
    sub = lambda t, i: Tile(k, t.h[i], t.name + f"_{i}")
    for g in range(2):
        k.begin_phase(f"l0a{g}")
        emit_phase_l0(k, P, S, x, pos, sub(wn, g), sub(wm, g), cw, consts, oT_s, row_nsa=256 * g, row_moba=512 + 256 * g)
        k.end_phase()
    k.begin_phase("l0b")
    emit_phase_b(k, P, S, SG, oT_s, x, x1_s, sub(w_out, 0), sub(lnp, 0), sub(wr, 0), sub(br, 0), sub(wg, 0), sub(wu, 0),
                 sub(wd, 0), consts["ident"])
    k.end_phase()
    for g in range(2):
        k.begin_phase(f"l1a{g}")
        emit_phase_sb(k, P, S, x1_s, sub(wsb, g), oT_s, consts["ident"], row0=512 * g)
        k.end_phase()
    k.begin_phase("l1b")
    emit_phase_b(k, P, S, SG, oT_s, x1_s, out, sub(w_out, 1), sub(lnp, 1), sub(wr, 1), sub(br, 1), sub(wg, 1), sub(wu, 1),
                 sub(wd, 1), consts["ident"])
    k.end_phase()
    k.finish([out])
    return nc


def host_inputs(inp, b, S):
    w_in = inp["ab_w_in"][0]
    c_qa, c_kc, c_vc, c_ksl, c_vsl, c_kw, c_vw, c_ga, c_qm, c_km, c_vm = _SP
    wns, wms, wsbs = [], [], []
    w_sb = inp["sb_w_in"][0]
    for g in range(2):
        kvc = lambda base: w_in[:, base + g * 64: base + (g + 1) * 64]
        wns.append(np.concatenate([w_in[:, c_qa + 256 * g: c_qa + 256 * (g + 1)], kvc(c_ksl), kvc(c_ksl), kvc(c_kw), kvc(c_kw),
                                   kvc(c_kc), kvc(c_vc), kvc(c_vsl), kvc(c_vw), w_in[:, c_ga + 12 * g: c_ga + 12 * (g + 1)]], axis=1))
        wms.append(np.concatenate([w_in[:, c_qm + 256 * g: c_qm + 256 * (g + 1)], w_in[:, c_km + 256 * g: c_km + 256 * (g + 1)],
                                   w_in[:, c_vm + 256 * g: c_vm + 256 * (g + 1)]], axis=1))
        wsbs.append(np.concatenate([w_sb[:, j * 1024 + 512 * g: j * 1024 + 512 * (g + 1)] for j in range(3)], axis=1))
    m = dict(x=np.ascontiguousarray(inp["x"][b]).astype(np.float32),
             pos=np.ascontiguousarray(inp["positions"][b].reshape(S // 128, 128).T.astype(np.int32)),
             wn=np.ascontiguousarray(np.stack(wns)), wm=np.ascontiguousarray(np.stack(wms)), wsb=np.ascontiguousarray(np.stack(wsbs)),
             w1k=inp["nsa_cmp_w1_k"][0], w1v=inp["nsa_cmp_w1_v"][0], w2k=inp["nsa_cmp_w2_k"][0], w2v=inp["nsa_cmp_w2_v"][0],
             posT=np.ascontiguousarray(np.concatenate([inp["nsa_cmp_pos_k"][0].T, inp["nsa_cmp_pos_v"][0].T], axis=0)),
             w_out=np.ascontiguousarray(np.stack([inp["ab_w_out"][0], inp["sb_w_out"][0]])),
             lnp=np.ascontiguousarray(np.stack([np.stack([inp["ln_mix_g"][l], inp["ln_mix_b"][l], inp["ln_ffn_g"][l], inp["ln_ffn_b"][l]]) for l in range(2)])),
             wr=np.ascontiguousarray(np.stack([np.concatenate([inp["moe_w_grp"][l], inp["moe_w_rt"][l].transpose(1, 0, 2).reshape(1024, 16)], axis=1) for l in range(2)])),
             br=np.ascontiguousarray(np.stack([np.concatenate([inp["moe_b_grp"][l], inp["moe_b_rt"][l].reshape(16)]) for l in range(2)])),
             w_gate=inp["moe_w_gate"], w_up=inp["moe_w_up"], w_down=inp["moe_w_down"])
    m.update(_consts(S))
    return m


def kernel(**inputs):
    inp = {k_: np.asarray(v) for k_, v in inputs.items()}
    B, S, _ = inp["x"].shape
    SG = min(2048, S)
    key = ("fused", S, SG)
    if key not in _PROGS:
        _PROGS[key] = build_fused(S, SG)
    nc = _PROGS[key]
    in_maps = [host_inputs(inp, b, S) for b in range(B)]
    res = run_bass_kernel_spmd(nc, in_maps, core_ids=list(range(B)))
    return np.stack([res.results[b]["out"] for b in range(B)]).astype(np.float32)
```

```python
import math
import bisect
from contextlib import ExitStack
import numpy as np
import concourse.bass as bass
import concourse.mybir as mybir

F32 = mybir.dt.float32
BF16 = mybir.dt.bfloat16
I32 = mybir.dt.int32
AF = mybir.ActivationFunctionType
ALU = mybir.AluOpType
AX = mybir.AxisListType


class V:
    __slots__ = ("tile", "ap")

    def __init__(self, tile, ap):
        self.tile = tile
        self.ap = ap

    def __getitem__(self, idx):
        return V(self.tile, self.ap[idx])

    def bc(self, shape):
        return V(self.tile, self.ap.broadcast_to(shape))

    def unsq(self, d):
        return V(self.tile, self.ap.unsqueeze(d))

    def re(self, s, **kw):
        return V(self.tile, self.ap.rearrange(s, **kw))

    def bitcast(self, dt):
        return V(self.tile, self.ap.bitcast(dt))


class Tile:
    def __init__(self, k, h, name):
        self.k = k
        self.h = h
        self.name = name
        self.w = None
        self.r = {}
        self.excl = False

    def __getitem__(self, idx):
        return V(self, self.h[idx])

    @property
    def a(self):
        return V(self, self.h[:])


class EngState:
    def __init__(self, k, name, eng, is_compute=True):
        self.k = k
        self.name = name
        self.eng = eng
        self.sem = k.nc.alloc_semaphore("s_" + name)
        self.n = 0
        self.last = None
        self.marks_idx = []
        self.marks_val = []
        self.val = 0
        self.waited = {}
        self.nwaits = 0


class DmaSem:
    def __init__(self, k, name):
        self.sem = k.nc.alloc_semaphore("d_" + name)
        self.cnt = 0
        self.name = name


class K:
    def __init__(self, nc, same_engine_sync=True):
        self.nc = nc
        self.E = {
            "pe": EngState(self, "pe", nc.tensor),
            "act": EngState(self, "act", nc.scalar),
            "dve": EngState(self, "dve", nc.vector),
            "pool": EngState(self, "pool", nc.gpsimd),
            "sp": EngState(self, "sp", nc.sync),
        }
        self.same_engine_sync = same_engine_sync
        self.dsems = {}
        self.rings = {}
        self.ring_pos = {}
        self.RING = 16
        self.n_tiles = 0
        self.stack = ExitStack()
        self.pname = ""

    def sb(self, shape, dt, name=None):
        self.n_tiles += 1
        name = "sb_" + self.pname + (name or f"t{self.n_tiles}")
        h = self.stack.enter_context(self.nc.sbuf_tensor(name, list(shape), dt))
        return Tile(self, h, name)

    def ps(self, shape, dt=F32, name=None):
        self.n_tiles += 1
        name = name or f"p{self.n_tiles}"
        h = self.nc.alloc_psum_tensor(name, list(shape), dt)
        t = Tile(self, h, name)
        t.excl = True
        return t

    def dram(self, name, shape, dt, kind="Internal"):
        h = self.nc.dram_tensor(name, list(shape), dt, kind=kind)
        return Tile(self, h, name)

    def sub(self, v, name="sub"):
        return Tile(self, v.ap, name)

    def dsem(self, name):
        if name not in self.dsems:
            self.dsems[name] = DmaSem(self, name)
        return self.dsems[name]

    def _token_value(self, tok):
        kind, obj, idx = tok
        if kind == "dma":
            return obj.sem, idx
        es = obj
        if es.marks_idx and es.marks_idx[-1] >= idx:
            j = bisect.bisect_left(es.marks_idx, idx)
            return es.sem, es.marks_val[j]
        es.val += 1
        es.last.then_inc(es.sem, 1)
        es.marks_idx.append(es.n - 1)
        es.marks_val.append(es.val)
        return es.sem, es.val

    def _wait(self, es, tok):
        if tok is None:
            return
        kind, obj, idx = tok
        if kind == "eng" and obj is es:
            if not self.same_engine_sync or es.name == "pe":
                return
        sem, val = self._token_value(tok)
        key = id(sem) if kind == "dma" else obj.name
        if es.waited.get(key, 0) >= val:
            return
        es.waited[key] = val
        es.eng.wait_ge(sem, val)
        es.nwaits += 1

    def _deps(self, es, reads, writes):
        for v in reads:
            if v is None:
                continue
            self._wait(es, v.tile.w)
            if v.tile.excl:
                for tok in v.tile.r.values():
                    self._wait(es, tok)
        for v in writes:
            if v is None:
                continue
            t = v.tile
            self._wait(es, t.w)
            for tok in t.r.values():
                self._wait(es, tok)

    def _commit(self, tok, reads, writes, rkey):
        for v in reads:
            if v is None:
                continue
            v.tile.r[rkey] = tok
        for v in writes:
            if v is None:
                continue
            v.tile.w = tok
            v.tile.r = {}

    OUT_KEYS = ("out", "accum_out", "out_ap")

    def op(self, en, fn, **kw):
        es = self.E[en]
        reads, writes = [], []
        extra_r = kw.pop("_reads", [])
        extra_w = kw.pop("_writes", [])
        mark = kw.pop("mark", None)
        if mark is None:
            mark = en != "pe"
        args = {}
        for key, val in kw.items():
            if isinstance(val, V):
                (writes if key in self.OUT_KEYS else reads).append(val)
                args[key] = val.ap
            else:
                args[key] = val
        reads += extra_r
        writes += extra_w
        self._deps(es, reads, writes)
        inst = getattr(es.eng, fn)(**args)
        es.last = inst
        es.n += 1
        tok = ("eng", es, es.n - 1)
        if mark:
            es.val += 1
            inst.then_inc(es.sem, 1)
            es.marks_idx.append(es.n - 1)
            es.marks_val.append(es.val)
        self._commit(tok, reads, writes, en)
        return inst

    def dma(self, qn, out, in_, ds=None, **kw):
        es = self.E[qn]
        ring = self.rings.setdefault(qn, [])
        pos = self.ring_pos.get(qn, 0)
        if len(ring) < self.RING:
            ring.append(self.dsem(f"{qn}_r{len(ring)}"))
        ds = ring[pos % self.RING]
        self.ring_pos[qn] = pos + 1
        if ds.cnt:
            self._wait(es, ("dma", ds, ds.cnt))
        self._deps(es, [in_], [out])
        inst = es.eng.dma_start(out=out.ap, in_=in_.ap, **kw)
        inst.then_inc(ds.sem, 16)
        ds.cnt += 16
        tok = ("dma", ds, ds.cnt)
        self._commit(tok, [in_], [out], "dma_" + ds.name)
        return inst

    def begin_phase(self, name):
        self.stack = ExitStack()
        self.pname = name + "_"

    def barrier(self):
        targets = []
        for n in ("pe", "act", "dve", "pool"):
            es = self.E[n]
            if es.n == 0:
                continue
            if not es.marks_idx or es.marks_idx[-1] < es.n - 1:
                es.val += 1
                es.last.then_inc(es.sem, 1)
                es.marks_idx.append(es.n - 1)
                es.marks_val.append(es.val)
            targets.append((n, es.sem, es.val))
        for en, es in self.E.items():
            for (n, sem, val) in targets:
                if es.waited.get(n, 0) < val:
                    es.eng.wait_ge(sem, val)
                    es.waited[n] = val
            for ds in self.dsems.values():
                if ds.cnt and es.waited.get(id(ds.sem), 0) < ds.cnt:
                    es.eng.wait_ge(ds.sem, ds.cnt)
                    es.waited[id(ds.sem)] = ds.cnt

    def end_phase(self):
        self.barrier()
        self.stack.close()
        self.stack = ExitStack()

    def finish(self, out_tiles):
        es = self.E["sp"]
        for t in out_tiles:
            self._wait(es, t.w)
        for ds in self.dsems.values():
            if ds.cnt:
                key = id(ds.sem)
                if es.waited.get(key, 0) < ds.cnt:
                    es.eng.wait_ge(ds.sem, ds.cnt)
                    es.waited[key] = ds.cnt

    def stats(self):
        return {n: (e.n, e.nwaits, e.val) for n, e in self.E.items()}


D = 1024
ALPHA = float(4 ** 0.25)
EPS = 1e-5
NE = 16
FH = 256


class Psum:
    def __init__(self, k):
        self.k = k
        self.t = k.nc.alloc_psum_tensor("psum_all", [128, 8, 512], F32)
        self.b = [Tile(k, self.t[:, i, :], f"bank{i}") for i in range(8)]
        for t in self.b:
            t.excl = True

    def v(self, i, n=1):
        if n == 1:
            return V(self.b[i], self.t[:, i, :]), []
        ap = self.t[:, i:i + n, :].rearrange("p a b -> p (a b)")
        return V(self.b[i], ap), [self.b[j].a for j in range(i + 1, i + n)]


def layer_norm_tile(k, src, dst, g_t, b_t, eps_t, tmp, st, mv, rstd, nmr):
    for i in range(2):
        k.op("dve", "bn_stats", out=st[:, i, :], in_=src[:, i * 512:(i + 1) * 512])
    k.op("dve", "bn_aggr", out=mv.a, in_=st.a.re("p a b -> p (a b)"))
    k.op("act", "activation", out=rstd.a, in_=mv[:, 1:2], func=AF.Ln, bias=eps_t.a, scale=1.0)
    k.op("act", "activation", out=rstd.a, in_=rstd.a, func=AF.Exp, scale=-0.5)
    k.op("dve", "scalar_tensor_tensor", out=nmr.a, in0=mv[:, 0:1], scalar=-1.0, in1=rstd.a,
         op0=ALU.mult, op1=ALU.mult)
    k.op("act", "activation", out=dst, in_=src, func=AF.Identity, bias=nmr.a, scale=rstd.a)
    k.op("pool", "tensor_tensor", out=dst, in0=dst, in1=g_t.a, op=ALU.mult)
    k.op("pool", "tensor_tensor", out=dst, in0=dst, in1=b_t.a, op=ALU.add)


def emit_phase_b(k, P, NT, SG, oT, xres, xo, w_out, lnp, wr, br, w_gate, w_up, w_down, ident_d):
    nsg = NT // SG
    ntile = SG // 128
    ngrp = SG // 512
    idf = k.sb([128, 128], F32, "idf")
    k.dma("sp", idf.a, ident_d.a, "ld_c")
    woT = k.sb([128, 8, 1024], BF16, "woT")
    k.dma("pool", woT.a, V(w_out, w_out.h[:, :].rearrange("(c p) n -> p c n", p=128)), "ld_c")
    lnt = []
    for i in range(4):
        t = k.sb([128, 1024], F32, f"ln{i}")
        k.dma("sp", t.a, V(lnp, lnp.h[i].partition_broadcast(128)), "ld_c")
        lnt.append(t)
    WR = k.sb([128, 8, 20], F32, "WR")
    if True:
        k.dma("sp", WR.a, V(wr, wr.h[:, :].rearrange("(c p) n -> p c n", p=128)), "ld_c")
    BR = k.sb([128, 20], F32, "BR")
    if True:
      k.dma("sp", BR.a, V(br, br.h[:].partition_broadcast(128)), "ld_c")
    eps_t = k.sb([128, 1], F32, "eps")
    k.op("pool", "memset", ap=eps_t.a, constant=EPS, _writes=[eps_t.a])
    SELT = k.sb([16, 16, 128], F32, "SELT")
    k.op("pool", "memset", ap=SELT.a, constant=1.0, _writes=[SELT.a])
    ones16 = SELT
    if True:
      k.op("pool", "affine_select", out=SELT.a, in_=ones16.a, pattern=[[-1, 16], [0, 128]],
         compare_op=ALU.is_equal, fill=0.0, base=0, channel_multiplier=1)

    acc = [k.sb([128, 1024], F32, f"acc{i}") for i in range(ntile)]
    x1T = k.sb([128, 8, SG], BF16, "x1T")
    CT = k.sb([16, SG], F32, "CT")
    oTt = [k.sb([128, 8, 128], BF16, f"oTt{i}") for i in range(2)]
    xrt = [k.sb([128, 1024], F32, f"xrt{i}") for i in range(2)]
    yt = k.sb([128, 1024], F32, "yt")
    tmp = yt
    x1t = k.sb([128, 1024], F32, "x1t")
    x1Tf = k.sb([128, 8, 128], F32, "x1Tf")
    st = k.sb([128, 2, 6], F32, "st")
    mv = k.sb([128, 2], F32, "mv")
    rstd = k.sb([128, 1], F32, "rstd")
    nmr = k.sb([128, 1], F32, "nmr")
    L = k.sb([128, 20], F32, "L")
    sm = k.sb([128, 16], F32, "sm")
    r4 = k.sb([128, 8], F32, "r4")
    r16 = [k.sb([128, 16], F32, f"r16_{i}") for i in range(4)]
    comb = k.sb([128, 16], F32, "comb")
    combAll = k.sb([128, ntile, 16], F32, "combAll")
    wgb = [k.sb([128, 8, FH], BF16, f"wgb{i}") for i in range(2)]
    wub = [k.sb([128, 8, FH], BF16, f"wub{i}") for i in range(2)]
    wdb = [k.sb([128, 2, 1024], BF16, f"wdb{i}") for i in range(2)]
    hT = [k.sb([128, 2, 512], BF16, f"hT{i}") for i in range(2)]
    sgt = [k.sb([128, 512], F32, f"sgt{i}") for i in range(2)]
    tt_ = sgt
    ot = xrt

    oT_v = oT.h[:, :].rearrange("(c p) t -> p c t", p=128)
    dcount = [0]
    ld_i = 0
    for sg in range(nsg):
        for ti in range(ntile if 9.0 >= 1 else 0):
            tok0 = sg * SG + ti * 128
            o_t = oTt[ld_i % 2]
            x_t = xrt[ld_i % 2]
            ld_i += 1
            k.dma("sp", o_t.a, V(oT, oT_v[:, :, tok0:tok0 + 128]), f"ld_o{ld_i % 2}")
            k.dma("sp", x_t.a, V(xres, xres.h[tok0:tok0 + 128, :]), f"ld_x{ld_i % 2}")
            pm, pm_x = P.v(0, 2)
            for half in range(2):
                for c in range(8):
                    k.op("pe", "matmul", out=pm[:, half * 512:(half + 1) * 512], lhsT=o_t[:, c, :],
                         rhs=woT[:, c, half * 512:(half + 1) * 512], start=(c == 0), stop=(c == 7),
                         _writes=pm_x, mark=(c == 7 and half == 1))
            k.op("dve", "scalar_tensor_tensor", out=yt.a, in0=x_t.a, scalar=ALPHA, in1=pm, op0=ALU.mult,
                 op1=ALU.add, _reads=pm_x)
            if 9.0 < 1.2:
                continue
            layer_norm_tile(k, yt.a, x1t.a, lnt[0], lnt[1], eps_t, tmp.a, st, mv, rstd, nmr)
            k.op("act", "mul", out=acc[ti].a, in_=x1t.a, mul=ALPHA)
            if 9.0 < 1.4:
                continue
            pT, pT_x = P.v(2, 2)
            for c in range(8):
                k.op("pe", "transpose", out=pT[:, c * 128:(c + 1) * 128], in_=x1t[:, c * 128:(c + 1) * 128],
                     identity=idf.a, _writes=pT_x, mark=(c == 7))
            if True:
                k.op("act", "copy", out=x1Tf.a.re("p c t -> p (c t)"), in_=pT, _reads=pT_x)
            if True:
                k.op("dve", "tensor_copy", out=x1T[:, :, ti * 128:(ti + 1) * 128],
                     in_=pT.re("p (c t) -> p c t", c=8), _reads=pT_x)
            if 9.0 < 2:
                continue
            pr, _ = P.v(7)
            for c in range(8):
                k.op("pe", "matmul", out=pr[:, 0:20], lhsT=x1Tf[:, c, :], rhs=WR[:, c, :], start=(c == 0),
                     stop=(c == 7), mark=(c == 7))
            k.op("dve", "tensor_tensor", out=L.a, in0=pr[:, 0:20], in1=BR.a, op=ALU.add)
            gmax, ngmax, sumg, wg_, m1, nm1, m2, ssum, coef = [sm[:, i:i + 1] for i in range(9)]
            k.op("dve", "reduce_max", out=gmax, in_=L[:, 0:4], axis=AX.X)
            k.op("dve", "tensor_scalar", out=ngmax, in0=gmax, scalar1=-1.0, scalar2=None, op0=ALU.mult)
            k.op("act", "activation", out=r4[:, 0:4], in_=L[:, 0:4], func=AF.Exp, bias=ngmax, scale=1.0,
                 accum_out=sumg)
            k.op("dve", "reciprocal", out=wg_, in_=sumg)
            k.op("dve", "tensor_scalar", out=r4[:, 4:8], in0=L[:, 0:4], scalar1=gmax, scalar2=None,
                 op0=ALU.is_equal)
            k.op("dve", "tensor_scalar", out=r4[:, 4:8], in0=r4[:, 4:8], scalar1=-1.0, scalar2=1e30,
                 op0=ALU.add, op1=ALU.mult)
            lm = r16[0]
            k.op("dve", "tensor_tensor", out=lm.a.re("p (g e) -> p g e", g=4),
                 in0=L[:, 4:20].re("p (g e) -> p g e", g=4), in1=r4[:, 4:8].unsq(2).bc([128, 4, 4]),
                 op=ALU.add)
            k.op("dve", "reduce_max", out=m1, in_=lm.a, axis=AX.X)
            k.op("dve", "tensor_scalar", out=r16[1].a, in0=lm.a, scalar1=m1, scalar2=None, op0=ALU.is_equal)
            k.op("dve", "scalar_tensor_tensor", out=r16[1].a, in0=r16[1].a, scalar=-1e30, in1=lm.a,
                 op0=ALU.mult, op1=ALU.add)
            k.op("dve", "reduce_max", out=m2, in_=r16[1].a, axis=AX.X)
            k.op("dve", "tensor_scalar", out=r16[2].a, in0=lm.a, scalar1=m2, scalar2=None, op0=ALU.is_ge)
            k.op("dve", "tensor_scalar", out=nm1, in0=m1, scalar1=-1.0, scalar2=None, op0=ALU.mult)
            k.op("act", "activation", out=r16[3].a, in_=lm.a, func=AF.Exp, bias=nm1, scale=1.0)
            k.op("dve", "tensor_tensor", out=r16[3].a, in0=r16[3].a, in1=r16[2].a, op=ALU.mult)
            k.op("dve", "reduce_sum", out=ssum, in_=r16[3].a, axis=AX.X)
            k.op("dve", "reciprocal", out=ssum, in_=ssum)
            k.op("dve", "tensor_tensor", out=coef, in0=ssum, in1=wg_, op=ALU.mult)
            k.op("dve", "tensor_scalar", out=comb.a, in0=r16[3].a, scalar1=coef, scalar2=None, op0=ALU.mult)
            k.op("dve", "tensor_copy", out=combAll[:, ti, :], in_=comb.a)
        items = [(e, grp) for e in range(NE) for grp in range(ngrp)]

        def load_w(e):
            s_ = e % 2
            k.dma("pool", wgb[s_].a, V(w_gate, w_gate.h[e].rearrange("(c p) f -> p c f", p=128)))
            k.dma("pool", wub[s_].a, V(w_up, w_up.h[e].rearrange("(c p) f -> p c f", p=128)))
            k.dma("pool", wdb[s_].a, V(w_down, w_down.h[e].rearrange("(c p) n -> p c n", p=128)))

        def gu_part(it, f, which):
            e, grp = it
            s_ = e % 2
            tc0 = grp * 512
            pb_, _ = P.v(2 + 2 * f + which)
            wt = wgb[s_] if which == 0 else wub[s_]
            for c in range(8):
                k.op("pe", "matmul", out=pb_, lhsT=wt[:, c, f * 128:(f + 1) * 128],
                     rhs=x1T[:, c, tc0:tc0 + 512], start=(c == 0), stop=(c == 7), mark=(c == 7))

        def elem_part(idx, f):
            h_t = hT[idx % 2]
            pcb, _ = P.v(6)
            pg, _ = P.v(2 + 2 * f)
            pu, _ = P.v(3 + 2 * f)
            k.op("act", "activation", out=sgt[f].a, in_=pg, func=AF.Silu)
            k.op("dve", "tensor_tensor", out=h_t[:, f, :], in0=sgt[f].a, in1=pu, op=ALU.mult)

        def down_unit(idx, it, u):
            e, grp = it
            s_ = e % 2
            h_t = hT[idx % 2]
            t4, half = u // 2, u % 2
            ti = grp * 4 + t4
            pd, _ = P.v((0, 1, 7)[dcount[0] % 3])
            dcount[0] += 1
            for f in range(2):
                k.op("pe", "matmul", out=pd, lhsT=h_t[:, f, t4 * 128:(t4 + 1) * 128],
                     rhs=wdb[s_][:, f, half * 512:(half + 1) * 512], start=(f == 0), stop=(f == 1), mark=(f == 1))
            k.op("dve", "scalar_tensor_tensor", out=acc[ti][:, half * 512:(half + 1) * 512], in0=pd,
                 scalar=combAll[:, ti, e:e + 1], in1=acc[ti][:, half * 512:(half + 1) * 512], op0=ALU.mult, op1=ALU.add)

        load_w(0)
        for f in range(2):
            gu_part(items[0], f, 0)
            gu_part(items[0], f, 1)
            elem_part(0, f)
        for idx, it in enumerate(items):
            nxt = items[idx + 1] if idx + 1 < len(items) else None
            if nxt is not None and nxt[0] != it[0]:
                load_w(nxt[0])
            u = 0
            for f in range(2):
                for which in range(2):
                    if nxt is not None:
                        gu_part(nxt, f, which)
                    if which == 1 and nxt is not None:
                        elem_part(idx + 1, f)
                    down_unit(idx, it, u)
                    down_unit(idx, it, u + 1)
                    u += 2
        for ti in range(ntile):
            tok0 = sg * SG + ti * 128
            o_ = ot[ti % 2]
            layer_norm_tile(k, acc[ti].a, o_.a, lnt[2], lnt[3], eps_t, tmp.a, st, mv, rstd, nmr)
            dst_ap = xo.h[tok0:tok0 + 128, :]
            k.dma("sp", V(Tile(k, dst_ap, "xo_part"), dst_ap), o_.a)


def load_xT_group(k, P, x_d, tok0, ntok, xin, xbf, xT, idb, bank):
    for t in range(ntok // 128):
        xb = xbf[t % 2]
        k.dma("pool", xb.a, V(x_d, x_d.h[tok0 + t * 128: tok0 + (t + 1) * 128, :]))
        pt, _ = P.v(bank + (t % 2))
        ptb = pt.bitcast(BF16)
        for c in range(8):
            k.op("pe", "transpose", out=ptb[:, c * 128:(c + 1) * 128], in_=xb[:, c * 128:(c + 1) * 128],
                 identity=idb.a, mark=(c == 7))
        k.op("dve", "tensor_copy", out=xT[:, :, t * 128:(t + 1) * 128], in_=ptb.re("p (c t) -> p c t", c=8))


def run_streams_sb(gens, nslot):
    pending = list(gens)
    active = [None] * nslot
    while True:
        progressed = False
        for s_ in range(nslot):
            if active[s_] is None and pending:
                active[s_] = pending.pop(0)(s_)
            if active[s_] is not None:
                try:
                    next(active[s_])
                except StopIteration:
                    active[s_] = None
                progressed = True
        if not progressed and not pending:
            break


def sb_stream(k, P, slot, h, qb, qt, kt, r0, Vt, Oa, NTRI, LSTR, e_s, p_s, a_s, w_s):
    zb = P.v(0 + slot)[0]
    lab = P.v(2 + slot)[0]
    ob = P.v(4 + slot)[0]
    chunks = [(4 * qb + j, 128 * j) for j in (3, 2, 1, 0)] + [(kc, 0) for kc in range(4 * qb - 1, -1, -1)]

    def zmm(kc, c0):
        k.op("pe", "matmul", out=zb[:, c0:512], lhsT=kt[r0:r0 + 64, kc * 128:(kc + 1) * 128],
             rhs=qt[r0:r0 + 64, qb * 512 + c0:(qb + 1) * 512], start=True, stop=True, mark=True)
    zmm(*chunks[0])
    yield
    prev = None
    first = True
    for ci, (kc, c0) in enumerate(chunks):
        e = e_s[ci % 2]
        p_cur = p_s[ci % 2]
        a = a_s[ci % 2]
        w = w_s[ci % 2]
        k.op("act", "activation", out=e[:, c0:512], in_=zb[:, c0:512], func=AF.Exp, scale=0.125)
        if kc >= 4 * qb:
            k.op("pool", "affine_select", out=e[:, c0:c0 + 128], in_=e[:, c0:c0 + 128],
                 pattern=[[1, 128]], compare_op=ALU.is_gt, fill=0.0, base=0, channel_multiplier=-1)
        k.op("act", "activation", out=p_cur[:, c0:512], in_=e[:, c0:512], func=AF.Ln, bias=1.0, scale=1.0)
        yield
        if prev is not None:
            pp, pc0 = prev
            k.op("pe", "matmul", out=lab[:, pc0:512], lhsT=LSTR.a, rhs=pp[:, pc0:512], start=False,
                 stop=False, skip_group_check=True)
        k.op("pe", "matmul", out=lab[:, c0:512], lhsT=NTRI.a, rhs=p_cur[:, c0:512],
             start=first, stop=True, skip_group_check=True, mark=True)
        if ci + 1 < len(chunks):
            zmm(*chunks[ci + 1])
        yield
        k.op("act", "activation", out=a[:, c0:512], in_=lab[:, c0:512], func=AF.Exp)
        k.op("dve", "tensor_tensor", out=w[:, c0:512], in0=a[:, c0:512], in1=e[:, c0:512], op=ALU.mult)
        yield
        for i in range(c0 // 128, 4):
            k.op("pe", "matmul", out=ob[:, i * 64:(i + 1) * 64], lhsT=w[:, i * 128:(i + 1) * 128],
                 rhs=Vt[:, kc, h * 64:(h + 1) * 64], start=(first and i == c0 // 128),
                 stop=True, skip_group_check=True, mark=(i == 3))
        first = False
        prev = (p_cur, c0)
        yield
    k.op("act", "copy", out=Oa[:, qb * 4:(qb + 1) * 4, h * 64:(h + 1) * 64],
         in_=ob[:, 0:256].re("p (i d) -> p i d", i=4))
    yield


def sb_wide(k, P, hp, qb, qt, kt, Vt, Oa, NTRI, LSTR, e_b, p_b, a_b, w_b):
    chunks = [(4 * qb + j, 128 * j) for j in (3, 2, 1, 0)] + [(kc, 0) for kc in range(4 * qb - 1, -1, -1)]
    n = len(chunks)
    T = P.t

    def pair(b0, c0):
        ap = T[:, b0:b0 + 2, c0:512]
        return V(P.b[b0], ap), [P.b[b0 + 1].a]

    def zmm(i):
        kc, c0 = chunks[i]
        b0 = 2 * (i % 2)
        for hh in range(2):
            r0 = hh * 64
            zb = V(P.b[b0 + hh], T[:, b0 + hh, c0:512])
            k.op("pe", "matmul", out=zb, lhsT=kt[r0:r0 + 64, kc * 128:(kc + 1) * 128],
                 rhs=qt[r0:r0 + 64, qb * 512 + c0:(qb + 1) * 512], start=True, stop=True, mark=(hh == 1))

    def ep(i):
        kc, c0 = chunks[i]
        zv, zx = pair(2 * (i % 2), c0)
        e = e_b[i % 3]
        k.op("act", "activation", out=e[:, :, c0:512], in_=zv, func=AF.Exp, scale=0.125, _reads=zx)
        if kc >= 4 * qb:
            k.op("pool", "affine_select", out=e[:, :, c0:c0 + 128], in_=e[:, :, c0:c0 + 128],
                 pattern=[[0, 2], [1, 128]], compare_op=ALU.is_gt, fill=0.0, base=0, channel_multiplier=-1)
        k.op("act", "activation", out=p_b[i % 3][:, :, c0:512], in_=e[:, :, c0:512], func=AF.Ln, bias=1.0, scale=1.0)

    def la(i):
        kc, c0 = chunks[i]
        for hh in range(2):
            lab = V(P.b[4 + hh], T[:, 4 + hh, :])
            if i > 0:
                pc0 = chunks[i - 1][1]
                k.op("pe", "matmul", out=lab[:, pc0:512], lhsT=LSTR.a, rhs=p_b[(i - 1) % 3][:, hh, pc0:512], start=False,
                     stop=False, skip_group_check=True)
            k.op("pe", "matmul", out=lab[:, c0:512], lhsT=NTRI.a, rhs=p_b[i % 3][:, hh, c0:512],
                 start=(i == 0), stop=True, skip_group_check=True, mark=(hh == 1))

    def aw(i):
        kc, c0 = chunks[i]
        lv, lx = pair(4, c0)
        k.op("act", "activation", out=a_b[i % 2][:, :, c0:512], in_=lv, func=AF.Exp, _reads=lx)
        k.op("dve", "tensor_tensor", out=w_b[i % 2][:, :, c0:512], in0=a_b[i % 2][:, :, c0:512],
             in1=e_b[i % 3][:, :, c0:512], op=ALU.mult)

    def pv(i):
        kc, c0 = chunks[i]
        ob = V(P.b[6], T[:, 6, :])
        for hh in range(2):
            h = 2 * hp + hh
            for j in range(c0 // 128, 4):
                col = (hh * 4 + j) * 64
                k.op("pe", "matmul", out=ob[:, col:col + 64], lhsT=w_b[i % 2][:, hh, j * 128:(j + 1) * 128],
                     rhs=Vt[:, kc, h * 64:(h + 1) * 64], start=(i == 0 and hh == 0 and j == c0 // 128),
                     stop=True, skip_group_check=True, mark=(hh == 1 and j == 3))

    zmm(0)
    ep(0)
    if n > 1:
        zmm(1)
    la(0)
    for i in range(n):
        if i + 1 < n:
            ep(i + 1)
        if i + 2 < n:
            zmm(i + 2)
        aw(i)
        if i + 1 < n:
            la(i + 1)
        pv(i)
    ob = V(P.b[6], T[:, 6, :])
    for hh in range(2):
        h = 2 * hp + hh
        k.op("act", "copy", out=Oa[:, qb * 4:(qb + 1) * 4, h * 64:(h + 1) * 64],
             in_=ob[:, hh * 256:(hh + 1) * 256].re("p (i d) -> p i d", i=4))


def emit_phase_sb(k, P, S, x_d, w_d, oT_d, ident_d, nsub=2, row0=0):
    NTILE = S // 128
    NQB = S // 512
    idf = k.sb([128, 128], F32, "idf")
    k.dma("sp", idf.a, ident_d.a, "ld_c")
    idb = k.sb([128, 128], BF16, "idb")
    k.op("dve", "tensor_copy", out=idb.a, in_=idf.a)
    negones = k.sb([128, 128], BF16, "negones")
    k.op("pool", "memset", ap=negones.a, constant=-1.0, _writes=[negones.a])
    NTRI = k.sb([128, 128], BF16, "NTRI")
    k.op("pool", "affine_select", out=NTRI.a, in_=negones.a, pattern=[[-1, 128]], compare_op=ALU.is_ge,
         fill=0.0, base=0, channel_multiplier=1)
    LSTR = k.sb([128, 128], BF16, "LSTR")
    k.op("pool", "affine_select", out=LSTR.a, in_=negones.a, pattern=[[1, 128]], compare_op=ALU.is_gt,
         fill=0.0, base=0, channel_multiplier=-1)

    W = k.sb([128, 8, 768], BF16, "Wsb")
    QT = [k.sb([128, S], BF16, f"QT{i}") for i in range(2)]
    KT = [k.sb([128, S], BF16, f"KT{i}") for i in range(2)]
    Vt = k.sb([128, NTILE, 256], BF16, "Vt")
    Oa = k.sb([128, NTILE, 256], BF16, "Oa")
    xin = None
    xbf = [k.sb([128, 1024], BF16, f"xbf{i}") for i in range(2)]
    xT = [k.sb([128, 8, 512], BF16, f"xT{i}") for i in range(2)]
    NS = 2
    e_b = [k.sb([128, 2, 512], F32, f"e{j}") for j in range(3)]
    p_b = [k.sb([128, 2, 512], BF16, f"p{j}") for j in range(3)]
    a_b = [k.sb([128, 2, 512], BF16, f"a{j}") for j in range(2)]
    w_b = [k.sb([128, 2, 512], BF16, f"w{j}") for j in range(2)]
    oTs = [k.sb([128, 2, 128], BF16, f"oTs{i}") for i in range(4)]

    wv = w_d.h[:, :].rearrange("(c p) n -> p c n", p=128)
    for sub in range(nsub):
        for j, base in enumerate((0, 512, 1024)):
            k.dma("pool", W[:, :, j * 256:(j + 1) * 256],
                  V(w_d, wv[:, :, base + sub * 256: base + (sub + 1) * 256]), "ld_w")
        for g4 in range(S // 512):
            tok0 = g4 * 512
            xT_g = xT[g4 % 2]
            load_xT_group(k, P, x_d, tok0, 512, xin, xbf, xT_g, idb, 0)
            for j in range(4):
                pq, _ = P.v(2 + (j % 2))
                for c in range(8):
                    k.op("pe", "matmul", out=pq, lhsT=W[:, c, j * 128:(j + 1) * 128], rhs=xT_g[:, c, :],
                         start=(c == 0), stop=(c == 7), mark=(c == 7))
                dst = (QT if j < 2 else KT)[j % 2]
                k.op("act", "copy", out=dst[:, tok0:tok0 + 512], in_=pq)
            for t in range(4):
                pv, _ = P.v(4 + (t % 2))
                for c in range(8):
                    k.op("pe", "matmul", out=pv[:, 0:256], lhsT=xT_g[:, c, t * 128:(t + 1) * 128],
                         rhs=W[:, c, 512:768], start=(c == 0), stop=(c == 7), mark=(c == 7))
                k.op("dve", "tensor_copy", out=Vt[:, g4 * 4 + t, :], in_=pv[:, 0:256])
        for qb in range(NQB):
            for hp in range(2):
                sb_wide(k, P, hp, qb, QT[hp], KT[hp], Vt, Oa, NTRI, LSTR, e_b, p_b, a_b, w_b)
        for t in range(NTILE):
            pt, _ = P.v(6 + (t % 2))
            ptb = pt.bitcast(BF16)
            for c in range(2):
                k.op("pe", "transpose", out=ptb[:, c * 128:(c + 1) * 128], in_=Oa[:, t, c * 128:(c + 1) * 128],
                     identity=idb.a, mark=(c == 1))
            o_ = oTs[t % 4]
            k.op("dve", "tensor_copy", out=o_.a, in_=ptb[:, 0:256].re("p (c t) -> p c t", c=2))
            dst_ap = oT_d.h[row0 + sub * 256:row0 + (sub + 1) * 256, t * 128:(t + 1) * 128].rearrange("(c p) t -> p c t", p=128)
            k.dma("sp", V(Tile(k, dst_ap, "oT_part"), dst_ap), o_.a)


import math

SCALE = 0.125
NEG = -30000.0


def run_streams(gens, nslot):
    pending = list(gens)
    active = [None] * nslot
    while True:
        progressed = False
        for s in range(nslot):
            if active[s] is None and pending:
                active[s] = pending.pop(0)(s)
            if active[s] is not None:
                try:
                    next(active[s])
                except StopIteration:
                    active[s] = None
                progressed = True
        if not progressed and not pending:
            break


def flash_stream(k, slot, zb, ob, e_tiles, qv_fn, chunks, nv, negM, epilogue):
    first = True
    for ci, ch in enumerate(chunks):
        c0, c1 = ch["c0"], ch["c1"]
        e = e_tiles[ci % 2]
        has_mask = ch.get("mask") is not None
        k.op("pe", "matmul", out=zb[:, c0:c1], lhsT=ch["kT"], rhs=qv_fn(c0, c1), start=True, stop=not has_mask,
             mark=not has_mask, skip_group_check=True)
        if has_mask:
            ml, mr = ch["mask"]
            k.op("pe", "matmul", out=zb[:, c0:c1], lhsT=ml, rhs=mr(c0, c1), start=False, stop=True, mark=True,
                 skip_group_check=True)
        yield
        k.op("act", "activation", out=e[:, c0:c1], in_=zb[:, c0:c1], func=AF.Exp, scale=SCALE, bias=negM)
        for (lo, hi, base, cm, step, op) in ch.get("aff", []):
            k.op("pool", "affine_select", out=e[:, lo:hi], in_=e[:, lo:hi], pattern=[[step, hi - lo]],
                 compare_op=op, fill=0.0, base=base, channel_multiplier=cm)
        yield
        subs = list(range(c0 // 128, (c1 + 127) // 128))
        for i in subs:
            k.op("pe", "matmul", out=ob[:, i * nv:(i + 1) * nv], lhsT=e[:, i * 128:(i + 1) * 128], rhs=ch["v"],
                 start=first, stop=True, skip_group_check=True, mark=(i == subs[-1]))
            first = False
        yield
    epilogue()
    yield


def rope_group(k, pos_t, invf, g4, tmps):
    posf, y, yy, ki, kf, outs = tmps
    k.op("dve", "tensor_copy", out=posf.a, in_=pos_t[:, g4 * 4:(g4 + 1) * 4])
    k.op("dve", "tensor_tensor", out=y.a, in0=posf.a.unsq(2).bc([128, 4, 32]),
         in1=invf.a.unsq(1).bc([128, 4, 32]), op=ALU.mult)
    res = []
    for j, shift in enumerate((0.0, 0.25)):
        k.op("dve", "tensor_scalar", out=yy.a, in0=y.a, scalar1=shift, scalar2=None, op0=ALU.add)
        k.op("dve", "tensor_copy", out=ki.a, in_=yy.a)
        k.op("dve", "tensor_copy", out=kf.a, in_=ki.a)
        k.op("dve", "tensor_tensor", out=yy.a, in0=yy.a, in1=kf.a, op=ALU.subtract)
        k.op("dve", "tensor_scalar", out=kf.a, in0=yy.a, scalar1=0.5, scalar2=None, op0=ALU.is_gt)
        k.op("dve", "tensor_tensor", out=yy.a, in0=yy.a, in1=kf.a, op=ALU.subtract)
        k.op("dve", "tensor_scalar", out=kf.a, in0=yy.a, scalar1=-0.5, scalar2=None, op0=ALU.is_lt)
        k.op("dve", "tensor_tensor", out=yy.a, in0=yy.a, in1=kf.a, op=ALU.add)
        t = outs[g4 % 2][j]
        k.op("act", "activation", out=t.a, in_=yy.a, func=AF.Sin, scale=2.0 * math.pi * (1 - 1e-6))
        res.append(t)
    return res[1], res[0]


def rope_tmps(k):
    posf = k.sb([128, 4], F32, "posf")
    y = k.sb([128, 4, 32], F32, "rope_y")
    yy = k.sb([128, 4, 32], F32, "rope_yy")
    ki = k.sb([128, 4, 32], I32, "rope_ki")
    kf = k.sb([128, 4, 32], F32, "rope_kf")
    outs = [[k.sb([128, 4, 32], F32, f"rope_o{i}_{j}") for j in range(2)] for i in range(2)]
    return (posf, y, yy, ki, kf, outs)


def rope_apply(k, pf, nh, cos, sin, t1, t2, out_bf):
    pr = pf.re("p (h t d) -> p h t d", h=nh, t=2)
    t1v = t1[:, 0:nh * 64].re("p (h t d) -> p h t d", h=nh, t=2)
    t2v = t2[:, 0:nh * 64].re("p (h t d) -> p h t d", h=nh, t=2)
    ob = out_bf.re("p (h t d) -> p h t d", h=nh, t=2)
    cb = cos.unsq(1).unsq(1).bc([128, nh, 2, 32])
    sb_ = sin.unsq(1).bc([128, nh, 32])
    k.op("dve", "tensor_tensor", out=t1v, in0=pr, in1=cb, op=ALU.mult)
    k.op("pool", "tensor_tensor", out=t2v[:, :, 0, :], in0=pr[:, :, 1, :], in1=sb_, op=ALU.mult)
    k.op("pool", "tensor_tensor", out=t2v[:, :, 1, :], in0=pr[:, :, 0, :], in1=sb_, op=ALU.mult)
    k.op("dve", "tensor_tensor", out=ob[:, :, 0, :], in0=t1v[:, :, 0, :], in1=t2v[:, :, 0, :], op=ALU.subtract)
    k.op("dve", "tensor_tensor", out=ob[:, :, 1, :], in0=t1v[:, :, 1, :], in1=t2v[:, :, 1, :], op=ALU.add)


def bound_negM(k, P, nq_t, nk_t, idf, ones1, out_negM, bank, scratch):
    mq, mm, row = scratch
    k.op("dve", "reduce_max", out=mq[:, 0:1], in_=nq_t, axis=AX.X)
    k.op("dve", "reduce_max", out=mq[:, 1:2], in_=nk_t, axis=AX.X)
    pb, _ = P.v(bank)
    k.op("pe", "transpose", out=pb[0:2, 0:128], in_=mq.a, identity=idf.a, mark=True)
    k.op("dve", "reduce_max", out=mm.a, in_=pb[0:2, 0:128], axis=AX.X)
    k.op("pe", "transpose", out=pb[0:1, 128:130], in_=mm.a, identity=idf[0:2, 0:2], mark=True)
    k.op("dve", "tensor_copy", out=row[:, 0:2], in_=pb[0:1, 128:130])
    k.op("dve", "tensor_tensor", out=row[:, 2:3], in0=row[:, 0:1], in1=row[:, 1:2], op=ALU.mult)
    k.op("act", "activation", out=row[:, 2:3], in_=row[:, 2:3], func=AF.Ln)
    k.op("act", "activation", out=row[:, 2:3], in_=row[:, 2:3], func=AF.Exp, scale=0.5)
    k.op("dve", "tensor_scalar", out=row[:, 3:4], in0=row[:, 2:3], scalar1=-SCALE * 1.02, scalar2=None, op0=ALU.mult)
    k.op("pe", "matmul", out=pb[:, 132:133], lhsT=ones1.a, rhs=row[:, 3:4], start=True, stop=True, mark=True)
    k.op("dve", "tensor_copy", out=out_negM, in_=pb[:, 132:133])


def emit_out_T(k, P, OUT_bf, oT_d, row0, tok0, idb, oTs, bank, idx):
    dst_ap = oT_d.h[row0:row0 + 256, tok0:tok0 + 128].rearrange("(c p) t -> p c t", p=128)
    pt, _ = P.v(bank)
    ptb = pt.bitcast(BF16)
    for c in range(2):
        k.op("pe", "transpose", out=ptb[:, c * 128:(c + 1) * 128], in_=OUT_bf[:, c * 128:(c + 1) * 128],
             identity=idb.a, mark=(c == 1))
    o_ = oTs[idx % 4]
    k.op("dve", "tensor_copy", out=o_.a, in_=ptb[:, 0:256].re("p (c t) -> p c t", c=2))
    k.dma("sp", V(Tile(k, dst_ap, "oT_part"), dst_ap), o_.a)


def emit_phase_l0(k, P, S, x_d, pos_d, wn_d, wm_d, cw, consts, oT_d, do_nsa=True, do_moba=True, row_nsa=0, row_moba=256):
    NTILE = S // 128
    NQB = S // 512
    NCMP = (S - 32) // 16 + 1
    NCC = (NCMP + 127) // 128
    NSLC = S // 64
    NBLK = S // 256
    idf = k.sb([128, 128], F32, "idf")
    k.dma("sp", idf.a, consts["ident"].a, "ld_c")
    idb = k.sb([128, 128], BF16, "idb")
    k.op("dve", "tensor_copy", out=idb.a, in_=idf.a)
    ones1 = k.sb([1, 128], F32, "ones1")
    k.op("pool", "memset", ap=ones1.a, constant=1.0, _writes=[ones1.a])
    invf = k.sb([128, 32], F32, "invf")
    k.dma("sp", invf.a, consts["invf"].a, "ld_c")
    pos_t = k.sb([128, NTILE], I32, "pos_t")
    k.dma("sp", pos_t.a, pos_d.a, "ld_c")
    rtm = rope_tmps(k)
    xin = None
    xbf = [k.sb([128, 1024], BF16, f"xbf{i}") for i in range(2)]
    xT = [k.sb([128, 8, 512], BF16, "xT0")] * 2
    pf = k.sb([128, 780], F32, "pf")
    t1 = k.sb([128, 576], F32, "rt1")
    t2 = k.sb([128, 576], F32, "rt2")
    Rb = k.sb([128, 640], BF16, "Rb")
    sqj = k.sb([128, 640], F32, "sqj")
    e_t = [[k.sb([128, 512], BF16, f"e{s}_{j}") for j in range(2)] for s in range(4)]
    oTs = [k.sb([128, 2, 128], BF16, f"oTs{i}") for i in range(4)]
    OUT = k.sb([128, 4, 256], F32, "OUT")
    OUTb = k.sb([128, 4, 256], BF16, "OUTb")
    negM = [k.sb([128, 1], F32, f"negM{i}") for i in range(4)]
    sc_mq = k.sb([128, 2], F32, "sc_mq")
    sc_mk = k.sb([2, 1], F32, "sc_mk")
    sc_row = k.sb([1, 4], F32, "sc_row")
    rz = k.sb([128, 4 * 4], F32, "rz")
    coef = k.sb([128, 4 * 4], F32, "coef")
    QT = [k.sb([128, S], BF16, f"QT{i}") for i in range(2)]
    KT = [k.sb([128, S], BF16, f"KT{i}") for i in range(2)]
    CT = k.sb([128, S], BF16, "CT")
    Vall = k.sb([128, NTILE, 260], BF16, "Vall")
    k.op("pool", "memset", ap=Vall.a, constant=1.0, _writes=[Vall.a])
    Wall = k.sb([128, 8, 780], BF16, "Wall")

    if do_nsa:
        W = Wall
        k.dma("pool", W.a, V(wn_d, wn_d.h[:, :].rearrange("(c p) n -> p c n", p=128)), "ld_w")
        VS = V(Vall, Vall.h[:, :, 0:130].rearrange("p t (a d) -> p t a d", a=2))
        GL = k.sb([128, NTILE, 12], F32, "GL")
        NQ = k.sb([128, 3, NTILE], F32, "NQ")
        for g4 in range(S // 512):
            xT_g = xT[g4 % 2]
            load_xT_group(k, P, x_d, g4 * 512, 512, xin, xbf, xT_g, idb, 0)
            COS4, SIN4 = rope_group(k, pos_t, invf, g4, rtm)
            for t in range(4):
                ti = g4 * 4 + t
                pp, pp_x = P.v(2 + 2 * (t % 2), 2)
                for (a, b) in ((0, 512), (512, 780)):
                    for c in range(8):
                        k.op("pe", "matmul", out=pp[:, a:b], lhsT=xT_g[:, c, t * 128:(t + 1) * 128], rhs=W[:, c, a:b],
                             start=(c == 0), stop=(c == 7), _writes=pp_x, mark=(c == 7 and a == 512))
                k.op("act", "copy", out=pf[:, 0:780], in_=pp[:, 0:780], _reads=pp_x)
                rope_apply(k, pf[:, 0:576], 9, COS4[:, t, :], SIN4[:, t, :], t1, t2, Rb[:, 0:576])
                k.op("pool", "tensor_copy", out=Rb[:, 576:640], in_=pf[:, 576:640])
                k.op("pool", "tensor_copy", out=VS[:, ti, :, 0:64], in_=pf[:, 640:768].re("p (a d) -> p a d", a=2))
                k.op("pool", "tensor_copy", out=GL[:, ti, :], in_=pf[:, 768:780])
                for j, (a, b) in enumerate(((0, 256), (320, 448), (512, 576))):
                    k.op("act", "activation", out=sqj[:, a:b], in_=Rb[:, a:b], func=AF.Square, accum_out=NQ[:, j, ti:ti + 1])
                pt, _ = P.v(6 + (t % 2))
                ptb = pt.bitcast(BF16)
                for j in range(5):
                    k.op("pe", "transpose", out=ptb[:, j * 128:(j + 1) * 128], in_=Rb[:, j * 128:(j + 1) * 128],
                         identity=idb.a, mark=(j == 4))
                for j, dst in enumerate((QT[0], QT[1], KT[0], KT[1], CT)):
                    k.op("act" if j % 2 else "dve", "copy" if j % 2 else "tensor_copy", out=dst[:, ti * 128:(ti + 1) * 128],
                         in_=ptb[:, j * 128:(j + 1) * 128])
        G = GL
        k.op("act", "activation", out=G.a, in_=GL.a, func=AF.Sigmoid)
        W1 = k.sb([128, 32, 128], BF16, "W1")
        k.dma("pool", W1[0:64], V(cw["w1k"], cw["w1k"].h[:, :].rearrange("(l d) h -> d l h", d=64)), "ld_w")
        k.dma("pool", W1[64:128], V(cw["w1v"], cw["w1v"].h[:, :].rearrange("(l d) h -> d l h", d=64)), "ld_w")
        PF = k.sb([128, 32], BF16, "PF")
        k.dma("pool", PF.a, cw["posT"].a)
        W2 = [k.sb([128, 128], BF16, "W2k2"), k.sb([128, 64], BF16, "W2v")]
        k.dma("pool", W2[0][:, 0:64], cw["w2k"].a, "ld_w")
        k.dma("pool", W2[0][:, 64:128], cw["w2k"].a, "ld_w")
        k.dma("pool", W2[1].a, cw["w2v"].a, "ld_w")
        KCT = k.sb([128, NCC * 128], BF16, "KCT")
        k.op("pool", "memset", ap=KCT.a, constant=0.0, _writes=[KCT.a])
        NV = 193
        VCA = k.sb([128, NCC, NV], BF16, "VCA")
        k.op("pool", "memset", ap=VCA.a, constant=1.0, _writes=[VCA.a])
        for c in range(NCC):
            k.op("pool", "affine_select", out=VCA[:, c, 65:193], in_=VCA[:, c, 65:193], pattern=[[-4, 128]],
                 compare_op=ALU.is_ge, fill=0.0, base=128 * c + 1, channel_multiplier=1)
            k.op("pool", "affine_select", out=VCA[:, c, 65:193], in_=VCA[:, c, 65:193], pattern=[[4, 128]],
                 compare_op=ALU.is_ge, fill=0.0, base=3 - 128 * c, channel_multiplier=-1)
        cb = k.sb([128, 2], F32, "cbias")
        gu = [k.sb([128, 512], F32, f"gu{i}") for i in range(3)]
        gact = [k.sb([128, 512], BF16, f"gact{i}") for i in range(2)]
        k.op("pool", "memset", ap=gact[1].a, constant=0.0, _writes=[gact[1].a])
        for kv in range(2):
            r0 = kv * 64
            ph, _ = P.v(0 + kv)
            for l in range(32):
                k.op("pe", "matmul", out=ph[:, 0:NCMP], lhsT=W1[r0:r0 + 64, l, :],
                     rhs=CT[r0:r0 + 64, l:l + 16 * (NCMP - 1) + 1:16], start=(l == 0), stop=(l == 31), mark=(l == 31))
            pbias, _ = P.v(2 + kv)
            for l in range(32):
                k.op("pe", "matmul", out=pbias[:, 0:1], lhsT=W1[r0:r0 + 64, l, :], rhs=PF[r0:r0 + 64, l:l + 1], start=(l == 0),
                     stop=(l == 31), mark=(l == 31))
            k.op("dve", "tensor_copy", out=cb[:, kv:kv + 1], in_=pbias[:, 0:1])
            u, u2, th = gu
            n_ = NCMP
            k.op("dve", "tensor_scalar", out=u[:, 0:n_], in0=ph[:, 0:n_], scalar1=cb[:, kv:kv + 1], scalar2=None, op0=ALU.add)
            k.op("dve", "tensor_tensor", out=u2[:, 0:n_], in0=u[:, 0:n_], in1=u[:, 0:n_], op=ALU.mult)
            k.op("dve", "tensor_scalar", out=u2[:, 0:n_], in0=u2[:, 0:n_], scalar1=0.044715, scalar2=1.0, op0=ALU.mult, op1=ALU.add)
            k.op("dve", "tensor_tensor", out=u2[:, 0:n_], in0=u2[:, 0:n_], in1=u[:, 0:n_], op=ALU.mult)
            k.op("act", "activation", out=th[:, 0:n_], in_=u2[:, 0:n_], func=AF.Tanh, scale=0.7978845608028654)
            k.op("dve", "tensor_scalar", out=th[:, 0:n_], in0=th[:, 0:n_], scalar1=0.5, scalar2=0.5, op0=ALU.mult, op1=ALU.add)
            k.op("dve", "tensor_tensor", out=gact[kv][:, 0:n_], in0=th[:, 0:n_], in1=u[:, 0:n_], op=ALU.mult)
            if kv == 0:
                pk, _ = P.v(4)
                k.op("pe", "matmul", out=pk[:, 0:n_], lhsT=W2[0].a, rhs=gact[0][:, 0:n_], start=True, stop=True, mark=True)
                k.op("dve", "tensor_copy", out=KCT[:, 0:n_], in_=pk[:, 0:n_])
                k.op("act", "activation", out=sqj[0:64, 0:n_], in_=pk[0:64, 0:n_], func=AF.Square)
            else:
                for c in range(NCC):
                    pvv, _ = P.v(5)
                    k.op("pe", "matmul", out=pvv[:, 0:64], lhsT=gact[1][:, c * 128:(c + 1) * 128], rhs=W2[1].a, start=True,
                         stop=True, mark=True)
                    k.op("dve", "tensor_copy", out=VCA[:, c, 0:64], in_=pvv[:, 0:64])
        ones64 = k.sb([64, 1], F32, "ones64")
        k.op("pool", "memset", ap=ones64.a, constant=1.0, _writes=[ones64.a])
        pn, _ = P.v(6)
        k.op("pe", "matmul", out=pn[0:1, 0:NCMP], lhsT=ones64.a, rhs=sqj[0:64, 0:NCMP], start=True, stop=True, mark=True)
        nkc = k.sb([128, 1], F32, "nkc")
        k.op("pool", "memset", ap=nkc.a, constant=0.0, _writes=[nkc.a])
        k.op("dve", "reduce_max", out=nkc[0:1, 0:1], in_=pn[0:1, 0:NCMP], axis=AX.X)
        bound_negM(k, P, NQ[:, 0, :], NQ[:, 1, :], idf, ones1, negM[0].a, 7, (sc_mq, sc_mk, sc_row))
        bound_negM(k, P, NQ[:, 0, :], nkc.a, idf, ones1, negM[1].a, 7, (sc_mq, sc_mk, sc_row))
        WV = k.sb([128, 256], F32, "WV")
        WADD = k.sb([128, 256], F32, "WADD")
        k.dma("sp", WV.a, consts["wv"].a, "ld_c")
        k.dma("sp", WADD.a, consts["wadd"].a, "ld_c")
        G64 = CT
        k.op("pool", "memset", ap=G64.a, constant=0.0, _writes=[G64.a])
        k.dma("sp", G64[0:NSLC], consts["g64"].a, "ld_c")
        IMP = k.sb([128, 4, 128], F32, "IMP")
        NMT = k.sb([128, 512], BF16, "NMT")
        k.op("pool", "memset", ap=NMT.a, constant=0.0, _writes=[NMT.a])
        sc_s = [k.sb([128, 128], F32, f"sc_s{i}") for i in range(2)]
        m8 = k.sb([128, 16], F32, "m8")
        nmb = k.sb([128, 128], BF16, "nmb")
        for qb in range(NQB):
            q0 = qb * 512
            first_touch = {"imp": [True] * 4, "out": [[True] * 4 for _ in range(4)]}

            def mk_cmp(h, half, qb=qb, q0=q0):
                def fac(slot):
                    zb, _ = P.v(slot * 2)
                    ob, _ = P.v(slot * 2 + 1)
                    qt = QT[h // 2]
                    r0 = (h % 2) * 64
                    cbase = q0 + half * 256
                    chunks = []
                    for c in range(NCC):
                        if 16 * (128 * c) + 31 > cbase + 255:
                            continue
                        chunks.append(dict(kT=KCT[r0:r0 + 64, c * 128:(c + 1) * 128], v=VCA[:, c, :], c0=0, c1=256,
                                           aff=[(0, 256, cbase - 2048 * c - 31, -16, 1, ALU.is_ge)]))

                    def epi():
                        for i in range(2):
                            sub = half * 2 + i
                            ti = qb * 4 + sub
                            rzv = rz[:, slot * 4 + i: slot * 4 + i + 1]
                            cfv = coef[:, slot * 4 + i: slot * 4 + i + 1]
                            if not chunks:
                                if first_touch["imp"][sub]:
                                    k.op("pool", "memset", ap=IMP[:, sub, :], constant=0.0, _writes=[IMP.a])
                                    first_touch["imp"][sub] = False
                                if first_touch["out"][sub][h]:
                                    k.op("pool", "memset", ap=OUT[:, sub, h * 64:(h + 1) * 64], constant=0.0, _writes=[OUT.a])
                                    first_touch["out"][sub][h] = False
                                continue
                            acc = ob[:, i * NV:(i + 1) * NV]
                            k.op("dve", "tensor_scalar", out=rzv, in0=acc[:, 64:65], scalar1=1e-30, scalar2=None, op0=ALU.max)
                            k.op("dve", "reciprocal", out=rzv, in_=rzv)
                            k.op("dve", "tensor_tensor", out=cfv, in0=rzv, in1=G[:, ti, h * 3:h * 3 + 1], op=ALU.mult)
                            k.op("dve", "tensor_scalar", out=OUT[:, sub, h * 64:(h + 1) * 64], in0=acc[:, 0:64], scalar1=cfv,
                                 scalar2=None, op0=ALU.mult)
                            first_touch["out"][sub][h] = False
                            if first_touch["imp"][sub]:
                                k.op("dve", "tensor_scalar", out=IMP[:, sub, :], in0=acc[:, 65:193], scalar1=rzv, scalar2=None,
                                     op0=ALU.mult)
                                first_touch["imp"][sub] = False
                            else:
                                k.op("dve", "scalar_tensor_tensor", out=IMP[:, sub, :], in0=acc[:, 65:193], scalar=rzv,
                                     in1=IMP[:, sub, :], op0=ALU.mult, op1=ALU.add)
                    qv = lambda c0, c1: qt[r0:r0 + 64, cbase + c0:cbase + c1]
                    return flash_stream(k, slot, zb, ob, e_t[slot], qv, chunks, NV, negM[1].a, epi)
                return fac
            run_streams([mk_cmp(h, half) for h in range(4) for half in range(2)], 4)
            for sub in range(4):
                qt_i = qb * 4 + sub
                sc0, sc1 = sc_s
                lo = 128 - 2 * qt_i
                k.op("dve", "tensor_tensor", out=sc0.a, in0=IMP[:, sub, :], in1=WV[:, lo:lo + 128], op=ALU.mult)
                k.op("dve", "tensor_tensor", out=sc0.a, in0=sc0.a, in1=WADD[:, lo:lo + 128], op=ALU.add)
                if qt_i >= 1:
                    k.op("dve", "tensor_scalar", out=sc0[:, 0:1], in0=sc0[:, 0:1], scalar1=1.0e4, scalar2=None, op0=ALU.add)
                k.op("dve", "max", out=m8[:, 0:8], in_=sc0.a)
                k.op("dve", "match_replace", out=sc1.a, in_to_replace=m8[:, 0:8], in_values=sc0.a, imm_value=-1e30)
                k.op("dve", "max", out=m8[:, 8:16], in_=sc1.a)
                k.op("dve", "tensor_scalar", out=sc1.a, in0=sc0.a, scalar1=m8[:, 15:16], scalar2=None, op0=ALU.is_ge)
                k.op("dve", "tensor_tensor", out=sc1.a, in0=sc1.a, in1=WV[:, lo:lo + 128], op=ALU.mult)
                k.op("dve", "tensor_scalar", out=nmb.a, in0=sc1.a, scalar1=-NEG, scalar2=NEG, op0=ALU.mult, op1=ALU.add)
                pt, _ = P.v(7)
                ptb = pt.bitcast(BF16)
                k.op("pe", "transpose", out=ptb[:, 0:128], in_=nmb.a, identity=idb.a, mark=True)
                k.op("act", "copy", out=NMT[:, sub * 128:(sub + 1) * 128], in_=ptb[:, 0:128])
            def mk_flash(h, br, qb=qb, q0=q0):
                def fac(slot):
                    zb, _ = P.v(slot * 2)
                    ob, _ = P.v(slot * 2 + 1)
                    qt = QT[h // 2]
                    r0 = (h % 2) * 64
                    chunks = []
                    if br == 1:
                        for kc in range(0, 4 * qb + 4):
                            j = kc - 4 * qb
                            c0 = 128 * j if j > 0 else 0
                            aff = [(c0, c0 + 128, 0, -1, 1, ALU.is_ge)] if j >= 0 else []
                            chunks.append(dict(kT=KT[0][r0:r0 + 64, kc * 128:(kc + 1) * 128], v=VS[:, kc, 0, :], c0=c0, c1=512, aff=aff,
                                               mask=(G64[:, kc * 128:(kc + 1) * 128], lambda a, b: NMT[:, a:b])))
                    else:
                        for kc in range(max(0, 4 * qb - 4), 4 * qb + 4):
                            j = kc - 4 * qb
                            if j >= 0:
                                c0, c1 = 128 * j, 512
                                aff = [(c0, c0 + 128, 0, -1, 1, ALU.is_ge)]
                            else:
                                jp = j + 4
                                c0, c1 = 0, 128 * (jp + 1)
                                aff = [(128 * jp, 128 * jp + 128, -1, 1, -1, ALU.is_ge)]
                            chunks.append(dict(kT=KT[1][r0:r0 + 64, kc * 128:(kc + 1) * 128], v=VS[:, kc, 1, :], c0=c0, c1=c1, aff=aff))

                    def epi():
                        for sub in range(4):
                            ti = qb * 4 + sub
                            rzv = rz[:, slot * 4 + sub: slot * 4 + sub + 1]
                            cfv = coef[:, slot * 4 + sub: slot * 4 + sub + 1]
                            acc = ob[:, sub * 65:(sub + 1) * 65]
                            k.op("dve", "tensor_scalar", out=rzv, in0=acc[:, 64:65], scalar1=1e-30, scalar2=None, op0=ALU.max)
                            k.op("dve", "reciprocal", out=rzv, in_=rzv)
                            k.op("dve", "tensor_tensor", out=cfv, in0=rzv, in1=G[:, ti, h * 3 + br:h * 3 + br + 1], op=ALU.mult)
                            k.op("dve", "scalar_tensor_tensor", out=OUT[:, sub, h * 64:(h + 1) * 64], in0=acc[:, 0:64], scalar=cfv,
                                 in1=OUT[:, sub, h * 64:(h + 1) * 64], op0=ALU.mult, op1=ALU.add)
                    qv = lambda c0, c1: qt[r0:r0 + 64, q0 + c0:q0 + c1]
                    return flash_stream(k, slot, zb, ob, e_t[slot], qv, chunks, 65, negM[0].a, epi)
                return fac
            run_streams([mk_flash(h, br) for h in range(4) for br in (1, 2)], 4)
            k.op("act", "copy", out=OUTb.a, in_=OUT.a)
            for sub in range(4):
                emit_out_T(k, P, OUTb[:, sub, :], oT_d, row_nsa, (qb * 4 + sub) * 128, idb, oTs, 7, sub)

    if do_moba:
        NB = max(NBLK, 8)
        Wm = Wall
        k.dma("pool", Wm[:, :, 0:768], V(wm_d, wm_d.h[:, :].rearrange("(c p) n -> p c n", p=128)))
        VB = V(Vall, Vall.h[:, :, :].rearrange("p t (a d) -> p t a d", a=4))
        k.op("pool", "memset", ap=Vall.a, constant=1.0, _writes=[Vall.a])
        NQm = k.sb([128, 2, NTILE], F32, "NQm")
        for g4 in range(S // 512):
            xT_g = xT[g4 % 2]
            load_xT_group(k, P, x_d, g4 * 512, 512, xin, xbf, xT_g, idb, 0)
            COS4, SIN4 = rope_group(k, pos_t, invf, g4, rtm)
            for t in range(4):
                ti = g4 * 4 + t
                pp, pp_x = P.v(2 + 2 * (t % 2), 2)
                for (a, b) in ((0, 512), (512, 768)):
                    for c in range(8):
                        k.op("pe", "matmul", out=pp[:, a:b], lhsT=xT_g[:, c, t * 128:(t + 1) * 128], rhs=Wm[:, c, a:b],
                             start=(c == 0), stop=(c == 7), _writes=pp_x, mark=(c == 7 and a == 512))
                k.op("act", "copy", out=pf[:, 0:768], in_=pp[:, 0:768], _reads=pp_x)
                rope_apply(k, pf[:, 0:512], 8, COS4[:, t, :], SIN4[:, t, :], t1, t2, Rb[:, 0:512])
                k.op("pool", "tensor_copy", out=VB[:, ti, :, 0:64], in_=pf[:, 512:768].re("p (a d) -> p a d", a=4))
                for j, (a, b) in enumerate(((0, 256), (256, 512))):
                    k.op("act", "activation", out=sqj[:, a:b], in_=Rb[:, a:b], func=AF.Square, accum_out=NQm[:, j, ti:ti + 1])
                pt, _ = P.v(6 + (t % 2))
                ptb = pt.bitcast(BF16)
                for j in range(4):
                    k.op("pe", "transpose", out=ptb[:, j * 128:(j + 1) * 128], in_=Rb[:, j * 128:(j + 1) * 128],
                         identity=idb.a, mark=(j == 3))
                for j, dst in enumerate((QT[0], QT[1], KT[0], KT[1])):
                    k.op("act" if j % 2 else "dve", "copy" if j % 2 else "tensor_copy", out=dst[:, ti * 128:(ti + 1) * 128],
                         in_=ptb[:, j * 128:(j + 1) * 128])
        bound_negM(k, P, NQm[:, 0, :], NQm[:, 1, :], idf, ones1, negM[2].a, 7, (sc_mq, sc_mk, sc_row))
        KM = k.sb([128, 32], F32, "KM")
        KMt = k.sb([128, 32], F32, "KMt")
        KMhi = [k.sb([128, 32], BF16, f"KMhi{i}") for i in range(2)]
        KMlo = [k.sb([128, 32], BF16, f"KMlo{i}") for i in range(2)]
        for i in range(2):
            k.op("pool", "memset", ap=KMhi[i].a, constant=0.0, _writes=[KMhi[i].a])
            k.op("pool", "memset", ap=KMlo[i].a, constant=0.0, _writes=[KMlo[i].a])
            k.op("dve", "tensor_reduce", out=KM[:, 0:NBLK], in_=KT[i].a.re("p (n l) -> p n l", l=256), axis=AX.X, op=ALU.add)
            k.op("dve", "tensor_scalar", out=KM[:, 0:NBLK], in0=KM[:, 0:NBLK], scalar1=1.0 / 256, scalar2=None, op0=ALU.mult)
            k.op("dve", "tensor_copy", out=KMhi[i][:, 0:NBLK], in_=KM[:, 0:NBLK])
            k.op("dve", "tensor_tensor", out=KMt[:, 0:NBLK], in0=KM[:, 0:NBLK], in1=KMhi[i][:, 0:NBLK], op=ALU.subtract)
            k.op("dve", "tensor_copy", out=KMlo[i][:, 0:NBLK], in_=KMt[:, 0:NBLK])
        G256 = CT
        k.op("pool", "memset", ap=CT.a, constant=0.0, _writes=[CT.a])
        k.dma("sp", G256[0:NBLK], consts["g256"].a)
        PM = k.sb([128, 64], F32, "PM")
        k.dma("sp", PM.a, consts["pm"].a)
        NMTm = [k.sb([128, 512], BF16, f"NMTm{h}") for h in range(4)]
        for h in range(4):
            k.op("pool", "memset", ap=NMTm[h].a, constant=0.0, _writes=[NMTm[h].a])
        wk4 = [k.sb([128, 32], F32, f"wk4_{h}") for h in range(4)]
        nm4 = [k.sb([128, 32], F32, f"nm4_{h}") for h in range(4)]
        nmb4 = [k.sb([128, 32], BF16, f"nmb4_{h}") for h in range(4)]
        for h in range(4):
            k.op("pool", "memset", ap=nmb4[h].a, constant=0.0, _writes=[nmb4[h].a])
        m84 = [k.sb([128, 8], F32, f"m84_{h}") for h in range(4)]
        thr4 = [k.sb([128, 1], F32, f"thr4_{h}") for h in range(4)]
        for qb in range(NQB):
            q0 = qb * 512
            for sub in range(4):
                qt_i = qb * 4 + sub
                cur = qt_i // 2
                pgs = [P.v(2 + h)[0] for h in range(4)]
                for h in range(4):
                    r0 = (h % 2) * 64
                    k.op("pe", "matmul", out=pgs[h][:, 0:32], lhsT=QT[h // 2][r0:r0 + 64, qt_i * 128:(qt_i + 1) * 128],
                         rhs=KMhi[h // 2][r0:r0 + 64, :], start=True, stop=False)
                    k.op("pe", "matmul", out=pgs[h][:, 0:32], lhsT=QT[h // 2][r0:r0 + 64, qt_i * 128:(qt_i + 1) * 128],
                         rhs=KMlo[h // 2][r0:r0 + 64, :], start=False, stop=True, mark=True)
                for h in range(4):
                    k.op("dve", "tensor_tensor", out=wk4[h][:, 0:NB], in0=pgs[h][:, 0:NB], in1=PM[:, 32 - cur:32 - cur + NB], op=ALU.add)
                for h in range(4):
                    k.op("dve", "max", out=m84[h].a, in_=wk4[h][:, 0:NB])
                for h in range(4):
                    k.op("dve", "tensor_scalar", out=thr4[h].a, in0=m84[h][:, 2:3], scalar1=-1e29, scalar2=None, op0=ALU.max)
                for h in range(4):
                    k.op("dve", "tensor_scalar", out=nm4[h][:, 0:NB], in0=wk4[h][:, 0:NB], scalar1=thr4[h].a, scalar2=None, op0=ALU.is_ge)
                for h in range(4):
                    k.op("dve", "tensor_scalar", out=nm4[h][:, 0:NB], in0=nm4[h][:, 0:NB], scalar1=-NEG, scalar2=NEG, op0=ALU.mult, op1=ALU.add)
                for h in range(4):
                    k.op("dve", "memset", ap=nm4[h][:, cur:cur + 1], constant=0.0, _writes=[nm4[h].a])
                for h in range(4):
                    k.op("dve", "tensor_copy", out=nmb4[h][:, 0:NB], in_=nm4[h][:, 0:NB])
                pt, _ = P.v(7)
                ptb = pt.bitcast(BF16)
                for h in range(4):
                    k.op("pe", "transpose", out=ptb[0:32, h * 128:(h + 1) * 128], in_=nmb4[h].a, identity=idb.a, mark=(h == 3))
                for h in range(4):
                    k.op("act", "copy", out=NMTm[h][0:32, sub * 128:(sub + 1) * 128], in_=ptb[0:32, h * 128:(h + 1) * 128])

            def mk_moba(h, qb=qb, q0=q0):
                def fac(slot):
                    zb, _ = P.v(slot * 2)
                    ob, _ = P.v(slot * 2 + 1)
                    qt = QT[h // 2]
                    r0 = (h % 2) * 64
                    chunks = []
                    for kc in range(0, 4 * qb + 4):
                        j = kc - 4 * qb
                        c0 = 128 * j if j > 0 else 0
                        aff = [(c0, c0 + 128, 0, -1, 1, ALU.is_ge)] if j >= 0 else []
                        chunks.append(dict(kT=KT[h // 2][r0:r0 + 64, kc * 128:(kc + 1) * 128], v=VB[:, kc, h, :], c0=c0, c1=512, aff=aff,
                                           mask=(G256[:, kc * 128:(kc + 1) * 128], lambda a, b, h=h: NMTm[h][:, a:b])))

                    def epi():
                        for sub in range(4):
                            rzv = rz[:, slot * 4 + sub: slot * 4 + sub + 1]
                            acc = ob[:, sub * 65:(sub + 1) * 65]
                            k.op("dve", "tensor_scalar", out=rzv, in0=acc[:, 64:65], scalar1=1e-30, scalar2=None, op0=ALU.max)
                            k.op("dve", "reciprocal", out=rzv, in_=rzv)
                            k.op("dve", "tensor_scalar", out=OUT[:, sub, h * 64:(h + 1) * 64], in0=acc[:, 0:64], scalar1=rzv,
                                 scalar2=None, op0=ALU.mult)
                    qv = lambda c0, c1: qt[r0:r0 + 64, q0 + c0:q0 + c1]
                    return flash_stream(k, slot, zb, ob, e_t[slot], qv, chunks, 65, negM[2].a, epi)
                return fac
            run_streams([mk_moba(h) for h in range(4)], 4)
            k.op("act", "copy", out=OUTb.a, in_=OUT.a)
            for sub in range(4):
                emit_out_T(k, P, OUTb[:, sub, :], oT_d, row_moba, (qb * 4 + sub) * 128, idb, oTs, 7, sub)


import ml_dtypes
from concourse.bass_utils import run_bass_kernel_spmd

_PROGS = {}
EVEN_WIDTHS = (512, 128, 128, 128, 128, 128, 128, 24, 512, 512, 512)
_SP = [0] + [int(v) for v in np.cumsum(EVEN_WIDTHS)[:-1]]


def _consts(S):
    half = 32
    invf = (10000.0 ** (-np.arange(half, dtype=np.float32) / half)).astype(np.float32)
    invf_t = np.tile((invf / np.float32(2 * np.pi)).astype(np.float32)[None, :], (128, 1))
    p = np.arange(128)[:, None]
    m = np.arange(256)[None, :]
    hp = (p >= 64).astype(np.int64)
    d = m - 128
    wv = (d <= hp).astype(np.float32)
    wadd = (1e4 * ((d == hp) | (d == hp - 1)) - 1.0 * (d > hp)).astype(np.float32)
    g64 = (np.arange(S)[None, :] // 64 == np.arange(S // 64)[:, None]).astype(ml_dtypes.bfloat16)
    g256 = (np.arange(S)[None, :] // 256 == np.arange(S // 256)[:, None]).astype(ml_dtypes.bfloat16)
    pm = np.where(np.arange(64)[None, :] < 32, 0.0, -1e30).astype(np.float32) * np.ones((128, 1), np.float32)
    return dict(ident=np.eye(128, dtype=np.float32), invf=invf_t, wv=wv, wadd=wadd, g64=g64, g256=g256, pm=pm)


def build_fused(S, SG):
    nc = bass.Bass("TRN2", target_bir_lowering=False)
    k = K(nc)
    P = Psum(k)
    EI = dict(kind="ExternalInput")
    x = k.dram("x", [S, 1024], F32, **EI)
    pos = k.dram("pos", [128, S // 128], I32, **EI)
    wn = k.dram("wn", [2, 1024, 780], F32, **EI)
    wm = k.dram("wm", [2, 1024, 768], F32, **EI)
    wsb = k.dram("wsb", [2, 1024, 1536], F32, **EI)
    cw = {n: k.dram(n, sh, F32, **EI) for n, sh in
          (("w1k", [2048, 128]), ("w1v", [2048, 128]), ("w2k", [128, 64]), ("w2v", [128, 64]), ("posT", [128, 32]))}
    consts = {"ident": k.dram("ident", [128, 128], F32, **EI), "invf": k.dram("invf", [128, 32], F32, **EI),
              "wv": k.dram("wv", [128, 256], F32, **EI), "wadd": k.dram("wadd", [128, 256], F32, **EI),
              "g64": k.dram("g64", [S // 64, S], BF16, **EI), "g256": k.dram("g256", [S // 256, S], BF16, **EI),
              "pm": k.dram("pm", [128, 64], F32, **EI)}
    w_out = k.dram("w_out", [2, 1024, 1024], F32, **EI)
    lnp = k.dram("lnp", [2, 4, 1024], F32, **EI)
    wr = k.dram("wr", [2, 1024, 20], F32, **EI)
    br = k.dram("br", [2, 20], F32, **EI)
    wg = k.dram("w_gate", [2, 16, 1024, 256], F32, **EI)
    wu = k.dram("w_up", [2, 16, 1024, 256], F32, **EI)
    wd = k.dram("w_down", [2, 16, 256, 1024], F32, **EI)
    out = k.dram("out", [S, 1024], F32, kind="ExternalOutput")
    oT_s = k.dram("oT_s", [1024, S], BF16, kind="Internal")
    x1_s = k.dram("x1_s", [S, 1024], F32, kind="Internal")
    sub = lambda t, i: Tile(k, t.h[i], t.name + f"_{i}")
    for g in range(2):
        k.begin_phase(f"l0a{g}")
        emit_phase_l0(k, P, S, x, pos, sub(wn, g), sub(wm, g), cw, consts, oT_s, row_nsa=256 * g, row_moba=512 + 256 * g)
        k.end_phase()
    k.begin_phase("l0b")
    emit_phase_b(k, P, S, SG, oT_s, x, x1_s, sub(w_out, 0), sub(lnp, 0), sub(wr, 0), sub(br, 0), sub(wg, 0), sub(wu, 0),
                 sub(wd, 0), consts["ident"])
    k.end_phase()
    for g in range(2):
        k.begin_phase(f"l1a{g}")
        emit_phase_sb(k, P, S, x1_s, sub(wsb, g), oT_s, consts["ident"], row0=512 * g)
        k.end_phase()
    k.begin_phase("l1b")
    emit_phase_b(k, P, S, SG, oT_s, x1_s, out, sub(w_out, 1), sub(lnp, 1), sub(wr, 1), sub(br, 1), sub(wg, 1), sub(wu, 1),
                 sub(wd, 1), consts["ident"])
    k.end_phase()
    k.finish([out])
    return nc


def host_inputs(inp, b, S):
    w_in = inp["ab_w_in"][0]
    c_qa, c_kc, c_vc, c_ksl, c_vsl, c_kw, c_vw, c_ga, c_qm, c_km, c_vm = _SP
    wns, wms, wsbs = [], [], []
    w_sb = inp["sb_w_in"][0]
    for g in range(2):
        kvc = lambda base: w_in[:, base + g * 64: base + (g + 1) * 64]
        wns.append(np.concatenate([w_in[:, c_qa + 256 * g: c_qa + 256 * (g + 1)], kvc(c_ksl), kvc(c_ksl), kvc(c_kw), kvc(c_kw),
                                   kvc(c_kc), kvc(c_vc), kvc(c_vsl), kvc(c_vw), w_in[:, c_ga + 12 * g: c_ga + 12 * (g + 1)]], axis=1))
        wms.append(np.concatenate([w_in[:, c_qm + 256 * g: c_qm + 256 * (g + 1)], w_in[:, c_km + 256 * g: c_km + 256 * (g + 1)],
                                   w_in[:, c_vm + 256 * g: c_vm + 256 * (g + 1)]], axis=1))
        wsbs.append(np.concatenate([w_sb[:, j * 1024 + 512 * g: j * 1024 + 512 * (g + 1)] for j in range(3)], axis=1))
    m = dict(x=np.ascontiguousarray(inp["x"][b]).astype(np.float32),
             pos=np.ascontiguousarray(inp["positions"][b].reshape(S // 128, 128).T.astype(np.int32)),
             wn=np.ascontiguousarray(np.stack(wns)), wm=np.ascontiguousarray(np.stack(wms)), wsb=np.ascontiguousarray(np.stack(wsbs)),
             w1k=inp["nsa_cmp_w1_k"][0], w1v=inp["nsa_cmp_w1_v"][0], w2k=inp["nsa_cmp_w2_k"][0], w2v=inp["nsa_cmp_w2_v"][0],
             posT=np.ascontiguousarray(np.concatenate([inp["nsa_cmp_pos_k"][0].T, inp["nsa_cmp_pos_v"][0].T], axis=0)),
             w_out=np.ascontiguousarray(np.stack([inp["ab_w_out"][0], inp["sb_w_out"][0]])),
             lnp=np.ascontiguousarray(np.stack([np.stack([inp["ln_mix_g"][l], inp["ln_mix_b"][l], inp["ln_ffn_g"][l], inp["ln_ffn_b"][l]]) for l in range(2)])),
             wr=np.ascontiguousarray(np.stack([np.concatenate([inp["moe_w_grp"][l], inp["moe_w_rt"][l].transpose(1, 0, 2).reshape(1024, 16)], axis=1) for l in range(2)])),
             br=np.ascontiguousarray(np.stack([np.concatenate([inp["moe_b_grp"][l], inp["moe_b_rt"][l].reshape(16)]) for l in range(2)])),
             w_gate=inp["moe_w_gate"], w_up=inp["moe_w_up"], w_down=inp["moe_w_down"])
    m.update(_consts(S))
    return m


def kernel(**inputs):
    inp = {k_: np.asarray(v) for k_, v in inputs.items()}
    B, S, _ = inp["x"].shape
    SG = min(2048, S)
    key = ("fused", S, SG)
    if key not in _PROGS:
        _PROGS[key] = build_fused(S, SG)
    nc = _PROGS[key]
    in_maps = [host_inputs(inp, b, S) for b in range(B)]
    res = run_bass_kernel_spmd(nc, in_maps, core_ids=list(range(B)))
    return np.stack([res.results[b]["out"] for b in range(B)]).astype(np.float32)
```
